# Optimizing a Trainium2 kernel written in Bass

```python
import math
import jax
import jax.numpy as jnp
from jax import lax
import numpy as np

D_MODEL = 2048
BATCH = 4
SEQ = 2048
DEPTH = 2

GRID_W = 64
CTX_LEN = 256
EPS = 1e-6
F32 = jnp.float32

ML_HEADS = 4
ML_HD = 128
ML_W = ML_HEADS * ML_HD
ML_CHUNK = 64

SSD_HEADS = 8
SSD_HD = 64
SSD_W = SSD_HEADS * SSD_HD
SSD_GROUPS = 2
SSD_STATE = 128
SSD_CONV = 5
SSD_CONV_CH = SSD_W + 2 * SSD_GROUPS * SSD_STATE
SSD_CHUNK = 64

MLA_HEADS = 4
MLA_NOPE = 128
MLA_ROPE = 64
MLA_QK = MLA_NOPE + MLA_ROPE
MLA_V = 128
MLA_Q_RANK = 448
MLA_KV_RANK = 160
MLA_W = MLA_HEADS * MLA_V
ROPE_THETA = 10000.0
ATT_QBLOCK = 128

NA_HEADS = 4
NA_HD = 128
NA_W = NA_HEADS * NA_HD
NA_KH = 8
NA_KW = 16

MIX_W = ML_W + SSD_W + MLA_W + NA_W

IN_SPLITS = (ML_W, ML_W, ML_W, ML_W, 2 * ML_HEADS, 2 * ML_HEADS,
             SSD_W, SSD_CONV_CH, 2 * SSD_HEADS,
             MLA_Q_RANK, MLA_KV_RANK, MLA_ROPE,
             NA_W, NA_W, NA_W)
D_IN = sum(IN_SPLITS)

FFN_DENSE = 5632
N_EXPERTS = 8
TOP_K = 2
FFN_EXPERT = 7168
MOE_BLOCK = 128

kernel_name = 'hybrid_dit_mlstm_ssd_mla_natten_moe'


def rms_norm(x, g):
    xf = x.astype(F32)
    y = xf * lax.rsqrt(jnp.mean(xf * xf, axis=-1, keepdims=True) + EPS)
    return (y * g.astype(F32)).astype(x.dtype)


def split_cols(p):
    offs = np.cumsum(IN_SPLITS)[:-1].tolist()
    return jnp.split(p, offs, axis=-1)


def axial_rope(n_tok, rot_dim):
    t = jnp.arange(n_tok)
    row = (t // GRID_W).astype(F32)
    col = (t % GRID_W).astype(F32)
    n_freq = rot_dim // 4
    freqs = ROPE_THETA ** (-jnp.arange(n_freq, dtype=F32) / n_freq)
    ang = jnp.concatenate([row[:, None] * freqs, col[:, None] * freqs], axis=-1)
    return jnp.cos(ang), jnp.sin(ang)


def apply_rope(u, cos, sin):
    uf = u.astype(F32)
    half = u.shape[-1] // 2
    u1, u2 = uf[..., :half], uf[..., half:]
    cs, sn = cos[:, None, :], sin[:, None, :]
    return jnp.concatenate([u1 * cs - u2 * sn, u1 * sn + u2 * cs], axis=-1).astype(u.dtype)


def dense_attention(q, k, v, scale):
    s = jnp.einsum('bhqd,bhkd->bhqk', q, k).astype(F32) * scale
    p = jax.nn.softmax(s, axis=-1).astype(v.dtype)
    return jnp.einsum('bhqk,bhkd->bhqd', p, v)


def blocked_attention(q, k, v, scale):
    B, H, T, dk = q.shape
    nb = T // ATT_QBLOCK
    qb = jnp.moveaxis(q.reshape(B, H, nb, ATT_QBLOCK, dk), 2, 0)
    ob = lax.map(lambda qq: dense_attention(qq, k, v, scale), qb)
    return jnp.moveaxis(ob, 0, 2).reshape(B, H, T, v.shape[-1])


def mlstm_chunked(q, k, v, log_i, log_f, state):
    B, H, T, d = q.shape
    L = ML_CHUNK
    nc = T // L

    def chunks(u):
        return jnp.moveaxis(u.reshape(B, H, nc, L, *u.shape[3:]), 2, 0)

    causal = jnp.tril(jnp.ones((L, L), bool))

    def step(carry, inp):
        C, n, m = carry
        qc, kc, vc, ic, fc = inp
        b = jnp.cumsum(fc, axis=-1)
        dmat = jnp.where(causal, b[..., :, None] - b[..., None, :] + ic[..., None, :], -jnp.inf)
        inter = b + m[..., None]
        mt = jnp.maximum(inter, jnp.max(dmat, axis=-1))
        w_intra = jnp.exp(dmat - mt[..., None])
        w_state = jnp.exp(inter - mt)
        s = jnp.einsum('bhtd,bhsd->bhts', qc, kc) * w_intra
        num = jnp.einsum('bhts,bhsd->bhtd', s, vc) + w_state[..., None] * jnp.einsum('bhde,bhte->bhtd', C, qc)
        den = jnp.sum(s, axis=-1) + w_state * jnp.einsum('bhd,bhtd->bht', n, qc)
        h = num / jnp.maximum(jnp.abs(den), jnp.exp(-mt))[..., None]
        bl = b[..., -1]
        g = bl[..., None] - b + ic
        m_new = jnp.maximum(bl + m, jnp.max(g, axis=-1))
        wg = jnp.exp(g - m_new[..., None])
        wc = jnp.exp(bl + m - m_new)
        C_new = wc[..., None, None] * C + jnp.einsum('bhs,bhsd,bhse->bhde', wg, vc, kc)
        n_new = wc[..., None] * n + jnp.einsum('bhs,bhsd->bhd', wg, kc)
        return (C_new, n_new, m_new), h

    state, hs = lax.scan(step, state, (chunks(q), chunks(k), chunks(v), chunks(log_i), chunks(log_f)))
    return state, jnp.moveaxis(hs, 0, 2).reshape(B, H, T, d)


def mlstm_mixer(lat, ctx, i_bias, f_bias, norm_g, with_ctx_out):
    def prep(parts):
        q, k, v, o, ig, fg = parts
        B, T, _ = q.shape
        heads = lambda u: u.astype(F32).reshape(B, T, ML_HEADS, ML_HD).transpose(0, 2, 1, 3)
        gate = lambda u, bias: jnp.transpose(u.astype(F32).reshape(B, T, 2, ML_HEADS) + bias.astype(F32), (2, 0, 3, 1))
        return (heads(q) * ML_HD ** -0.5, heads(k), heads(v), o,
                gate(ig, i_bias), jax.nn.log_sigmoid(gate(fg, f_bias)))

    ql, kl, vl, ol, il, fl = prep(lat)
    qc, kc, vc, oc, ic, fc = prep(ctx)
    B = ql.shape[0]
    zero = (jnp.zeros((B, ML_HEADS, ML_HD, ML_HD), F32), jnp.zeros((B, ML_HEADS, ML_HD), F32),
            jnp.zeros((B, ML_HEADS), F32))
    rev = lambda u: jnp.flip(u, axis=2)
    st_f, hc_f = mlstm_chunked(qc, kc, vc, ic[0], fc[0], zero)
    _, hl_f = mlstm_chunked(ql, kl, vl, il[0], fl[0], st_f)
    st_b, hc_b = mlstm_chunked(rev(qc), rev(kc), rev(vc), rev(ic[1]), rev(fc[1]), zero)
    _, hl_b = mlstm_chunked(rev(ql), rev(kl), rev(vl), rev(il[1]), rev(fl[1]), st_b)

    def finish(h, o):
        B_, H_, T_, d_ = h.shape
        h = rms_norm(h.transpose(0, 2, 1, 3), norm_g.reshape(ML_HEADS, ML_HD))
        out = h * jax.nn.sigmoid(o.astype(F32)).reshape(B_, T_, ML_HEADS, ML_HD)
        return out.reshape(B_, T_, ML_W).astype(o.dtype)

    out_l = finish(hl_f + rev(hl_b), ol)
    out_c = finish(hc_f + rev(hc_b), oc) if with_ctx_out else None
    return out_l, out_c


def ssd_chunked(x, dt, A, Bm, Cm, state):
    B, T, H, P = x.shape
    L = SSD_CHUNK
    nc = T // L

    def chunks(u):
        return jnp.moveaxis(u.reshape(B, nc, L, *u.shape[2:]), 1, 0)

    causal = jnp.tril(jnp.ones((L, L), bool))[None, :, :, None]

    def step(h, inp):
        xc, dtc, bc, cc = inp
        cum = jnp.cumsum(dtc * A, axis=1)
        decay = jnp.exp(jnp.where(causal, cum[:, :, None, :] - cum[:, None, :, :], -jnp.inf))
        w = jnp.einsum('bthn,bshn->btsh', cc, bc) * decay * dtc[:, None, :, :]
        y = (jnp.einsum('btsh,bshp->bthp', w, xc)
             + jnp.einsum('bthn,bhpn->bthp', cc, h) * jnp.exp(cum)[..., None])
        tail = jnp.exp(cum[:, -1:, :] - cum) * dtc
        h_new = h * jnp.exp(cum[:, -1, :])[..., None, None] + jnp.einsum('bsh,bshp,bshn->bhpn', tail, xc, bc)
        return h_new, y

    state, ys = lax.scan(step, state, (chunks(x), chunks(dt), chunks(Bm), chunks(Cm)))
    return state, jnp.moveaxis(ys, 0, 1).reshape(B, T, H, P)


def dwconv_centered(u, w, b):
    K = w.shape[0]
    out = lax.conv_general_dilated(u, w[:, None, :].astype(u.dtype), (1,), [(K // 2, K // 2)],
                                   dimension_numbers=('NWC', 'WIO', 'NWC'), feature_group_count=u.shape[-1])
    return out + b.astype(u.dtype)


def ssd_mixer(lat, ctx, conv_w, conv_b, dt_bias, A_log, D_skip, norm_g, with_ctx_out):
    A = -jnp.exp(A_log.astype(F32))
    rep = SSD_HEADS // SSD_GROUPS

    def prep(parts):
        z, xbc, dtr = parts
        B, T, _ = z.shape
        xbc = jax.nn.silu(dwconv_centered(xbc, conv_w, conv_b)).astype(F32)
        xs, bm, cm = jnp.split(xbc, [SSD_W, SSD_W + SSD_GROUPS * SSD_STATE], axis=-1)
        grp = lambda u: jnp.repeat(u.reshape(B, T, SSD_GROUPS, SSD_STATE), rep, axis=2)
        dt = jax.nn.softplus(dtr.astype(F32).reshape(B, T, 2, SSD_HEADS) + dt_bias.astype(F32))
        return z, xs.reshape(B, T, SSD_HEADS, SSD_HD), grp(bm), grp(cm), dt

    zl, xl, bl, cl, dtl = prep(lat)
    zc, xc, bc, cc, dtc = prep(ctx)
    B = xl.shape[0]
    zero = jnp.zeros((B, SSD_HEADS, SSD_HD, SSD_STATE), F32)
    rev = lambda u: jnp.flip(u, axis=1)
    st_f, yc_f = ssd_chunked(xc, dtc[:, :, 0], A[0], bc, cc, zero)
    _, yl_f = ssd_chunked(xl, dtl[:, :, 0], A[0], bl, cl, st_f)
    st_b, yc_b = ssd_chunked(rev(xc), rev(dtc[:, :, 1]), A[1], rev(bc), rev(cc), zero)
    _, yl_b = ssd_chunked(rev(xl), rev(dtl[:, :, 1]), A[1], rev(bl), rev(cl), st_b)

    def finish(yf, yb, xs, z):
        B_, T_ = z.shape[:2]
        y = yf + rev(yb) + D_skip.astype(F32)[:, None] * xs
        y = y.reshape(B_, T_, SSD_W) * jax.nn.silu(z.astype(F32))
        return rms_norm(y, norm_g).astype(z.dtype)

    out_l = finish(yl_f, yl_b, xl, zl)
    out_c = finish(yc_f, yc_b, xc, zc) if with_ctx_out else None
    return out_l, out_c


def mla_mixer(lat, ctx, q_norm, w_qb, kv_norm, w_kvb, gq, gk, rope, with_ctx_out):
    def prep(parts, rot):
        cq, ckv, kr = parts
        B, T, _ = cq.shape
        q = jnp.dot(rms_norm(cq, q_norm), w_qb).reshape(B, T, MLA_HEADS, MLA_QK)
        kv = jnp.dot(rms_norm(ckv, kv_norm), w_kvb).reshape(B, T, MLA_HEADS, MLA_NOPE + MLA_V)
        k_nope, v = jnp.split(kv, [MLA_NOPE], axis=-1)
        k = jnp.concatenate([k_nope, jnp.broadcast_to(kr[:, :, None, :], (B, T, MLA_HEADS, MLA_ROPE))], axis=-1)
        q = rms_norm(q, gq)
        k = rms_norm(k, gk)
        if rot is not None:
            cos, sin = rot
            q = jnp.concatenate([q[..., :MLA_NOPE], apply_rope(q[..., MLA_NOPE:], cos, sin)], axis=-1)
            k = jnp.concatenate([k[..., :MLA_NOPE], apply_rope(k[..., MLA_NOPE:], cos, sin)], axis=-1)
        t = lambda u: u.transpose(0, 2, 1, 3)
        return t(q), t(k), t(v)

    ql, kl, vl = prep(lat, rope)
    qc, kc, vc = prep(ctx, None)
    scale = MLA_QK ** -0.5
    k_all = jnp.concatenate([kl, kc], axis=2)
    v_all = jnp.concatenate([vl, vc], axis=2)
    merge = lambda o: o.transpose(0, 2, 1, 3).reshape(o.shape[0], o.shape[2], MLA_W)
    out_l = merge(blocked_attention(ql, k_all, v_all, scale))
    out_c = merge(dense_attention(qc, kc, vc, scale)) if with_ctx_out else None
    return out_l, out_c


def na_indices(rows, kh, kw):
    r = jnp.arange(rows)
    cidx = jnp.arange(GRID_W)
    r0 = jnp.clip(r - kh // 2, 0, rows - kh)
    c0 = jnp.clip(cidx - kw // 2, 0, GRID_W - kw)
    key_r = r0[:, None] + jnp.arange(kh)
    key_c = c0[:, None] + jnp.arange(kw)
    idx = key_r[:, None, :, None] * GRID_W + key_c[None, :, None, :]
    dr = key_r - r[:, None] + NA_KH - 1
    dc = key_c - cidx[:, None] + NA_KW - 1
    return idx.reshape(rows, GRID_W, kh * kw), dr, dc


def na_mixer(lat, ctx, gq, gk, rpb, with_ctx_out):
    def prep(parts):
        q, k, v = parts
        B, T, _ = q.shape
        heads = lambda u: u.reshape(B, T, NA_HEADS, NA_HD)
        t = lambda u: u.transpose(0, 2, 1, 3)
        return t(rms_norm(heads(q), gq)), t(rms_norm(heads(k), gk)), t(heads(v))

    ql, kl, vl = prep(lat)
    qc, kc, vc = prep(ctx)
    B, H, T, d = ql.shape
    rows = T // GRID_W
    kh = min(NA_KH, rows)
    nk = kh * NA_KW
    idx, dr, dc = na_indices(rows, kh, NA_KW)
    bias = rpb[:, dr[:, None, :, None], dc[None, :, None, :]]
    bias = bias.reshape(H, rows, GRID_W, nk).transpose(1, 0, 2, 3).astype(F32)
    scale = NA_HD ** -0.5
    q_rows = jnp.moveaxis(ql.reshape(B, H, rows, GRID_W, d), 2, 0)

    def row_step(args):
        q_row, idx_row, b_row = args
        k_nb = kl[:, :, idx_row]
        v_nb = vl[:, :, idx_row]
        s_nb = jnp.einsum('bhwd,bhwkd->bhwk', q_row, k_nb).astype(F32) * scale + b_row
        s_cx = jnp.einsum('bhwd,bhcd->bhwc', q_row, kc).astype(F32) * scale
        p = jax.nn.softmax(jnp.concatenate([s_nb, s_cx], axis=-1), axis=-1).astype(v_nb.dtype)
        return (jnp.einsum('bhwk,bhwkd->bhwd', p[..., :nk], v_nb)
                + jnp.einsum('bhwc,bhcd->bhwd', p[..., nk:], vc))

    o_rows = lax.map(row_step, (q_rows, idx, bias))
    merge = lambda o: o.transpose(0, 2, 1, 3).reshape(o.shape[0], o.shape[2], NA_W)
    out_l = merge(jnp.moveaxis(o_rows, 0, 2).reshape(B, H, T, d))
    out_c = merge(dense_attention(qc, kc, vc, scale)) if with_ctx_out else None
    return out_l, out_c


def swiglu(h, w1, w3, w2):
    return jnp.dot(jax.nn.silu(jnp.dot(h, w1)) * jnp.dot(h, w3), w2)


def moe_swiglu(h, router, w1, w3, w2):
    n_tok, d = h.shape
    n_exp = w1.shape[0]
    logits = jnp.dot(h, router).astype(F32)
    top_v, top_e = lax.top_k(logits, TOP_K)
    gates = jax.nn.softmax(top_v, axis=-1)
    flat_e = top_e.reshape(-1)
    flat_t = jnp.repeat(jnp.arange(n_tok, dtype=jnp.int32), TOP_K)
    flat_g = gates.reshape(-1)
    order = jnp.argsort(flat_e)
    se = flat_e[order]
    counts = jnp.bincount(flat_e, length=n_exp)
    padded = (counts + MOE_BLOCK - 1) // MOE_BLOCK * MOE_BLOCK
    pend = jnp.cumsum(padded)
    pstart = pend - padded
    sstart = jnp.cumsum(counts) - counts
    slot = pstart[se] + jnp.arange(flat_e.shape[0]) - sstart[se]
    n_blk = -(-flat_e.shape[0] // MOE_BLOCK) + n_exp
    n_slot = n_blk * MOE_BLOCK
    slot_tok = jnp.full((n_slot,), n_tok, jnp.int32).at[slot].set(flat_t[order])
    slot_gate = jnp.zeros((n_slot,), F32).at[slot].set(flat_g[order])
    blk_e = jnp.minimum(jnp.searchsorted(pend, jnp.arange(n_blk) * MOE_BLOCK, side='right'), n_exp - 1)
    h_pad = jnp.concatenate([h, jnp.zeros((1, d), h.dtype)], axis=0)
    xb = h_pad[slot_tok].reshape(n_blk, MOE_BLOCK, d)

    def expert_block(args):
        xe, e = args
        return jnp.dot(jax.nn.silu(jnp.dot(xe, w1[e])) * jnp.dot(xe, w3[e]), w2[e])

    yb = lax.map(expert_block, (xb, blk_e)).reshape(n_slot, d)
    out = jax.ops.segment_sum(yb * slot_gate[:, None].astype(yb.dtype), slot_tok, num_segments=n_tok + 1)
    return out[:n_tok]


def setup_inputs(seed: int = 0) -> dict:
    key = jax.random.key(seed)
    ks = iter(jax.random.split(key, 48))
    nrm = lambda shape, s: jax.random.normal(next(ks), shape, F32) * s
    L, D = DEPTH, D_MODEL
    nd, nm = (DEPTH + 1) // 2, DEPTH // 2
    x = nrm((BATCH, SEQ, D), 1.0)
    c = nrm((BATCH, D), 1.0)
    ctx = nrm((BATCH, CTX_LEN, D), 1.0)
    c_ctx = nrm((D,), 1.0)
    mod_w = nrm((L, D, 6 * D), 0.5 * D ** -0.5)
    mod_b = nrm((L, 6 * D), 0.02)
    norm1 = 1.0 + nrm((L, D), 0.05)
    w_in = nrm((L, D, D_IN), D ** -0.5)
    w_out = nrm((L, MIX_W, D), MIX_W ** -0.5)
    ml_i_bias = nrm((L, 2, ML_HEADS), 0.1)
    ml_f_bias = 3.0 + 3.0 * jax.random.uniform(next(ks), (L, 2, ML_HEADS), F32)
    ml_norm = 1.0 + nrm((L, ML_W), 0.05)
    ssd_conv_w = nrm((L, SSD_CONV, SSD_CONV_CH), SSD_CONV ** -0.5)
    ssd_conv_b = nrm((L, SSD_CONV_CH), 0.02)
    dt0 = jnp.exp(jax.random.uniform(next(ks), (L, 2, SSD_HEADS), F32, math.log(1e-3), math.log(1e-1)))
    ssd_dt_bias = dt0 + jnp.log(-jnp.expm1(-dt0))
    ssd_A_log = jnp.log(jax.random.uniform(next(ks), (L, 2, SSD_HEADS), F32, 1.0, 16.0))
    ssd_D = 1.0 + nrm((L, SSD_HEADS), 0.1)
    ssd_norm = 1.0 + nrm((L, SSD_W), 0.05)
    mla_q_norm = 1.0 + nrm((L, MLA_Q_RANK), 0.05)
    mla_w_qb = nrm((L, MLA_Q_RANK, MLA_HEADS * MLA_QK), MLA_Q_RANK ** -0.5)
    mla_kv_norm = 1.0 + nrm((L, MLA_KV_RANK), 0.05)
    mla_w_kvb = nrm((L, MLA_KV_RANK, MLA_HEADS * (MLA_NOPE + MLA_V)), MLA_KV_RANK ** -0.5)
    mla_gq = 1.0 + nrm((L, MLA_QK), 0.05)
    mla_gk = 1.0 + nrm((L, MLA_QK), 0.05)
    na_gq = 1.0 + nrm((L, NA_HD), 0.05)
    na_gk = 1.0 + nrm((L, NA_HD), 0.05)
    na_rpb = nrm((L, NA_HEADS, 2 * NA_KH - 1, 2 * NA_KW - 1), 0.1)
    norm2 = 1.0 + nrm((L, D), 0.05)
    ffn_w1 = nrm((nd, D, FFN_DENSE), D ** -0.5)
    ffn_w3 = nrm((nd, D, FFN_DENSE), D ** -0.5)
    ffn_w2 = nrm((nd, FFN_DENSE, D), FFN_DENSE ** -0.5)
    moe_router = nrm((nm, D, N_EXPERTS), D ** -0.5)
    moe_w1 = nrm((nm, N_EXPERTS, D, FFN_EXPERT), D ** -0.5)
    moe_w3 = nrm((nm, N_EXPERTS, D, FFN_EXPERT), D ** -0.5)
    moe_w2 = nrm((nm, N_EXPERTS, FFN_EXPERT, D), FFN_EXPERT ** -0.5)
    return {'x': x, 'c': c, 'ctx': ctx, 'c_ctx': c_ctx, 'mod_w': mod_w, 'mod_b': mod_b,
            'norm1': norm1, 'w_in': w_in, 'w_out': w_out,
            'ml_i_bias': ml_i_bias, 'ml_f_bias': ml_f_bias, 'ml_norm': ml_norm,
            'ssd_conv_w': ssd_conv_w, 'ssd_conv_b': ssd_conv_b, 'ssd_dt_bias': ssd_dt_bias,
            'ssd_A_log': ssd_A_log, 'ssd_D': ssd_D, 'ssd_norm': ssd_norm,
            'mla_q_norm': mla_q_norm, 'mla_w_qb': mla_w_qb, 'mla_kv_norm': mla_kv_norm,
            'mla_w_kvb': mla_w_kvb, 'mla_gq': mla_gq, 'mla_gk': mla_gk,
            'na_gq': na_gq, 'na_gk': na_gk, 'na_rpb': na_rpb, 'norm2': norm2,
            'ffn_w1': ffn_w1, 'ffn_w3': ffn_w3, 'ffn_w2': ffn_w2,
            'moe_router': moe_router, 'moe_w1': moe_w1, 'moe_w3': moe_w3, 'moe_w2': moe_w2}


def reference(x, c, ctx, c_ctx, mod_w, mod_b, norm1, w_in, w_out,
              ml_i_bias, ml_f_bias, ml_norm,
              ssd_conv_w, ssd_conv_b, ssd_dt_bias, ssd_A_log, ssd_D, ssd_norm,
              mla_q_norm, mla_w_qb, mla_kv_norm, mla_w_kvb, mla_gq, mla_gk,
              na_gq, na_gk, na_rpb, norm2,
              ffn_w1, ffn_w3, ffn_w2, moe_router, moe_w1, moe_w3, moe_w2):
    B, T, D = x.shape
    n_ctx = ctx.shape[1]
    rope = axial_rope(T, MLA_ROPE)
    silu_c = jax.nn.silu(c)
    silu_cc = jax.nn.silu(c_ctx)
    xc = ctx
    for l in range(DEPTH):
        with_ctx = l < DEPTH - 1
        mod = (jnp.dot(silu_c, mod_w[l]) + mod_b[l])[:, None, :]
        modc = (jnp.dot(silu_cc, mod_w[l]) + mod_b[l])[None, None, :]
        sh1, sc1, g1, sh2, sc2, g2 = jnp.split(mod, 6, axis=-1)
        sh1c, sc1c, g1c, sh2c, sc2c, g2c = jnp.split(modc, 6, axis=-1)

        h = rms_norm(x, norm1[l]) * (1.0 + sc1) + sh1
        hc = rms_norm(xc, norm1[l]) * (1.0 + sc1c) + sh1c
        pl = split_cols(jnp.dot(h, w_in[l]))
        pc = split_cols(jnp.dot(hc, w_in[l]))
        ml_l, ml_c = mlstm_mixer(pl[0:6], pc[0:6], ml_i_bias[l], ml_f_bias[l], ml_norm[l], with_ctx)
        ss_l, ss_c = ssd_mixer(pl[6:9], pc[6:9], ssd_conv_w[l], ssd_conv_b[l], ssd_dt_bias[l],
                               ssd_A_log[l], ssd_D[l], ssd_norm[l], with_ctx)
        la_l, la_c = mla_mixer(pl[9:12], pc[9:12], mla_q_norm[l], mla_w_qb[l], mla_kv_norm[l],
                               mla_w_kvb[l], mla_gq[l], mla_gk[l], rope, with_ctx)
        na_l, na_c = na_mixer(pl[12:15], pc[12:15], na_gq[l], na_gk[l], na_rpb[l], with_ctx)
        y = jnp.dot(jnp.concatenate([ml_l, ss_l, la_l, na_l], axis=-1), w_out[l])
        x = x + g1 * y
        if with_ctx:
            yc = jnp.dot(jnp.concatenate([ml_c, ss_c, la_c, na_c], axis=-1), w_out[l])
            xc = xc + g1c * yc

        h2 = rms_norm(x, norm2[l]) * (1.0 + sc2) + sh2
        tokens = h2.reshape(B * T, D)
        if with_ctx:
            h2c = rms_norm(xc, norm2[l]) * (1.0 + sc2c) + sh2c
            tokens = jnp.concatenate([tokens, h2c.reshape(B * n_ctx, D)], axis=0)
        if l % 2 == 0:
            f = swiglu(tokens, ffn_w1[l // 2], ffn_w3[l // 2], ffn_w2[l // 2])
        else:
            f = moe_swiglu(tokens, moe_router[l // 2], moe_w1[l // 2], moe_w3[l // 2], moe_w2[l // 2])
        x = x + g2 * f[:B * T].reshape(B, T, D)
        if with_ctx:
            xc = xc + g2c * f[B * T:].reshape(B, n_ctx, D)
    return x
```

```python
import contextlib
import numpy as np
import concourse.bass as bass
import concourse.mybir as mybir
from concourse.bass_utils import run_bass_kernel_spmd

F32 = mybir.dt.float32
BF16 = mybir.dt.bfloat16
AF = mybir.ActivationFunctionType
ALU = mybir.AluOpType
AX = mybir.AxisListType
NCORES = 8


class Trk:
    __slots__ = ("w", "r")

    def __init__(self):
        self.w = {}
        self.r = {}


class V:
    __slots__ = ("ap", "trks")

    def __init__(self, ap, trks):
        self.ap = ap
        self.trks = trks

    def __getitem__(self, idx):
        return V(self.ap[idx], self.trks)

    def bc(self, shape):
        return V(self.ap.to_broadcast(shape), self.trks)


class Buf:
    def __init__(self, t, nreg=1):
        self.t = t
        self.regs = [Trk() for _ in range(nreg)]

    def __getitem__(self, idx):
        return V(self.t[idx], self.regs)

    def reg(self, i, idx=None):
        ap = self.t[:] if idx is None else self.t[idx]
        return V(ap, [self.regs[i]])


class Eng:
    def __init__(self, k, name, e):
        self.name = name
        self.e = e
        self.sem = k.newsem("c_" + name)
        self.cnt = 0
        self.waited = {}
        self.dsems = []
        self.dvals = []
        self.dnext = 0


class KB:
    def __init__(self, same_engine_sync=True):
        self.nc = bass.Bass("TRN2", target_bir_lowering=False)
        self.es = contextlib.ExitStack()
        self.es.__enter__()
        self.lp = self.nc.allow_low_precision("bf16 matmul operands, fp32 accumulate")
        self.lp.__enter__()
        self.ncd = self.nc.allow_non_contiguous_dma("tiny per-partition parameter loads")
        self.ncd.__enter__()
        self.nsem = 0
        self.allsems = []
        self.same = same_engine_sync
        nc = self.nc
        self.pe = Eng(self, "pe", nc.tensor)
        self.act = Eng(self, "act", nc.scalar)
        self.dve = Eng(self, "dve", nc.vector)
        self.pool = Eng(self, "pool", nc.gpsimd)
        self.sp = Eng(self, "sp", nc.sync)
        self.engs = [self.pe, self.act, self.dve, self.pool, self.sp]
        for q in (self.sp, self.pool, self.act):
            for i in range(8):
                q.dsems.append(self.newsem("d_%s%d" % (q.name, i)))
                q.dvals.append(0)
        self.nid = 0

    def newsem(self, name):
        s = self.es.enter_context(self.nc.semaphore(name))
        self.allsems.append([s, 0])
        return s

    def dram(self, name, shape, dtype, kind):
        t = self.nc.dram_tensor(name, list(shape), dtype, kind=kind).ap()
        return Buf(t)

    def din(self, name, shape, dtype=F32):
        return self.dram(name, shape, dtype, "ExternalInput")

    def dout(self, name, shape, dtype=F32):
        return self.dram(name, shape, dtype, "ExternalOutput")

    def sb(self, shape, dtype=F32, nreg=1, name=None, stack=None):
        self.nid += 1
        name = name or ("sb%d" % self.nid)
        t = (stack or self.es).enter_context(self.nc.sbuf_tensor(name, list(shape), dtype))
        return Buf(t, nreg)

    def ps(self, shape, dtype=F32, nreg=1, name=None, stack=None):
        self.nid += 1
        name = name or ("ps%d" % self.nid)
        t = (stack or self.es).enter_context(self.nc.psum_tensor(name, list(shape), dtype))
        return Buf(t, nreg)

    @contextlib.contextmanager
    def scope(self):
        st = contextlib.ExitStack()
        with st:
            yield st
            self.barrier()

    def _semval(self, sem):
        for sv in self.allsems:
            if sv[0] is sem:
                return sv
        raise KeyError

    def _wait(self, eng, ev):
        sem, val = ev
        if eng.waited.get(id(sem), 0) >= val:
            return
        if sem is eng.sem and not self.same:
            return
        if sem is eng.sem and eng is self.pe:
            return
        eng.e.wait_ge(sem, val)
        eng.waited[id(sem)] = val

    def emit(self, eng, fn, reads, writes, dma=False, waw=True):
        evs = []
        for v in reads:
            for t in v.trks:
                evs.extend(t.w.values())
        for v in writes:
            for t in v.trks:
                if waw:
                    evs.extend(t.w.values())
                evs.extend(t.r.values())
        if dma:
            i = eng.dnext
            eng.dnext = (i + 1) % len(eng.dsems)
            sem = eng.dsems[i]
            if eng.dvals[i] > 0:
                evs.append((sem, eng.dvals[i]))
        for ev in evs:
            self._wait(eng, ev)
        ins = fn()
        if dma:
            eng.dvals[i] += 16
            ins.then_inc(sem, 16)
            ev = (sem, eng.dvals[i])
            self._semval(sem)[1] = eng.dvals[i]
        else:
            eng.cnt += 1
            ins.then_inc(eng.sem, 1)
            ev = (eng.sem, eng.cnt)
            self._semval(eng.sem)[1] = eng.cnt
        for v in reads:
            for t in v.trks:
                t.r[id(ev[0])] = ev
        for v in writes:
            for t in v.trks:
                if waw:
                    t.w = {}
                    t.r = {}
                t.w[id(ev[0])] = ev
        return ins

    def barrier(self):
        for eng in self.engs:
            for sem, val in self.allsems:
                if val > 0:
                    self._wait_force(eng, (sem, val))

    def _wait_force(self, eng, ev):
        sem, val = ev
        if eng.waited.get(id(sem), 0) >= val:
            return
        eng.e.wait_ge(sem, val)
        eng.waited[id(sem)] = val

    def finish(self):
        self.barrier()
        self.ncd.__exit__(None, None, None)
        self.lp.__exit__(None, None, None)
        self.es.__exit__(None, None, None)
        return self.nc

    def _E(self, eng):
        return {"pe": self.pe, "act": self.act, "dve": self.dve, "pool": self.pool, "sp": self.sp}[eng]

    def dma(self, out, in_, q="sp", waw=True):
        e = self._E(q)
        return self.emit(e, lambda: e.e.dma_start(out=out.ap, in_=in_.ap), [in_], [out], dma=True, waw=waw)

    def mm(self, out, lhsT, rhs, start=True, stop=True):
        return self.emit(self.pe, lambda: self.nc.tensor.matmul(out.ap, lhsT.ap, rhs.ap, start=start, stop=stop),
                         [lhsT, rhs], [out])

    def transpose(self, out, in_, ident):
        return self.emit(self.pe, lambda: self.nc.tensor.transpose(out.ap, in_.ap, ident.ap), [in_, ident], [out])

    def actf(self, out, in_, func, bias=None, scale=1.0, accum=None):
        reads = [in_]
        kw = {}
        if isinstance(bias, V):
            reads.append(bias)
            kw["bias"] = bias.ap
        elif bias is not None:
            kw["bias"] = float(bias)
        if isinstance(scale, V):
            reads.append(scale)
            kw["scale"] = scale.ap
        else:
            kw["scale"] = float(scale)
        writes = [out]
        if accum is not None:
            writes.append(accum)
            kw["accum_out"] = accum.ap
        return self.emit(self.act, lambda: self.nc.scalar.activation(out=out.ap, in_=in_.ap, func=func, **kw),
                         reads, writes)

    def _vec(self, eng):
        e = self._E(eng)
        return e, e.e

    def tt(self, out, in0, in1, op, eng="dve"):
        e, x = self._vec(eng)
        return self.emit(e, lambda: x.tensor_tensor(out=out.ap, in0=in0.ap, in1=in1.ap, op=op), [in0, in1], [out])

    def ts(self, out, in0, s1, op0, s2=None, op1=None, eng="dve", accum=None):
        e, x = self._vec(eng)
        reads = [in0]
        a1 = s1.ap if isinstance(s1, V) else float(s1)
        if isinstance(s1, V):
            reads.append(s1)
        a2 = None
        if s2 is not None:
            a2 = s2.ap if isinstance(s2, V) else float(s2)
            if isinstance(s2, V):
                reads.append(s2)
        kw = {}
        if op1 is not None:
            kw["op1"] = op1
        writes = [out]
        if accum is not None:
            kw["accum_out"] = accum.ap
            writes.append(accum)
        return self.emit(e, lambda: x.tensor_scalar(out=out.ap, in0=in0.ap, scalar1=a1, scalar2=a2, op0=op0, **kw),
                         reads, writes)

    def stt(self, out, in0, scalar, in1, op0, op1, eng="dve"):
        e, x = self._vec(eng)
        reads = [in0, in1]
        sc = scalar.ap if isinstance(scalar, V) else float(scalar)
        if isinstance(scalar, V):
            reads.append(scalar)
        return self.emit(e, lambda: x.scalar_tensor_tensor(out=out.ap, in0=in0.ap, scalar=sc, in1=in1.ap,
                                                           op0=op0, op1=op1), reads, [out])

    def copy(self, out, in_, eng="dve"):
        if eng == "act":
            return self.actf(out, in_, AF.Copy)
        e, x = self._vec(eng)
        return self.emit(e, lambda: x.tensor_copy(out=out.ap, in_=in_.ap), [in_], [out])

    def recip(self, out, in_):
        return self.emit(self.dve, lambda: self.nc.vector.reciprocal(out=out.ap, in_=in_.ap), [in_], [out])

    def reduce(self, out, in_, op, axis=AX.X, eng="dve"):
        e, x = self._vec(eng)
        return self.emit(e, lambda: x.tensor_reduce(out=out.ap, in_=in_.ap, axis=axis, op=op), [in_], [out])

    def memset(self, out, val, eng="dve"):
        e, x = self._vec(eng)
        return self.emit(e, lambda: x.memset(out.ap, val), [], [out])


def run(nc, in_maps):
    res = run_bass_kernel_spmd(nc, in_maps, core_ids=list(range(len(in_maps))))
    return res.results


def _v_rr(self, pattern, **kw):
    return V(self.ap.rearrange(pattern, **kw), self.trks)


V.rr = _v_rr


def _v_unsq(self, axis):
    return V(self.ap.unsqueeze(axis), self.trks)


V.unsq = _v_unsq


D = 2048
DIN = 5824
EPS = 1e-6
TILES = [(0, 512, 0), (512, 512, 0), (1024, 128, 1)]


def norm_mod(k, xT, hT, ones, gT, scT, shT, pss, tmp, tiles=TILES, stack=None):
    A = k.sb([128, 16, 2], stack=stack)
    for c in range(2):
        k.ts(A[:, :, c], scT[:, :, c], 1.0, ALU.add)
        k.tt(A[:, :, c], A[:, :, c], gT[:, :], ALU.mult)
    for (t0, n, col) in tiles:
        ps = pss[0]
        for kc in range(16):
            sq = tmp[kc % 2]
            k.actf(sq[:, 0:n], xT[:, kc, t0:t0 + n], AF.Square)
            k.mm(ps[:, 0:n], ones[:, :], sq[:, 0:n], start=(kc == 0), stop=(kc == 15))
        rs = tmp[2]
        k.ts(rs[:, 0:n], ps[:, 0:n], 1.0 / D, ALU.mult, EPS, ALU.add)
        k.actf(rs[:, 0:n], rs[:, 0:n], AF.Sqrt)
        k.recip(rs[:, 0:n], rs[:, 0:n])
        for kc in range(16):
            t = tmp[kc % 2]
            k.tt(t[:, 0:n], xT[:, kc, t0:t0 + n], rs[:, 0:n], ALU.mult)
            k.ts(hT[:, kc, t0:t0 + n], t[:, 0:n], A[:, kc, col:col + 1], ALU.mult, shT[:, kc, col:col + 1], ALU.add,
                 eng="pool" if kc % 2 else "dve")


def build_k1(NT=1152):
    k = KB()
    xTd = k.din("xT", [128, 16, NT])
    gTd = k.din("gT", [128, 16]); scTd = k.din("scT", [128, 16, 2]); shTd = k.din("shT", [128, 16, 2])
    w = k.din("w", [D, DIN])
    p = k.dout("p", [NT, DIN])
    xT = k.sb([128, 16, NT]); hT = k.sb([128, 16, NT], BF16)
    gT = k.sb([128, 16]); scT = k.sb([128, 16, 2]); shT = k.sb([128, 16, 2])
    ones = k.sb([128, 128]); k.memset(ones[:], 1.0)
    tmp = [k.sb([128, 512]) for _ in range(3)]
    pss = [k.ps([128, 512]) for _ in range(4)]
    for q in range(4):
        k.dma(xT[:, q * 4:(q + 1) * 4, :], xTd[:, q * 4:(q + 1) * 4, :])
    k.dma(gT[:], gTd[:]); k.dma(scT[:], scTd[:]); k.dma(shT[:], shTd[:])
    wsb = [k.sb([128, 16, 512], BF16) for _ in range(2)]
    wv = w[:].rr("(kc p) n -> p kc n", p=128)
    groups = [(c0, min(512, DIN - c0)) for c0 in range(0, DIN, 512)]

    def loadw(gi):
        c0, cn = groups[gi]
        for q in range(4):
            k.dma(wsb[gi % 2][:, q * 4:(q + 1) * 4, 0:cn], wv[:, q * 4:(q + 1) * 4, c0:c0 + cn], q="pool")
    loadw(0)
    norm_mod(k, xT, hT, ones, gT, scT, shT, pss, tmp)
    stg = [k.sb([128, 512]) for _ in range(3)]
    it = 0
    for gi, (c0, cn) in enumerate(groups):
        if gi + 1 < len(groups):
            loadw(gi + 1)
        for tb in range(NT // 128):
            ps = pss[it % 4]; st = stg[it % 3]
            for kc in range(16):
                k.mm(ps[:, 0:cn], hT[:, kc, tb * 128:(tb + 1) * 128], wsb[gi % 2][:, kc, 0:cn],
                     start=(kc == 0), stop=(kc == 15))
            if it % 2:
                k.copy(st[:, 0:cn], ps[:, 0:cn], eng="dve")
            else:
                k.copy(st[:, 0:cn], ps[:, 0:cn], eng="act")
            k.dma(p[tb * 128:(tb + 1) * 128, c0:c0 + cn], st[:, 0:cn], q="sp")
            it += 1
    return k.finish()


def to_fm(a):
    return np.ascontiguousarray(a.T.reshape(16, 128, a.shape[0]).transpose(1, 0, 2))


def vec_fm(v):
    return np.ascontiguousarray(v.reshape(16, 128).T)


def core_tokens(x, xc, i):
    b, s = i // 2, i % 2
    return np.concatenate([x[b, s * 1024:(s + 1) * 1024], xc[b, s * 128:(s + 1) * 128]], 0)


def mod_cols(mod_l, which, b):
    sl = mod_l[:, which * D:(which + 1) * D]
    return np.ascontiguousarray(np.stack([vec_fm(sl[b]), vec_fm(sl[4])], -1))


def run_k1(xT_list, mod_l, norm1_l, w_in_l, nc=None):
    maps = []
    for i in range(NCORES):
        b = i // 2
        maps.append({"xT": xT_list[i], "gT": vec_fm(norm1_l), "scT": mod_cols(mod_l, 1, b),
                     "shT": mod_cols(mod_l, 0, b), "w": w_in_l})
    res = run(nc or build_k1(), maps)
    return [r["p"] for r in res]


D = 2048
EPS = 1e-6


def ffn(k, h2T, tiles, w1d, w3d, w2d, F, acc_fn, pss, FG=256, stack=None, gate=None):
    NT = sum(n for _, n, _ in tiles)
    nfc = FG // 128
    w1s = [k.sb([128, 16, FG], BF16, stack=stack) for _ in range(2)]
    w3s = [k.sb([128, 16, FG], BF16, stack=stack) for _ in range(2)]
    w2s = [k.sb([128, nfc, D], BF16, stack=stack) for _ in range(2)]
    aT = [k.sb([128, nfc, NT], BF16, stack=stack) for _ in range(2)]
    tmp = [k.sb([128, 512], stack=stack) for _ in range(2)]
    w1v = w1d.rr("(kc p) f -> p kc f", p=128)
    w3v = w3d.rr("(kc p) f -> p kc f", p=128)
    w2v = w2d.rr("(fc p) d -> p fc d", p=128)
    ng = F // FG

    def loadA(g):
        f0 = g * FG
        for q in range(2):
            k.dma(w1s[g % 2][:, q * 8:(q + 1) * 8, :], w1v[:, q * 8:(q + 1) * 8, f0:f0 + FG], q="pool")
            k.dma(w3s[g % 2][:, q * 8:(q + 1) * 8, :], w3v[:, q * 8:(q + 1) * 8, f0:f0 + FG], q="pool")

    def loadB(g):
        k.dma(w2s[g % 2][:, :, :], w2v[:, g * nfc:(g + 1) * nfc, :], q="pool")
    cnt = [0, 0]

    def up(g, fc, t0, n):
        it = cnt[0]; cnt[0] += 1
        p1 = pss[(it % 2) * 2]; p3 = pss[(it % 2) * 2 + 1]; tm = tmp[it % 2]
        for kc in range(16):
            k.mm(p1[:, 0:n], w1s[g % 2][:, kc, fc * 128:(fc + 1) * 128], h2T[:, kc, t0:t0 + n], start=(kc == 0), stop=(kc == 15))
        for kc in range(16):
            k.mm(p3[:, 0:n], w3s[g % 2][:, kc, fc * 128:(fc + 1) * 128], h2T[:, kc, t0:t0 + n], start=(kc == 0), stop=(kc == 15))
        k.actf(tm[:, 0:n], p1[:, 0:n], AF.Silu)
        if gate is not None:
            k.tt(tm[:, 0:n], tm[:, 0:n], gate[:, t0:t0 + n], ALU.mult, eng="pool")
        k.tt(aT[g % 2][:, fc, t0:t0 + n], tm[:, 0:n], p3[:, 0:n], ALU.mult)

    def down(g, dc, t0, n):
        jt = cnt[1]; cnt[1] += 1
        po = pss[4 + jt % 4]
        for fc in range(nfc):
            k.mm(po[:, 0:n], w2s[g % 2][:, fc, dc * 128:(dc + 1) * 128], aT[g % 2][:, fc, t0:t0 + n],
                 start=(fc == 0), stop=(fc == nfc - 1))
        acc_fn(po[:, 0:n], dc, t0, n)
    ups = [(fc, t0, n) for fc in range(nfc) for (t0, n, _) in tiles]
    downs = [(dc, t0, n) for dc in range(16) for (t0, n, _) in tiles]
    loadA(0); loadB(0)
    if ng > 1:
        loadA(1)
    for u in ups:
        up(0, *u)
    per = -(-len(downs) // len(ups))
    for g in range(ng):
        if g + 2 < ng:
            loadA(g + 2)
        if g + 1 < ng:
            loadB(g + 1)
        di = 0
        for u in (ups if g + 1 < ng else []):
            up(g + 1, *u)
            for dd in downs[di:di + per]:
                down(g, *dd)
            di += per
        for dd in downs[di:]:
            down(g, *dd)


def ffn_simple(k, h2T, tiles, w1d, w3d, w2d, F, acc_fn, pss, FG=512, stack=None, gate=None):
    NT = sum(n for _, n, _ in tiles)
    nfc = FG // 128
    w1s = [k.sb([128, 16, FG], BF16, stack=stack) for _ in range(2)]
    w3s = [k.sb([128, 16, FG], BF16, stack=stack) for _ in range(2)]
    w2s = k.sb([128, nfc, D], BF16, stack=stack)
    a = k.sb([128, nfc, NT], BF16, stack=stack)
    tmp = [k.sb([128, 512], stack=stack) for _ in range(2)]
    w1v = w1d.rr("(kc p) f -> p kc f", p=128)
    w3v = w3d.rr("(kc p) f -> p kc f", p=128)
    w2v = w2d.rr("(fc p) d -> p fc d", p=128)
    ng = F // FG

    def loadA(g):
        f0 = g * FG
        for q in range(2):
            k.dma(w1s[g % 2][:, q * 8:(q + 1) * 8, :], w1v[:, q * 8:(q + 1) * 8, f0:f0 + FG], q="pool")
            k.dma(w3s[g % 2][:, q * 8:(q + 1) * 8, :], w3v[:, q * 8:(q + 1) * 8, f0:f0 + FG], q="pool")

    def loadB(g):
        for q in range(nfc // 2):
            k.dma(w2s[:, q * 2:(q + 1) * 2, :], w2v[:, g * nfc + q * 2:g * nfc + (q + 1) * 2, :], q="pool")
    loadA(0); loadB(0)
    it = 0
    jt = 0
    for g in range(ng):
        if g + 1 < ng:
            loadA(g + 1)
        for (t0, n, _) in tiles:
            for fc in range(nfc):
                p1 = pss[(it % 2) * 2]; p3 = pss[(it % 2) * 2 + 1]; tm = tmp[it % 2]
                for kc in range(16):
                    k.mm(p1[:, 0:n], w1s[g % 2][:, kc, fc * 128:(fc + 1) * 128], h2T[:, kc, t0:t0 + n],
                         start=(kc == 0), stop=(kc == 15))
                for kc in range(16):
                    k.mm(p3[:, 0:n], w3s[g % 2][:, kc, fc * 128:(fc + 1) * 128], h2T[:, kc, t0:t0 + n],
                         start=(kc == 0), stop=(kc == 15))
                k.actf(tm[:, 0:n], p1[:, 0:n], AF.Silu)
                if gate is not None:
                    k.tt(tm[:, 0:n], tm[:, 0:n], gate[:, t0:t0 + n], ALU.mult, eng="pool")
                k.tt(a[:, fc, t0:t0 + n], tm[:, 0:n], p3[:, 0:n], ALU.mult)
                it += 1
        for (t0, n, _) in tiles:
            for dc in range(16):
                po = pss[4 + jt % 4]
                for fc in range(nfc):
                    k.mm(po[:, 0:n], w2s[:, fc, dc * 128:(dc + 1) * 128], a[:, fc, t0:t0 + n],
                         start=(fc == 0), stop=(fc == nfc - 1))
                acc_fn(po[:, 0:n], dc, t0, n)
                jt += 1
        if g + 1 < ng:
            loadB(g + 1)


def build_k3(NT=1152, F=5632, tiles=TILES):
    k = KB()
    mixd = k.din("mixT", [128, 16, NT])
    xTd = k.din("xT", [128, 16, NT])
    wo = k.din("w_out", [D, D])
    g1d = k.din("g1T", [128, 16, 2]); g2d = k.din("g2T", [128, 16, 2])
    n2d = k.din("n2T", [128, 16]); sc2d = k.din("sc2T", [128, 16, 2]); sh2d = k.din("sh2T", [128, 16, 2])
    gsd = k.din("gsT", [128, 4])
    w1d = k.din("w1", [D, F]); w3d = k.din("w3", [D, F]); w2d = k.din("w2", [F, D])
    xo = k.dout("xo", [128, 16, NT])
    xT = k.sb([128, 16, NT])
    g1 = k.sb([128, 16, 2]); g2 = k.sb([128, 16, 2]); n2 = k.sb([128, 16]); sc2 = k.sb([128, 16, 2])
    sh2 = k.sb([128, 16, 2]); gs = k.sb([128, 4])
    ones = k.sb([128, 128]); k.memset(ones[:], 1.0)
    pss = [k.ps([128, 512]) for _ in range(8)]
    for q in range(4):
        k.dma(xT[:, q * 4:(q + 1) * 4, :], xTd[:, q * 4:(q + 1) * 4, :])
    for a, b in ((g1, g1d), (g2, g2d), (n2, n2d), (sc2, sc2d), (sh2, sh2d), (gs, gsd)):
        k.dma(a[:], b[:])
    with k.scope() as st:
        mixb = k.sb([128, 16, NT], BF16, stack=st)
        ssd = k.sb([128, 4, NT], stack=st)
        tmp = [k.sb([128, 512], stack=st) for _ in range(3)]
        wos = [k.sb([128, 16, 512], BF16, stack=st) for _ in range(2)]
        wov = wo[:].rr("(kc p) n -> p kc n", p=128)

        def loadwo(g):
            for q in range(2):
                k.dma(wos[g % 2][:, q * 8:(q + 1) * 8, :], wov[:, q * 8:(q + 1) * 8, g * 512:(g + 1) * 512], q="pool")
        k.dma(mixb[:, 0:4, :], mixd[:, 0:4, :], q="pool")
        k.dma(mixb[:, 8:12, :], mixd[:, 8:12, :], q="pool")
        k.dma(mixb[:, 12:16, :], mixd[:, 12:16, :], q="pool")
        k.dma(ssd[:], mixd[:, 4:8, :])
        loadwo(0)
        for (t0, n, _) in tiles:
            ps = pss[0]
            for c in range(4):
                sq = tmp[c % 2]
                k.actf(sq[:, 0:n], ssd[:, c, t0:t0 + n], AF.Square)
                k.mm(ps[:, 0:n], ones[:, :], sq[:, 0:n], start=(c == 0), stop=(c == 3))
            rs = tmp[2]
            k.ts(rs[:, 0:n], ps[:, 0:n], 1.0 / 512, ALU.mult, EPS, ALU.add)
            k.actf(rs[:, 0:n], rs[:, 0:n], AF.Sqrt)
            k.recip(rs[:, 0:n], rs[:, 0:n])
            for c in range(4):
                k.stt(mixb[:, 4 + c, t0:t0 + n], ssd[:, c, t0:t0 + n], gs[:, c:c + 1], rs[:, 0:n], ALU.mult, ALU.mult)
        jt = 0
        for g in range(4):
            if g + 1 < 4:
                loadwo(g + 1)
            for dl in range(4):
                dc = g * 4 + dl
                for (t0, n, col) in tiles:
                    po = pss[4 + jt % 4]
                    for kc in range(16):
                        k.mm(po[:, 0:n], wos[g % 2][:, kc, dl * 128:(dl + 1) * 128], mixb[:, kc, t0:t0 + n],
                             start=(kc == 0), stop=(kc == 15))
                    k.stt(xT[:, dc, t0:t0 + n], po[:, 0:n], g1[:, dc, col:col + 1], xT[:, dc, t0:t0 + n],
                          ALU.mult, ALU.add)
                    jt += 1
    h2T = k.sb([128, 16, NT], BF16)
    with k.scope() as st:
        tmp = [k.sb([128, 512], stack=st) for _ in range(3)]
        norm_mod(k, xT, h2T, ones, n2, sc2, sh2, pss, tmp, tiles, stack=st)
    colof = {t0: col for (t0, n, col) in tiles}

    def acc(po, dc, t0, n):
        col = colof[t0]
        k.stt(xT[:, dc, t0:t0 + n], po, g2[:, dc, col:col + 1], xT[:, dc, t0:t0 + n], ALU.mult, ALU.add)
    ffn(k, h2T, tiles, w1d[:], w3d[:], w2d[:], F, acc, pss)
    for q in range(4):
        k.dma(xo[:, q * 4:(q + 1) * 4, :], xT[:, q * 4:(q + 1) * 4, :])
    return k.finish()


def run_k3(mixT_list, xT_list, mod_l, l, z, nc=None):
    maps = []
    for i in range(NCORES):
        b = i // 2
        maps.append({"mixT": mixT_list[i], "xT": xT_list[i], "w_out": np.ascontiguousarray(z['w_out'][l]),
                     "g1T": mod_cols(mod_l, 2, b), "g2T": mod_cols(mod_l, 5, b), "n2T": vec_fm(z['norm2'][l]),
                     "sc2T": mod_cols(mod_l, 4, b), "sh2T": mod_cols(mod_l, 3, b),
                     "gsT": np.ascontiguousarray(z['ssd_norm'][l].reshape(4, 128).T),
                     "w1": np.ascontiguousarray(z['ffn_w1'][0]), "w3": np.ascontiguousarray(z['ffn_w3'][0]),
                     "w2": np.ascontiguousarray(z['ffn_w2'][0])})
    res = run(nc or build_k3(), maps)
    return [r["xo"] for r in res]


S = 2304
TT = [(0, 512), (512, 512), (1024, 512), (1536, 512), (2048, 256)]
EPS = 1e-6


def headnorm_fm(k, srcT, dstT, h, gcol, ones, ps, tmp, extra_scale=1.0, nfeat=128):
    for (t0, n) in TT:
        sq = tmp[0]
        k.actf(sq[:, 0:n], srcT[:, h, t0:t0 + n], AF.Square)
        k.mm(ps[:, 0:n], ones[:, :], sq[:, 0:n])
        rs = tmp[1]
        k.ts(rs[:, 0:n], ps[:, 0:n], 1.0 / nfeat, ALU.mult, EPS, ALU.add)
        k.actf(rs[:, 0:n], rs[:, 0:n], AF.Sqrt)
        k.recip(rs[:, 0:n], rs[:, 0:n])
        k.tt(rs[:, 0:n], srcT[:, h, t0:t0 + n], rs[:, 0:n], ALU.mult)
        k.ts(dstT[:, h, t0:t0 + n], rs[:, 0:n], gcol, ALU.mult, extra_scale, ALU.mult)


def build_na():
    k = KB()
    qd = k.din("qT", [128, 2, S]); kd = k.din("kT", [128, 2, S])
    vAd = k.din("vA", [128, 18, 256]); vBd = k.din("vB", [128, 15, 256])
    gqd = k.din("gq", [128, 1]); gkd = k.din("gk", [128, 1])
    btd = k.din("bt", [128, 2, 14, 64])
    od = k.dout("oT", [128, 2, S])
    qf = k.sb([128, 2, S]); kf = k.sb([128, 2, S])
    qb = k.sb([128, 2, S], BF16); kb = k.sb([128, 2, S], BF16)
    vA = k.sb([128, 18, 256], BF16); vB = k.sb([128, 15, 256], BF16)
    gq = k.sb([128, 1]); gk = k.sb([128, 1]); bt = k.sb([128, 2, 14, 64])
    oT = k.sb([128, 2, S])
    ones = k.sb([128, 128]); k.memset(ones[:], 1.0)
    onesb = k.sb([128, 128], BF16); k.memset(onesb[:], 1.0)
    tmp = [k.sb([128, 512]) for _ in range(3)]
    pss = [k.ps([128, 512]) for _ in range(8)]
    k.dma(qf[:], qd[:]); k.dma(kf[:], kd[:])
    k.dma(vA[:], vAd[:], q="pool"); k.dma(vB[:], vBd[:], q="pool")
    k.dma(gq[:], gqd[:]); k.dma(gk[:], gkd[:]); k.dma(bt[:], btd[:])
    scale = 128 ** -0.5
    for h in range(2):
        headnorm_fm(k, qf, qb, h, gq[:, 0:1], ones, pss[0], tmp, extra_scale=scale)
        headnorm_fm(k, kf, kb, h, gk[:, 0:1], ones, pss[1], tmp)
    pT = [k.sb([128, 384], BF16) for _ in range(3)]
    it = 0
    for h in range(2):
        for rg in range(4):
            po = pss[4 + (it % 2) * 2]; pd = pss[5 + (it % 2) * 2]
            it += 1
            for rl in range(8):
                r = rg * 8 + rl
                r0 = min(max(r - 4, 0), 24)
                q0 = 256 + r * 64
                ps_s = pss[r % 4]
                vblks = []
                for j in range(4):
                    kt0 = 256 + r0 * 64 + 128 * j
                    k.mm(ps_s[:, j * 64:(j + 1) * 64], kb[:, h, kt0:kt0 + 128], qb[:, h, q0:q0 + 64])
                    if r0 % 2 == 0:
                        vblks.append(vA[:, 2 + r0 // 2 + j, h * 128:(h + 1) * 128])
                    else:
                        vblks.append(vB[:, (r0 - 1) // 2 + j, h * 128:(h + 1) * 128])
                for j in range(2):
                    k.mm(ps_s[:, 256 + j * 64:256 + (j + 1) * 64], kb[:, h, j * 128:(j + 1) * 128], qb[:, h, q0:q0 + 64])
                    vblks.append(vA[:, j, h * 128:(h + 1) * 128])
                m0 = r0 - r + 7
                tm = tmp[r % 2]
                k.tt(tm[:, 0:256].rr("p (j c) -> p j c", j=4), ps_s[:, 0:256].rr("p (j c) -> p j c", j=4),
                     bt[:, h, m0:m0 + 7:2, :], ALU.add)
                p = pT[r % 3]
                k.actf(p[:, 0:256], tm[:, 0:256], AF.Exp)
                k.actf(p[:, 256:384], ps_s[:, 256:384], AF.Exp)
                for j in range(6):
                    k.mm(po[:, rl * 64:(rl + 1) * 64], vblks[j], p[:, j * 64:(j + 1) * 64], start=(j == 0), stop=(j == 5))
                for j in range(6):
                    k.mm(pd[:, rl * 64:(rl + 1) * 64], onesb[:, :], p[:, j * 64:(j + 1) * 64], start=(j == 0), stop=(j == 5))
            rd = tmp[2]
            k.recip(rd[:, :], pd[:, :])
            k.tt(oT[:, h, 256 + rg * 512:256 + (rg + 1) * 512], po[:, :], rd[:, :], ALU.mult)
        ps_s = pss[0]; po = pss[4]; pd = pss[5]
        p = k.sb([128, 512], BF16)
        for j in range(2):
            k.mm(ps_s[:, j * 256:(j + 1) * 256], kb[:, h, j * 128:(j + 1) * 128], qb[:, h, 0:256])
        k.actf(p[:, :], ps_s[:, :], AF.Exp)
        for j in range(2):
            k.mm(po[:, 0:256], vA[:, j, h * 128:(h + 1) * 128], p[:, j * 256:(j + 1) * 256], start=(j == 0), stop=(j == 1))
        for j in range(2):
            k.mm(pd[:, 0:256], onesb[:, :], p[:, j * 256:(j + 1) * 256], start=(j == 0), stop=(j == 1))
        rd = tmp[2]
        k.recip(rd[:, 0:256], pd[:, 0:256])
        k.tt(oT[:, h, 0:256], po[:, 0:256], rd[:, 0:256], ALU.mult)
    k.dma(od[:], oT[:])
    return k.finish()


def na_bias_tiles(rpb_l, heads):
    bt = np.full((2, 64, len(heads), 14, 64), -30000.0, np.float32)
    qc = np.arange(64)
    c0 = np.clip(qc - 8, 0, 48)
    for kc in range(64):
        ok = (kc >= c0) & (kc < c0 + 16)
        dc = kc - qc + 15
        for a in range(2):
            for m in range(14):
                for hi, h in enumerate(heads):
                    bt[a, kc, hi, m, ok] = rpb_l[h, m + a, dc[ok]]
    return np.ascontiguousarray(bt.reshape(128, len(heads), 14, 64))


def fm_heads(a, nh=2, hd=128):
    return np.ascontiguousarray(a.reshape(a.shape[0], nh, hd).transpose(2, 1, 0))


def na_inputs(P_b, s, z, l):
    hs = [2 * s, 2 * s + 1]
    c = 4288
    q = P_b[:, c + s * 256: c + (s + 1) * 256]
    kk = P_b[:, c + 512 + s * 256: c + 512 + (s + 1) * 256]
    v = P_b[:, c + 1024 + s * 256: c + 1024 + (s + 1) * 256]
    vA = np.ascontiguousarray(v.reshape(18, 128, 256).transpose(1, 0, 2))
    vB = np.ascontiguousarray(v[256 + 64:256 + 64 + 15 * 128].reshape(15, 128, 256).transpose(1, 0, 2))
    return {"qT": fm_heads(q), "kT": fm_heads(kk), "vA": vA, "vB": vB,
            "gq": np.ascontiguousarray(z['na_gq'][l].reshape(128, 1)), "gk": np.ascontiguousarray(z['na_gk'][l].reshape(128, 1)),
            "bt": na_bias_tiles(z['na_rpb'][l], hs)}


def seq_P(r, l, b):
    return np.concatenate([r[f'p_c{l}'][b], r[f'p_l{l}'][b]], 0)


EPS = 1e-6


def rope_tables():
    t = np.arange(2048)
    row = (t // 64).astype(np.float32); col = (t % 64).astype(np.float32)
    freqs = (10000.0 ** (-np.arange(16, dtype=np.float32) / 16)).astype(np.float32)
    ang = np.concatenate([row[:, None] * freqs, col[:, None] * freqs], -1)
    cos = np.cos(ang).T; sin = np.sin(ang).T
    cosF = np.concatenate([cos, cos], 0).astype(np.float32)
    sinF = np.concatenate([sin, sin], 0).astype(np.float32)
    R = np.zeros((64, 64), np.float32)
    for i in range(32):
        R[i, 32 + i] = -1.0
        R[32 + i, i] = 1.0
    return np.ascontiguousarray(cosF), np.ascontiguousarray(sinF), np.ascontiguousarray(R.T)


def build_mla():
    k = KB()
    cqd = k.din("cqT", [128, 4, S]); ckvd = k.din("ckvT", [128, 2, S]); krd = k.din("krT", [64, S])
    wqd = k.din("wq", [128, 4, 384]); wkvd = k.din("wkv", [128, 2, 512])
    qnd = k.din("qn", [128, 4]); kvnd = k.din("kvn", [128, 2])
    gqd = k.din("gq", [128, 2]); gkd = k.din("gk", [128, 2])
    cosd = k.din("cosF", [64, 2048]); sind = k.din("sinF", [64, 2048]); rtd = k.din("RT", [64, 64])
    od = k.dout("oT", [128, 2, S])
    ones = k.sb([128, 128]); k.memset(ones[:], 1.0)
    onesb = k.sb([128, 128], BF16); k.memset(onesb[:], 1.0)
    wq = k.sb([128, 4, 384], BF16); wkv = k.sb([128, 2, 512], BF16)
    qn = k.sb([128, 4]); kvn = k.sb([128, 2]); gq = k.sb([128, 2]); gk = k.sb([128, 2])
    cosF = k.sb([64, 2048]); sinF = k.sb([64, 2048]); RT = k.sb([64, 64])
    krf = k.sb([64, S])
    cqn = k.sb([128, 4, S], BF16); ckvn = k.sb([128, 2, S], BF16)
    oT = k.sb([128, 2, S])
    pss = [k.ps([128, 512]) for _ in range(8)]
    tmp = [k.sb([128, 512]) for _ in range(4)]
    k.dma(wq[:], wqd[:], q="pool"); k.dma(wkv[:], wkvd[:], q="pool")
    for a, b in ((qn, qnd), (kvn, kvnd), (gq, gqd), (gk, gkd), (cosF, cosd), (sinF, sind), (RT, rtd), (krf, krd)):
        k.dma(a[:], b[:])
    with k.scope() as st:
        cq = k.sb([128, 4, S], stack=st); ckv = k.sb([128, 2, S], stack=st)
        k.dma(cq[:], cqd[:]); k.dma(ckv[:], ckvd[:])
        for (src, dst, nch, nfeat, gn) in ((cq, cqn, 4, 448, qn), (ckv, ckvn, 2, 160, kvn)):
            for (t0, n) in TT:
                ps = pss[0]
                for c in range(nch):
                    sq = tmp[c % 2]
                    k.actf(sq[:, 0:n], src[:, c, t0:t0 + n], AF.Square)
                    k.mm(ps[:, 0:n], ones[:, :], sq[:, 0:n], start=(c == 0), stop=(c == nch - 1))
                rs = tmp[2]
                k.ts(rs[:, 0:n], ps[:, 0:n], 1.0 / nfeat, ALU.mult, EPS, ALU.add)
                k.actf(rs[:, 0:n], rs[:, 0:n], AF.Sqrt)
                k.recip(rs[:, 0:n], rs[:, 0:n])
                for c in range(nch):
                    k.stt(dst[:, c, t0:t0 + n], src[:, c, t0:t0 + n], gn[:, c:c + 1], rs[:, 0:n], ALU.mult, ALU.mult)
    scale = 192 ** -0.5
    qA = k.sb([128, S], BF16); qB = k.sb([64, S], BF16)
    kA = k.sb([128, S], BF16); kB = k.sb([64, S], BF16)
    vT = k.sb([128, 18, 128], BF16)
    pT = [k.sb([128, 512], BF16) for _ in range(3)]
    for h in range(2):
        for which in range(2):
            dA, dB = (qA, qB) if which == 0 else (kA, kB)
            g = gq if which == 0 else gk
            sc = scale if which == 0 else 1.0
            for (t0, n) in TT:
                pa = pss[0]; pb = pss[1]; pn = pss[2]; pr = pss[3]
                if which == 0:
                    for c in range(4):
                        k.mm(pa[:, 0:n], wq[:, c, h * 192:h * 192 + 128], cqn[:, c, t0:t0 + n], start=(c == 0), stop=(c == 3))
                    for c in range(4):
                        k.mm(pb[0:64, 0:n], wq[:, c, h * 192 + 128:h * 192 + 192], cqn[:, c, t0:t0 + n], start=(c == 0), stop=(c == 3))
                    srcB = pb[0:64, 0:n]
                else:
                    for c in range(2):
                        k.mm(pa[:, 0:n], wkv[:, c, h * 256:h * 256 + 128], ckvn[:, c, t0:t0 + n], start=(c == 0), stop=(c == 1))
                    srcB = krf[:, t0:t0 + n]
                sa = tmp[0]; sbq = tmp[1]
                k.actf(sa[:, 0:n], pa[:, 0:n], AF.Square)
                k.actf(sbq[0:64, 0:n], srcB, AF.Square)
                k.mm(pn[:, 0:n], ones[:, :], sa[:, 0:n], start=True, stop=False)
                k.mm(pn[:, 0:n], ones[0:64, :], sbq[0:64, 0:n], start=False, stop=True)
                rs = tmp[2]
                k.ts(rs[:, 0:n], pn[:, 0:n], 1.0 / 192, ALU.mult, EPS, ALU.add)
                k.actf(rs[:, 0:n], rs[:, 0:n], AF.Sqrt)
                k.recip(rs[:, 0:n], rs[:, 0:n])
                k.tt(sa[:, 0:n], pa[:, 0:n], rs[:, 0:n], ALU.mult)
                k.ts(dA[:, t0:t0 + n], sa[:, 0:n], g[:, 0:1], ALU.mult, sc, ALU.mult)
                ub = tmp[3]
                k.tt(sbq[0:64, 0:n], srcB, rs[0:64, 0:n], ALU.mult)
                k.ts(ub[0:64, 0:n], sbq[0:64, 0:n], g[0:64, 1:2], ALU.mult, sc, ALU.mult)
                l0 = max(t0, 256)
                if l0 > t0:
                    k.copy(dB[:, t0:l0], ub[0:64, 0:l0 - t0])
                nl = t0 + n - l0
                o = l0 - t0
                k.mm(pr[0:64, 0:nl], RT[:, :], ub[0:64, o:o + nl])
                k.tt(sbq[0:64, 0:nl], pr[0:64, 0:nl], sinF[:, l0 - 256:l0 - 256 + nl], ALU.mult)
                k.tt(ub[0:64, o:o + nl], ub[0:64, o:o + nl], cosF[:, l0 - 256:l0 - 256 + nl], ALU.mult)
                k.tt(dB[:, l0:l0 + nl], ub[0:64, o:o + nl], sbq[0:64, 0:nl], ALU.add)
        for blk in range(18):
            pv = pss[blk % 2]
            for c in range(2):
                k.mm(pv[:, 0:128], ckvn[:, c, blk * 128:(blk + 1) * 128], wkv[:, c, h * 256 + 128:h * 256 + 256],
                     start=(c == 0), stop=(c == 1))
            k.copy(vT[:, blk, :], pv[:, 0:128])
        jobs = [(256 + qt * 512, 512, list(range(18))) for qt in range(4)] + [(0, 256, [0, 1])]
        for ji, (q0, nq, blks) in enumerate(jobs):
            po = pss[4 + (ji % 2) * 2]; pd = pss[5 + (ji % 2) * 2]
            for bi, blk in enumerate(blks):
                ps_s = pss[bi % 4]
                k.mm(ps_s[:, 0:nq], kA[:, blk * 128:(blk + 1) * 128], qA[:, q0:q0 + nq], start=True, stop=False)
                k.mm(ps_s[:, 0:nq], kB[:, blk * 128:(blk + 1) * 128], qB[:, q0:q0 + nq], start=False, stop=True)
                p = pT[bi % 3]
                k.actf(p[:, 0:nq], ps_s[:, 0:nq], AF.Exp)
                k.mm(po[:, 0:nq], vT[:, blk, :], p[:, 0:nq], start=(bi == 0), stop=(bi == len(blks) - 1))
                k.mm(pd[:, 0:nq], onesb[:, :], p[:, 0:nq], start=(bi == 0), stop=(bi == len(blks) - 1))
            rd = tmp[2]
            k.recip(rd[:, 0:nq], pd[:, 0:nq])
            k.tt(oT[:, h, q0:q0 + nq], po[:, 0:nq], rd[:, 0:nq], ALU.mult)
    k.dma(od[:], oT[:])
    return k.finish()


def fm_pad(a, nch):
    o = np.zeros((nch * 128, a.shape[0]), np.float32)
    o[:a.shape[1]] = a.T
    return np.ascontiguousarray(o.reshape(nch, 128, a.shape[0]).transpose(1, 0, 2))


def vec_pad(v, nch):
    o = np.zeros(nch * 128, np.float32); o[:v.shape[0]] = v
    return np.ascontiguousarray(o.reshape(nch, 128).T)


def w_pad(w, nch):
    o = np.zeros((nch * 128, w.shape[1]), np.float32); o[:w.shape[0]] = w
    return np.ascontiguousarray(o.reshape(nch, 128, w.shape[1]).transpose(1, 0, 2))


ROPE = rope_tables()


def mla_inputs(P_b, s, z, l):
    c = 3616
    cq = P_b[:, c:c + 448]; ckv = P_b[:, c + 448:c + 608]; kr = P_b[:, c + 608:c + 672]
    wq = z['mla_w_qb'][l][:, s * 384:(s + 1) * 384]
    wkv = z['mla_w_kvb'][l][:, s * 512:(s + 1) * 512]
    return {"cqT": fm_pad(cq, 4), "ckvT": fm_pad(ckv, 2), "krT": np.ascontiguousarray(kr.T),
            "wq": w_pad(wq, 4), "wkv": w_pad(wkv, 2),
            "qn": vec_pad(z['mla_q_norm'][l], 4), "kvn": vec_pad(z['mla_kv_norm'][l], 2),
            "gq": vec_pad(z['mla_gq'][l], 2), "gk": vec_pad(z['mla_gk'][l], 2),
            "cosF": ROPE[0], "sinF": ROPE[1], "RT": ROPE[2]}


EPS = 1e-6
NB = 18
FWD_ORDER = list(range(18))
BWD_ORDER = [1, 0] + list(range(17, 1, -1))


def tri_consts():
    r = np.arange(128)
    U = (r[:, None] <= r[None, :]).astype(np.float32)
    L = np.ascontiguousarray(U.T)
    return np.ascontiguousarray(np.stack([U, U, L, L], 1))


def build_ml():
    k = KB()
    qTd = k.din("qT", [128, 2, S]); kTd = k.din("kT", [128, 2, S])
    kMd = k.din("kTM", [128, NB, 256]); vMd = k.din("vTM", [128, NB, 256]); oMd = k.din("oTM", [128, NB, 256])
    gid = k.din("gi", [128, NB, 4]); gfd = k.din("gf", [128, NB, 4])
    bid = k.din("bi", [128, 4]); bfd = k.din("bf", [128, 4])
    gnd = k.din("gn", [128, 256])
    uld = k.din("UL4", [128, 4, 128])
    od = k.dout("oTM_out", [128, NB, 256])
    ones = k.sb([128, 128]); k.memset(ones[:], 1.0)
    UL4 = k.sb([128, 4, 128]); k.dma(UL4[:], uld[:])
    qTb = k.sb([128, 2, S], BF16); kTb = k.sb([128, 2, S], BF16)
    kTM = k.sb([128, NB, 256]); vext = k.sb([128, NB, 2, 129], BF16)
    osg = k.sb([128, NB, 256])
    gi = k.sb([128, NB, 4]); gf = k.sb([128, NB, 4]); bi = k.sb([128, 4]); bfb = k.sb([128, 4]); gn = k.sb([128, 256])
    pss = [k.ps([128, 512]) for _ in range(8)]
    k.dma(kTM[:], kMd[:]); k.dma(osg[:], oMd[:])
    for a, b in ((gi, gid), (gf, gfd), (bi, bid), (bfb, bfd), (gn, gnd)):
        k.dma(a[:], b[:])
    k.memset(vext[:, :, :, 128:129], 1.0)
    scale = 128 ** -0.5
    with k.scope() as st:
        qf = k.sb([128, 2, S], stack=st); vf = k.sb([128, NB, 256], stack=st)
        k.dma(qf[:], qTd[:]); k.dma(vf[:], vMd[:])
        k.dma(kTb[:], kTd[:], q="pool")
        k.ts(qTb[:], qf[:], scale, ALU.mult)
        k.copy(vext[:, :, :, 0:128], vf[:].rr("p b (h d) -> p b h d", h=2))
    k.actf(osg[:], osg[:], AF.Sigmoid)
    lf = k.sb([128, NB, 4]); li = k.sb([128, NB, 4])
    for c in range(4):
        k.ts(lf[:, :, c], gf[:, :, c], bfb[:, c:c + 1], ALU.add)
        k.ts(li[:, :, c], gi[:, :, c], bi[:, c:c + 1], ALU.add)
    k.actf(lf[:], lf[:], AF.Exp, scale=-1.0)
    k.actf(lf[:], lf[:], AF.Ln, bias=1.0)
    k.ts(lf[:], lf[:], -1.0, ALU.mult)
    bcol = k.sb([128, NB, 4]); acol = k.sb([128, NB, 4]); wg = k.sb([128, NB, 4]); ebtot = k.sb([128, NB, 4])
    lf2 = lf[:].rr("p b c -> p (b c)")
    pc = pss[0]
    k.mm(pc[:, 0:72], UL4[:, 0, :], lf2)
    k.mm(pc[:, 128:200], UL4[:, 2, :], lf2)
    k.copy(bcol[:, :, 0:2], pc[:, 0:72].rr("p (b c) -> p b c", c=4)[:, :, 0:2])
    k.copy(bcol[:, :, 2:4], pc[:, 128:200].rr("p (b c) -> p b c", c=4)[:, :, 2:4])
    k.tt(acol[:], li[:], bcol[:], ALU.subtract)
    k.mm(pc[:, 256:328], ones[:, :], lf2)
    btot = pc[:, 256:328].rr("p (b c) -> p b c", c=4)
    k.tt(wg[:], acol[:], btot, ALU.add)
    k.actf(wg[:], wg[:], AF.Exp)
    k.actf(ebtot[:], btot, AF.Exp)
    PT4 = k.sb([128, NB, 4, 128], BF16); QT4 = k.sb([128, NB, 4, 128], BF16)
    X = [k.sb([128, 4, 128]) for _ in range(2)]
    Dm = [k.sb([128, 4, 128]) for _ in range(2)]
    Eb = [k.sb([128, 4, 128]) for _ in range(2)]
    for blk in range(NB):
        x = X[blk % 2]; dm = Dm[blk % 2]; eb = Eb[blk % 2]
        pb = pss[1 + blk % 2]; psc = pss[3 + blk % 2]
        k.tt(x[:], UL4[:], lf[:, blk, :].unsq(2).bc([128, 4, 128]), ALU.mult)
        k.mm(pb[:, :], ones[:, :], x[:].rr("p c t -> p (c t)"))
        pb4 = pb[:, :].rr("p (c t) -> p c t", c=4)
        k.tt(dm[:], pb4, bcol[:, blk, :].unsq(2).bc([128, 4, 128]), ALU.subtract)
        k.tt(dm[:], dm[:], UL4[:], ALU.mult)
        k.tt(dm[:], dm[:], li[:, blk, :].unsq(2).bc([128, 4, 128]), ALU.add)
        k.actf(dm[:], dm[:], AF.Exp)
        k.tt(dm[:], dm[:], UL4[:], ALU.mult)
        for hl in range(2):
            k.mm(psc[:, hl * 128:(hl + 1) * 128], kTb[:, hl, blk * 128:(blk + 1) * 128], qTb[:, hl, blk * 128:(blk + 1) * 128])
        for d in range(2):
            k.tt(PT4[:, blk, d * 2:d * 2 + 2, :], dm[:, d * 2:d * 2 + 2, :],
                 psc[:, 0:256].rr("p (h t) -> p h t", h=2), ALU.mult)
        k.actf(eb[:], pb4, AF.Exp)
        for d in range(2):
            k.tt(QT4[:, blk, d * 2:d * 2 + 2, :], eb[:, d * 2:d * 2 + 2, :],
                 qTb[:, :, blk * 128:(blk + 1) * 128], ALU.mult)
    CT = k.sb([128, 4, 129]); CTb = k.sb([128, 4, 129], BF16)
    k.memset(CT[:], 0.0); k.memset(CTb[:], 0.0)
    H = [k.sb([128, NB, 2, 128]) for _ in range(2)]
    kw = [k.sb([128, 128], BF16) for _ in range(4)]
    dn = [k.sb([128, 1]) for _ in range(4)]
    for step in range(NB):
        for c in range(4):
            d, hl = c // 2, c % 2
            blk = FWD_ORDER[step] if d == 0 else BWD_ORDER[step]
            pn = pss[c % 2]; pcs = pss[2 + c % 2]
            k.mm(pn[:, 0:129], PT4[:, blk, c, :], vext[:, blk, hl, :], start=True, stop=False)
            k.mm(pn[:, 0:129], QT4[:, blk, c, :], CTb[:, c, :], start=False, stop=True)
            k.actf(dn[c][:], pn[:, 128:129], AF.Abs)
            k.ts(dn[c][:], dn[c][:], 1.0, ALU.max)
            k.recip(dn[c][:], dn[c][:])
            k.ts(H[d][:, blk, hl, :], pn[:, 0:128], dn[c][:, 0:1], ALU.mult)
            if step < NB - 1:
                k.ts(kw[c][:], kTM[:, blk, hl * 128:(hl + 1) * 128], wg[:, blk, c:c + 1], ALU.mult, eng="pool")
                k.mm(pcs[:, 0:129], kw[c][:], vext[:, blk, hl, :])
                k.stt(CT[:, c, :], CT[:, c, :], ebtot[:, blk, c:c + 1], pcs[:, 0:129], ALU.mult, ALU.add)
                k.copy(CTb[:, c, :], CT[:, c, :], eng="act")
    Hs = H[0]
    k.tt(Hs[:], H[0][:], H[1][:], ALU.add)
    sq = H[1]
    k.tt(sq[:], Hs[:], Hs[:], ALU.mult)
    ss = k.sb([128, NB * 2])
    k.reduce(ss[:], sq[:].rr("p b h d -> p (b h) d"), ALU.add, AX.X)
    k.ts(ss[:], ss[:], 1.0 / 128, ALU.mult, EPS, ALU.add)
    k.actf(ss[:], ss[:], AF.Sqrt)
    k.recip(ss[:], ss[:])
    outb = sq
    for blk in range(NB):
        for hl in range(2):
            k.stt(outb[:, blk, hl, :], Hs[:, blk, hl, :], ss[:, blk * 2 + hl:blk * 2 + hl + 1],
                  gn[:, hl * 128:(hl + 1) * 128], ALU.mult, ALU.mult)
    k.tt(osg[:], osg[:], outb[:].rr("p b h d -> p b (h d)"), ALU.mult)
    k.dma(od[:], osg[:])
    return k.finish()


def tm_blocks(a):
    return np.ascontiguousarray(a.reshape(NB, 128, a.shape[1]).transpose(1, 0, 2))


UL4 = tri_consts()


def ml_inputs(P_b, s, z, l):
    q = P_b[:, s * 256:(s + 1) * 256]; kk = P_b[:, 512 + s * 256:512 + (s + 1) * 256]
    v = P_b[:, 1024 + s * 256:1024 + (s + 1) * 256]; o = P_b[:, 1536 + s * 256:1536 + (s + 1) * 256]
    ig = P_b[:, 2048:2056].reshape(S, 2, 4)[:, :, 2 * s:2 * s + 2].reshape(S, 4)
    fg = P_b[:, 2056:2064].reshape(S, 2, 4)[:, :, 2 * s:2 * s + 2].reshape(S, 4)
    bi = z['ml_i_bias'][l][:, 2 * s:2 * s + 2].reshape(1, 4); bf = z['ml_f_bias'][l][:, 2 * s:2 * s + 2].reshape(1, 4)
    gn = z['ml_norm'][l][s * 256:(s + 1) * 256].reshape(1, 256)
    return {"qT": fm_heads(q), "kT": fm_heads(kk), "kTM": tm_blocks(kk), "vTM": tm_blocks(v), "oTM": tm_blocks(o),
            "gi": tm_blocks(ig), "gf": tm_blocks(fg),
            "bi": np.ascontiguousarray(np.repeat(bi, 128, 0)), "bf": np.ascontiguousarray(np.repeat(bf, 128, 0)),
            "gn": np.ascontiguousarray(np.repeat(gn, 128, 0)), "UL4": UL4}


EPS = 1e-6


def tri8():
    r = np.arange(128)
    U = (r[:, None] <= r[None, :]).astype(np.float32)
    L = np.ascontiguousarray(U.T)
    return np.ascontiguousarray(np.stack([U] * 4 + [L] * 4, 1))


def build_ssd():
    k = KB()
    xbcd = k.din("xbcT", [128, 4, S]); cwd_ = k.din("cw", [128, 4, 5]); cbd = k.din("cb", [128, 4])
    zd = k.din("zTM", [128, NB, 256]); dtd = k.din("dtr", [128, NB, 8])
    dbd = k.din("dtb", [128, 8]); ald = k.din("alog", [128, 8]); dsd = k.din("dskip", [128, 256])
    uld = k.din("UL8", [128, 8, 128]); idd = k.din("ident", [128, 128])
    od = k.dout("yTM", [128, NB, 256])
    ones = k.sb([128, 128]); k.memset(ones[:], 1.0)
    UL8 = k.sb([128, 8, 128]); ident = k.sb([128, 128])
    cw = k.sb([128, 4, 5]); cb = k.sb([128, 4]); dtb = k.sb([128, 8]); alog = k.sb([128, 8]); dsk = k.sb([128, 256])
    zs = k.sb([128, NB, 256]); dt = k.sb([128, NB, 8])
    for a, b in ((UL8, uld), (ident, idd), (cw, cwd_), (cb, cbd), (dtb, dbd), (alog, ald), (dsk, dsd), (zs, zd), (dt, dtd)):
        k.dma(a[:], b[:])
    pss = [k.ps([128, 512]) for _ in range(8)]
    BTb = k.sb([128, S], BF16); CTb = k.sb([128, S], BF16)
    xTM = k.sb([128, NB, 256]); BTM = k.sb([128, NB, 128])
    with k.scope() as st:
        u = k.sb([128, 4, S], stack=st); acc = k.sb([128, 4, S], stack=st)
        xs = u
        k.dma(u[:], xbcd[:])
        for c in range(4):
            e = "dve" if c % 2 == 0 else "pool"
            k.ts(acc[:, c, :], u[:, c, :], cw[:, c, 2:3], ALU.mult, eng=e)
            for j in (0, 1, 3, 4):
                d = j - 2
                for (a, b) in ((0, 256), (256, S)):
                    lo = a + max(0, -d); hi = b - max(0, d)
                    k.stt(acc[:, c, lo:hi], u[:, c, lo + d:hi + d], cw[:, c, j:j + 1], acc[:, c, lo:hi],
                          ALU.mult, ALU.add)
        for c in range(2):
            k.actf(xs[:, c, :], acc[:, c, :], AF.Silu, bias=cb[:, c:c + 1])
        k.actf(acc[:, 2, :], acc[:, 2, :], AF.Silu, bias=cb[:, 2:3])
        k.actf(CTb[:], acc[:, 3, :], AF.Silu, bias=cb[:, 3:4])
        k.copy(BTb[:], acc[:, 2, :])
        for blk in range(NB):
            pt = pss[blk % 4]
            for c in range(2):
                k.transpose(pt[:, c * 128:(c + 1) * 128], xs[:, c, blk * 128:(blk + 1) * 128], ident[:, :])
            k.transpose(pt[:, 256:384], acc[:, 2, blk * 128:(blk + 1) * 128], ident[:, :])
            k.copy(xTM[:, blk, :], pt[:, 0:256], eng="act")
            k.copy(BTM[:, blk, :], pt[:, 256:384])
    A = k.sb([128, 8])
    k.actf(A[:], alog[:], AF.Exp)
    k.ts(A[:], A[:], -1.0, ALU.mult)
    for c in range(8):
        k.ts(dt[:, :, c], dt[:, :, c], dtb[:, c:c + 1], ALU.add)
    k.actf(dt[:], dt[:], AF.Exp)
    k.actf(dt[:], dt[:], AF.Ln, bias=1.0)
    av = k.sb([128, NB, 8])
    for c in range(8):
        k.ts(av[:, :, c], dt[:, :, c], A[:, c:c + 1], ALU.mult)
    bcol = k.sb([128, NB, 8]); wtail = k.sb([128, NB, 8]); ebtot = k.sb([128, NB, 8])
    a2 = av[:].rr("p b c -> p (b c)")
    pc = pss[0]
    k.mm(pc[:, 0:144], UL8[:, 0, :], a2)
    k.mm(pc[:, 160:304], UL8[:, 4, :], a2)
    k.copy(bcol[:, :, 0:4], pc[:, 0:144].rr("p (b c) -> p b c", c=8)[:, :, 0:4])
    k.copy(bcol[:, :, 4:8], pc[:, 160:304].rr("p (b c) -> p b c", c=8)[:, :, 4:8])
    k.mm(pc[:, 320:464], ones[:, :], a2)
    btot = pc[:, 320:464].rr("p (b c) -> p b c", c=8)
    k.tt(wtail[:], btot, bcol[:], ALU.subtract)
    k.actf(wtail[:], wtail[:], AF.Exp)
    k.actf(ebtot[:], btot, AF.Exp)
    xt = k.sb([128, NB, 8, 64], BF16)
    for blk in range(NB):
        for d in range(2):
            k.tt(xt[:, blk, d * 4:(d + 1) * 4, :], xTM[:, blk, :].rr("p (h e) -> p h e", h=4),
                 dt[:, blk, d * 4:(d + 1) * 4].unsq(2).bc([128, 4, 64]), ALU.mult, eng="pool" if d else "dve")
    PT8 = k.sb([128, NB, 8, 128], BF16); CT8 = k.sb([128, NB, 8, 128], BF16)
    pre = k.scope(); st2 = pre.__enter__()
    X = [k.sb([128, 8, 128], stack=st2) for _ in range(2)]
    Dm = [k.sb([128, 8, 128], stack=st2) for _ in range(2)]
    for blk in range(NB):
        x = X[blk % 2]; dm = Dm[blk % 2]
        pb0 = pss[1 + (blk % 2) * 2]; pb1 = pss[2 + (blk % 2) * 2]; pg = pss[5 + blk % 2]
        k.tt(x[:], UL8[:], av[:, blk, :].unsq(2).bc([128, 8, 128]), ALU.mult)
        k.mm(pb0[:, :], ones[:, :], x[:, 0:4, :].rr("p c t -> p (c t)"))
        k.mm(pb1[:, :], ones[:, :], x[:, 4:8, :].rr("p c t -> p (c t)"))
        k.mm(pg[:, 0:128], BTb[:, blk * 128:(blk + 1) * 128], CTb[:, blk * 128:(blk + 1) * 128])
        for d, pb in enumerate((pb0, pb1)):
            pb4 = pb[:, :].rr("p (c t) -> p c t", c=4)
            sl = slice(d * 4, (d + 1) * 4)
            k.tt(dm[:, sl, :], pb4, bcol[:, blk, sl].unsq(2).bc([128, 4, 128]), ALU.subtract)
            k.tt(dm[:, sl, :], dm[:, sl, :], UL8[:, sl, :], ALU.mult)
            k.actf(dm[:, sl, :], dm[:, sl, :], AF.Exp)
            k.tt(dm[:, sl, :], dm[:, sl, :], UL8[:, sl, :], ALU.mult, eng="pool")
            k.tt(PT8[:, blk, sl, :], dm[:, sl, :], pg[:, 0:128].unsq(1).bc([128, 4, 128]), ALU.mult)
            k.actf(x[:, sl, :], pb4, AF.Exp)
            k.tt(CT8[:, blk, sl, :], x[:, sl, :], CTb[:, blk * 128:(blk + 1) * 128].unsq(1).bc([128, 4, 128]),
                 ALU.mult, eng="pool")
    pre.__exit__(None, None, None)
    HT = k.sb([128, 8, 64]); HTb = k.sb([128, 8, 64], BF16)
    k.memset(HT[:], 0.0); k.memset(HTb[:], 0.0)
    Y = [k.sb([128, NB, 256]) for _ in range(2)]
    Bw = [k.sb([128, 128], BF16) for _ in range(8)]
    for step in range(NB):
        for d in range(2):
            blk = FWD_ORDER[step] if d == 0 else BWD_ORDER[step]
            py = pss[d]
            for hh in range(4):
                c = d * 4 + hh
                k.mm(py[:, hh * 64:(hh + 1) * 64], PT8[:, blk, c, :], xt[:, blk, c, :], start=True, stop=False)
                k.mm(py[:, hh * 64:(hh + 1) * 64], CT8[:, blk, c, :], HTb[:, c, :], start=False, stop=True)
            k.copy(Y[d][:, blk, :], py[:, 0:256], eng="act")
            if step < NB - 1:
                ph = pss[2 + d]
                for hh in range(4):
                    c = d * 4 + hh
                    k.ts(Bw[c][:], BTM[:, blk, :], wtail[:, blk, c:c + 1], ALU.mult, eng="pool")
                    k.mm(ph[:, hh * 64:(hh + 1) * 64], Bw[c][:], xt[:, blk, c, :])
                    k.stt(HT[:, c, :], HT[:, c, :], ebtot[:, blk, c:c + 1], ph[:, hh * 64:(hh + 1) * 64], ALU.mult, ALU.add)
                k.copy(HTb[:, d * 4:(d + 1) * 4, :], HT[:, d * 4:(d + 1) * 4, :], eng="act")
    k.tt(Y[0][:], Y[0][:], Y[1][:], ALU.add)
    k.tt(xTM[:], xTM[:], dsk[:, :].unsq(1).bc([128, NB, 256]), ALU.mult)
    k.tt(Y[0][:], Y[0][:], xTM[:], ALU.add)
    k.actf(zs[:], zs[:], AF.Silu)
    k.tt(Y[0][:], Y[0][:], zs[:], ALU.mult)
    k.dma(od[:], Y[0][:])
    return k.finish()


UL8 = tri8()
IDENT = np.eye(128, dtype=np.float32)


def ssd_inputs(P_b, s, z, l):
    c = 2064
    zz = P_b[:, c + s * 256:c + (s + 1) * 256]
    xb = 2576
    xx = P_b[:, xb + s * 256:xb + (s + 1) * 256]
    Bm = P_b[:, xb + 512 + s * 128:xb + 512 + (s + 1) * 128]
    Cm = P_b[:, xb + 768 + s * 128:xb + 768 + (s + 1) * 128]
    chans = np.concatenate([np.arange(s * 256, (s + 1) * 256), 512 + np.arange(s * 128, (s + 1) * 128),
                            768 + np.arange(s * 128, (s + 1) * 128)])
    xbc = np.concatenate([xx, Bm, Cm], 1)
    xbcT = np.ascontiguousarray(xbc.T.reshape(4, 128, S).transpose(1, 0, 2))
    cw = np.ascontiguousarray(z['ssd_conv_w'][l][:, chans].T.reshape(4, 128, 5).transpose(1, 0, 2))
    cb = np.ascontiguousarray(z['ssd_conv_b'][l][chans].reshape(4, 128).T)
    dtr = P_b[:, 3600:3616].reshape(S, 2, 8)[:, :, 4 * s:4 * s + 4].reshape(S, 8)
    rep = lambda v: np.ascontiguousarray(np.repeat(v.reshape(1, -1), 128, 0).astype(np.float32))
    dtb = rep(z['ssd_dt_bias'][l][:, 4 * s:4 * s + 4].reshape(8))
    alog = rep(z['ssd_A_log'][l][:, 4 * s:4 * s + 4].reshape(8))
    dsk = rep(np.repeat(z['ssd_D'][l][4 * s:4 * s + 4], 64))
    return {"xbcT": xbcT, "cw": cw, "cb": cb, "zTM": tm_blocks(zz), "dtr": tm_blocks(dtr), "dtb": dtb, "alog": alog,
            "dskip": dsk, "UL8": UL8, "ident": IDENT}


D = 2048
DIN = 5824
S = 2304
NB = 18
EPS = 1e-6
FD = 5632
FE = 7168
SEQ_TILES = [(0, 256, 1), (256, 512, 0), (768, 512, 0), (1280, 512, 0), (1792, 512, 0)]
TT5 = [(t0, n) for (t0, n, _) in SEQ_TILES]
CGROUPS = [(0, 512, 1, 0), (512, 512, 1, 1), (1024, 512, 0, 1), (1536, 512, 0, 1), (2048, 512, 0, 1), (2560, 16, 0, 1),
           (2576, 512, 1, 0), (3088, 512, 1, 0), (3600, 16, 0, 1), (3616, 512, 1, 0), (4128, 160, 1, 0),
           (4288, 512, 1, 0), (4800, 512, 1, 0), (5312, 512, 0, 1)]


class Consts:
    pass


def rstd_from_ps(k, rs, ps, n, nfeat):
    k.ts(rs[:, 0:n], ps[:, 0:n], 1.0 / nfeat, ALU.mult, EPS, ALU.add)
    k.actf(rs[:, 0:n], rs[:, 0:n], AF.Sqrt)
    k.recip(rs[:, 0:n], rs[:, 0:n])


def phase_mod(k, C, ccT_d, modw_d, modb_d, modsb, pss):
    with k.scope() as st:
        cc = k.sb([128, 16, 2], stack=st); scb = k.sb([128, 16, 2], BF16, stack=st); bsb = k.sb([128, 2, 96], stack=st)
        wsb = [k.sb([128, 16, 1536], BF16, stack=st) for _ in range(2)]
        k.dma(cc[:], ccT_d[:]); k.dma(bsb[:], modb_d[:])
        k.actf(scb[:], cc[:], AF.Silu)
        groups = [(l, g) for l in range(2) for g in range(8)]

        def load(i):
            l, g = groups[i]
            wv = modw_d[l].rr("(kc p) n -> p kc n", p=128)
            for q in range(4):
                k.dma(wsb[i % 2][:, q * 4:(q + 1) * 4, :], wv[:, q * 4:(q + 1) * 4, g * 1536:(g + 1) * 1536], q="pool")
        load(0)
        for i, (l, g) in enumerate(groups):
            if i + 1 < len(groups):
                load(i + 1)
            for j in range(12):
                p = pss[j % 2]
                for kc in range(16):
                    k.mm(p[:, 0:2], wsb[i % 2][:, kc, j * 128:(j + 1) * 128], scb[:, kc, :], start=(kc == 0), stop=(kc == 15))
                ch = g * 12 + j
                k.ts(modsb[:, l, ch, :], p[:, 0:2], bsb[:, l, ch:ch + 1], ALU.add)


def norm_tile(k, C, x, h, n, A, sh, col, ps, tmp):
    for kc in range(16):
        sq = tmp[kc % 2]
        k.actf(sq[:, 0:n], x[:, kc, 0:n], AF.Square)
        k.mm(ps[:, 0:n], C.ones[:, :], sq[:, 0:n], start=(kc == 0), stop=(kc == 15))
    rs = tmp[2]
    rstd_from_ps(k, rs, ps, n, D)
    for kc in range(16):
        t = tmp[kc % 2]
        k.tt(t[:, 0:n], x[:, kc, 0:n], rs[:, 0:n], ALU.mult)
        k.ts(h[:, kc, 0:n], t[:, 0:n], A[:, kc, col:col + 1], ALU.mult, sh[:, kc, col:col + 1], ALU.add)


def make_A(k, A, gT, scT):
    for c in range(2):
        k.ts(A[:, :, c], scT[:, :, c], 1.0, ALU.add)
        k.tt(A[:, :, c], A[:, :, c], gT[:, :], ALU.mult)


def phase_inproj(k, C, xsrc, gT, scT, shT, w_d, PT, PM, pss):
    with k.scope() as st:
        hT = k.sb([128, 16, S], BF16, stack=st)
        A = k.sb([128, 16, 2], stack=st)
        make_A(k, A, gT, scT)
        with k.scope() as st2:
            xt = [k.sb([128, 16, 512], stack=st2) for _ in range(2)]
            tmp = [k.sb([128, 512], stack=st2) for _ in range(3)]
            for ti, (t0, n, col) in enumerate(SEQ_TILES):
                x = xt[ti % 2]
                for q in range(4):
                    k.dma(x[:, q * 4:(q + 1) * 4, 0:n], xsrc[:, q * 4:(q + 1) * 4, t0:t0 + n])
                norm_tile(k, C, x, hT[:, :, t0:t0 + n], n, A, shT, col, pss[ti % 2], tmp)
        wsb = [k.sb([128, 16, 512], BF16, stack=st) for _ in range(2)]
        stg = [k.sb([128, 512], stack=st) for _ in range(4)]
        wv = w_d.rr("(kc p) n -> p kc n", p=128)

        def loadw(gi):
            c0, cn, _, _ = CGROUPS[gi]
            for q in range(4):
                k.dma(wsb[gi % 2][:, q * 4:(q + 1) * 4, 0:cn], wv[:, q * 4:(q + 1) * 4, c0:c0 + cn], q="pool")
        loadw(0)
        it = 0
        for gi, (c0, cn, fm, tm) in enumerate(CGROUPS):
            if gi + 1 < len(CGROUPS):
                loadw(gi + 1)
            w = wsb[gi % 2]
            if tm:
                for tb in range(NB):
                    ps = pss[it % 4]; sg = stg[it % 4]
                    for kc in range(16):
                        k.mm(ps[:, 0:cn], hT[:, kc, tb * 128:(tb + 1) * 128], w[:, kc, 0:cn], start=(kc == 0), stop=(kc == 15))
                    k.copy(sg[:, 0:cn], ps[:, 0:cn], eng="act" if it % 2 else "dve")
                    k.dma(PM[tb * 128:(tb + 1) * 128, c0:c0 + cn], sg[:, 0:cn], waw=False)
                    it += 1
            if fm:
                for m0 in range(0, cn, 128):
                    mn = min(128, cn - m0)
                    for (t0, n, _) in SEQ_TILES:
                        ps = pss[it % 4]; sg = stg[it % 4]
                        for kc in range(16):
                            k.mm(ps[0:mn, 0:n], w[:, kc, m0:m0 + mn], hT[:, kc, t0:t0 + n], start=(kc == 0), stop=(kc == 15))
                        k.copy(sg[0:mn, 0:n], ps[0:mn, 0:n], eng="act" if it % 2 else "dve")
                        k.dma(PT[c0 + m0:c0 + m0 + mn, t0:t0 + n], sg[0:mn, 0:n], waw=False)
                        it += 1


def headnorm_fm(k, C, srcT, dstT, h, gcol, ps, tmp, extra_scale=1.0, nfeat=128):
    for (t0, n) in TT5:
        sq = tmp[0]
        k.actf(sq[:, 0:n], srcT[:, h, t0:t0 + n], AF.Square)
        k.mm(ps[:, 0:n], C.ones[:, :], sq[:, 0:n])
        rs = tmp[1]
        rstd_from_ps(k, rs, ps, n, nfeat)
        k.tt(rs[:, 0:n], srcT[:, h, t0:t0 + n], rs[:, 0:n], ALU.mult)
        k.ts(dstT[:, h, t0:t0 + n], rs[:, 0:n], gcol, ALU.mult, extra_scale, ALU.mult)


def body_na(k, C, PT, PM, s, gq_d, gk_d, bt_d, MIXT):
    with k.scope() as st:
        sb = lambda shp, dt=F32: k.sb(shp, dt, stack=st)
        qd = PT[4288 + s * 256:4288 + (s + 1) * 256, :].rr("(h p) t -> p h t", p=128)
        kd = PT[4800 + s * 256:4800 + (s + 1) * 256, :].rr("(h p) t -> p h t", p=128)
        vc = slice(5312 + s * 256, 5312 + (s + 1) * 256)
        vAd = PM[:, vc].rr("(j p) c -> p j c", p=128)
        vBd = PM[320:320 + 15 * 128, vc].rr("(j p) c -> p j c", p=128)
        qf = sb([128, 2, S]); kf = sb([128, 2, S])
        qb = sb([128, 2, S], BF16); kb = sb([128, 2, S], BF16)
        vA = sb([128, 18, 256], BF16); vB = sb([128, 15, 256], BF16)
        gq = sb([128, 1]); gk = sb([128, 1]); bt = sb([128, 2, 14, 64])
        oT = sb([128, 2, S])
        tmp = [sb([128, 512]) for _ in range(3)]
        pss = [k.ps([128, 512], stack=st) for _ in range(8)]
        k.dma(qf[:], qd); k.dma(kf[:], kd)
        k.dma(vA[:], vAd, q="pool"); k.dma(vB[:], vBd, q="pool")
        k.dma(gq[:], gq_d); k.dma(gk[:], gk_d); k.dma(bt[:], bt_d)
        scale = 128 ** -0.5
        for h in range(2):
            headnorm_fm(k, C, qf, qb, h, gq[:, 0:1], pss[0], tmp, extra_scale=scale)
            headnorm_fm(k, C, kf, kb, h, gk[:, 0:1], pss[1], tmp)
        pT = [sb([128, 384], BF16) for _ in range(3)]
        pc = sb([128, 512], BF16)
        it = 0
        for h in range(2):
            def s_stage(r, h=h):
                r0 = min(max(r - 4, 0), 24)
                q0 = 256 + r * 64
                ps_s = pss[r % 4]
                vblks = []
                for j in range(4):
                    kt0 = 256 + r0 * 64 + 128 * j
                    k.mm(ps_s[:, j * 64:(j + 1) * 64], kb[:, h, kt0:kt0 + 128], qb[:, h, q0:q0 + 64])
                    if r0 % 2 == 0:
                        vblks.append(vA[:, 2 + r0 // 2 + j, h * 128:(h + 1) * 128])
                    else:
                        vblks.append(vB[:, (r0 - 1) // 2 + j, h * 128:(h + 1) * 128])
                for j in range(2):
                    k.mm(ps_s[:, 256 + j * 64:256 + (j + 1) * 64], kb[:, h, j * 128:(j + 1) * 128], qb[:, h, q0:q0 + 64])
                    vblks.append(vA[:, j, h * 128:(h + 1) * 128])
                return vblks, r0 - r + 7
            cur = s_stage(0)
            for r in range(32):
                rg, rl = r // 8, r % 8
                if rl == 0:
                    po = pss[4 + (it % 2) * 2]; pd = pss[5 + (it % 2) * 2]
                    it += 1
                vblks, m0 = cur
                ps_s = pss[r % 4]
                tm = tmp[r % 2]
                k.tt(tm[:, 0:256].rr("p (j c) -> p j c", j=4), ps_s[:, 0:256].rr("p (j c) -> p j c", j=4),
                     bt[:, h, m0:m0 + 7:2, :], ALU.add)
                p = pT[r % 3]
                k.actf(p[:, 0:256], tm[:, 0:256], AF.Exp)
                k.actf(p[:, 256:384], ps_s[:, 256:384], AF.Exp)
                if r + 1 < 32:
                    cur = s_stage(r + 1)
                for j in range(6):
                    k.mm(po[:, rl * 64:(rl + 1) * 64], vblks[j], p[:, j * 64:(j + 1) * 64], start=(j == 0), stop=(j == 5))
                for j in range(6):
                    k.mm(pd[:, rl * 64:(rl + 1) * 64], C.onesb[:, :], p[:, j * 64:(j + 1) * 64], start=(j == 0), stop=(j == 5))
                if rl == 7:
                    rd = tmp[2]
                    k.recip(rd[:, :], pd[:, :])
                    k.tt(oT[:, h, 256 + rg * 512:256 + (rg + 1) * 512], po[:, :], rd[:, :], ALU.mult)
            ps_s = pss[0]; po = pss[4]; pd = pss[5]
            for j in range(2):
                k.mm(ps_s[:, j * 256:(j + 1) * 256], kb[:, h, j * 128:(j + 1) * 128], qb[:, h, 0:256])
            k.actf(pc[:, :], ps_s[:, :], AF.Exp)
            for j in range(2):
                k.mm(po[:, 0:256], vA[:, j, h * 128:(h + 1) * 128], pc[:, j * 256:(j + 1) * 256], start=(j == 0), stop=(j == 1))
            for j in range(2):
                k.mm(pd[:, 0:256], C.onesb[:, :], pc[:, j * 256:(j + 1) * 256], start=(j == 0), stop=(j == 1))
            rd = tmp[2]
            k.recip(rd[:, 0:256], pd[:, 0:256])
            k.tt(oT[:, h, 0:256], po[:, 0:256], rd[:, 0:256], ALU.mult)
        r0_ = 1536 + s * 256
        k.dma(MIXT[r0_:r0_ + 256, :].rr("(h p) t -> p h t", p=128), oT[:], waw=False)


def body_mla(k, C, PT, s, P, MIXT):
    with k.scope() as st:
        sb = lambda shp, dt=F32: k.sb(shp, dt, stack=st)
        wq = sb([128, 4, 384], BF16); wkv = sb([128, 2, 512], BF16)
        qn = sb([128, 4]); kvn = sb([128, 2]); gq = sb([128, 2]); gk = sb([128, 2])
        cosF = sb([64, 2048]); sinF = sb([64, 2048]); RT = sb([64, 64])
        krf = sb([64, S])
        cqn = sb([128, 4, S], BF16); ckvn = sb([128, 2, S], BF16)
        oT = sb([128, 2, S])
        pss = [k.ps([128, 512], stack=st) for _ in range(8)]
        tmp = [sb([128, 512]) for _ in range(8)]
        k.dma(wq[:], P["wq"], q="pool"); k.dma(wkv[:], P["wkv"], q="pool")
        for a, b in ((qn, "qn"), (kvn, "kvn"), (gq, "gq"), (gk, "gk"), (cosF, "cosF"), (sinF, "sinF"), (RT, "RT")):
            k.dma(a[:], P[b])
        k.dma(krf[:], PT[4224:4288, :])
        with k.scope() as st2:
            cq = k.sb([128, 4, S], stack=st2); ckv = k.sb([128, 2, S], stack=st2)
            k.memset(cq[:, 3, :], 0.0); k.memset(ckv[:, 1, :], 0.0)
            k.dma(cq[:, 0:3, :], PT[3616:3616 + 384, :].rr("(c p) t -> p c t", p=128))
            k.dma(cq[0:64, 3, :], PT[3616 + 384:3616 + 448, :])
            k.dma(ckv[:, 0, :], PT[4064:4064 + 128, :])
            k.dma(ckv[0:32, 1, :], PT[4064 + 128:4064 + 160, :])
            for (src, dst, nch, nfeat, gn) in ((cq, cqn, 4, 448, qn), (ckv, ckvn, 2, 160, kvn)):
                for (t0, n) in TT5:
                    ps = pss[0]
                    for c in range(nch):
                        sq = tmp[c % 2]
                        k.actf(sq[:, 0:n], src[:, c, t0:t0 + n], AF.Square)
                        k.mm(ps[:, 0:n], C.ones[:, :], sq[:, 0:n], start=(c == 0), stop=(c == nch - 1))
                    rs = tmp[2]
                    rstd_from_ps(k, rs, ps, n, nfeat)
                    for c in range(nch):
                        k.stt(dst[:, c, t0:t0 + n], src[:, c, t0:t0 + n], gn[:, c:c + 1], rs[:, 0:n], ALU.mult, ALU.mult)
        scale = 192 ** -0.5
        qA = sb([128, S], BF16); qB = sb([64, S], BF16)
        kA = sb([128, S], BF16); kB = sb([64, S], BF16)
        vT = sb([128, 18, 128], BF16)
        pT = [sb([128, 512], BF16) for _ in range(3)]
        pit = [0]
        for h in range(2):
            for which in range(2):
                dA, dB = (qA, qB) if which == 0 else (kA, kB)
                g = gq if which == 0 else gk
                sc = scale if which == 0 else 1.0
                for (t0, n) in TT5:
                    pit[0] += 1
                    o4 = (pit[0] % 2) * 4
                    pa = pss[o4]; pb = pss[o4 + 1]; pn = pss[o4 + 2]; pr = pss[o4 + 3]
                    if which == 0:
                        for c in range(4):
                            k.mm(pa[:, 0:n], wq[:, c, h * 192:h * 192 + 128], cqn[:, c, t0:t0 + n], start=(c == 0), stop=(c == 3))
                        for c in range(4):
                            k.mm(pb[0:64, 0:n], wq[:, c, h * 192 + 128:h * 192 + 192], cqn[:, c, t0:t0 + n], start=(c == 0), stop=(c == 3))
                        srcB = pb[0:64, 0:n]
                    else:
                        for c in range(2):
                            k.mm(pa[:, 0:n], wkv[:, c, h * 256:h * 256 + 128], ckvn[:, c, t0:t0 + n], start=(c == 0), stop=(c == 1))
                        srcB = krf[:, t0:t0 + n]
                    sa = tmp[o4]; sbq = tmp[o4 + 1]
                    k.actf(sa[:, 0:n], pa[:, 0:n], AF.Square)
                    k.actf(sbq[0:64, 0:n], srcB, AF.Square)
                    k.mm(pn[:, 0:n], C.ones[:, :], sa[:, 0:n], start=True, stop=False)
                    k.mm(pn[:, 0:n], C.ones[0:64, :], sbq[0:64, 0:n], start=False, stop=True)
                    rs = tmp[o4 + 2]
                    rstd_from_ps(k, rs, pn, n, 192)
                    k.tt(sa[:, 0:n], pa[:, 0:n], rs[:, 0:n], ALU.mult)
                    k.ts(dA[:, t0:t0 + n], sa[:, 0:n], g[:, 0:1], ALU.mult, sc, ALU.mult)
                    ub = tmp[o4 + 3]
                    k.tt(sbq[0:64, 0:n], srcB, rs[0:64, 0:n], ALU.mult)
                    k.ts(ub[0:64, 0:n], sbq[0:64, 0:n], g[0:64, 1:2], ALU.mult, sc, ALU.mult)
                    l0 = max(t0, 256)
                    if l0 > t0:
                        k.copy(dB[:, t0:l0], ub[0:64, 0:l0 - t0])
                    nl = t0 + n - l0
                    if nl <= 0:
                        continue
                    o = l0 - t0
                    k.mm(pr[0:64, 0:nl], RT[:, :], ub[0:64, o:o + nl])
                    k.tt(sbq[0:64, 0:nl], pr[0:64, 0:nl], sinF[:, l0 - 256:l0 - 256 + nl], ALU.mult)
                    k.tt(ub[0:64, o:o + nl], ub[0:64, o:o + nl], cosF[:, l0 - 256:l0 - 256 + nl], ALU.mult)
                    k.tt(dB[:, l0:l0 + nl], ub[0:64, o:o + nl], sbq[0:64, 0:nl], ALU.add)
            for blk in range(18):
                pv = pss[blk % 2]
                for c in range(2):
                    k.mm(pv[:, 0:128], ckvn[:, c, blk * 128:(blk + 1) * 128], wkv[:, c, h * 256 + 128:h * 256 + 256],
                         start=(c == 0), stop=(c == 1))
                k.copy(vT[:, blk, :], pv[:, 0:128])
            jobs = [(256 + qt * 512, 512, list(range(18))) for qt in range(4)] + [(0, 256, [0, 1])]
            for ji, (q0, nq, blks) in enumerate(jobs):
                po = pss[4 + (ji % 2) * 2]; pd = pss[5 + (ji % 2) * 2]

                def smm(bi):
                    blk = blks[bi]
                    ps_s = pss[bi % 4]
                    k.mm(ps_s[:, 0:nq], kA[:, blk * 128:(blk + 1) * 128], qA[:, q0:q0 + nq], start=True, stop=False)
                    k.mm(ps_s[:, 0:nq], kB[:, blk * 128:(blk + 1) * 128], qB[:, q0:q0 + nq], start=False, stop=True)
                for bi in range(min(2, len(blks))):
                    smm(bi)
                for bi, blk in enumerate(blks):
                    p = pT[bi % 3]
                    k.actf(p[:, 0:nq], pss[bi % 4][:, 0:nq], AF.Exp)
                    if bi + 2 < len(blks):
                        smm(bi + 2)
                    k.mm(po[:, 0:nq], vT[:, blk, :], p[:, 0:nq], start=(bi == 0), stop=(bi == len(blks) - 1))
                    k.mm(pd[:, 0:nq], C.onesb[:, :], p[:, 0:nq], start=(bi == 0), stop=(bi == len(blks) - 1))
                rd = tmp[2]
                k.recip(rd[:, 0:nq], pd[:, 0:nq])
                k.tt(oT[:, h, q0:q0 + nq], po[:, 0:nq], rd[:, 0:nq], ALU.mult)
        r0_ = 1024 + s * 256
        k.dma(MIXT[r0_:r0_ + 256, :].rr("(h p) t -> p h t", p=128), oT[:], waw=False)


def tm_to_mixt(k, C, src, MIXT, r0_, pss, stg):
    it = 0
    for c in range(2):
        for g in range(5):
            nb = min(4, NB - g * 4)
            ps = pss[it % 2]; sg = stg[it % 2]
            for j in range(nb):
                blk = g * 4 + j
                k.transpose(ps[:, j * 128:(j + 1) * 128], src[:, blk, c * 128:(c + 1) * 128], C.ident[:, :])
            k.copy(sg[:, 0:nb * 128], ps[:, 0:nb * 128], eng="act" if it % 2 else "dve")
            k.dma(MIXT[r0_ + c * 128:r0_ + (c + 1) * 128, g * 512:g * 512 + nb * 128], sg[:, 0:nb * 128], waw=False)
            it += 1


def body_ml(k, C, PT, PM, s, P, MIXT):
    with k.scope() as st:
        sb = lambda shp, dt=F32: k.sb(shp, dt, stack=st)
        hv = lambda r: PT[r + s * 256:r + (s + 1) * 256, :].rr("(h p) t -> p h t", p=128)
        tv = lambda c: PM[:, c + s * 256:c + (s + 1) * 256].rr("(j p) c -> p j c", p=128)
        UL4 = sb([128, 4, 128]); k.dma(UL4[:], P["UL4"])
        qTb = sb([128, 2, S], BF16); kTb = sb([128, 2, S], BF16)
        kTM = sb([128, NB, 256]); vext = sb([128, NB, 2, 129], BF16)
        osg = sb([128, NB, 256])
        g16 = sb([128, NB, 16]); bi = sb([128, 4]); bfb = sb([128, 4]); gn = sb([128, 256])
        pss = [k.ps([128, 512], stack=st) for _ in range(8)]
        k.dma(kTM[:], tv(512)); k.dma(osg[:], tv(1536))
        k.dma(g16[:], PM[:, 2048:2064].rr("(j p) c -> p j c", p=128))
        for a, b in ((bi, "bi"), (bfb, "bf"), (gn, "gn")):
            k.dma(a[:], P[b])
        k.memset(vext[:, :, :, 128:129], 1.0)
        scale = 128 ** -0.5
        with k.scope() as st2:
            qf = k.sb([128, 2, S], stack=st2); vf = k.sb([128, NB, 256], stack=st2)
            k.dma(qf[:], hv(0)); k.dma(vf[:], tv(1024))
            k.dma(kTb[:], hv(512), q="pool")
            k.ts(qTb[:], qf[:], scale, ALU.mult)
            k.copy(vext[:, :, :, 0:128], vf[:].rr("p b (h d) -> p b h d", h=2))
        k.actf(osg[:], osg[:], AF.Sigmoid)
        lf = sb([128, NB, 4]); li = sb([128, NB, 4])
        for c in range(4):
            d, hl = c // 2, c % 2
            gc = d * 4 + 2 * s + hl
            k.ts(lf[:, :, c], g16[:, :, 8 + gc], bfb[:, c:c + 1], ALU.add)
            k.ts(li[:, :, c], g16[:, :, gc], bi[:, c:c + 1], ALU.add)
        k.actf(lf[:], lf[:], AF.Exp, scale=-1.0)
        k.actf(lf[:], lf[:], AF.Ln, bias=1.0)
        k.ts(lf[:], lf[:], -1.0, ALU.mult)
        bcol = sb([128, NB, 4]); acol = sb([128, NB, 4]); wg = sb([128, NB, 4]); ebtot = sb([128, NB, 4])
        lf2 = lf[:].rr("p b c -> p (b c)")
        pc = pss[0]
        k.mm(pc[:, 0:72], UL4[:, 0, :], lf2)
        k.mm(pc[:, 128:200], UL4[:, 2, :], lf2)
        k.copy(bcol[:, :, 0:2], pc[:, 0:72].rr("p (b c) -> p b c", c=4)[:, :, 0:2])
        k.copy(bcol[:, :, 2:4], pc[:, 128:200].rr("p (b c) -> p b c", c=4)[:, :, 2:4])
        k.tt(acol[:], li[:], bcol[:], ALU.subtract)
        k.mm(pc[:, 256:328], C.ones[:, :], lf2)
        btot = pc[:, 256:328].rr("p (b c) -> p b c", c=4)
        k.tt(wg[:], acol[:], btot, ALU.add)
        k.actf(wg[:], wg[:], AF.Exp)
        k.actf(ebtot[:], btot, AF.Exp)
        PT4 = sb([128, NB, 4, 128], BF16); QT4 = sb([128, NB, 4, 128], BF16)
        KW = sb([128, NB, 4, 128], BF16)
        with k.scope() as st3:
            X = [k.sb([128, 4, 128], stack=st3) for _ in range(2)]
            Dm = [k.sb([128, 4, 128], stack=st3) for _ in range(2)]
            Eb = [k.sb([128, 4, 128], stack=st3) for _ in range(2)]
            for blk in range(NB):
                x = X[blk % 2]; dm = Dm[blk % 2]; eb = Eb[blk % 2]
                pb = pss[1 + blk % 2]; psc = pss[3 + blk % 2]
                k.tt(x[:], UL4[:], lf[:, blk, :].unsq(2).bc([128, 4, 128]), ALU.mult)
                k.mm(pb[:, :], C.ones[:, :], x[:].rr("p c t -> p (c t)"))
                pb4 = pb[:, :].rr("p (c t) -> p c t", c=4)
                k.tt(dm[:], pb4, bcol[:, blk, :].unsq(2).bc([128, 4, 128]), ALU.subtract)
                k.tt(dm[:], dm[:], UL4[:], ALU.mult)
                k.tt(dm[:], dm[:], li[:, blk, :].unsq(2).bc([128, 4, 128]), ALU.add)
                k.actf(dm[:], dm[:], AF.Exp)
                k.tt(dm[:], dm[:], UL4[:], ALU.mult)
                for hl in range(2):
                    k.mm(psc[:, hl * 128:(hl + 1) * 128], kTb[:, hl, blk * 128:(blk + 1) * 128], qTb[:, hl, blk * 128:(blk + 1) * 128])
                for d in range(2):
                    k.tt(PT4[:, blk, d * 2:d * 2 + 2, :], dm[:, d * 2:d * 2 + 2, :],
                         psc[:, 0:256].rr("p (h t) -> p h t", h=2), ALU.mult)
                k.actf(eb[:], pb4, AF.Exp)
                for d in range(2):
                    k.tt(QT4[:, blk, d * 2:d * 2 + 2, :], eb[:, d * 2:d * 2 + 2, :],
                         qTb[:, :, blk * 128:(blk + 1) * 128], ALU.mult)
                for c in range(4):
                    hl = c % 2
                    k.ts(KW[:, blk, c, :], kTM[:, blk, hl * 128:(hl + 1) * 128], wg[:, blk, c:c + 1], ALU.mult)
        CT = sb([128, 4, 129]); CTb = sb([128, 4, 129], BF16)
        k.memset(CT[:], 0.0); k.memset(CTb[:], 0.0)
        H = [sb([128, NB, 2, 128]) for _ in range(2)]
        dn = [sb([128, 1]) for _ in range(4)]
        chains = [(c, c // 2, c % 2) for c in range(4)]
        for step in range(NB):
            blks = [FWD_ORDER[step] if d == 0 else BWD_ORDER[step] for (c, d, hl) in chains]
            for (c, d, hl), blk in zip(chains, blks):
                pn = pss[c]
                k.mm(pn[:, 0:129], PT4[:, blk, c, :], vext[:, blk, hl, :], start=True, stop=False)
                k.mm(pn[:, 0:129], QT4[:, blk, c, :], CTb[:, c, :], start=False, stop=True)
            if step < NB - 1:
                for (c, d, hl), blk in zip(chains, blks):
                    k.mm(pss[4 + c][:, 0:129], KW[:, blk, c, :], vext[:, blk, hl, :])
                for (c, d, hl), blk in zip(chains, blks):
                    k.stt(CT[:, c, :], CT[:, c, :], ebtot[:, blk, c:c + 1], pss[4 + c][:, 0:129], ALU.mult, ALU.add)
                for (c, d, hl), blk in zip(chains, blks):
                    k.copy(CTb[:, c, :], CT[:, c, :], eng="act")
            for (c, d, hl), blk in zip(chains, blks):
                k.actf(dn[c][:], pss[c][:, 128:129], AF.Abs)
            for (c, d, hl), blk in zip(chains, blks):
                k.ts(dn[c][:], dn[c][:], 1.0, ALU.max)
            for (c, d, hl), blk in zip(chains, blks):
                k.recip(dn[c][:], dn[c][:])
            for (c, d, hl), blk in zip(chains, blks):
                k.ts(H[d][:, blk, hl, :], pss[c][:, 0:128], dn[c][:, 0:1], ALU.mult)
        Hs = H[0]
        k.tt(Hs[:], H[0][:], H[1][:], ALU.add)
        sq = H[1]
        k.tt(sq[:], Hs[:], Hs[:], ALU.mult)
        ss = sb([128, NB * 2])
        k.reduce(ss[:], sq[:].rr("p b h d -> p (b h) d"), ALU.add, AX.X)
        k.ts(ss[:], ss[:], 1.0 / 128, ALU.mult, EPS, ALU.add)
        k.actf(ss[:], ss[:], AF.Sqrt)
        k.recip(ss[:], ss[:])
        outb = sq
        for blk in range(NB):
            for hl in range(2):
                k.stt(outb[:, blk, hl, :], Hs[:, blk, hl, :], ss[:, blk * 2 + hl:blk * 2 + hl + 1],
                      gn[:, hl * 128:(hl + 1) * 128], ALU.mult, ALU.mult)
        k.tt(osg[:], osg[:], outb[:].rr("p b h d -> p b (h d)"), ALU.mult)
        stg = [sb([128, 512]) for _ in range(2)]
        tm_to_mixt(k, C, osg, MIXT, s * 256, pss[4:6], stg)


def body_ssd(k, C, PT, PM, s, P, MIXT):
    with k.scope() as st:
        sb = lambda shp, dt=F32: k.sb(shp, dt, stack=st)
        UL8 = sb([128, 8, 128])
        cw = sb([128, 4, 5]); cb = sb([128, 4]); dtb = sb([128, 8]); alog = sb([128, 8]); dsk = sb([128, 256])
        zs = sb([128, NB, 256]); d16 = sb([128, NB, 16]); dt = sb([128, NB, 8])
        for a, b in ((UL8, "UL8"), (cw, "cw"), (cb, "cb"), (dtb, "dtb"), (alog, "alog"), (dsk, "dskip")):
            k.dma(a[:], P[b])
        k.dma(zs[:], PM[:, 2064 + s * 256:2064 + (s + 1) * 256].rr("(j p) c -> p j c", p=128))
        k.dma(d16[:], PM[:, 3600:3616].rr("(j p) c -> p j c", p=128))
        pss = [k.ps([128, 512], stack=st) for _ in range(8)]
        BTb = sb([128, S], BF16); CTb = sb([128, S], BF16)
        xTM = sb([128, NB, 256]); BTM = sb([128, NB, 128])
        with k.scope() as st2:
            u = k.sb([128, 4, S], stack=st2); acc = k.sb([128, 4, S], stack=st2)
            xs = u
            k.dma(u[:, 0:2, :], PT[2576 + s * 256:2576 + (s + 1) * 256, :].rr("(c p) t -> p c t", p=128))
            k.dma(u[:, 2, :], PT[3088 + s * 128:3088 + (s + 1) * 128, :])
            k.dma(u[:, 3, :], PT[3344 + s * 128:3344 + (s + 1) * 128, :])
            for c in range(4):
                k.ts(acc[:, c, :], u[:, c, :], cw[:, c, 2:3], ALU.mult)
                for j in (0, 1, 3, 4):
                    d = j - 2
                    for (a, b) in ((0, 256), (256, S)):
                        lo = a + max(0, -d); hi = b - max(0, d)
                        k.stt(acc[:, c, lo:hi], u[:, c, lo + d:hi + d], cw[:, c, j:j + 1], acc[:, c, lo:hi], ALU.mult, ALU.add)
            for c in range(2):
                k.actf(xs[:, c, :], acc[:, c, :], AF.Silu, bias=cb[:, c:c + 1])
            k.actf(acc[:, 2, :], acc[:, 2, :], AF.Silu, bias=cb[:, 2:3])
            k.actf(CTb[:], acc[:, 3, :], AF.Silu, bias=cb[:, 3:4])
            k.copy(BTb[:], acc[:, 2, :])
            for blk in range(NB):
                pt = pss[blk % 4]
                for c in range(2):
                    k.transpose(pt[:, c * 128:(c + 1) * 128], xs[:, c, blk * 128:(blk + 1) * 128], C.ident[:, :])
                k.transpose(pt[:, 256:384], acc[:, 2, blk * 128:(blk + 1) * 128], C.ident[:, :])
                k.copy(xTM[:, blk, :], pt[:, 0:256], eng="act")
                k.copy(BTM[:, blk, :], pt[:, 256:384])
        A = sb([128, 8])
        k.actf(A[:], alog[:], AF.Exp)
        k.ts(A[:], A[:], -1.0, ALU.mult)
        for c in range(8):
            d, hh = c // 4, c % 4
            k.ts(dt[:, :, c], d16[:, :, d * 8 + 4 * s + hh], dtb[:, c:c + 1], ALU.add)
        k.actf(dt[:], dt[:], AF.Exp)
        k.actf(dt[:], dt[:], AF.Ln, bias=1.0)
        av = sb([128, NB, 8])
        for c in range(8):
            k.ts(av[:, :, c], dt[:, :, c], A[:, c:c + 1], ALU.mult)
        bcol = sb([128, NB, 8]); wtail = sb([128, NB, 8]); ebtot = sb([128, NB, 8])
        a2 = av[:].rr("p b c -> p (b c)")
        pc = pss[0]
        k.mm(pc[:, 0:144], UL8[:, 0, :], a2)
        k.mm(pc[:, 160:304], UL8[:, 4, :], a2)
        k.copy(bcol[:, :, 0:4], pc[:, 0:144].rr("p (b c) -> p b c", c=8)[:, :, 0:4])
        k.copy(bcol[:, :, 4:8], pc[:, 160:304].rr("p (b c) -> p b c", c=8)[:, :, 4:8])
        k.mm(pc[:, 320:464], C.ones[:, :], a2)
        btot = pc[:, 320:464].rr("p (b c) -> p b c", c=8)
        k.tt(wtail[:], btot, bcol[:], ALU.subtract)
        k.actf(wtail[:], wtail[:], AF.Exp)
        k.actf(ebtot[:], btot, AF.Exp)
        xt = sb([128, NB, 8, 64], BF16)
        for blk in range(NB):
            for d in range(2):
                k.tt(xt[:, blk, d * 4:(d + 1) * 4, :], xTM[:, blk, :].rr("p (h e) -> p h e", h=4),
                     dt[:, blk, d * 4:(d + 1) * 4].unsq(2).bc([128, 4, 64]), ALU.mult, eng="pool" if d else "dve")
        PT8 = sb([128, NB, 8, 128], BF16); CT8 = sb([128, NB, 8, 128], BF16)
        with k.scope() as st3:
            X = [k.sb([128, 8, 128], stack=st3) for _ in range(2)]
            Dm = [k.sb([128, 8, 128], stack=st3) for _ in range(2)]
            for blk in range(NB):
                x = X[blk % 2]; dm = Dm[blk % 2]
                pb0 = pss[1 + (blk % 2) * 2]; pb1 = pss[2 + (blk % 2) * 2]; pg = pss[5 + blk % 2]
                k.tt(x[:], UL8[:], av[:, blk, :].unsq(2).bc([128, 8, 128]), ALU.mult)
                k.mm(pb0[:, :], C.ones[:, :], x[:, 0:4, :].rr("p c t -> p (c t)"))
                k.mm(pb1[:, :], C.ones[:, :], x[:, 4:8, :].rr("p c t -> p (c t)"))
                k.mm(pg[:, 0:128], BTb[:, blk * 128:(blk + 1) * 128], CTb[:, blk * 128:(blk + 1) * 128])
                for d, pb in enumerate((pb0, pb1)):
                    pb4 = pb[:, :].rr("p (c t) -> p c t", c=4)
                    sl = slice(d * 4, (d + 1) * 4)
                    k.tt(dm[:, sl, :], pb4, bcol[:, blk, sl].unsq(2).bc([128, 4, 128]), ALU.subtract)
                    k.tt(dm[:, sl, :], dm[:, sl, :], UL8[:, sl, :], ALU.mult)
                    k.actf(dm[:, sl, :], dm[:, sl, :], AF.Exp)
                    k.tt(dm[:, sl, :], dm[:, sl, :], UL8[:, sl, :], ALU.mult, eng="pool")
                    k.tt(PT8[:, blk, sl, :], dm[:, sl, :], pg[:, 0:128].unsq(1).bc([128, 4, 128]), ALU.mult)
                    k.actf(x[:, sl, :], pb4, AF.Exp)
                    k.tt(CT8[:, blk, sl, :], x[:, sl, :], CTb[:, blk * 128:(blk + 1) * 128].unsq(1).bc([128, 4, 128]),
                         ALU.mult, eng="pool")
        HT = sb([128, 8, 64]); HTb = sb([128, 8, 64], BF16)
        k.memset(HT[:], 0.0); k.memset(HTb[:], 0.0)
        Y = [sb([128, NB, 256]) for _ in range(2)]
        Bw = [sb([128, 128], BF16) for _ in range(8)]
        for step in range(NB):
            bl = [FWD_ORDER[step], BWD_ORDER[step]]
            if step < NB - 1:
                for d in range(2):
                    for hh in range(4):
                        c = d * 4 + hh
                        k.ts(Bw[c][:], BTM[:, bl[d], :], wtail[:, bl[d], c:c + 1], ALU.mult)
            for d in range(2):
                for hh in range(4):
                    c = d * 4 + hh
                    k.mm(pss[d][:, hh * 64:(hh + 1) * 64], PT8[:, bl[d], c, :], xt[:, bl[d], c, :], start=True, stop=False)
                    k.mm(pss[d][:, hh * 64:(hh + 1) * 64], CT8[:, bl[d], c, :], HTb[:, c, :], start=False, stop=True)
            if step < NB - 1:
                for d in range(2):
                    for hh in range(4):
                        c = d * 4 + hh
                        k.mm(pss[2 + d][:, hh * 64:(hh + 1) * 64], Bw[c][:], xt[:, bl[d], c, :])
                for d in range(2):
                    k.tt(HT[:, d * 4:(d + 1) * 4, :], HT[:, d * 4:(d + 1) * 4, :],
                         ebtot[:, bl[d], d * 4:(d + 1) * 4].unsq(2).bc([128, 4, 64]), ALU.mult)
                    k.tt(HT[:, d * 4:(d + 1) * 4, :], HT[:, d * 4:(d + 1) * 4, :],
                         pss[2 + d][:, 0:256].rr("p (h e) -> p h e", h=4), ALU.add)
                for d in range(2):
                    k.copy(HTb[:, d * 4:(d + 1) * 4, :], HT[:, d * 4:(d + 1) * 4, :], eng="act")
            for d in range(2):
                k.copy(Y[d][:, bl[d], :], pss[d][:, 0:256], eng="act")
        k.tt(Y[0][:], Y[0][:], Y[1][:], ALU.add)
        k.tt(xTM[:], xTM[:], dsk[:, :].unsq(1).bc([128, NB, 256]), ALU.mult)
        k.tt(Y[0][:], Y[0][:], xTM[:], ALU.add)
        k.actf(zs[:], zs[:], AF.Silu)
        k.tt(Y[0][:], Y[0][:], zs[:], ALU.mult)
        stg = [Y[1][:, 0:2, :].rr("p a c -> p (a c)"), Y[1][:, 2:4, :].rr("p a c -> p (a c)")]
        tm_to_mixt(k, C, Y[0], MIXT, 512 + s * 256, pss[4:6], stg)


def outproj_tiles(k, C, mixsrc, xT, tiles, NT, wo_d, g1, gs, pss, st):
    mixb = k.sb([128, 16, NT], BF16, stack=st); ssd = k.sb([128, 4, NT], stack=st)
    tmp = [k.sb([128, 512], stack=st) for _ in range(3)]
    wos = [k.sb([128, 16, 512], BF16, stack=st) for _ in range(2)]
    wov = wo_d.rr("(kc p) n -> p kc n", p=128)

    def loadwo(g):
        for q in range(2):
            k.dma(wos[g % 2][:, q * 8:(q + 1) * 8, :], wov[:, q * 8:(q + 1) * 8, g * 512:(g + 1) * 512], q="pool")
    k.dma(mixb[:, 0:4, :], mixsrc[:, 0:4, :], q="pool"); k.dma(mixb[:, 8:12, :], mixsrc[:, 8:12, :], q="pool")
    k.dma(mixb[:, 12:16, :], mixsrc[:, 12:16, :], q="pool"); k.dma(ssd[:], mixsrc[:, 4:8, :])
    loadwo(0)
    for (t0, n, _) in tiles:
        ps = pss[0]
        for c in range(4):
            sq = tmp[c % 2]
            k.actf(sq[:, 0:n], ssd[:, c, t0:t0 + n], AF.Square)
            k.mm(ps[:, 0:n], C.ones[:, :], sq[:, 0:n], start=(c == 0), stop=(c == 3))
        rs = tmp[2]
        rstd_from_ps(k, rs, ps, n, 512)
        for c in range(4):
            k.stt(mixb[:, 4 + c, t0:t0 + n], ssd[:, c, t0:t0 + n], gs[:, c:c + 1], rs[:, 0:n], ALU.mult, ALU.mult)
    jt = 0
    for g in range(4):
        if g + 1 < 4:
            loadwo(g + 1)
        for dl in range(4):
            dc = g * 4 + dl
            for (t0, n, col) in tiles:
                po = pss[4 + jt % 4]
                for kc in range(16):
                    k.mm(po[:, 0:n], wos[g % 2][:, kc, dl * 128:(dl + 1) * 128], mixb[:, kc, t0:t0 + n],
                         start=(kc == 0), stop=(kc == 15))
                k.stt(xT[:, dc, t0:t0 + n], po[:, 0:n], g1[:, dc, col:col + 1], xT[:, dc, t0:t0 + n], ALU.mult, ALU.add)
                jt += 1


def phase_ffn0(k, C, xsrc, MIXT, XR, tok0, tiles, modl, n2, gs, wo_d, w1d, w3d, w2d, pss):
    NT = sum(n for _, n, _ in tiles)
    g1 = modl(2); sh2 = modl(3); sc2 = modl(4); g2 = modl(5)
    mixv = MIXT.rr("(kc p) t -> p kc t", p=128)
    with k.scope() as st:
        xT = k.sb([128, 16, NT], stack=st)
        for q in range(4):
            k.dma(xT[:, q * 4:(q + 1) * 4, :], xsrc[:, q * 4:(q + 1) * 4, tok0:tok0 + NT])
        with k.scope() as st2:
            outproj_tiles(k, C, mixv[:, :, tok0:tok0 + NT], xT, tiles, NT, wo_d, g1, gs, pss, st2)
        h2T = k.sb([128, 16, NT], BF16, stack=st)
        with k.scope() as st2:
            tmp = [k.sb([128, 512], stack=st2) for _ in range(3)]
            A = k.sb([128, 16, 2], stack=st2)
            make_A(k, A, n2, sc2)
            for ti, (t0, n, col) in enumerate(tiles):
                norm_tile(k, C, xT[:, :, t0:t0 + n], h2T[:, :, t0:t0 + n], n, A, sh2, col, pss[ti % 2], tmp)
        colof = {t0: col for (t0, n, col) in tiles}

        def acc(po, dc, t0, n):
            col = colof[t0]
            k.stt(xT[:, dc, t0:t0 + n], po, g2[:, dc, col:col + 1], xT[:, dc, t0:t0 + n], ALU.mult, ALU.add)
        with k.scope() as st2:
            ffn_simple(k, h2T, tiles, w1d, w3d, w2d, FD, acc, pss, stack=st2)
        for q in range(4):
            k.dma(XR[:, q * 4:(q + 1) * 4, tok0:tok0 + NT], xT[:, q * 4:(q + 1) * 4, :], waw=False)


def phase_moe(k, C, XO, MIXO, modl, n2, gs, wo_d, rt_d, sel_d, w1d, w3d, w2d, xout, pss):
    NT = 1024
    tiles = [(0, 512, 0), (512, 512, 0)]
    g1 = modl(2); sh2 = modl(3); sc2 = modl(4); g2 = modl(5)
    with k.scope() as st:
        xT = k.sb([128, 16, NT], stack=st)
        for q in range(4):
            k.dma(xT[:, q * 4:(q + 1) * 4, :], XO[:, q * 4:(q + 1) * 4, :])
        with k.scope() as st2:
            outproj_tiles(k, C, MIXO, xT, tiles, NT, wo_d, g1, gs, pss, st2)
        h2B = k.sb([128, 16, NT], BF16, stack=st)
        gT8 = k.sb([8, NT], stack=st)
        sel = k.sb([8, 8, 128], stack=st)
        k.dma(sel[:], sel_d)
        with k.scope() as st2:
            h2F = k.sb([128, 16, NT], stack=st2)
            tmp = [k.sb([128, 512], stack=st2) for _ in range(3)]
            A = k.sb([128, 16, 2], stack=st2)
            rt = k.sb([128, 16, 8], stack=st2)
            k.dma(rt[:], rt_d)
            make_A(k, A, n2, sc2)
            for ti, (t0, n, col) in enumerate(tiles):
                norm_tile(k, C, xT[:, :, t0:t0 + n], h2F[:, :, t0:t0 + n], n, A, sh2, col, pss[ti % 2], tmp)
            for q in range(4):
                k.copy(h2B[:, q * 4:(q + 1) * 4, :], h2F[:, q * 4:(q + 1) * 4, :], eng="act" if q % 2 else "dve")
            lg = k.sb([128, 8], stack=st2); m1 = k.sb([128, 1], stack=st2); m2 = k.sb([128, 1], stack=st2)
            t8 = k.sb([128, 8], stack=st2); sl = k.sb([128, 8], stack=st2); sm = k.sb([128, 1], stack=st2)
            gts = k.sb([128, 8], stack=st2)
            for tb in range(NT // 128):
                pl = pss[tb % 2]; pt = pss[2 + tb % 2]
                for kc in range(16):
                    k.mm(pl[:, 0:8], h2F[:, kc, tb * 128:(tb + 1) * 128], rt[:, kc, :], start=(kc == 0), stop=(kc == 15))
                k.copy(lg[:], pl[:, 0:8])
                k.reduce(m1[:], lg[:], ALU.max)
                k.ts(t8[:], lg[:], m1[:, 0:1], ALU.is_ge, -1e30, ALU.mult)
                k.tt(t8[:], t8[:], lg[:], ALU.add)
                k.reduce(m2[:], t8[:], ALU.max)
                k.ts(sl[:], lg[:], m2[:, 0:1], ALU.is_ge)
                k.ts(t8[:], lg[:], m1[:, 0:1], ALU.subtract)
                k.actf(t8[:], t8[:], AF.Exp)
                k.tt(t8[:], t8[:], sl[:], ALU.mult)
                k.reduce(sm[:], t8[:], ALU.add)
                k.recip(sm[:], sm[:])
                k.ts(gts[:], t8[:], sm[:, 0:1], ALU.mult)
                k.transpose(pt[0:8, 0:128], gts[:, :], C.ident[:, :])
                k.copy(gT8[:, tb * 128:(tb + 1) * 128], pt[0:8, 0:128])
        gbc = k.sb([128, NT], stack=st)
        for e in range(8):
            for (t0, n, _) in tiles:
                pg = pss[0]
                k.mm(pg[:, 0:n], sel[:, e, :], gT8[:, t0:t0 + n])
                k.copy(gbc[:, t0:t0 + n], pg[:, 0:n])

            def acc(po, dc, t0, n):
                k.stt(xT[:, dc, t0:t0 + n], po, g2[:, dc, 0:1], xT[:, dc, t0:t0 + n], ALU.mult, ALU.add)
            with k.scope() as st2:
                ffn_simple(k, h2B, tiles, w1d[e], w3d[e], w2d[e], FE, acc, pss, stack=st2, gate=gbc)
        for q in range(4):
            k.dma(xout[:, q * 4:(q + 1) * 4, :], xT[:, q * 4:(q + 1) * 4, :])


def build_mega(stop_after=None):
    k = KB()
    C = Consts()
    xin = k.din("xT", [128, 16, S])
    ccT_d = k.din("ccT", [128, 16, 2]); modw_d = k.din("mod_w", [2, D, 6 * D]); modb_d = k.din("mod_bT", [128, 2, 96])
    n1_d = k.din("n1T", [128, 2, 16]); n2_d = k.din("n2T", [128, 2, 16]); gs_d = k.din("gsT", [128, 2, 4])
    win_d = k.din("w_in", [2, D, DIN]); wout_d = k.din("w_out", [2, D, D])
    ident_d = k.din("ident", [128, 128])
    na_g = k.din("na_g", [128, 2, 2]); na_bt = k.din("na_bt", [128, 2, 2, 2, 14, 64])
    mla_wq = k.din("mla_wq", [2, 2, 128, 4, 384]); mla_wkv = k.din("mla_wkv", [2, 2, 128, 2, 512])
    mla_v = k.din("mla_vec", [128, 2, 10]); rope_c = k.din("cosF", [64, 2048]); rope_s = k.din("sinF", [64, 2048])
    rope_r = k.din("RT", [64, 64])
    ml_b = k.din("ml_b", [128, 2, 2, 8]); ml_gn = k.din("ml_gn", [128, 2, 2, 256]); ul4_d = k.din("UL4", [128, 4, 128])
    ssd_cw = k.din("ssd_cw", [128, 2, 2, 4, 5]); ssd_cb = k.din("ssd_cb", [128, 2, 2, 4])
    ssd_v = k.din("ssd_vec", [128, 2, 2, 16]); ssd_dk = k.din("ssd_dsk", [128, 2, 2, 256]); ul8_d = k.din("UL8", [128, 8, 128])
    if stop_after is None or stop_after[0] != "mix" or stop_after[1] > 0:
        fw1 = k.din("ffn_w1", [D, FD]); fw3 = k.din("ffn_w3", [D, FD]); fw2 = k.din("ffn_w2", [FD, D])
    if stop_after is None:
        rt_d = k.din("router", [128, 16, 8]); sel_d = k.din("sel", [8, 8, 128]); selv_d = k.din("selv", [128, 2])
        mw1 = k.din("moe_w1", [8, D, FE]); mw3 = k.din("moe_w3", [8, D, FE]); mw2 = k.din("moe_w2", [8, FE, D])
        xout = k.dout("xo", [128, 16, 1024])
        XO = k.dram("XO", [128, 16, 1024], F32, "Internal"); MIXO = k.dram("MIXO", [128, 16, 1024], F32, "Internal")
    PT = k.dram("PT", [DIN, S], F32, "Internal"); PM = k.dram("PM", [S, DIN], F32, "Internal")
    MIXT = k.dram("MIXT", [D, S], F32, "Internal"); XR = k.dram("XR", [128, 16, S], F32, "Internal")
    C.ones = k.sb([128, 128]); k.memset(C.ones[:], 1.0)
    C.onesb = k.sb([128, 128], BF16); k.memset(C.onesb[:], 1.0)
    C.ident = k.sb([128, 128]); k.dma(C.ident[:], ident_d[:])
    modsb = k.sb([128, 2, 96, 2])
    n1 = k.sb([128, 2, 16]); n2 = k.sb([128, 2, 16]); gs = k.sb([128, 2, 4])
    k.dma(n1[:], n1_d[:]); k.dma(n2[:], n2_d[:]); k.dma(gs[:], gs_d[:])
    with k.scope() as st:
        pss = [k.ps([128, 512], stack=st) for _ in range(8)]
        phase_mod(k, C, ccT_d, modw_d, modb_d, modsb, pss)
    for l in range(2):
        modl = lambda which, l=l: modsb[:, l, which * 16:(which + 1) * 16, :]
        xsrc = xin[:] if l == 0 else XR[:]
        with k.scope() as st:
            pss = [k.ps([128, 512], stack=st) for _ in range(8)]
            phase_inproj(k, C, xsrc, n1[:, l, :], modl(1), modl(0), win_d[l], PT, PM, pss)
        for s in range(2):
            body_ml(k, C, PT, PM, s, {"bi": ml_b[:, l, s, 0:4], "bf": ml_b[:, l, s, 4:8], "gn": ml_gn[:, l, s, :],
                                      "UL4": ul4_d[:]}, MIXT)
            body_ssd(k, C, PT, PM, s, {"cw": ssd_cw[:, l, s], "cb": ssd_cb[:, l, s], "dtb": ssd_v[:, l, s, 0:8],
                                       "alog": ssd_v[:, l, s, 8:16], "dskip": ssd_dk[:, l, s, :], "UL8": ul8_d[:]}, MIXT)
            body_mla(k, C, PT, s, {"wq": mla_wq[l, s], "wkv": mla_wkv[l, s], "qn": mla_v[:, l, 0:4], "kvn": mla_v[:, l, 4:6],
                                   "gq": mla_v[:, l, 6:8], "gk": mla_v[:, l, 8:10], "cosF": rope_c[:], "sinF": rope_s[:],
                                   "RT": rope_r[:]}, MIXT)
            body_na(k, C, PT, PM, s, na_g[:, l, 0:1], na_g[:, l, 1:2], na_bt[:, l, s], MIXT)
        if stop_after == ("mix", l):
            dbg = k.dout("dbg", [D, S])
            with k.scope() as st:
                t = k.sb([128, 16, S], stack=st)
                k.dma(t[:], MIXT[:].rr("(kc p) t -> p kc t", p=128))
                k.dma(dbg[:].rr("(kc p) t -> p kc t", p=128), t[:])
            return k.finish()
        with k.scope() as st:
            pss = [k.ps([128, 512], stack=st) for _ in range(8)]
            if l == 0:
                phase_ffn0(k, C, xin[:], MIXT[:], XR, 0, [(0, 256, 1), (256, 512, 0), (768, 384, 0)], modl, n2[:, l, :],
                           gs[:, l, :], wout_d[l], fw1[:], fw3[:], fw2[:], pss)
                phase_ffn0(k, C, xin[:], MIXT[:], XR, 1152, [(0, 512, 0), (512, 512, 0), (1024, 128, 0)], modl, n2[:, l, :],
                           gs[:, l, :], wout_d[l], fw1[:], fw3[:], fw2[:], pss)
            else:
                phase_select(k, XR[:], MIXT[:].rr("(kc p) t -> p kc t", p=128), XO, MIXO, selv_d)
                phase_moe(k, C, XO[:], MIXO[:], modl, n2[:, l, :], gs[:, l, :], wout_d[l], rt_d[:], sel_d[:],
                          mw1, mw3, mw2, xout, pss)
    return k.finish()


def phase_select(k, xr, mixv, XO, MIXO, selv_d):
    with k.scope() as st:
        sv = k.sb([128, 2], stack=st)
        k.dma(sv[:], selv_d[:])
        a = [k.sb([128, 4, 1024], stack=st) for _ in range(2)]
        b = [k.sb([128, 4, 1024], stack=st) for _ in range(2)]
        it = 0
        for src, dst in ((xr, XO), (mixv, MIXO)):
            for q in range(4):
                ta = a[it % 2]; tb = b[it % 2]; it += 1
                k.dma(ta[:], src[:, q * 4:(q + 1) * 4, 256:1280])
                k.dma(tb[:], src[:, q * 4:(q + 1) * 4, 1280:2304])
                k.ts(ta[:], ta[:], sv[:, 0:1], ALU.mult)
                k.stt(ta[:], tb[:], sv[:, 1:2], ta[:], ALU.mult, ALU.add)
                k.dma(dst[:, q * 4:(q + 1) * 4, :], ta[:], waw=False)


ROPE = rope_tables()
UL4c = tri_consts()
UL8c = tri8()


def _rep(v):
    return np.ascontiguousarray(np.repeat(np.asarray(v, np.float32).reshape(1, -1), 128, 0))


def mega_inputs(z, full=True):
    L = 2
    shared = {}
    shared["mod_w"] = np.ascontiguousarray(z['mod_w'])
    shared["mod_bT"] = np.ascontiguousarray(z['mod_b'].reshape(2, 96, 128).transpose(2, 0, 1))
    shared["n1T"] = np.ascontiguousarray(z['norm1'].reshape(2, 16, 128).transpose(2, 0, 1))
    shared["n2T"] = np.ascontiguousarray(z['norm2'].reshape(2, 16, 128).transpose(2, 0, 1))
    shared["gsT"] = np.ascontiguousarray(z['ssd_norm'].reshape(2, 4, 128).transpose(2, 0, 1))
    shared["w_in"] = np.ascontiguousarray(z['w_in']); shared["w_out"] = np.ascontiguousarray(z['w_out'])
    shared["ident"] = np.eye(128, dtype=np.float32)
    shared["na_g"] = np.ascontiguousarray(np.stack([z['na_gq'], z['na_gk']], -1).transpose(1, 0, 2))
    shared["na_bt"] = np.ascontiguousarray(np.stack(
        [np.stack([na_bias_tiles(z['na_rpb'][l], [2 * s, 2 * s + 1]) for s in range(2)], 1) for l in range(L)], 1))
    shared["mla_wq"] = np.ascontiguousarray(np.stack(
        [np.stack([w_pad(z['mla_w_qb'][l][:, s * 384:(s + 1) * 384], 4) for s in range(2)], 0) for l in range(L)], 0))
    shared["mla_wkv"] = np.ascontiguousarray(np.stack(
        [np.stack([w_pad(z['mla_w_kvb'][l][:, s * 512:(s + 1) * 512], 2) for s in range(2)], 0) for l in range(L)], 0))
    shared["mla_vec"] = np.ascontiguousarray(np.stack(
        [np.concatenate([vec_pad(z['mla_q_norm'][l], 4), vec_pad(z['mla_kv_norm'][l], 2), vec_pad(z['mla_gq'][l], 2),
                         vec_pad(z['mla_gk'][l], 2)], 1) for l in range(L)], 1))
    shared["cosF"], shared["sinF"], shared["RT"] = ROPE
    shared["ml_b"] = np.ascontiguousarray(np.stack(
        [np.stack([_rep(np.concatenate([z['ml_i_bias'][l][:, 2 * s:2 * s + 2].reshape(4),
                                        z['ml_f_bias'][l][:, 2 * s:2 * s + 2].reshape(4)])) for s in range(2)], 1)
         for l in range(L)], 1))
    shared["ml_gn"] = np.ascontiguousarray(np.stack(
        [np.stack([_rep(z['ml_norm'][l][s * 256:(s + 1) * 256]) for s in range(2)], 1) for l in range(L)], 1))
    shared["UL4"] = UL4c; shared["UL8"] = UL8c
    cw_l, cb_l, v_l, dk_l = [], [], [], []
    for l in range(L):
        cw_s, cb_s, v_s, dk_s = [], [], [], []
        for s in range(2):
            chans = np.concatenate([np.arange(s * 256, (s + 1) * 256), 512 + np.arange(s * 128, (s + 1) * 128),
                                    768 + np.arange(s * 128, (s + 1) * 128)])
            cw_s.append(z['ssd_conv_w'][l][:, chans].T.reshape(4, 128, 5).transpose(1, 0, 2))
            cb_s.append(z['ssd_conv_b'][l][chans].reshape(4, 128).T)
            v_s.append(_rep(np.concatenate([z['ssd_dt_bias'][l][:, 4 * s:4 * s + 4].reshape(8),
                                            z['ssd_A_log'][l][:, 4 * s:4 * s + 4].reshape(8)])))
            dk_s.append(_rep(np.repeat(z['ssd_D'][l][4 * s:4 * s + 4], 64)))
        cw_l.append(np.stack(cw_s, 1)); cb_l.append(np.stack(cb_s, 1)); v_l.append(np.stack(v_s, 1)); dk_l.append(np.stack(dk_s, 1))
    shared["ssd_cw"] = np.ascontiguousarray(np.stack(cw_l, 1)); shared["ssd_cb"] = np.ascontiguousarray(np.stack(cb_l, 1))
    shared["ssd_vec"] = np.ascontiguousarray(np.stack(v_l, 1)); shared["ssd_dsk"] = np.ascontiguousarray(np.stack(dk_l, 1))
    if full:
        shared["ffn_w1"] = np.ascontiguousarray(z['ffn_w1'][0]); shared["ffn_w3"] = np.ascontiguousarray(z['ffn_w3'][0])
        shared["ffn_w2"] = np.ascontiguousarray(z['ffn_w2'][0])
        shared["router"] = np.ascontiguousarray(z['moe_router'][0].reshape(16, 128, 8).transpose(1, 0, 2))
        sel = np.zeros((8, 8, 128), np.float32)
        for e in range(8):
            sel[e, e, :] = 1.0
        shared["sel"] = sel
        shared["moe_w1"] = np.ascontiguousarray(z['moe_w1'][0]); shared["moe_w3"] = np.ascontiguousarray(z['moe_w3'][0])
        shared["moe_w2"] = np.ascontiguousarray(z['moe_w2'][0])
    maps = []
    for i in range(NCORES):
        b, s = i // 2, i % 2
        m_ = dict(shared)
        m_["xT"] = to_fm(np.concatenate([z['ctx'][b], z['x'][b]], 0))
        cc = np.stack([z['c'][b], z['c_ctx']], 0)
        m_["ccT"] = np.ascontiguousarray(cc.T.reshape(16, 128, 2).transpose(1, 0, 2))
        if full:
            sv = np.zeros((128, 2), np.float32); sv[:, s] = 1.0
            m_["selv"] = sv
        maps.append(m_)
    return maps


def kernel(**inputs):
    z = {k_: np.asarray(v) for k_, v in inputs.items()}
    res = run(build_mega(), mega_inputs(z))
    out = np.zeros((4, 2048, 2048), np.float32)
    for i in range(NCORES):
        b, s = i // 2, i % 2
        out[b, s * 1024:(s + 1) * 1024] = res[i]["xo"].transpose(2, 1, 0).reshape(1024, 2048)
    return out
```

```python
import contextlib
import numpy as np
import concourse.bass as bass
import concourse.mybir as mybir
from concourse.bass_utils import run_bass_kernel_spmd

F32 = mybir.dt.float32
BF16 = mybir.dt.bfloat16
AF = mybir.ActivationFunctionType
ALU = mybir.AluOpType
AX = mybir.AxisListType
NCORES = 8


class Trk:
    __slots__ = ("w", "r")

    def __init__(self):
        self.w = {}
        self.r = {}


class V:
    __slots__ = ("ap", "trks")

    def __init__(self, ap, trks):
        self.ap = ap
        self.trks = trks

    def __getitem__(self, idx):
        return V(self.ap[idx], self.trks)

    def bc(self, shape):
        return V(self.ap.to_broadcast(shape), self.trks)


class Buf:
    def __init__(self, t, nreg=1):
        self.t = t
        self.regs = [Trk() for _ in range(nreg)]

    def __getitem__(self, idx):
        return V(self.t[idx], self.regs)

    def reg(self, i, idx=None):
        ap = self.t[:] if idx is None else self.t[idx]
        return V(ap, [self.regs[i]])


class Eng:
    def __init__(self, k, name, e):
        self.name = name
        self.e = e
        self.sem = k.newsem("c_" + name)
        self.cnt = 0
        self.waited = {}
        self.dsems = []
        self.dvals = []
        self.dnext = 0


class KB:
    def __init__(self, same_engine_sync=True):
        self.nc = bass.Bass("TRN2", target_bir_lowering=False)
        self.es = contextlib.ExitStack()
        self.es.__enter__()
        self.lp = self.nc.allow_low_precision("bf16 matmul operands, fp32 accumulate")
        self.lp.__enter__()
        self.ncd = self.nc.allow_non_contiguous_dma("tiny per-partition parameter loads")
        self.ncd.__enter__()
        self.nsem = 0
        self.allsems = []
        self.same = same_engine_sync
        nc = self.nc
        self.pe = Eng(self, "pe", nc.tensor)
        self.act = Eng(self, "act", nc.scalar)
        self.dve = Eng(self, "dve", nc.vector)
        self.pool = Eng(self, "pool", nc.gpsimd)
        self.sp = Eng(self, "sp", nc.sync)
        self.engs = [self.pe, self.act, self.dve, self.pool, self.sp]
        for q in (self.sp, self.pool, self.act):
            for i in range(8):
                q.dsems.append(self.newsem("d_%s%d" % (q.name, i)))
                q.dvals.append(0)
        self.nid = 0

    def newsem(self, name):
        s = self.es.enter_context(self.nc.semaphore(name))
        self.allsems.append([s, 0])
        return s

    def dram(self, name, shape, dtype, kind):
        t = self.nc.dram_tensor(name, list(shape), dtype, kind=kind).ap()
        return Buf(t)

    def din(self, name, shape, dtype=F32):
        return self.dram(name, shape, dtype, "ExternalInput")

    def dout(self, name, shape, dtype=F32):
        return self.dram(name, shape, dtype, "ExternalOutput")

    def sb(self, shape, dtype=F32, nreg=1, name=None, stack=None):
        self.nid += 1
        name = name or ("sb%d" % self.nid)
        t = (stack or self.es).enter_context(self.nc.sbuf_tensor(name, list(shape), dtype))
        return Buf(t, nreg)

    def ps(self, shape, dtype=F32, nreg=1, name=None, stack=None):
        self.nid += 1
        name = name or ("ps%d" % self.nid)
        t = (stack or self.es).enter_context(self.nc.psum_tensor(name, list(shape), dtype))
        return Buf(t, nreg)

    @contextlib.contextmanager
    def scope(self):
        st = contextlib.ExitStack()
        with st:
            yield st
            self.barrier()

    def _semval(self, sem):
        for sv in self.allsems:
            if sv[0] is sem:
                return sv
        raise KeyError

    def _wait(self, eng, ev):
        sem, val = ev
        if eng.waited.get(id(sem), 0) >= val:
            return
        if sem is eng.sem and not self.same:
            return
        if sem is eng.sem and eng is self.pe:
            return
        eng.e.wait_ge(sem, val)
        eng.waited[id(sem)] = val

    def emit(self, eng, fn, reads, writes, dma=False, waw=True):
        evs = []
        for v in reads:
            for t in v.trks:
                evs.extend(t.w.values())
        for v in writes:
            for t in v.trks:
                if waw:
                    evs.extend(t.w.values())
                evs.extend(t.r.values())
        if dma:
            i = eng.dnext
            eng.dnext = (i + 1) % len(eng.dsems)
            sem = eng.dsems[i]
            if eng.dvals[i] > 0:
                evs.append((sem, eng.dvals[i]))
        for ev in evs:
            self._wait(eng, ev)
        ins = fn()
        if dma:
            eng.dvals[i] += 16
            ins.then_inc(sem, 16)
            ev = (sem, eng.dvals[i])
            self._semval(sem)[1] = eng.dvals[i]
        else:
            eng.cnt += 1
            ins.then_inc(eng.sem, 1)
            ev = (eng.sem, eng.cnt)
            self._semval(eng.sem)[1] = eng.cnt
        for v in reads:
            for t in v.trks:
                t.r[id(ev[0])] = ev
        for v in writes:
            for t in v.trks:
                if waw:
                    t.w = {}
                    t.r = {}
                t.w[id(ev[0])] = ev
        return ins

    def barrier(self):
        for eng in self.engs:
            for sem, val in self.allsems:
                if val > 0:
                    self._wait_force(eng, (sem, val))

    def _wait_force(self, eng, ev):
        sem, val = ev
        if eng.waited.get(id(sem), 0) >= val:
            return
        eng.e.wait_ge(sem, val)
        eng.waited[id(sem)] = val

    def finish(self):
        self.barrier()
        self.ncd.__exit__(None, None, None)
        self.lp.__exit__(None, None, None)
        self.es.__exit__(None, None, None)
        return self.nc

    def _E(self, eng):
        return {"pe": self.pe, "act": self.act, "dve": self.dve, "pool": self.pool, "sp": self.sp}[eng]

    def dma(self, out, in_, q="sp", waw=True):
        e = self._E(q)
        return self.emit(e, lambda: e.e.dma_start(out=out.ap, in_=in_.ap), [in_], [out], dma=True, waw=waw)

    def mm(self, out, lhsT, rhs, start=True, stop=True):
        return self.emit(self.pe, lambda: self.nc.tensor.matmul(out.ap, lhsT.ap, rhs.ap, start=start, stop=stop),
                         [lhsT, rhs], [out])

    def transpose(self, out, in_, ident):
        return self.emit(self.pe, lambda: self.nc.tensor.transpose(out.ap, in_.ap, ident.ap), [in_, ident], [out])

    def actf(self, out, in_, func, bias=None, scale=1.0, accum=None):
        reads = [in_]
        kw = {}
        if isinstance(bias, V):
            reads.append(bias)
            kw["bias"] = bias.ap
        elif bias is not None:
            kw["bias"] = float(bias)
        if isinstance(scale, V):
            reads.append(scale)
            kw["scale"] = scale.ap
        else:
            kw["scale"] = float(scale)
        writes = [out]
        if accum is not None:
            writes.append(accum)
            kw["accum_out"] = accum.ap
        return self.emit(self.act, lambda: self.nc.scalar.activation(out=out.ap, in_=in_.ap, func=func, **kw),
                         reads, writes)

    def _vec(self, eng):
        e = self._E(eng)
        return e, e.e

    def tt(self, out, in0, in1, op, eng="dve"):
        e, x = self._vec(eng)
        return self.emit(e, lambda: x.tensor_tensor(out=out.ap, in0=in0.ap, in1=in1.ap, op=op), [in0, in1], [out])

    def ts(self, out, in0, s1, op0, s2=None, op1=None, eng="dve", accum=None):
        e, x = self._vec(eng)
        reads = [in0]
        a1 = s1.ap if isinstance(s1, V) else float(s1)
        if isinstance(s1, V):
            reads.append(s1)
        a2 = None
        if s2 is not None:
            a2 = s2.ap if isinstance(s2, V) else float(s2)
            if isinstance(s2, V):
                reads.append(s2)
        kw = {}
        if op1 is not None:
            kw["op1"] = op1
        writes = [out]
        if accum is not None:
            kw["accum_out"] = accum.ap
            writes.append(accum)
        return self.emit(e, lambda: x.tensor_scalar(out=out.ap, in0=in0.ap, scalar1=a1, scalar2=a2, op0=op0, **kw),
                         reads, writes)

    def stt(self, out, in0, scalar, in1, op0, op1, eng="dve"):
        e, x = self._vec(eng)
        reads = [in0, in1]
        sc = scalar.ap if isinstance(scalar, V) else float(scalar)
        if isinstance(scalar, V):
            reads.append(scalar)
        return self.emit(e, lambda: x.scalar_tensor_tensor(out=out.ap, in0=in0.ap, scalar=sc, in1=in1.ap,
                                                           op0=op0, op1=op1), reads, [out])

    def copy(self, out, in_, eng="dve"):
        if eng == "act":
            return self.actf(out, in_, AF.Copy)
        e, x = self._vec(eng)
        return self.emit(e, lambda: x.tensor_copy(out=out.ap, in_=in_.ap), [in_], [out])

    def recip(self, out, in_):
        return self.emit(self.dve, lambda: self.nc.vector.reciprocal(out=out.ap, in_=in_.ap), [in_], [out])

    def reduce(self, out, in_, op, axis=AX.X, eng="dve"):
        e, x = self._vec(eng)
        return self.emit(e, lambda: x.tensor_reduce(out=out.ap, in_=in_.ap, axis=axis, op=op), [in_], [out])

    def memset(self, out, val, eng="dve"):
        e, x = self._vec(eng)
        return self.emit(e, lambda: x.memset(out.ap, val), [], [out])


def run(nc, in_maps):
    res = run_bass_kernel_spmd(nc, in_maps, core_ids=list(range(len(in_maps))))
    return res.results


def _v_rr(self, pattern, **kw):
    return V(self.ap.rearrange(pattern, **kw), self.trks)


V.rr = _v_rr


def _v_unsq(self, axis):
    return V(self.ap.unsqueeze(axis), self.trks)


V.unsq = _v_unsq


D = 2048
DIN = 5824
EPS = 1e-6
TILES = [(0, 512, 0), (512, 512, 0), (1024, 128, 1)]


def norm_mod(k, xT, hT, ones, gT, scT, shT, pss, tmp, tiles=TILES, stack=None):
    A = k.sb([128, 16, 2], stack=stack)
    for c in range(2):
        k.ts(A[:, :, c], scT[:, :, c], 1.0, ALU.add)
        k.tt(A[:, :, c], A[:, :, c], gT[:, :], ALU.mult)
    for (t0, n, col) in tiles:
        ps = pss[0]
        for kc in range(16):
            sq = tmp[kc % 2]
            k.actf(sq[:, 0:n], xT[:, kc, t0:t0 + n], AF.Square)
            k.mm(ps[:, 0:n], ones[:, :], sq[:, 0:n], start=(kc == 0), stop=(kc == 15))
        rs = tmp[2]
        k.ts(rs[:, 0:n], ps[:, 0:n], 1.0 / D, ALU.mult, EPS, ALU.add)
        k.actf(rs[:, 0:n], rs[:, 0:n], AF.Sqrt)
        k.recip(rs[:, 0:n], rs[:, 0:n])
        for kc in range(16):
            t = tmp[kc % 2]
            k.tt(t[:, 0:n], xT[:, kc, t0:t0 + n], rs[:, 0:n], ALU.mult)
            k.ts(hT[:, kc, t0:t0 + n], t[:, 0:n], A[:, kc, col:col + 1], ALU.mult, shT[:, kc, col:col + 1], ALU.add,
                 eng="pool" if kc % 2 else "dve")


def build_k1(NT=1152):
    k = KB()
    xTd = k.din("xT", [128, 16, NT])
    gTd = k.din("gT", [128, 16]); scTd = k.din("scT", [128, 16, 2]); shTd = k.din("shT", [128, 16, 2])
    w = k.din("w", [D, DIN])
    p = k.dout("p", [NT, DIN])
    xT = k.sb([128, 16, NT]); hT = k.sb([128, 16, NT], BF16)
    gT = k.sb([128, 16]); scT = k.sb([128, 16, 2]); shT = k.sb([128, 16, 2])
    ones = k.sb([128, 128]); k.memset(ones[:], 1.0)
    tmp = [k.sb([128, 512]) for _ in range(3)]
    pss = [k.ps([128, 512]) for _ in range(4)]
    for q in range(4):
        k.dma(xT[:, q * 4:(q + 1) * 4, :], xTd[:, q * 4:(q + 1) * 4, :])
    k.dma(gT[:], gTd[:]); k.dma(scT[:], scTd[:]); k.dma(shT[:], shTd[:])
    wsb = [k.sb([128, 16, 512], BF16) for _ in range(2)]
    wv = w[:].rr("(kc p) n -> p kc n", p=128)
    groups = [(c0, min(512, DIN - c0)) for c0 in range(0, DIN, 512)]

    def loadw(gi):
        c0, cn = groups[gi]
        for q in range(4):
            k.dma(wsb[gi % 2][:, q * 4:(q + 1) * 4, 0:cn], wv[:, q * 4:(q + 1) * 4, c0:c0 + cn], q="pool")
    loadw(0)
    norm_mod(k, xT, hT, ones, gT, scT, shT, pss, tmp)
    stg = [k.sb([128, 512]) for _ in range(3)]
    it = 0
    for gi, (c0, cn) in enumerate(groups):
        if gi + 1 < len(groups):
            loadw(gi + 1)
        for tb in range(NT // 128):
            ps = pss[it % 4]; st = stg[it % 3]
            for kc in range(16):
                k.mm(ps[:, 0:cn], hT[:, kc, tb * 128:(tb + 1) * 128], wsb[gi % 2][:, kc, 0:cn],
                     start=(kc == 0), stop=(kc == 15))
            if it % 2:
                k.copy(st[:, 0:cn], ps[:, 0:cn], eng="dve")
            else:
                k.copy(st[:, 0:cn], ps[:, 0:cn], eng="act")
            k.dma(p[tb * 128:(tb + 1) * 128, c0:c0 + cn], st[:, 0:cn], q="sp")
            it += 1
    return k.finish()


def to_fm(a):
    return np.ascontiguousarray(a.T.reshape(16, 128, a.shape[0]).transpose(1, 0, 2))


def vec_fm(v):
    return np.ascontiguousarray(v.reshape(16, 128).T)


def core_tokens(x, xc, i):
    b, s = i // 2, i % 2
    return np.concatenate([x[b, s * 1024:(s + 1) * 1024], xc[b, s * 128:(s + 1) * 128]], 0)


def mod_cols(mod_l, which, b):
    sl = mod_l[:, which * D:(which + 1) * D]
    return np.ascontiguousarray(np.stack([vec_fm(sl[b]), vec_fm(sl[4])], -1))


def run_k1(xT_list, mod_l, norm1_l, w_in_l, nc=None):
    maps = []
    for i in range(NCORES):
        b = i // 2
        maps.append({"xT": xT_list[i], "gT": vec_fm(norm1_l), "scT": mod_cols(mod_l, 1, b),
                     "shT": mod_cols(mod_l, 0, b), "w": w_in_l})
    res = run(nc or build_k1(), maps)
    return [r["p"] for r in res]


D = 2048
EPS = 1e-6


def ffn(k, h2T, tiles, w1d, w3d, w2d, F, acc_fn, pss, FG=256, stack=None, gate=None):
    NT = sum(n for _, n, _ in tiles)
    nfc = FG // 128
    w1s = [k.sb([128, 16, FG], BF16, stack=stack) for _ in range(2)]
    w3s = [k.sb([128, 16, FG], BF16, stack=stack) for _ in range(2)]
    w2s = [k.sb([128, nfc, D], BF16, stack=stack) for _ in range(2)]
    aT = [k.sb([128, nfc, NT], BF16, stack=stack) for _ in range(2)]
    tmp = [k.sb([128, 512], stack=stack) for _ in range(2)]
    w1v = w1d.rr("(kc p) f -> p kc f", p=128)
    w3v = w3d.rr("(kc p) f -> p kc f", p=128)
    w2v = w2d.rr("(fc p) d -> p fc d", p=128)
    ng = F // FG

    def loadA(g):
        f0 = g * FG
        for q in range(2):
            k.dma(w1s[g % 2][:, q * 8:(q + 1) * 8, :], w1v[:, q * 8:(q + 1) * 8, f0:f0 + FG], q="pool")
            k.dma(w3s[g % 2][:, q * 8:(q + 1) * 8, :], w3v[:, q * 8:(q + 1) * 8, f0:f0 + FG], q="pool")

    def loadB(g):
        k.dma(w2s[g % 2][:, :, :], w2v[:, g * nfc:(g + 1) * nfc, :], q="pool")
    cnt = [0, 0]

    def up(g, fc, t0, n):
        it = cnt[0]; cnt[0] += 1
        p1 = pss[(it % 2) * 2]; p3 = pss[(it % 2) * 2 + 1]; tm = tmp[it % 2]
        for kc in range(16):
            k.mm(p1[:, 0:n], w1s[g % 2][:, kc, fc * 128:(fc + 1) * 128], h2T[:, kc, t0:t0 + n], start=(kc == 0), stop=(kc == 15))
        for kc in range(16):
            k.mm(p3[:, 0:n], w3s[g % 2][:, kc, fc * 128:(fc + 1) * 128], h2T[:, kc, t0:t0 + n], start=(kc == 0), stop=(kc == 15))
        k.actf(tm[:, 0:n], p1[:, 0:n], AF.Silu)
        if gate is not None:
            k.tt(tm[:, 0:n], tm[:, 0:n], gate[:, t0:t0 + n], ALU.mult, eng="pool")
        k.tt(aT[g % 2][:, fc, t0:t0 + n], tm[:, 0:n], p3[:, 0:n], ALU.mult)

    def down(g, dc, t0, n):
        jt = cnt[1]; cnt[1] += 1
        po = pss[4 + jt % 4]
        for fc in range(nfc):
            k.mm(po[:, 0:n], w2s[g % 2][:, fc, dc * 128:(dc + 1) * 128], aT[g % 2][:, fc, t0:t0 + n],
                 start=(fc == 0), stop=(fc == nfc - 1))
        acc_fn(po[:, 0:n], dc, t0, n)
    ups = [(fc, t0, n) for fc in range(nfc) for (t0, n, _) in tiles]
    downs = [(dc, t0, n) for dc in range(16) for (t0, n, _) in tiles]
    loadA(0); loadB(0)
    if ng > 1:
        loadA(1)
    for u in ups:
        up(0, *u)
    per = -(-len(downs) // len(ups))
    for g in range(ng):
        if g + 2 < ng:
            loadA(g + 2)
        if g + 1 < ng:
            loadB(g + 1)
        di = 0
        for u in (ups if g + 1 < ng else []):
            up(g + 1, *u)
            for dd in downs[di:di + per]:
                down(g, *dd)
            di += per
        for dd in downs[di:]:
            down(g, *dd)


def ffn_simple(k, h2T, tiles, w1d, w3d, w2d, F, acc_fn, pss, FG=512, stack=None, gate=None):
    NT = sum(n for _, n, _ in tiles)
    nfc = FG // 128
    w1s = [k.sb([128, 16, FG], BF16, stack=stack) for _ in range(2)]
    w3s = [k.sb([128, 16, FG], BF16, stack=stack) for _ in range(2)]
    w2s = k.sb([128, nfc, D], BF16, stack=stack)
    a = k.sb([128, nfc, NT], BF16, stack=stack)
    tmp = [k.sb([128, 512], stack=stack) for _ in range(2)]
    w1v = w1d.rr("(kc p) f -> p kc f", p=128)
    w3v = w3d.rr("(kc p) f -> p kc f", p=128)
    w2v = w2d.rr("(fc p) d -> p fc d", p=128)
    ng = F // FG

    def loadA(g):
        f0 = g * FG
        for q in range(2):
            k.dma(w1s[g % 2][:, q * 8:(q + 1) * 8, :], w1v[:, q * 8:(q + 1) * 8, f0:f0 + FG], q="pool")
            k.dma(w3s[g % 2][:, q * 8:(q + 1) * 8, :], w3v[:, q * 8:(q + 1) * 8, f0:f0 + FG], q="pool")

    def loadB(g):
        for q in range(nfc // 2):
            k.dma(w2s[:, q * 2:(q + 1) * 2, :], w2v[:, g * nfc + q * 2:g * nfc + (q + 1) * 2, :], q="pool")
    loadA(0); loadB(0)
    it = 0
    jt = 0
    for g in range(ng):
        if g + 1 < ng:
            loadA(g + 1)
        for (t0, n, _) in tiles:
            for fc in range(nfc):
                p1 = pss[(it % 2) * 2]; p3 = pss[(it % 2) * 2 + 1]; tm = tmp[it % 2]
                for kc in range(16):
                    k.mm(p1[:, 0:n], w1s[g % 2][:, kc, fc * 128:(fc + 1) * 128], h2T[:, kc, t0:t0 + n],
                         start=(kc == 0), stop=(kc == 15))
                for kc in range(16):
                    k.mm(p3[:, 0:n], w3s[g % 2][:, kc, fc * 128:(fc + 1) * 128], h2T[:, kc, t0:t0 + n],
                         start=(kc == 0), stop=(kc == 15))
                k.actf(tm[:, 0:n], p1[:, 0:n], AF.Silu)
                if gate is not None:
                    k.tt(tm[:, 0:n], tm[:, 0:n], gate[:, t0:t0 + n], ALU.mult, eng="pool")
                k.tt(a[:, fc, t0:t0 + n], tm[:, 0:n], p3[:, 0:n], ALU.mult)
                it += 1
        for (t0, n, _) in tiles:
            for dc in range(16):
                po = pss[4 + jt % 4]
                for fc in range(nfc):
                    k.mm(po[:, 0:n], w2s[:, fc, dc * 128:(dc + 1) * 128], a[:, fc, t0:t0 + n],
                         start=(fc == 0), stop=(fc == nfc - 1))
                acc_fn(po[:, 0:n], dc, t0, n)
                jt += 1
        if g + 1 < ng:
            loadB(g + 1)


def build_k3(NT=1152, F=5632, tiles=TILES):
    k = KB()
    mixd = k.din("mixT", [128, 16, NT])
    xTd = k.din("xT", [128, 16, NT])
    wo = k.din("w_out", [D, D])
    g1d = k.din("g1T", [128, 16, 2]); g2d = k.din("g2T", [128, 16, 2])
    n2d = k.din("n2T", [128, 16]); sc2d = k.din("sc2T", [128, 16, 2]); sh2d = k.din("sh2T", [128, 16, 2])
    gsd = k.din("gsT", [128, 4])
    w1d = k.din("w1", [D, F]); w3d = k.din("w3", [D, F]); w2d = k.din("w2", [F, D])
    xo = k.dout("xo", [128, 16, NT])
    xT = k.sb([128, 16, NT])
    g1 = k.sb([128, 16, 2]); g2 = k.sb([128, 16, 2]); n2 = k.sb([128, 16]); sc2 = k.sb([128, 16, 2])
    sh2 = k.sb([128, 16, 2]); gs = k.sb([128, 4])
    ones = k.sb([128, 128]); k.memset(ones[:], 1.0)
    pss = [k.ps([128, 512]) for _ in range(8)]
    for q in range(4):
        k.dma(xT[:, q * 4:(q + 1) * 4, :], xTd[:, q * 4:(q + 1) * 4, :])
    for a, b in ((g1, g1d), (g2, g2d), (n2, n2d), (sc2, sc2d), (sh2, sh2d), (gs, gsd)):
        k.dma(a[:], b[:])
    with k.scope() as st:
        mixb = k.sb([128, 16, NT], BF16, stack=st)
        ssd = k.sb([128, 4, NT], stack=st)
        tmp = [k.sb([128, 512], stack=st) for _ in range(3)]
        wos = [k.sb([128, 16, 512], BF16, stack=st) for _ in range(2)]
        wov = wo[:].rr("(kc p) n -> p kc n", p=128)

        def loadwo(g):
            for q in range(2):
                k.dma(wos[g % 2][:, q * 8:(q + 1) * 8, :], wov[:, q * 8:(q + 1) * 8, g * 512:(g + 1) * 512], q="pool")
        k.dma(mixb[:, 0:4, :], mixd[:, 0:4, :], q="pool")
        k.dma(mixb[:, 8:12, :], mixd[:, 8:12, :], q="pool")
        k.dma(mixb[:, 12:16, :], mixd[:, 12:16, :], q="pool")
        k.dma(ssd[:], mixd[:, 4:8, :])
        loadwo(0)
        for (t0, n, _) in tiles:
            ps = pss[0]
            for c in range(4):
                sq = tmp[c % 2]
                k.actf(sq[:, 0:n], ssd[:, c, t0:t0 + n], AF.Square)
                k.mm(ps[:, 0:n], ones[:, :], sq[:, 0:n], start=(c == 0), stop=(c == 3))
            rs = tmp[2]
            k.ts(rs[:, 0:n], ps[:, 0:n], 1.0 / 512, ALU.mult, EPS, ALU.add)
            k.actf(rs[:, 0:n], rs[:, 0:n], AF.Sqrt)
            k.recip(rs[:, 0:n], rs[:, 0:n])
            for c in range(4):
                k.stt(mixb[:, 4 + c, t0:t0 + n], ssd[:, c, t0:t0 + n], gs[:, c:c + 1], rs[:, 0:n], ALU.mult, ALU.mult)
        jt = 0
        for g in range(4):
            if g + 1 < 4:
                loadwo(g + 1)
            for dl in range(4):
                dc = g * 4 + dl
                for (t0, n, col) in tiles:
                    po = pss[4 + jt % 4]
                    for kc in range(16):
                        k.mm(po[:, 0:n], wos[g % 2][:, kc, dl * 128:(dl + 1) * 128], mixb[:, kc, t0:t0 + n],
                             start=(kc == 0), stop=(kc == 15))
                    k.stt(xT[:, dc, t0:t0 + n], po[:, 0:n], g1[:, dc, col:col + 1], xT[:, dc, t0:t0 + n],
                          ALU.mult, ALU.add)
                    jt += 1
    h2T = k.sb([128, 16, NT], BF16)
    with k.scope() as st:
        tmp = [k.sb([128, 512], stack=st) for _ in range(3)]
        norm_mod(k, xT, h2T, ones, n2, sc2, sh2, pss, tmp, tiles, stack=st)
    colof = {t0: col for (t0, n, col) in tiles}

    def acc(po, dc, t0, n):
        col = colof[t0]
        k.stt(xT[:, dc, t0:t0 + n], po, g2[:, dc, col:col + 1], xT[:, dc, t0:t0 + n], ALU.mult, ALU.add)
    ffn(k, h2T, tiles, w1d[:], w3d[:], w2d[:], F, acc, pss)
    for q in range(4):
        k.dma(xo[:, q * 4:(q + 1) * 4, :], xT[:, q * 4:(q + 1) * 4, :])
    return k.finish()


def run_k3(mixT_list, xT_list, mod_l, l, z, nc=None):
    maps = []
    for i in range(NCORES):
        b = i // 2
        maps.append({"mixT": mixT_list[i], "xT": xT_list[i], "w_out": np.ascontiguousarray(z['w_out'][l]),
                     "g1T": mod_cols(mod_l, 2, b), "g2T": mod_cols(mod_l, 5, b), "n2T": vec_fm(z['norm2'][l]),
                     "sc2T": mod_cols(mod_l, 4, b), "sh2T": mod_cols(mod_l, 3, b),
                     "gsT": np.ascontiguousarray(z['ssd_norm'][l].reshape(4, 128).T),
                     "w1": np.ascontiguousarray(z['ffn_w1'][0]), "w3": np.ascontiguousarray(z['ffn_w3'][0]),
                     "w2": np.ascontiguousarray(z['ffn_w2'][0])})
    res = run(nc or build_k3(), maps)
    return [r["xo"] for r in res]


S = 2304
TT = [(0, 512), (512, 512), (1024, 512), (1536, 512), (2048, 256)]
EPS = 1e-6


def headnorm_fm(k, srcT, dstT, h, gcol, ones, ps, tmp, extra_scale=1.0, nfeat=128):
    for (t0, n) in TT:
        sq = tmp[0]
        k.actf(sq[:, 0:n], srcT[:, h, t0:t0 + n], AF.Square)
        k.mm(ps[:, 0:n], ones[:, :], sq[:, 0:n])
        rs = tmp[1]
        k.ts(rs[:, 0:n], ps[:, 0:n], 1.0 / nfeat, ALU.mult, EPS, ALU.add)
        k.actf(rs[:, 0:n], rs[:, 0:n], AF.Sqrt)
        k.recip(rs[:, 0:n], rs[:, 0:n])
        k.tt(rs[:, 0:n], srcT[:, h, t0:t0 + n], rs[:, 0:n], ALU.mult)
        k.ts(dstT[:, h, t0:t0 + n], rs[:, 0:n], gcol, ALU.mult, extra_scale, ALU.mult)


def build_na():
    k = KB()
    qd = k.din("qT", [128, 2, S]); kd = k.din("kT", [128, 2, S])
    vAd = k.din("vA", [128, 18, 256]); vBd = k.din("vB", [128, 15, 256])
    gqd = k.din("gq", [128, 1]); gkd = k.din("gk", [128, 1])
    btd = k.din("bt", [128, 2, 14, 64])
    od = k.dout("oT", [128, 2, S])
    qf = k.sb([128, 2, S]); kf = k.sb([128, 2, S])
    qb = k.sb([128, 2, S], BF16); kb = k.sb([128, 2, S], BF16)
    vA = k.sb([128, 18, 256], BF16); vB = k.sb([128, 15, 256], BF16)
    gq = k.sb([128, 1]); gk = k.sb([128, 1]); bt = k.sb([128, 2, 14, 64])
    oT = k.sb([128, 2, S])
    ones = k.sb([128, 128]); k.memset(ones[:], 1.0)
    onesb = k.sb([128, 128], BF16); k.memset(onesb[:], 1.0)
    tmp = [k.sb([128, 512]) for _ in range(3)]
    pss = [k.ps([128, 512]) for _ in range(8)]
    k.dma(qf[:], qd[:]); k.dma(kf[:], kd[:])
    k.dma(vA[:], vAd[:], q="pool"); k.dma(vB[:], vBd[:], q="pool")
    k.dma(gq[:], gqd[:]); k.dma(gk[:], gkd[:]); k.dma(bt[:], btd[:])
    scale = 128 ** -0.5
    for h in range(2):
        headnorm_fm(k, qf, qb, h, gq[:, 0:1], ones, pss[0], tmp, extra_scale=scale)
        headnorm_fm(k, kf, kb, h, gk[:, 0:1], ones, pss[1], tmp)
    pT = [k.sb([128, 384], BF16) for _ in range(3)]
    it = 0
    for h in range(2):
        for rg in range(4):
            po = pss[4 + (it % 2) * 2]; pd = pss[5 + (it % 2) * 2]
            it += 1
            for rl in range(8):
                r = rg * 8 + rl
                r0 = min(max(r - 4, 0), 24)
                q0 = 256 + r * 64
                ps_s = pss[r % 4]
                vblks = []
                for j in range(4):
                    kt0 = 256 + r0 * 64 + 128 * j
                    k.mm(ps_s[:, j * 64:(j + 1) * 64], kb[:, h, kt0:kt0 + 128], qb[:, h, q0:q0 + 64])
                    if r0 % 2 == 0:
                        vblks.append(vA[:, 2 + r0 // 2 + j, h * 128:(h + 1) * 128])
                    else:
                        vblks.append(vB[:, (r0 - 1) // 2 + j, h * 128:(h + 1) * 128])
                for j in range(2):
                    k.mm(ps_s[:, 256 + j * 64:256 + (j + 1) * 64], kb[:, h, j * 128:(j + 1) * 128], qb[:, h, q0:q0 + 64])
                    vblks.append(vA[:, j, h * 128:(h + 1) * 128])
                m0 = r0 - r + 7
                tm = tmp[r % 2]
                k.tt(tm[:, 0:256].rr("p (j c) -> p j c", j=4), ps_s[:, 0:256].rr("p (j c) -> p j c", j=4),
                     bt[:, h, m0:m0 + 7:2, :], ALU.add)
                p = pT[r % 3]
                k.actf(p[:, 0:256], tm[:, 0:256], AF.Exp)
                k.actf(p[:, 256:384], ps_s[:, 256:384], AF.Exp)
                for j in range(6):
                    k.mm(po[:, rl * 64:(rl + 1) * 64], vblks[j], p[:, j * 64:(j + 1) * 64], start=(j == 0), stop=(j == 5))
                for j in range(6):
                    k.mm(pd[:, rl * 64:(rl + 1) * 64], onesb[:, :], p[:, j * 64:(j + 1) * 64], start=(j == 0), stop=(j == 5))
            rd = tmp[2]
            k.recip(rd[:, :], pd[:, :])
            k.tt(oT[:, h, 256 + rg * 512:256 + (rg + 1) * 512], po[:, :], rd[:, :], ALU.mult)
        ps_s = pss[0]; po = pss[4]; pd = pss[5]
        p = k.sb([128, 512], BF16)
        for j in range(2):
            k.mm(ps_s[:, j * 256:(j + 1) * 256], kb[:, h, j * 128:(j + 1) * 128], qb[:, h, 0:256])
        k.actf(p[:, :], ps_s[:, :], AF.Exp)
        for j in range(2):
            k.mm(po[:, 0:256], vA[:, j, h * 128:(h + 1) * 128], p[:, j * 256:(j + 1) * 256], start=(j == 0), stop=(j == 1))
        for j in range(2):
            k.mm(pd[:, 0:256], onesb[:, :], p[:, j * 256:(j + 1) * 256], start=(j == 0), stop=(j == 1))
        rd = tmp[2]
        k.recip(rd[:, 0:256], pd[:, 0:256])
        k.tt(oT[:, h, 0:256], po[:, 0:256], rd[:, 0:256], ALU.mult)
    k.dma(od[:], oT[:])
    return k.finish()


def na_bias_tiles(rpb_l, heads):
    bt = np.full((2, 64, len(heads), 14, 64), -30000.0, np.float32)
    qc = np.arange(64)
    c0 = np.clip(qc - 8, 0, 48)
    for kc in range(64):
        ok = (kc >= c0) & (kc < c0 + 16)
        dc = kc - qc + 15
        for a in range(2):
            for m in range(14):
                for hi, h in enumerate(heads):
                    bt[a, kc, hi, m, ok] = rpb_l[h, m + a, dc[ok]]
    return np.ascontiguousarray(bt.reshape(128, len(heads), 14, 64))


def fm_heads(a, nh=2, hd=128):
    return np.ascontiguousarray(a.reshape(a.shape[0], nh, hd).transpose(2, 1, 0))


def na_inputs(P_b, s, z, l):
    hs = [2 * s, 2 * s + 1]
    c = 4288
    q = P_b[:, c + s * 256: c + (s + 1) * 256]
    kk = P_b[:, c + 512 + s * 256: c + 512 + (s + 1) * 256]
    v = P_b[:, c + 1024 + s * 256: c + 1024 + (s + 1) * 256]
    vA = np.ascontiguousarray(v.reshape(18, 128, 256).transpose(1, 0, 2))
    vB = np.ascontiguousarray(v[256 + 64:256 + 64 + 15 * 128].reshape(15, 128, 256).transpose(1, 0, 2))
    return {"qT": fm_heads(q), "kT": fm_heads(kk), "vA": vA, "vB": vB,
            "gq": np.ascontiguousarray(z['na_gq'][l].reshape(128, 1)), "gk": np.ascontiguousarray(z['na_gk'][l].reshape(128, 1)),
            "bt": na_bias_tiles(z['na_rpb'][l], hs)}


def seq_P(r, l, b):
    return np.concatenate([r[f'p_c{l}'][b], r[f'p_l{l}'][b]], 0)


EPS = 1e-6


def rope_tables():
    t = np.arange(2048)
    row = (t // 64).astype(np.float32); col = (t % 64).astype(np.float32)
    freqs = (10000.0 ** (-np.arange(16, dtype=np.float32) / 16)).astype(np.float32)
    ang = np.concatenate([row[:, None] * freqs, col[:, None] * freqs], -1)
    cos = np.cos(ang).T; sin = np.sin(ang).T
    cosF = np.concatenate([cos, cos], 0).astype(np.float32)
    sinF = np.concatenate([sin, sin], 0).astype(np.float32)
    R = np.zeros((64, 64), np.float32)
    for i in range(32):
        R[i, 32 + i] = -1.0
        R[32 + i, i] = 1.0
    return np.ascontiguousarray(cosF), np.ascontiguousarray(sinF), np.ascontiguousarray(R.T)


def build_mla():
    k = KB()
    cqd = k.din("cqT", [128, 4, S]); ckvd = k.din("ckvT", [128, 2, S]); krd = k.din("krT", [64, S])
    wqd = k.din("wq", [128, 4, 384]); wkvd = k.din("wkv", [128, 2, 512])
    qnd = k.din("qn", [128, 4]); kvnd = k.din("kvn", [128, 2])
    gqd = k.din("gq", [128, 2]); gkd = k.din("gk", [128, 2])
    cosd = k.din("cosF", [64, 2048]); sind = k.din("sinF", [64, 2048]); rtd = k.din("RT", [64, 64])
    od = k.dout("oT", [128, 2, S])
    ones = k.sb([128, 128]); k.memset(ones[:], 1.0)
    onesb = k.sb([128, 128], BF16); k.memset(onesb[:], 1.0)
    wq = k.sb([128, 4, 384], BF16); wkv = k.sb([128, 2, 512], BF16)
    qn = k.sb([128, 4]); kvn = k.sb([128, 2]); gq = k.sb([128, 2]); gk = k.sb([128, 2])
    cosF = k.sb([64, 2048]); sinF = k.sb([64, 2048]); RT = k.sb([64, 64])
    krf = k.sb([64, S])
    cqn = k.sb([128, 4, S], BF16); ckvn = k.sb([128, 2, S], BF16)
    oT = k.sb([128, 2, S])
    pss = [k.ps([128, 512]) for _ in range(8)]
    tmp = [k.sb([128, 512]) for _ in range(4)]
    k.dma(wq[:], wqd[:], q="pool"); k.dma(wkv[:], wkvd[:], q="pool")
    for a, b in ((qn, qnd), (kvn, kvnd), (gq, gqd), (gk, gkd), (cosF, cosd), (sinF, sind), (RT, rtd), (krf, krd)):
        k.dma(a[:], b[:])
    with k.scope() as st:
        cq = k.sb([128, 4, S], stack=st); ckv = k.sb([128, 2, S], stack=st)
        k.dma(cq[:], cqd[:]); k.dma(ckv[:], ckvd[:])
        for (src, dst, nch, nfeat, gn) in ((cq, cqn, 4, 448, qn), (ckv, ckvn, 2, 160, kvn)):
            for (t0, n) in TT:
                ps = pss[0]
                for c in range(nch):
                    sq = tmp[c % 2]
                    k.actf(sq[:, 0:n], src[:, c, t0:t0 + n], AF.Square)
                    k.mm(ps[:, 0:n], ones[:, :], sq[:, 0:n], start=(c == 0), stop=(c == nch - 1))
                rs = tmp[2]
                k.ts(rs[:, 0:n], ps[:, 0:n], 1.0 / nfeat, ALU.mult, EPS, ALU.add)
                k.actf(rs[:, 0:n], rs[:, 0:n], AF.Sqrt)
                k.recip(rs[:, 0:n], rs[:, 0:n])
                for c in range(nch):
                    k.stt(dst[:, c, t0:t0 + n], src[:, c, t0:t0 + n], gn[:, c:c + 1], rs[:, 0:n], ALU.mult, ALU.mult)
    scale = 192 ** -0.5
    qA = k.sb([128, S], BF16); qB = k.sb([64, S], BF16)
    kA = k.sb([128, S], BF16); kB = k.sb([64, S], BF16)
    vT = k.sb([128, 18, 128], BF16)
    pT = [k.sb([128, 512], BF16) for _ in range(3)]
    for h in range(2):
        for which in range(2):
            dA, dB = (qA, qB) if which == 0 else (kA, kB)
            g = gq if which == 0 else gk
            sc = scale if which == 0 else 1.0
            for (t0, n) in TT:
                pa = pss[0]; pb = pss[1]; pn = pss[2]; pr = pss[3]
                if which == 0:
                    for c in range(4):
                        k.mm(pa[:, 0:n], wq[:, c, h * 192:h * 192 + 128], cqn[:, c, t0:t0 + n], start=(c == 0), stop=(c == 3))
                    for c in range(4):
                        k.mm(pb[0:64, 0:n], wq[:, c, h * 192 + 128:h * 192 + 192], cqn[:, c, t0:t0 + n], start=(c == 0), stop=(c == 3))
                    srcB = pb[0:64, 0:n]
                else:
                    for c in range(2):
                        k.mm(pa[:, 0:n], wkv[:, c, h * 256:h * 256 + 128], ckvn[:, c, t0:t0 + n], start=(c == 0), stop=(c == 1))
                    srcB = krf[:, t0:t0 + n]
                sa = tmp[0]; sbq = tmp[1]
                k.actf(sa[:, 0:n], pa[:, 0:n], AF.Square)
                k.actf(sbq[0:64, 0:n], srcB, AF.Square)
                k.mm(pn[:, 0:n], ones[:, :], sa[:, 0:n], start=True, stop=False)
                k.mm(pn[:, 0:n], ones[0:64, :], sbq[0:64, 0:n], start=False, stop=True)
                rs = tmp[2]
                k.ts(rs[:, 0:n], pn[:, 0:n], 1.0 / 192, ALU.mult, EPS, ALU.add)
                k.actf(rs[:, 0:n], rs[:, 0:n], AF.Sqrt)
                k.recip(rs[:, 0:n], rs[:, 0:n])
                k.tt(sa[:, 0:n], pa[:, 0:n], rs[:, 0:n], ALU.mult)
                k.ts(dA[:, t0:t0 + n], sa[:, 0:n], g[:, 0:1], ALU.mult, sc, ALU.mult)
                ub = tmp[3]
                k.tt(sbq[0:64, 0:n], srcB, rs[0:64, 0:n], ALU.mult)
                k.ts(ub[0:64, 0:n], sbq[0:64, 0:n], g[0:64, 1:2], ALU.mult, sc, ALU.mult)
                l0 = max(t0, 256)
                if l0 > t0:
                    k.copy(dB[:, t0:l0], ub[0:64, 0:l0 - t0])
                nl = t0 + n - l0
                o = l0 - t0
                k.mm(pr[0:64, 0:nl], RT[:, :], ub[0:64, o:o + nl])
                k.tt(sbq[0:64, 0:nl], pr[0:64, 0:nl], sinF[:, l0 - 256:l0 - 256 + nl], ALU.mult)
                k.tt(ub[0:64, o:o + nl], ub[0:64, o:o + nl], cosF[:, l0 - 256:l0 - 256 + nl], ALU.mult)
                k.tt(dB[:, l0:l0 + nl], ub[0:64, o:o + nl], sbq[0:64, 0:nl], ALU.add)
        for blk in range(18):
            pv = pss[blk % 2]
            for c in range(2):
                k.mm(pv[:, 0:128], ckvn[:, c, blk * 128:(blk + 1) * 128], wkv[:, c, h * 256 + 128:h * 256 + 256],
                     start=(c == 0), stop=(c == 1))
            k.copy(vT[:, blk, :], pv[:, 0:128])
        jobs = [(256 + qt * 512, 512, list(range(18))) for qt in range(4)] + [(0, 256, [0, 1])]
        for ji, (q0, nq, blks) in enumerate(jobs):
            po = pss[4 + (ji % 2) * 2]; pd = pss[5 + (ji % 2) * 2]
            for bi, blk in enumerate(blks):
                ps_s = pss[bi % 4]
                k.mm(ps_s[:, 0:nq], kA[:, blk * 128:(blk + 1) * 128], qA[:, q0:q0 + nq], start=True, stop=False)
                k.mm(ps_s[:, 0:nq], kB[:, blk * 128:(blk + 1) * 128], qB[:, q0:q0 + nq], start=False, stop=True)
                p = pT[bi % 3]
                k.actf(p[:, 0:nq], ps_s[:, 0:nq], AF.Exp)
                k.mm(po[:, 0:nq], vT[:, blk, :], p[:, 0:nq], start=(bi == 0), stop=(bi == len(blks) - 1))
                k.mm(pd[:, 0:nq], onesb[:, :], p[:, 0:nq], start=(bi == 0), stop=(bi == len(blks) - 1))
            rd = tmp[2]
            k.recip(rd[:, 0:nq], pd[:, 0:nq])
            k.tt(oT[:, h, q0:q0 + nq], po[:, 0:nq], rd[:, 0:nq], ALU.mult)
    k.dma(od[:], oT[:])
    return k.finish()


def fm_pad(a, nch):
    o = np.zeros((nch * 128, a.shape[0]), np.float32)
    o[:a.shape[1]] = a.T
    return np.ascontiguousarray(o.reshape(nch, 128, a.shape[0]).transpose(1, 0, 2))


def vec_pad(v, nch):
    o = np.zeros(nch * 128, np.float32); o[:v.shape[0]] = v
    return np.ascontiguousarray(o.reshape(nch, 128).T)


def w_pad(w, nch):
    o = np.zeros((nch * 128, w.shape[1]), np.float32); o[:w.shape[0]] = w
    return np.ascontiguousarray(o.reshape(nch, 128, w.shape[1]).transpose(1, 0, 2))


ROPE = rope_tables()


def mla_inputs(P_b, s, z, l):
    c = 3616
    cq = P_b[:, c:c + 448]; ckv = P_b[:, c + 448:c + 608]; kr = P_b[:, c + 608:c + 672]
    wq = z['mla_w_qb'][l][:, s * 384:(s + 1) * 384]
    wkv = z['mla_w_kvb'][l][:, s * 512:(s + 1) * 512]
    return {"cqT": fm_pad(cq, 4), "ckvT": fm_pad(ckv, 2), "krT": np.ascontiguousarray(kr.T),
            "wq": w_pad(wq, 4), "wkv": w_pad(wkv, 2),
            "qn": vec_pad(z['mla_q_norm'][l], 4), "kvn": vec_pad(z['mla_kv_norm'][l], 2),
            "gq": vec_pad(z['mla_gq'][l], 2), "gk": vec_pad(z['mla_gk'][l], 2),
            "cosF": ROPE[0], "sinF": ROPE[1], "RT": ROPE[2]}


EPS = 1e-6
NB = 18
FWD_ORDER = list(range(18))
BWD_ORDER = [1, 0] + list(range(17, 1, -1))


def tri_consts():
    r = np.arange(128)
    U = (r[:, None] <= r[None, :]).astype(np.float32)
    L = np.ascontiguousarray(U.T)
    return np.ascontiguousarray(np.stack([U, U, L, L], 1))


def build_ml():
    k = KB()
    qTd = k.din("qT", [128, 2, S]); kTd = k.din("kT", [128, 2, S])
    kMd = k.din("kTM", [128, NB, 256]); vMd = k.din("vTM", [128, NB, 256]); oMd = k.din("oTM", [128, NB, 256])
    gid = k.din("gi", [128, NB, 4]); gfd = k.din("gf", [128, NB, 4])
    bid = k.din("bi", [128, 4]); bfd = k.din("bf", [128, 4])
    gnd = k.din("gn", [128, 256])
    uld = k.din("UL4", [128, 4, 128])
    od = k.dout("oTM_out", [128, NB, 256])
    ones = k.sb([128, 128]); k.memset(ones[:], 1.0)
    UL4 = k.sb([128, 4, 128]); k.dma(UL4[:], uld[:])
    qTb = k.sb([128, 2, S], BF16); kTb = k.sb([128, 2, S], BF16)
    kTM = k.sb([128, NB, 256]); vext = k.sb([128, NB, 2, 129], BF16)
    osg = k.sb([128, NB, 256])
    gi = k.sb([128, NB, 4]); gf = k.sb([128, NB, 4]); bi = k.sb([128, 4]); bfb = k.sb([128, 4]); gn = k.sb([128, 256])
    pss = [k.ps([128, 512]) for _ in range(8)]
    k.dma(kTM[:], kMd[:]); k.dma(osg[:], oMd[:])
    for a, b in ((gi, gid), (gf, gfd), (bi, bid), (bfb, bfd), (gn, gnd)):
        k.dma(a[:], b[:])
    k.memset(vext[:, :, :, 128:129], 1.0)
    scale = 128 ** -0.5
    with k.scope() as st:
        qf = k.sb([128, 2, S], stack=st); vf = k.sb([128, NB, 256], stack=st)
        k.dma(qf[:], qTd[:]); k.dma(vf[:], vMd[:])
        k.dma(kTb[:], kTd[:], q="pool")
        k.ts(qTb[:], qf[:], scale, ALU.mult)
        k.copy(vext[:, :, :, 0:128], vf[:].rr("p b (h d) -> p b h d", h=2))
    k.actf(osg[:], osg[:], AF.Sigmoid)
    lf = k.sb([128, NB, 4]); li = k.sb([128, NB, 4])
    for c in range(4):
        k.ts(lf[:, :, c], gf[:, :, c], bfb[:, c:c + 1], ALU.add)
        k.ts(li[:, :, c], gi[:, :, c], bi[:, c:c + 1], ALU.add)
    k.actf(lf[:], lf[:], AF.Exp, scale=-1.0)
    k.actf(lf[:], lf[:], AF.Ln, bias=1.0)
    k.ts(lf[:], lf[:], -1.0, ALU.mult)
    bcol = k.sb([128, NB, 4]); acol = k.sb([128, NB, 4]); wg = k.sb([128, NB, 4]); ebtot = k.sb([128, NB, 4])
    lf2 = lf[:].rr("p b c -> p (b c)")
    pc = pss[0]
    k.mm(pc[:, 0:72], UL4[:, 0, :], lf2)
    k.mm(pc[:, 128:200], UL4[:, 2, :], lf2)
    k.copy(bcol[:, :, 0:2], pc[:, 0:72].rr("p (b c) -> p b c", c=4)[:, :, 0:2])
    k.copy(bcol[:, :, 2:4], pc[:, 128:200].rr("p (b c) -> p b c", c=4)[:, :, 2:4])
    k.tt(acol[:], li[:], bcol[:], ALU.subtract)
    k.mm(pc[:, 256:328], ones[:, :], lf2)
    btot = pc[:, 256:328].rr("p (b c) -> p b c", c=4)
    k.tt(wg[:], acol[:], btot, ALU.add)
    k.actf(wg[:], wg[:], AF.Exp)
    k.actf(ebtot[:], btot, AF.Exp)
    PT4 = k.sb([128, NB, 4, 128], BF16); QT4 = k.sb([128, NB, 4, 128], BF16)
    X = [k.sb([128, 4, 128]) for _ in range(2)]
    Dm = [k.sb([128, 4, 128]) for _ in range(2)]
    Eb = [k.sb([128, 4, 128]) for _ in range(2)]
    for blk in range(NB):
        x = X[blk % 2]; dm = Dm[blk % 2]; eb = Eb[blk % 2]
        pb = pss[1 + blk % 2]; psc = pss[3 + blk % 2]
        k.tt(x[:], UL4[:], lf[:, blk, :].unsq(2).bc([128, 4, 128]), ALU.mult)
        k.mm(pb[:, :], ones[:, :], x[:].rr("p c t -> p (c t)"))
        pb4 = pb[:, :].rr("p (c t) -> p c t", c=4)
        k.tt(dm[:], pb4, bcol[:, blk, :].unsq(2).bc([128, 4, 128]), ALU.subtract)
        k.tt(dm[:], dm[:], UL4[:], ALU.mult)
        k.tt(dm[:], dm[:], li[:, blk, :].unsq(2).bc([128, 4, 128]), ALU.add)
        k.actf(dm[:], dm[:], AF.Exp)
        k.tt(dm[:], dm[:], UL4[:], ALU.mult)
        for hl in range(2):
            k.mm(psc[:, hl * 128:(hl + 1) * 128], kTb[:, hl, blk * 128:(blk + 1) * 128], qTb[:, hl, blk * 128:(blk + 1) * 128])
        for d in range(2):
            k.tt(PT4[:, blk, d * 2:d * 2 + 2, :], dm[:, d * 2:d * 2 + 2, :],
                 psc[:, 0:256].rr("p (h t) -> p h t", h=2), ALU.mult)
        k.actf(eb[:], pb4, AF.Exp)
        for d in range(2):
            k.tt(QT4[:, blk, d * 2:d * 2 + 2, :], eb[:, d * 2:d * 2 + 2, :],
                 qTb[:, :, blk * 128:(blk + 1) * 128], ALU.mult)
    CT = k.sb([128, 4, 129]); CTb = k.sb([128, 4, 129], BF16)
    k.memset(CT[:], 0.0); k.memset(CTb[:], 0.0)
    H = [k.sb([128, NB, 2, 128]) for _ in range(2)]
    kw = [k.sb([128, 128], BF16) for _ in range(4)]
    dn = [k.sb([128, 1]) for _ in range(4)]
    for step in range(NB):
        for c in range(4):
            d, hl = c // 2, c % 2
            blk = FWD_ORDER[step] if d == 0 else BWD_ORDER[step]
            pn = pss[c % 2]; pcs = pss[2 + c % 2]
            k.mm(pn[:, 0:129], PT4[:, blk, c, :], vext[:, blk, hl, :], start=True, stop=False)
            k.mm(pn[:, 0:129], QT4[:, blk, c, :], CTb[:, c, :], start=False, stop=True)
            k.actf(dn[c][:], pn[:, 128:129], AF.Abs)
            k.ts(dn[c][:], dn[c][:], 1.0, ALU.max)
            k.recip(dn[c][:], dn[c][:])
            k.ts(H[d][:, blk, hl, :], pn[:, 0:128], dn[c][:, 0:1], ALU.mult)
            if step < NB - 1:
                k.ts(kw[c][:], kTM[:, blk, hl * 128:(hl + 1) * 128], wg[:, blk, c:c + 1], ALU.mult, eng="pool")
                k.mm(pcs[:, 0:129], kw[c][:], vext[:, blk, hl, :])
                k.stt(CT[:, c, :], CT[:, c, :], ebtot[:, blk, c:c + 1], pcs[:, 0:129], ALU.mult, ALU.add)
                k.copy(CTb[:, c, :], CT[:, c, :], eng="act")
    Hs = H[0]
    k.tt(Hs[:], H[0][:], H[1][:], ALU.add)
    sq = H[1]
    k.tt(sq[:], Hs[:], Hs[:], ALU.mult)
    ss = k.sb([128, NB * 2])
    k.reduce(ss[:], sq[:].rr("p b h d -> p (b h) d"), ALU.add, AX.X)
    k.ts(ss[:], ss[:], 1.0 / 128, ALU.mult, EPS, ALU.add)
    k.actf(ss[:], ss[:], AF.Sqrt)
    k.recip(ss[:], ss[:])
    outb = sq
    for blk in range(NB):
        for hl in range(2):
            k.stt(outb[:, blk, hl, :], Hs[:, blk, hl, :], ss[:, blk * 2 + hl:blk * 2 + hl + 1],
                  gn[:, hl * 128:(hl + 1) * 128], ALU.mult, ALU.mult)
    k.tt(osg[:], osg[:], outb[:].rr("p b h d -> p b (h d)"), ALU.mult)
    k.dma(od[:], osg[:])
    return k.finish()


def tm_blocks(a):
    return np.ascontiguousarray(a.reshape(NB, 128, a.shape[1]).transpose(1, 0, 2))


UL4 = tri_consts()


def ml_inputs(P_b, s, z, l):
    q = P_b[:, s * 256:(s + 1) * 256]; kk = P_b[:, 512 + s * 256:512 + (s + 1) * 256]
    v = P_b[:, 1024 + s * 256:1024 + (s + 1) * 256]; o = P_b[:, 1536 + s * 256:1536 + (s + 1) * 256]
    ig = P_b[:, 2048:2056].reshape(S, 2, 4)[:, :, 2 * s:2 * s + 2].reshape(S, 4)
    fg = P_b[:, 2056:2064].reshape(S, 2, 4)[:, :, 2 * s:2 * s + 2].reshape(S, 4)
    bi = z['ml_i_bias'][l][:, 2 * s:2 * s + 2].reshape(1, 4); bf = z['ml_f_bias'][l][:, 2 * s:2 * s + 2].reshape(1, 4)
    gn = z['ml_norm'][l][s * 256:(s + 1) * 256].reshape(1, 256)
    return {"qT": fm_heads(q), "kT": fm_heads(kk), "kTM": tm_blocks(kk), "vTM": tm_blocks(v), "oTM": tm_blocks(o),
            "gi": tm_blocks(ig), "gf": tm_blocks(fg),
            "bi": np.ascontiguousarray(np.repeat(bi, 128, 0)), "bf": np.ascontiguousarray(np.repeat(bf, 128, 0)),
            "gn": np.ascontiguousarray(np.repeat(gn, 128, 0)), "UL4": UL4}


EPS = 1e-6


def tri8():
    r = np.arange(128)
    U = (r[:, None] <= r[None, :]).astype(np.float32)
    L = np.ascontiguousarray(U.T)
    return np.ascontiguousarray(np.stack([U] * 4 + [L] * 4, 1))


def build_ssd():
    k = KB()
    xbcd = k.din("xbcT", [128, 4, S]); cwd_ = k.din("cw", [128, 4, 5]); cbd = k.din("cb", [128, 4])
    zd = k.din("zTM", [128, NB, 256]); dtd = k.din("dtr", [128, NB, 8])
    dbd = k.din("dtb", [128, 8]); ald = k.din("alog", [128, 8]); dsd = k.din("dskip", [128, 256])
    uld = k.din("UL8", [128, 8, 128]); idd = k.din("ident", [128, 128])
    od = k.dout("yTM", [128, NB, 256])
    ones = k.sb([128, 128]); k.memset(ones[:], 1.0)
    UL8 = k.sb([128, 8, 128]); ident = k.sb([128, 128])
    cw = k.sb([128, 4, 5]); cb = k.sb([128, 4]); dtb = k.sb([128, 8]); alog = k.sb([128, 8]); dsk = k.sb([128, 256])
    zs = k.sb([128, NB, 256]); dt = k.sb([128, NB, 8])
    for a, b in ((UL8, uld), (ident, idd), (cw, cwd_), (cb, cbd), (dtb, dbd), (alog, ald), (dsk, dsd), (zs, zd), (dt, dtd)):
        k.dma(a[:], b[:])
    pss = [k.ps([128, 512]) for _ in range(8)]
    BTb = k.sb([128, S], BF16); CTb = k.sb([128, S], BF16)
    xTM = k.sb([128, NB, 256]); BTM = k.sb([128, NB, 128])
    with k.scope() as st:
        u = k.sb([128, 4, S], stack=st); acc = k.sb([128, 4, S], stack=st)
        xs = u
        k.dma(u[:], xbcd[:])
        for c in range(4):
            e = "dve" if c % 2 == 0 else "pool"
            k.ts(acc[:, c, :], u[:, c, :], cw[:, c, 2:3], ALU.mult, eng=e)
            for j in (0, 1, 3, 4):
                d = j - 2
                for (a, b) in ((0, 256), (256, S)):
                    lo = a + max(0, -d); hi = b - max(0, d)
                    k.stt(acc[:, c, lo:hi], u[:, c, lo + d:hi + d], cw[:, c, j:j + 1], acc[:, c, lo:hi],
                          ALU.mult, ALU.add)
        for c in range(2):
            k.actf(xs[:, c, :], acc[:, c, :], AF.Silu, bias=cb[:, c:c + 1])
        k.actf(acc[:, 2, :], acc[:, 2, :], AF.Silu, bias=cb[:, 2:3])
        k.actf(CTb[:], acc[:, 3, :], AF.Silu, bias=cb[:, 3:4])
        k.copy(BTb[:], acc[:, 2, :])
        for blk in range(NB):
            pt = pss[blk % 4]
            for c in range(2):
                k.transpose(pt[:, c * 128:(c + 1) * 128], xs[:, c, blk * 128:(blk + 1) * 128], ident[:, :])
            k.transpose(pt[:, 256:384], acc[:, 2, blk * 128:(blk + 1) * 128], ident[:, :])
            k.copy(xTM[:, blk, :], pt[:, 0:256], eng="act")
            k.copy(BTM[:, blk, :], pt[:, 256:384])
    A = k.sb([128, 8])
    k.actf(A[:], alog[:], AF.Exp)
    k.ts(A[:], A[:], -1.0, ALU.mult)
    for c in range(8):
        k.ts(dt[:, :, c], dt[:, :, c], dtb[:, c:c + 1], ALU.add)
    k.actf(dt[:], dt[:], AF.Exp)
    k.actf(dt[:], dt[:], AF.Ln, bias=1.0)
    av = k.sb([128, NB, 8])
    for c in range(8):
        k.ts(av[:, :, c], dt[:, :, c], A[:, c:c + 1], ALU.mult)
    bcol = k.sb([128, NB, 8]); wtail = k.sb([128, NB, 8]); ebtot = k.sb([128, NB, 8])
    a2 = av[:].rr("p b c -> p (b c)")
    pc = pss[0]
    k.mm(pc[:, 0:144], UL8[:, 0, :], a2)
    k.mm(pc[:, 160:304], UL8[:, 4, :], a2)
    k.copy(bcol[:, :, 0:4], pc[:, 0:144].rr("p (b c) -> p b c", c=8)[:, :, 0:4])
    k.copy(bcol[:, :, 4:8], pc[:, 160:304].rr("p (b c) -> p b c", c=8)[:, :, 4:8])
    k.mm(pc[:, 320:464], ones[:, :], a2)
    btot = pc[:, 320:464].rr("p (b c) -> p b c", c=8)
    k.tt(wtail[:], btot, bcol[:], ALU.subtract)
    k.actf(wtail[:], wtail[:], AF.Exp)
    k.actf(ebtot[:], btot, AF.Exp)
    xt = k.sb([128, NB, 8, 64], BF16)
    for blk in range(NB):
        for d in range(2):
            k.tt(xt[:, blk, d * 4:(d + 1) * 4, :], xTM[:, blk, :].rr("p (h e) -> p h e", h=4),
                 dt[:, blk, d * 4:(d + 1) * 4].unsq(2).bc([128, 4, 64]), ALU.mult, eng="pool" if d else "dve")
    PT8 = k.sb([128, NB, 8, 128], BF16); CT8 = k.sb([128, NB, 8, 128], BF16)
    pre = k.scope(); st2 = pre.__enter__()
    X = [k.sb([128, 8, 128], stack=st2) for _ in range(2)]
    Dm = [k.sb([128, 8, 128], stack=st2) for _ in range(2)]
    for blk in range(NB):
        x = X[blk % 2]; dm = Dm[blk % 2]
        pb0 = pss[1 + (blk % 2) * 2]; pb1 = pss[2 + (blk % 2) * 2]; pg = pss[5 + blk % 2]
        k.tt(x[:], UL8[:], av[:, blk, :].unsq(2).bc([128, 8, 128]), ALU.mult)
        k.mm(pb0[:, :], ones[:, :], x[:, 0:4, :].rr("p c t -> p (c t)"))
        k.mm(pb1[:, :], ones[:, :], x[:, 4:8, :].rr("p c t -> p (c t)"))
        k.mm(pg[:, 0:128], BTb[:, blk * 128:(blk + 1) * 128], CTb[:, blk * 128:(blk + 1) * 128])
        for d, pb in enumerate((pb0, pb1)):
            pb4 = pb[:, :].rr("p (c t) -> p c t", c=4)
            sl = slice(d * 4, (d + 1) * 4)
            k.tt(dm[:, sl, :], pb4, bcol[:, blk, sl].unsq(2).bc([128, 4, 128]), ALU.subtract)
            k.tt(dm[:, sl, :], dm[:, sl, :], UL8[:, sl, :], ALU.mult)
            k.actf(dm[:, sl, :], dm[:, sl, :], AF.Exp)
            k.tt(dm[:, sl, :], dm[:, sl, :], UL8[:, sl, :], ALU.mult, eng="pool")
            k.tt(PT8[:, blk, sl, :], dm[:, sl, :], pg[:, 0:128].unsq(1).bc([128, 4, 128]), ALU.mult)
            k.actf(x[:, sl, :], pb4, AF.Exp)
            k.tt(CT8[:, blk, sl, :], x[:, sl, :], CTb[:, blk * 128:(blk + 1) * 128].unsq(1).bc([128, 4, 128]),
                 ALU.mult, eng="pool")
    pre.__exit__(None, None, None)
    HT = k.sb([128, 8, 64]); HTb = k.sb([128, 8, 64], BF16)
    k.memset(HT[:], 0.0); k.memset(HTb[:], 0.0)
    Y = [k.sb([128, NB, 256]) for _ in range(2)]
    Bw = [k.sb([128, 128], BF16) for _ in range(8)]
    for step in range(NB):
        for d in range(2):
            blk = FWD_ORDER[step] if d == 0 else BWD_ORDER[step]
            py = pss[d]
            for hh in range(4):
                c = d * 4 + hh
                k.mm(py[:, hh * 64:(hh + 1) * 64], PT8[:, blk, c, :], xt[:, blk, c, :], start=True, stop=False)
                k.mm(py[:, hh * 64:(hh + 1) * 64], CT8[:, blk, c, :], HTb[:, c, :], start=False, stop=True)
            k.copy(Y[d][:, blk, :], py[:, 0:256], eng="act")
            if step < NB - 1:
                ph = pss[2 + d]
                for hh in range(4):
                    c = d * 4 + hh
                    k.ts(Bw[c][:], BTM[:, blk, :], wtail[:, blk, c:c + 1], ALU.mult, eng="pool")
                    k.mm(ph[:, hh * 64:(hh + 1) * 64], Bw[c][:], xt[:, blk, c, :])
                    k.stt(HT[:, c, :], HT[:, c, :], ebtot[:, blk, c:c + 1], ph[:, hh * 64:(hh + 1) * 64], ALU.mult, ALU.add)
                k.copy(HTb[:, d * 4:(d + 1) * 4, :], HT[:, d * 4:(d + 1) * 4, :], eng="act")
    k.tt(Y[0][:], Y[0][:], Y[1][:], ALU.add)
    k.tt(xTM[:], xTM[:], dsk[:, :].unsq(1).bc([128, NB, 256]), ALU.mult)
    k.tt(Y[0][:], Y[0][:], xTM[:], ALU.add)
    k.actf(zs[:], zs[:], AF.Silu)
    k.tt(Y[0][:], Y[0][:], zs[:], ALU.mult)
    k.dma(od[:], Y[0][:])
    return k.finish()


UL8 = tri8()
IDENT = np.eye(128, dtype=np.float32)


def ssd_inputs(P_b, s, z, l):
    c = 2064
    zz = P_b[:, c + s * 256:c + (s + 1) * 256]
    xb = 2576
    xx = P_b[:, xb + s * 256:xb + (s + 1) * 256]
    Bm = P_b[:, xb + 512 + s * 128:xb + 512 + (s + 1) * 128]
    Cm = P_b[:, xb + 768 + s * 128:xb + 768 + (s + 1) * 128]
    chans = np.concatenate([np.arange(s * 256, (s + 1) * 256), 512 + np.arange(s * 128, (s + 1) * 128),
                            768 + np.arange(s * 128, (s + 1) * 128)])
    xbc = np.concatenate([xx, Bm, Cm], 1)
    xbcT = np.ascontiguousarray(xbc.T.reshape(4, 128, S).transpose(1, 0, 2))
    cw = np.ascontiguousarray(z['ssd_conv_w'][l][:, chans].T.reshape(4, 128, 5).transpose(1, 0, 2))
    cb = np.ascontiguousarray(z['ssd_conv_b'][l][chans].reshape(4, 128).T)
    dtr = P_b[:, 3600:3616].reshape(S, 2, 8)[:, :, 4 * s:4 * s + 4].reshape(S, 8)
    rep = lambda v: np.ascontiguousarray(np.repeat(v.reshape(1, -1), 128, 0).astype(np.float32))
    dtb = rep(z['ssd_dt_bias'][l][:, 4 * s:4 * s + 4].reshape(8))
    alog = rep(z['ssd_A_log'][l][:, 4 * s:4 * s + 4].reshape(8))
    dsk = rep(np.repeat(z['ssd_D'][l][4 * s:4 * s + 4], 64))
    return {"xbcT": xbcT, "cw": cw, "cb": cb, "zTM": tm_blocks(zz), "dtr": tm_blocks(dtr), "dtb": dtb, "alog": alog,
            "dskip": dsk, "UL8": UL8, "ident": IDENT}


D = 2048
DIN = 5824
S = 2304
NB = 18
EPS = 1e-6
FD = 5632
FE = 7168
SEQ_TILES = [(0, 256, 1), (256, 512, 0), (768, 512, 0), (1280, 512, 0), (1792, 512, 0)]
TT5 = [(t0, n) for (t0, n, _) in SEQ_TILES]
CGROUPS = [(0, 512, 1, 0), (512, 512, 1, 1), (1024, 512, 0, 1), (1536, 512, 0, 1), (2048, 512, 0, 1), (2560, 16, 0, 1),
           (2576, 512, 1, 0), (3088, 512, 1, 0), (3600, 16, 0, 1), (3616, 512, 1, 0), (4128, 160, 1, 0),
           (4288, 512, 1, 0), (4800, 512, 1, 0), (5312, 512, 0, 1)]


class Consts:
    pass


def rstd_from_ps(k, rs, ps, n, nfeat):
    k.ts(rs[:, 0:n], ps[:, 0:n], 1.0 / nfeat, ALU.mult, EPS, ALU.add)
    k.actf(rs[:, 0:n], rs[:, 0:n], AF.Sqrt)
    k.recip(rs[:, 0:n], rs[:, 0:n])


MG = 768
MNG = 12288 // MG


def mod_load(k, modw_d, wsb, i):
    l, g = divmod(i, MNG)
    wv = modw_d[l].rr("(kc p) n -> p kc n", p=128)
    for q in range(2):
        k.dma(wsb[i % 2][:, q * 8:(q + 1) * 8, :], wv[:, q * 8:(q + 1) * 8, g * MG:(g + 1) * MG], q="pool")


def mod_compute(k, wsb, scb, bsb, modsb, ps2, i):
    l, g = divmod(i, MNG)
    for j in range(MG // 128):
        p = ps2[j % 2]
        for kc in range(16):
            k.mm(p[:, 0:2], wsb[i % 2][:, kc, j * 128:(j + 1) * 128], scb[:, kc, :], start=(kc == 0), stop=(kc == 15))
        ch = g * (MG // 128) + j
        k.ts(modsb[:, l, ch, :], p[:, 0:2], bsb[:, l, ch:ch + 1], ALU.add)


MOD_FIRST = 6


def phase_mod(k, C, ccT_d, modw_d, modb_d, modsb, scb, bsb, pss):
    with k.scope() as st:
        cc = k.sb([128, 16, 2], stack=st)
        wsb = [k.sb([128, 16, MG], BF16, stack=st) for _ in range(2)]
        k.dma(cc[:], ccT_d[:]); k.dma(bsb[:], modb_d[:])
        k.actf(scb[:], cc[:], AF.Silu)
        mod_load(k, modw_d, wsb, 0)
        for i in range(MOD_FIRST):
            if i + 1 < MOD_FIRST:
                mod_load(k, modw_d, wsb, i + 1)
            mod_compute(k, wsb, scb, bsb, modsb, pss[0:2], i)


def norm_tile(k, C, x, h, n, A, sh, col, ps, tmp):
    for kc in range(16):
        sq = tmp[kc % 2]
        k.actf(sq[:, 0:n], x[:, kc, 0:n], AF.Square)
        k.mm(ps[:, 0:n], C.ones[:, :], sq[:, 0:n], start=(kc == 0), stop=(kc == 15))
    rs = tmp[2]
    rstd_from_ps(k, rs, ps, n, D)
    for kc in range(16):
        t = tmp[kc % 2]
        k.tt(t[:, 0:n], x[:, kc, 0:n], rs[:, 0:n], ALU.mult)
        k.ts(h[:, kc, 0:n], t[:, 0:n], A[:, kc, col:col + 1], ALU.mult, sh[:, kc, col:col + 1], ALU.add)


def make_A(k, A, gT, scT):
    for c in range(2):
        k.ts(A[:, :, c], scT[:, :, c], 1.0, ALU.add)
        k.tt(A[:, :, c], A[:, :, c], gT[:, :], ALU.mult)


def phase_inproj(k, C, xsrc, gT, scT, shT, w_d, PT, PM, pss, modctx=None):
    with k.scope() as st:
        hT = k.sb([128, 16, S], BF16, stack=st)
        A = k.sb([128, 16, 2], stack=st)
        make_A(k, A, gT, scT)
        with k.scope() as st2:
            xt = [k.sb([128, 16, 512], stack=st2) for _ in range(2)]
            tmp = [k.sb([128, 512], stack=st2) for _ in range(3)]
            for ti, (t0, n, col) in enumerate(SEQ_TILES):
                x = xt[ti % 2]
                for q in range(4):
                    k.dma(x[:, q * 4:(q + 1) * 4, 0:n], xsrc[:, q * 4:(q + 1) * 4, t0:t0 + n])
                norm_tile(k, C, x, hT[:, :, t0:t0 + n], n, A, shT, col, pss[ti % 2], tmp)
        wsb = [k.sb([128, 16, 512], BF16, stack=st) for _ in range(2)]
        stg = [k.sb([128, 512], stack=st) for _ in range(4)]
        wv = w_d.rr("(kc p) n -> p kc n", p=128)

        def loadw(gi):
            c0, cn, _, _ = CGROUPS[gi]
            for q in range(4):
                k.dma(wsb[gi % 2][:, q * 4:(q + 1) * 4, 0:cn], wv[:, q * 4:(q + 1) * 4, c0:c0 + cn], q="pool")
        loadw(0)
        if modctx is not None:
            modw_d, scb, bsb, modsb = modctx
            wsbm = [k.sb([128, 16, MG], BF16, stack=st) for _ in range(2)]
            mod_load(k, modw_d, wsbm, MOD_FIRST)
        it = 0
        for gi, (c0, cn, fm, tm) in enumerate(CGROUPS):
            if gi + 1 < len(CGROUPS):
                loadw(gi + 1)
            for mi in ((MOD_FIRST + 2 * gi, MOD_FIRST + 2 * gi + 1) if modctx is not None else ()):
                if mi < 2 * MNG:
                    if mi + 1 < 2 * MNG:
                        mod_load(k, modw_d, wsbm, mi + 1)
                    mod_compute(k, wsbm, scb, bsb, modsb, pss[6:8], mi)
            w = wsb[gi % 2]
            if tm:
                for tb in range(NB):
                    ps = pss[it % 4]; sg = stg[it % 4]
                    for kc in range(16):
                        k.mm(ps[:, 0:cn], hT[:, kc, tb * 128:(tb + 1) * 128], w[:, kc, 0:cn], start=(kc == 0), stop=(kc == 15))
                    k.copy(sg[:, 0:cn], ps[:, 0:cn], eng="act" if it % 2 else "dve")
                    k.dma(PM[tb * 128:(tb + 1) * 128, c0:c0 + cn], sg[:, 0:cn], waw=False)
                    it += 1
            if fm:
                for m0 in range(0, cn, 128):
                    mn = min(128, cn - m0)
                    for (t0, n, _) in SEQ_TILES:
                        ps = pss[it % 4]; sg = stg[it % 4]
                        for kc in range(16):
                            k.mm(ps[0:mn, 0:n], w[:, kc, m0:m0 + mn], hT[:, kc, t0:t0 + n], start=(kc == 0), stop=(kc == 15))
                        k.copy(sg[0:mn, 0:n], ps[0:mn, 0:n], eng="act" if it % 2 else "dve")
                        k.dma(PT[c0 + m0:c0 + m0 + mn, t0:t0 + n], sg[0:mn, 0:n], waw=False)
                        it += 1


def headnorm_fm(k, C, srcT, dstT, h, gcol, ps, tmp, extra_scale=1.0, nfeat=128):
    for (t0, n) in TT5:
        sq = tmp[0]
        k.actf(sq[:, 0:n], srcT[:, h, t0:t0 + n], AF.Square)
        k.mm(ps[:, 0:n], C.ones[:, :], sq[:, 0:n])
        rs = tmp[1]
        rstd_from_ps(k, rs, ps, n, nfeat)
        k.tt(rs[:, 0:n], srcT[:, h, t0:t0 + n], rs[:, 0:n], ALU.mult)
        k.ts(dstT[:, h, t0:t0 + n], rs[:, 0:n], gcol, ALU.mult, extra_scale, ALU.mult)


def body_na(k, C, PT, PM, s, gq_d, gk_d, bt_d, MIXT):
    with k.scope() as st:
        sb = lambda shp, dt=F32: k.sb(shp, dt, stack=st)
        qd = PT[4288 + s * 256:4288 + (s + 1) * 256, :].rr("(h p) t -> p h t", p=128)
        kd = PT[4800 + s * 256:4800 + (s + 1) * 256, :].rr("(h p) t -> p h t", p=128)
        vc = slice(5312 + s * 256, 5312 + (s + 1) * 256)
        vAd = PM[:, vc].rr("(j p) c -> p j c", p=128)
        vBd = PM[320:320 + 15 * 128, vc].rr("(j p) c -> p j c", p=128)
        qf = sb([128, 2, S]); kf = sb([128, 2, S])
        qb = sb([128, 2, S], BF16); kb = sb([128, 2, S], BF16)
        vA = sb([128, 18, 256], BF16); vB = sb([128, 15, 256], BF16)
        gq = sb([128, 1]); gk = sb([128, 1]); bt = sb([128, 2, 14, 64])
        oT = sb([128, 2, S])
        tmp = [sb([128, 512]) for _ in range(3)]
        pss = [k.ps([128, 512], stack=st) for _ in range(8)]
        k.dma(qf[:], qd); k.dma(kf[:], kd)
        k.dma(vA[:], vAd, q="pool"); k.dma(vB[:], vBd, q="pool")
        k.dma(gq[:], gq_d); k.dma(gk[:], gk_d); k.dma(bt[:], bt_d)
        scale = 128 ** -0.5
        for h in range(2):
            headnorm_fm(k, C, qf, qb, h, gq[:, 0:1], pss[0], tmp, extra_scale=scale)
            headnorm_fm(k, C, kf, kb, h, gk[:, 0:1], pss[1], tmp)
        pT = [sb([128, 384], BF16) for _ in range(3)]
        pc = sb([128, 512], BF16)
        it = 0
        for h in range(2):
            def s_stage(r, h=h):
                r0 = min(max(r - 4, 0), 24)
                q0 = 256 + r * 64
                ps_s = pss[r % 4]
                vblks = []
                for j in range(4):
                    kt0 = 256 + r0 * 64 + 128 * j
                    k.mm(ps_s[:, j * 64:(j + 1) * 64], kb[:, h, kt0:kt0 + 128], qb[:, h, q0:q0 + 64])
                    if r0 % 2 == 0:
                        vblks.append(vA[:, 2 + r0 // 2 + j, h * 128:(h + 1) * 128])
                    else:
                        vblks.append(vB[:, (r0 - 1) // 2 + j, h * 128:(h + 1) * 128])
                for j in range(2):
                    k.mm(ps_s[:, 256 + j * 64:256 + (j + 1) * 64], kb[:, h, j * 128:(j + 1) * 128], qb[:, h, q0:q0 + 64])
                    vblks.append(vA[:, j, h * 128:(h + 1) * 128])
                return vblks, r0 - r + 7
            cur = s_stage(0)
            for r in range(32):
                rg, rl = r // 8, r % 8
                if rl == 0:
                    po = pss[4 + (it % 2) * 2]; pd = pss[5 + (it % 2) * 2]
                    it += 1
                vblks, m0 = cur
                ps_s = pss[r % 4]
                tm = tmp[r % 2]
                k.tt(tm[:, 0:256].rr("p (j c) -> p j c", j=4), ps_s[:, 0:256].rr("p (j c) -> p j c", j=4),
                     bt[:, h, m0:m0 + 7:2, :], ALU.add)
                p = pT[r % 3]
                k.actf(p[:, 0:256], tm[:, 0:256], AF.Exp)
                k.actf(p[:, 256:384], ps_s[:, 256:384], AF.Exp)
                if r + 1 < 32:
                    cur = s_stage(r + 1)
                for j in range(6):
                    k.mm(po[:, rl * 64:(rl + 1) * 64], vblks[j], p[:, j * 64:(j + 1) * 64], start=(j == 0), stop=(j == 5))
                for j in range(6):
                    k.mm(pd[:, rl * 64:(rl + 1) * 64], C.onesb[:, :], p[:, j * 64:(j + 1) * 64], start=(j == 0), stop=(j == 5))
                if rl == 7:
                    rd = tmp[2]
                    k.recip(rd[:, :], pd[:, :])
                    k.tt(oT[:, h, 256 + rg * 512:256 + (rg + 1) * 512], po[:, :], rd[:, :], ALU.mult)
            ps_s = pss[0]; po = pss[4]; pd = pss[5]
            for j in range(2):
                k.mm(ps_s[:, j * 256:(j + 1) * 256], kb[:, h, j * 128:(j + 1) * 128], qb[:, h, 0:256])
            k.actf(pc[:, :], ps_s[:, :], AF.Exp)
            for j in range(2):
                k.mm(po[:, 0:256], vA[:, j, h * 128:(h + 1) * 128], pc[:, j * 256:(j + 1) * 256], start=(j == 0), stop=(j == 1))
            for j in range(2):
                k.mm(pd[:, 0:256], C.onesb[:, :], pc[:, j * 256:(j + 1) * 256], start=(j == 0), stop=(j == 1))
            rd = tmp[2]
            k.recip(rd[:, 0:256], pd[:, 0:256])
            k.tt(oT[:, h, 0:256], po[:, 0:256], rd[:, 0:256], ALU.mult)
        r0_ = 1536 + s * 256
        k.dma(MIXT[r0_:r0_ + 256, :].rr("(h p) t -> p h t", p=128), oT[:], waw=False)


def body_mla(k, C, PT, s, P, MIXT, own=None):
    with k.scope() as st:
        sb = lambda shp, dt=F32: k.sb(shp, dt, stack=st)
        wq = sb([128, 4, 384], BF16); wkv = sb([128, 2, 512], BF16)
        qn = sb([128, 4]); kvn = sb([128, 2]); gq = sb([128, 2]); gk = sb([128, 2])
        cosF = sb([64, 2048]); sinF = sb([64, 2048]); RT = sb([64, 64])
        krf = sb([64, S])
        cqn = sb([128, 4, S], BF16); ckvn = sb([128, 2, S], BF16)
        oT = sb([128, 2, S])
        pss = [k.ps([128, 512], stack=st) for _ in range(8)]
        tmp = [sb([128, 512]) for _ in range(8)]
        k.dma(wq[:], P["wq"], q="pool"); k.dma(wkv[:], P["wkv"], q="pool")
        for a, b in ((qn, "qn"), (kvn, "kvn"), (gq, "gq"), (gk, "gk"), (cosF, "cosF"), (sinF, "sinF"), (RT, "RT")):
            k.dma(a[:], P[b])
        k.dma(krf[:], PT[4224:4288, :])
        with k.scope() as st2:
            cq = k.sb([128, 4, S], stack=st2); ckv = k.sb([128, 2, S], stack=st2)
            k.memset(cq[:, 3, :], 0.0); k.memset(ckv[:, 1, :], 0.0)
            k.dma(cq[:, 0:3, :], PT[3616:3616 + 384, :].rr("(c p) t -> p c t", p=128))
            k.dma(cq[0:64, 3, :], PT[3616 + 384:3616 + 448, :])
            k.dma(ckv[:, 0, :], PT[4064:4064 + 128, :])
            k.dma(ckv[0:32, 1, :], PT[4064 + 128:4064 + 160, :])
            for (src, dst, nch, nfeat, gn) in ((cq, cqn, 4, 448, qn), (ckv, ckvn, 2, 160, kvn)):
                for (t0, n) in TT5:
                    ps = pss[0]
                    for c in range(nch):
                        sq = tmp[c % 2]
                        k.actf(sq[:, 0:n], src[:, c, t0:t0 + n], AF.Square)
                        k.mm(ps[:, 0:n], C.ones[:, :], sq[:, 0:n], start=(c == 0), stop=(c == nch - 1))
                    rs = tmp[2]
                    rstd_from_ps(k, rs, ps, n, nfeat)
                    for c in range(nch):
                        k.stt(dst[:, c, t0:t0 + n], src[:, c, t0:t0 + n], gn[:, c:c + 1], rs[:, 0:n], ALU.mult, ALU.mult)
        scale = 192 ** -0.5
        qA = sb([128, S], BF16); qB = sb([64, S], BF16)
        kA = sb([128, S], BF16); kB = sb([64, S], BF16)
        vT = sb([128, 18, 128], BF16)
        pT = [sb([128, 512], BF16) for _ in range(3)]
        if own is not None:
            sv = sb([128, 2]); k.dma(sv[:], own[0][:])
            qAo = sb([128, 1024], BF16); qBo = sb([64, 1024], BF16)
        pit = [0]
        for h in range(2):
            for which in range(2):
                dA, dB = (qA, qB) if which == 0 else (kA, kB)
                g = gq if which == 0 else gk
                sc = scale if which == 0 else 1.0
                for (t0, n) in TT5:
                    pit[0] += 1
                    o4 = (pit[0] % 2) * 4
                    pa = pss[o4]; pb = pss[o4 + 1]; pn = pss[o4 + 2]; pr = pss[o4 + 3]
                    if which == 0:
                        for c in range(4):
                            k.mm(pa[:, 0:n], wq[:, c, h * 192:h * 192 + 128], cqn[:, c, t0:t0 + n], start=(c == 0), stop=(c == 3))
                        for c in range(4):
                            k.mm(pb[0:64, 0:n], wq[:, c, h * 192 + 128:h * 192 + 192], cqn[:, c, t0:t0 + n], start=(c == 0), stop=(c == 3))
                        srcB = pb[0:64, 0:n]
                    else:
                        for c in range(2):
                            k.mm(pa[:, 0:n], wkv[:, c, h * 256:h * 256 + 128], ckvn[:, c, t0:t0 + n], start=(c == 0), stop=(c == 1))
                        srcB = krf[:, t0:t0 + n]
                    sa = tmp[o4]; sbq = tmp[o4 + 1]
                    k.actf(sa[:, 0:n], pa[:, 0:n], AF.Square)
                    k.actf(sbq[0:64, 0:n], srcB, AF.Square)
                    k.mm(pn[:, 0:n], C.ones[:, :], sa[:, 0:n], start=True, stop=False)
                    k.mm(pn[:, 0:n], C.ones[0:64, :], sbq[0:64, 0:n], start=False, stop=True)
                    rs = tmp[o4 + 2]
                    rstd_from_ps(k, rs, pn, n, 192)
                    k.tt(sa[:, 0:n], pa[:, 0:n], rs[:, 0:n], ALU.mult)
                    k.ts(dA[:, t0:t0 + n], sa[:, 0:n], g[:, 0:1], ALU.mult, sc, ALU.mult)
                    ub = tmp[o4 + 3]
                    k.tt(sbq[0:64, 0:n], srcB, rs[0:64, 0:n], ALU.mult)
                    k.ts(ub[0:64, 0:n], sbq[0:64, 0:n], g[0:64, 1:2], ALU.mult, sc, ALU.mult)
                    l0 = max(t0, 256)
                    if l0 > t0:
                        k.copy(dB[:, t0:l0], ub[0:64, 0:l0 - t0])
                    nl = t0 + n - l0
                    if nl <= 0:
                        continue
                    o = l0 - t0
                    k.mm(pr[0:64, 0:nl], RT[:, :], ub[0:64, o:o + nl])
                    k.tt(sbq[0:64, 0:nl], pr[0:64, 0:nl], sinF[:, l0 - 256:l0 - 256 + nl], ALU.mult)
                    k.tt(ub[0:64, o:o + nl], ub[0:64, o:o + nl], cosF[:, l0 - 256:l0 - 256 + nl], ALU.mult)
                    k.tt(dB[:, l0:l0 + nl], ub[0:64, o:o + nl], sbq[0:64, 0:nl], ALU.add)
            for blk in range(18):
                pv = pss[blk % 2]
                for c in range(2):
                    k.mm(pv[:, 0:128], ckvn[:, c, blk * 128:(blk + 1) * 128], wkv[:, c, h * 256 + 128:h * 256 + 256],
                         start=(c == 0), stop=(c == 1))
                k.copy(vT[:, blk, :], pv[:, 0:128])
            if own is None:
                jobs = [(256 + qt * 512, 512, list(range(18))) for qt in range(4)] + [(0, 256, [0, 1])]
                qsA, qsB = qA, qB
            else:
                k.ts(qAo[:], qA[:, 256:1280], sv[:, 0:1], ALU.mult)
                k.stt(qAo[:], qA[:, 1280:2304], sv[:, 1:2], qAo[:], ALU.mult, ALU.add)
                k.ts(qBo[:], qB[:, 256:1280], sv[0:64, 0:1], ALU.mult)
                k.stt(qBo[:], qB[:, 1280:2304], sv[0:64, 1:2], qBo[:], ALU.mult, ALU.add)
                jobs = [(qt * 512, 512, list(range(18))) for qt in range(2)]
                qsA, qsB = qAo, qBo
            for ji, (q0, nq, blks) in enumerate(jobs):
                po = pss[4 + (ji % 2) * 2]; pd = pss[5 + (ji % 2) * 2]

                def smm(bi):
                    blk = blks[bi]
                    ps_s = pss[bi % 4]
                    k.mm(ps_s[:, 0:nq], kA[:, blk * 128:(blk + 1) * 128], qsA[:, q0:q0 + nq], start=True, stop=False)
                    k.mm(ps_s[:, 0:nq], kB[:, blk * 128:(blk + 1) * 128], qsB[:, q0:q0 + nq], start=False, stop=True)
                for bi in range(min(2, len(blks))):
                    smm(bi)
                for bi, blk in enumerate(blks):
                    p = pT[bi % 3]
                    k.actf(p[:, 0:nq], pss[bi % 4][:, 0:nq], AF.Exp)
                    if bi + 2 < len(blks):
                        smm(bi + 2)
                    k.mm(po[:, 0:nq], vT[:, blk, :], p[:, 0:nq], start=(bi == 0), stop=(bi == len(blks) - 1))
                    k.mm(pd[:, 0:nq], C.onesb[:, :], p[:, 0:nq], start=(bi == 0), stop=(bi == len(blks) - 1))
                rd = tmp[2]
                k.recip(rd[:, 0:nq], pd[:, 0:nq])
                k.tt(oT[:, h, q0:q0 + nq], po[:, 0:nq], rd[:, 0:nq], ALU.mult)
        if own is not None:
            k.dma(own[1][:, 8 + 2 * s:8 + 2 * s + 2, :], oT[:, :, 0:1024], waw=False)
            return
        r0_ = 1024 + s * 256
        k.dma(MIXT[r0_:r0_ + 256, :].rr("(h p) t -> p h t", p=128), oT[:], waw=False)


def tm_to_mixt(k, C, src, MIXT, r0_, pss, stg):
    it = 0
    for c in range(2):
        for g in range(5):
            nb = min(4, NB - g * 4)
            ps = pss[it % 2]; sg = stg[it % 2]
            for j in range(nb):
                blk = g * 4 + j
                k.transpose(ps[:, j * 128:(j + 1) * 128], src[:, blk, c * 128:(c + 1) * 128], C.ident[:, :])
            k.copy(sg[:, 0:nb * 128], ps[:, 0:nb * 128], eng="act" if it % 2 else "dve")
            k.dma(MIXT[r0_ + c * 128:r0_ + (c + 1) * 128, g * 512:g * 512 + nb * 128], sg[:, 0:nb * 128], waw=False)
            it += 1


def body_ml(k, C, PT, PM, s, P, MIXT):
    with k.scope() as st:
        sb = lambda shp, dt=F32: k.sb(shp, dt, stack=st)
        hv = lambda r: PT[r + s * 256:r + (s + 1) * 256, :].rr("(h p) t -> p h t", p=128)
        tv = lambda c: PM[:, c + s * 256:c + (s + 1) * 256].rr("(j p) c -> p j c", p=128)
        UL4 = sb([128, 4, 128]); k.dma(UL4[:], P["UL4"])
        qTb = sb([128, 2, S], BF16); kTb = sb([128, 2, S], BF16)
        kTM = sb([128, NB, 256]); vext = sb([128, NB, 2, 129], BF16)
        osg = sb([128, NB, 256])
        g16 = sb([128, NB, 16]); bi = sb([128, 4]); bfb = sb([128, 4]); gn = sb([128, 256])
        pss = [k.ps([128, 512], stack=st) for _ in range(8)]
        k.dma(kTM[:], tv(512)); k.dma(osg[:], tv(1536))
        k.dma(g16[:], PM[:, 2048:2064].rr("(j p) c -> p j c", p=128))
        for a, b in ((bi, "bi"), (bfb, "bf"), (gn, "gn")):
            k.dma(a[:], P[b])
        k.memset(vext[:, :, :, 128:129], 1.0)
        scale = 128 ** -0.5
        with k.scope() as st2:
            qf = k.sb([128, 2, S], stack=st2); vf = k.sb([128, NB, 256], stack=st2)
            k.dma(qf[:], hv(0)); k.dma(vf[:], tv(1024))
            k.dma(kTb[:], hv(512), q="pool")
            k.ts(qTb[:], qf[:], scale, ALU.mult)
            k.copy(vext[:, :, :, 0:128], vf[:].rr("p b (h d) -> p b h d", h=2))
        k.actf(osg[:], osg[:], AF.Sigmoid)
        lf = sb([128, NB, 4]); li = sb([128, NB, 4])
        for c in range(4):
            d, hl = c // 2, c % 2
            gc = d * 4 + 2 * s + hl
            k.ts(lf[:, :, c], g16[:, :, 8 + gc], bfb[:, c:c + 1], ALU.add)
            k.ts(li[:, :, c], g16[:, :, gc], bi[:, c:c + 1], ALU.add)
        k.actf(lf[:], lf[:], AF.Exp, scale=-1.0)
        k.actf(lf[:], lf[:], AF.Ln, bias=1.0)
        k.ts(lf[:], lf[:], -1.0, ALU.mult)
        bcol = sb([128, NB, 4]); acol = sb([128, NB, 4]); wg = sb([128, NB, 4]); ebtot = sb([128, NB, 4])
        lf2 = lf[:].rr("p b c -> p (b c)")
        pc = pss[0]
        k.mm(pc[:, 0:72], UL4[:, 0, :], lf2)
        k.mm(pc[:, 128:200], UL4[:, 2, :], lf2)
        k.copy(bcol[:, :, 0:2], pc[:, 0:72].rr("p (b c) -> p b c", c=4)[:, :, 0:2])
        k.copy(bcol[:, :, 2:4], pc[:, 128:200].rr("p (b c) -> p b c", c=4)[:, :, 2:4])
        k.tt(acol[:], li[:], bcol[:], ALU.subtract)
        k.mm(pc[:, 256:328], C.ones[:, :], lf2)
        btot = pc[:, 256:328].rr("p (b c) -> p b c", c=4)
        k.tt(wg[:], acol[:], btot, ALU.add)
        k.actf(wg[:], wg[:], AF.Exp)
        k.actf(ebtot[:], btot, AF.Exp)
        PT4 = sb([128, NB, 4, 128], BF16); QT4 = sb([128, NB, 4, 128], BF16)
        KW = sb([128, NB, 4, 128], BF16)
        with k.scope() as st3:
            X = [k.sb([128, 4, 128], stack=st3) for _ in range(2)]
            Dm = [k.sb([128, 4, 128], stack=st3) for _ in range(2)]
            Eb = [k.sb([128, 4, 128], stack=st3) for _ in range(2)]
            for blk in range(NB):
                x = X[blk % 2]; dm = Dm[blk % 2]; eb = Eb[blk % 2]
                pb = pss[1 + blk % 2]; psc = pss[3 + blk % 2]
                k.tt(x[:], UL4[:], lf[:, blk, :].unsq(2).bc([128, 4, 128]), ALU.mult)
                k.mm(pb[:, :], C.ones[:, :], x[:].rr("p c t -> p (c t)"))
                pb4 = pb[:, :].rr("p (c t) -> p c t", c=4)
                k.tt(dm[:], pb4, bcol[:, blk, :].unsq(2).bc([128, 4, 128]), ALU.subtract)
                k.tt(dm[:], dm[:], UL4[:], ALU.mult)
                k.tt(dm[:], dm[:], li[:, blk, :].unsq(2).bc([128, 4, 128]), ALU.add)
                k.actf(dm[:], dm[:], AF.Exp)
                k.tt(dm[:], dm[:], UL4[:], ALU.mult)
                for hl in range(2):
                    k.mm(psc[:, hl * 128:(hl + 1) * 128], kTb[:, hl, blk * 128:(blk + 1) * 128], qTb[:, hl, blk * 128:(blk + 1) * 128])
                for d in range(2):
                    k.tt(PT4[:, blk, d * 2:d * 2 + 2, :], dm[:, d * 2:d * 2 + 2, :],
                         psc[:, 0:256].rr("p (h t) -> p h t", h=2), ALU.mult)
                k.actf(eb[:], pb4, AF.Exp)
                for d in range(2):
                    k.tt(QT4[:, blk, d * 2:d * 2 + 2, :], eb[:, d * 2:d * 2 + 2, :],
                         qTb[:, :, blk * 128:(blk + 1) * 128], ALU.mult)
                for c in range(4):
                    hl = c % 2
                    k.ts(KW[:, blk, c, :], kTM[:, blk, hl * 128:(hl + 1) * 128], wg[:, blk, c:c + 1], ALU.mult)
        CT = sb([128, 4, 129]); CTb = sb([128, 4, 129], BF16)
        k.memset(CT[:], 0.0); k.memset(CTb[:], 0.0)
        H = [sb([128, NB, 2, 128]) for _ in range(2)]
        dn = [sb([128, 1]) for _ in range(4)]
        chains = [(c, c // 2, c % 2) for c in range(4)]
        for step in range(NB):
            blks = [FWD_ORDER[step] if d == 0 else BWD_ORDER[step] for (c, d, hl) in chains]
            for (c, d, hl), blk in zip(chains, blks):
                pn = pss[c]
                k.mm(pn[:, 0:129], PT4[:, blk, c, :], vext[:, blk, hl, :], start=True, stop=False)
                k.mm(pn[:, 0:129], QT4[:, blk, c, :], CTb[:, c, :], start=False, stop=True)
            if step < NB - 1:
                for (c, d, hl), blk in zip(chains, blks):
                    k.mm(pss[4 + c][:, 0:129], KW[:, blk, c, :], vext[:, blk, hl, :])
                for (c, d, hl), blk in zip(chains, blks):
                    k.stt(CT[:, c, :], CT[:, c, :], ebtot[:, blk, c:c + 1], pss[4 + c][:, 0:129], ALU.mult, ALU.add)
                for (c, d, hl), blk in zip(chains, blks):
                    k.copy(CTb[:, c, :], CT[:, c, :], eng="act")
            for (c, d, hl), blk in zip(chains, blks):
                k.actf(dn[c][:], pss[c][:, 128:129], AF.Abs)
            for (c, d, hl), blk in zip(chains, blks):
                k.ts(dn[c][:], dn[c][:], 1.0, ALU.max)
            for (c, d, hl), blk in zip(chains, blks):
                k.recip(dn[c][:], dn[c][:])
            for (c, d, hl), blk in zip(chains, blks):
                k.ts(H[d][:, blk, hl, :], pss[c][:, 0:128], dn[c][:, 0:1], ALU.mult)
        Hs = H[0]
        k.tt(Hs[:], H[0][:], H[1][:], ALU.add)
        sq = H[1]
        k.tt(sq[:], Hs[:], Hs[:], ALU.mult)
        ss = sb([128, NB * 2])
        k.reduce(ss[:], sq[:].rr("p b h d -> p (b h) d"), ALU.add, AX.X)
        k.ts(ss[:], ss[:], 1.0 / 128, ALU.mult, EPS, ALU.add)
        k.actf(ss[:], ss[:], AF.Sqrt)
        k.recip(ss[:], ss[:])
        outb = sq
        for blk in range(NB):
            for hl in range(2):
                k.stt(outb[:, blk, hl, :], Hs[:, blk, hl, :], ss[:, blk * 2 + hl:blk * 2 + hl + 1],
                      gn[:, hl * 128:(hl + 1) * 128], ALU.mult, ALU.mult)
        k.tt(osg[:], osg[:], outb[:].rr("p b h d -> p b (h d)"), ALU.mult)
        stg = [sb([128, 512]) for _ in range(2)]
        tm_to_mixt(k, C, osg, MIXT, s * 256, pss[4:6], stg)


def body_ssd(k, C, PT, PM, s, P, MIXT):
    with k.scope() as st:
        sb = lambda shp, dt=F32: k.sb(shp, dt, stack=st)
        UL8 = sb([128, 8, 128])
        cw = sb([128, 4, 5]); cb = sb([128, 4]); dtb = sb([128, 8]); alog = sb([128, 8]); dsk = sb([128, 256])
        zs = sb([128, NB, 256]); d16 = sb([128, NB, 16]); dt = sb([128, NB, 8])
        for a, b in ((UL8, "UL8"), (cw, "cw"), (cb, "cb"), (dtb, "dtb"), (alog, "alog"), (dsk, "dskip")):
            k.dma(a[:], P[b])
        k.dma(zs[:], PM[:, 2064 + s * 256:2064 + (s + 1) * 256].rr("(j p) c -> p j c", p=128))
        k.dma(d16[:], PM[:, 3600:3616].rr("(j p) c -> p j c", p=128))
        pss = [k.ps([128, 512], stack=st) for _ in range(8)]
        BTb = sb([128, S], BF16); CTb = sb([128, S], BF16)
        xTM = sb([128, NB, 256]); BTM = sb([128, NB, 128])
        with k.scope() as st2:
            u = k.sb([128, 4, S], stack=st2); acc = k.sb([128, 4, S], stack=st2)
            xs = u
            k.dma(u[:, 0:2, :], PT[2576 + s * 256:2576 + (s + 1) * 256, :].rr("(c p) t -> p c t", p=128))
            k.dma(u[:, 2, :], PT[3088 + s * 128:3088 + (s + 1) * 128, :])
            k.dma(u[:, 3, :], PT[3344 + s * 128:3344 + (s + 1) * 128, :])
            for c in range(4):
                k.ts(acc[:, c, :], u[:, c, :], cw[:, c, 2:3], ALU.mult)
                for j in (0, 1, 3, 4):
                    d = j - 2
                    for (a, b) in ((0, 256), (256, S)):
                        lo = a + max(0, -d); hi = b - max(0, d)
                        k.stt(acc[:, c, lo:hi], u[:, c, lo + d:hi + d], cw[:, c, j:j + 1], acc[:, c, lo:hi], ALU.mult, ALU.add)
            for c in range(2):
                k.actf(xs[:, c, :], acc[:, c, :], AF.Silu, bias=cb[:, c:c + 1])
            k.actf(acc[:, 2, :], acc[:, 2, :], AF.Silu, bias=cb[:, 2:3])
            k.actf(CTb[:], acc[:, 3, :], AF.Silu, bias=cb[:, 3:4])
            k.copy(BTb[:], acc[:, 2, :])
            for blk in range(NB):
                pt = pss[blk % 4]
                for c in range(2):
                    k.transpose(pt[:, c * 128:(c + 1) * 128], xs[:, c, blk * 128:(blk + 1) * 128], C.ident[:, :])
                k.transpose(pt[:, 256:384], acc[:, 2, blk * 128:(blk + 1) * 128], C.ident[:, :])
                k.copy(xTM[:, blk, :], pt[:, 0:256], eng="act")
                k.copy(BTM[:, blk, :], pt[:, 256:384])
        A = sb([128, 8])
        k.actf(A[:], alog[:], AF.Exp)
        k.ts(A[:], A[:], -1.0, ALU.mult)
        for c in range(8):
            d, hh = c // 4, c % 4
            k.ts(dt[:, :, c], d16[:, :, d * 8 + 4 * s + hh], dtb[:, c:c + 1], ALU.add)
        k.actf(dt[:], dt[:], AF.Exp)
        k.actf(dt[:], dt[:], AF.Ln, bias=1.0)
        av = sb([128, NB, 8])
        for c in range(8):
            k.ts(av[:, :, c], dt[:, :, c], A[:, c:c + 1], ALU.mult)
        bcol = sb([128, NB, 8]); wtail = sb([128, NB, 8]); ebtot = sb([128, NB, 8])
        a2 = av[:].rr("p b c -> p (b c)")
        pc = pss[0]
        k.mm(pc[:, 0:144], UL8[:, 0, :], a2)
        k.mm(pc[:, 160:304], UL8[:, 4, :], a2)
        k.copy(bcol[:, :, 0:4], pc[:, 0:144].rr("p (b c) -> p b c", c=8)[:, :, 0:4])
        k.copy(bcol[:, :, 4:8], pc[:, 160:304].rr("p (b c) -> p b c", c=8)[:, :, 4:8])
        k.mm(pc[:, 320:464], C.ones[:, :], a2)
        btot = pc[:, 320:464].rr("p (b c) -> p b c", c=8)
        k.tt(wtail[:], btot, bcol[:], ALU.subtract)
        k.actf(wtail[:], wtail[:], AF.Exp)
        k.actf(ebtot[:], btot, AF.Exp)
        xt = sb([128, NB, 8, 64], BF16)
        for blk in range(NB):
            for d in range(2):
                k.tt(xt[:, blk, d * 4:(d + 1) * 4, :], xTM[:, blk, :].rr("p (h e) -> p h e", h=4),
                     dt[:, blk, d * 4:(d + 1) * 4].unsq(2).bc([128, 4, 64]), ALU.mult, eng="pool" if d else "dve")
        PT8 = sb([128, NB, 8, 128], BF16); CT8 = sb([128, NB, 8, 128], BF16)
        with k.scope() as st3:
            X = [k.sb([128, 8, 128], stack=st3) for _ in range(2)]
            Dm = [k.sb([128, 8, 128], stack=st3) for _ in range(2)]
            for blk in range(NB):
                x = X[blk % 2]; dm = Dm[blk % 2]
                pb0 = pss[1 + (blk % 2) * 2]; pb1 = pss[2 + (blk % 2) * 2]; pg = pss[5 + blk % 2]
                k.tt(x[:], UL8[:], av[:, blk, :].unsq(2).bc([128, 8, 128]), ALU.mult)
                k.mm(pb0[:, :], C.ones[:, :], x[:, 0:4, :].rr("p c t -> p (c t)"))
                k.mm(pb1[:, :], C.ones[:, :], x[:, 4:8, :].rr("p c t -> p (c t)"))
                k.mm(pg[:, 0:128], BTb[:, blk * 128:(blk + 1) * 128], CTb[:, blk * 128:(blk + 1) * 128])
                for d, pb in enumerate((pb0, pb1)):
                    pb4 = pb[:, :].rr("p (c t) -> p c t", c=4)
                    sl = slice(d * 4, (d + 1) * 4)
                    k.tt(dm[:, sl, :], pb4, bcol[:, blk, sl].unsq(2).bc([128, 4, 128]), ALU.subtract)
                    k.tt(dm[:, sl, :], dm[:, sl, :], UL8[:, sl, :], ALU.mult)
                    k.actf(dm[:, sl, :], dm[:, sl, :], AF.Exp)
                    k.tt(dm[:, sl, :], dm[:, sl, :], UL8[:, sl, :], ALU.mult, eng="pool")
                    k.tt(PT8[:, blk, sl, :], dm[:, sl, :], pg[:, 0:128].unsq(1).bc([128, 4, 128]), ALU.mult)
                    k.actf(x[:, sl, :], pb4, AF.Exp)
                    k.tt(CT8[:, blk, sl, :], x[:, sl, :], CTb[:, blk * 128:(blk + 1) * 128].unsq(1).bc([128, 4, 128]),
                         ALU.mult, eng="pool")
        HT = sb([128, 8, 64]); HTb = sb([128, 8, 64], BF16)
        k.memset(HT[:], 0.0); k.memset(HTb[:], 0.0)
        Y = [sb([128, NB, 256]) for _ in range(2)]
        Bw = [sb([128, 128], BF16) for _ in range(8)]
        for step in range(NB):
            bl = [FWD_ORDER[step], BWD_ORDER[step]]
            if step < NB - 1:
                for d in range(2):
                    for hh in range(4):
                        c = d * 4 + hh
                        k.ts(Bw[c][:], BTM[:, bl[d], :], wtail[:, bl[d], c:c + 1], ALU.mult)
            for d in range(2):
                for hh in range(4):
                    c = d * 4 + hh
                    k.mm(pss[d][:, hh * 64:(hh + 1) * 64], PT8[:, bl[d], c, :], xt[:, bl[d], c, :], start=True, stop=False)
                    k.mm(pss[d][:, hh * 64:(hh + 1) * 64], CT8[:, bl[d], c, :], HTb[:, c, :], start=False, stop=True)
            if step < NB - 1:
                for d in range(2):
                    for hh in range(4):
                        c = d * 4 + hh
                        k.mm(pss[2 + d][:, hh * 64:(hh + 1) * 64], Bw[c][:], xt[:, bl[d], c, :])
                for d in range(2):
                    k.tt(HT[:, d * 4:(d + 1) * 4, :], HT[:, d * 4:(d + 1) * 4, :],
                         ebtot[:, bl[d], d * 4:(d + 1) * 4].unsq(2).bc([128, 4, 64]), ALU.mult)
                    k.tt(HT[:, d * 4:(d + 1) * 4, :], HT[:, d * 4:(d + 1) * 4, :],
                         pss[2 + d][:, 0:256].rr("p (h e) -> p h e", h=4), ALU.add)
                for d in range(2):
                    k.copy(HTb[:, d * 4:(d + 1) * 4, :], HT[:, d * 4:(d + 1) * 4, :], eng="act")
            for d in range(2):
                k.copy(Y[d][:, bl[d], :], pss[d][:, 0:256], eng="act")
        k.tt(Y[0][:], Y[0][:], Y[1][:], ALU.add)
        k.tt(xTM[:], xTM[:], dsk[:, :].unsq(1).bc([128, NB, 256]), ALU.mult)
        k.tt(Y[0][:], Y[0][:], xTM[:], ALU.add)
        k.actf(zs[:], zs[:], AF.Silu)
        k.tt(Y[0][:], Y[0][:], zs[:], ALU.mult)
        stg = [Y[1][:, 0:2, :].rr("p a c -> p (a c)"), Y[1][:, 2:4, :].rr("p a c -> p (a c)")]
        tm_to_mixt(k, C, Y[0], MIXT, 512 + s * 256, pss[4:6], stg)


def outproj_tiles(k, C, mixsrc, xT, tiles, NT, wo_d, g1, gs, pss, st):
    mixb = k.sb([128, 16, NT], BF16, stack=st); ssd = k.sb([128, 4, NT], stack=st)
    tmp = [k.sb([128, 512], stack=st) for _ in range(3)]
    wos = [k.sb([128, 16, 512], BF16, stack=st) for _ in range(2)]
    wov = wo_d.rr("(kc p) n -> p kc n", p=128)

    def loadwo(g):
        for q in range(2):
            k.dma(wos[g % 2][:, q * 8:(q + 1) * 8, :], wov[:, q * 8:(q + 1) * 8, g * 512:(g + 1) * 512], q="pool")
    k.dma(mixb[:, 0:4, :], mixsrc[:, 0:4, :], q="pool"); k.dma(mixb[:, 8:12, :], mixsrc[:, 8:12, :], q="pool")
    k.dma(mixb[:, 12:16, :], mixsrc[:, 12:16, :], q="pool"); k.dma(ssd[:], mixsrc[:, 4:8, :])
    loadwo(0)
    for (t0, n, _) in tiles:
        ps = pss[0]
        for c in range(4):
            sq = tmp[c % 2]
            k.actf(sq[:, 0:n], ssd[:, c, t0:t0 + n], AF.Square)
            k.mm(ps[:, 0:n], C.ones[:, :], sq[:, 0:n], start=(c == 0), stop=(c == 3))
        rs = tmp[2]
        rstd_from_ps(k, rs, ps, n, 512)
        for c in range(4):
            k.stt(mixb[:, 4 + c, t0:t0 + n], ssd[:, c, t0:t0 + n], gs[:, c:c + 1], rs[:, 0:n], ALU.mult, ALU.mult)
    jt = 0
    for g in range(4):
        if g + 1 < 4:
            loadwo(g + 1)
        for dl in range(4):
            dc = g * 4 + dl
            for (t0, n, col) in tiles:
                po = pss[4 + jt % 4]
                for kc in range(16):
                    k.mm(po[:, 0:n], wos[g % 2][:, kc, dl * 128:(dl + 1) * 128], mixb[:, kc, t0:t0 + n],
                         start=(kc == 0), stop=(kc == 15))
                k.stt(xT[:, dc, t0:t0 + n], po[:, 0:n], g1[:, dc, col:col + 1], xT[:, dc, t0:t0 + n], ALU.mult, ALU.add)
                jt += 1


def phase_ffn0(k, C, xsrc, MIXT, XR, tok0, tiles, modl, n2, gs, wo_d, w1d, w3d, w2d, pss):
    NT = sum(n for _, n, _ in tiles)
    g1 = modl(2); sh2 = modl(3); sc2 = modl(4); g2 = modl(5)
    mixv = MIXT.rr("(kc p) t -> p kc t", p=128)
    with k.scope() as st:
        xT = k.sb([128, 16, NT], stack=st)
        for q in range(4):
            k.dma(xT[:, q * 4:(q + 1) * 4, :], xsrc[:, q * 4:(q + 1) * 4, tok0:tok0 + NT])
        with k.scope() as st2:
            outproj_tiles(k, C, mixv[:, :, tok0:tok0 + NT], xT, tiles, NT, wo_d, g1, gs, pss, st2)
        h2T = k.sb([128, 16, NT], BF16, stack=st)
        with k.scope() as st2:
            tmp = [k.sb([128, 512], stack=st2) for _ in range(3)]
            A = k.sb([128, 16, 2], stack=st2)
            make_A(k, A, n2, sc2)
            for ti, (t0, n, col) in enumerate(tiles):
                norm_tile(k, C, xT[:, :, t0:t0 + n], h2T[:, :, t0:t0 + n], n, A, sh2, col, pss[ti % 2], tmp)
        colof = {t0: col for (t0, n, col) in tiles}

        def acc(po, dc, t0, n):
            col = colof[t0]
            k.stt(xT[:, dc, t0:t0 + n], po, g2[:, dc, col:col + 1], xT[:, dc, t0:t0 + n], ALU.mult, ALU.add)
        with k.scope() as st2:
            ffn_simple(k, h2T, tiles, w1d, w3d, w2d, FD, acc, pss, stack=st2)
        for q in range(4):
            k.dma(XR[:, q * 4:(q + 1) * 4, tok0:tok0 + NT], xT[:, q * 4:(q + 1) * 4, :], waw=False)


def phase_moe(k, C, XO, MIXO, modl, n2, gs, wo_d, rt_d, sel_d, w1d, w3d, w2d, xout, pss):
    NT = 1024
    tiles = [(0, 512, 0), (512, 512, 0)]
    g1 = modl(2); sh2 = modl(3); sc2 = modl(4); g2 = modl(5)
    with k.scope() as st:
        xT = k.sb([128, 16, NT], stack=st)
        for q in range(4):
            k.dma(xT[:, q * 4:(q + 1) * 4, :], XO[:, q * 4:(q + 1) * 4, :])
        with k.scope() as st2:
            outproj_tiles(k, C, MIXO, xT, tiles, NT, wo_d, g1, gs, pss, st2)
        h2B = k.sb([128, 16, NT], BF16, stack=st)
        gT8 = k.sb([8, NT], stack=st)
        sel = k.sb([8, 8, 128], stack=st)
        k.dma(sel[:], sel_d)
        with k.scope() as st2:
            h2F = k.sb([128, 16, NT], stack=st2)
            tmp = [k.sb([128, 512], stack=st2) for _ in range(3)]
            A = k.sb([128, 16, 2], stack=st2)
            rt = k.sb([128, 16, 8], stack=st2)
            k.dma(rt[:], rt_d)
            make_A(k, A, n2, sc2)
            for ti, (t0, n, col) in enumerate(tiles):
                norm_tile(k, C, xT[:, :, t0:t0 + n], h2F[:, :, t0:t0 + n], n, A, sh2, col, pss[ti % 2], tmp)
            for q in range(4):
                k.copy(h2B[:, q * 4:(q + 1) * 4, :], h2F[:, q * 4:(q + 1) * 4, :], eng="act" if q % 2 else "dve")
            lg = k.sb([128, 8], stack=st2); m1 = k.sb([128, 1], stack=st2); m2 = k.sb([128, 1], stack=st2)
            t8 = k.sb([128, 8], stack=st2); sl = k.sb([128, 8], stack=st2); sm = k.sb([128, 1], stack=st2)
            gts = k.sb([128, 8], stack=st2)
            for tb in range(NT // 128):
                pl = pss[tb % 2]; pt = pss[2 + tb % 2]
                for kc in range(16):
                    k.mm(pl[:, 0:8], h2F[:, kc, tb * 128:(tb + 1) * 128], rt[:, kc, :], start=(kc == 0), stop=(kc == 15))
                k.copy(lg[:], pl[:, 0:8])
                k.reduce(m1[:], lg[:], ALU.max)
                k.ts(t8[:], lg[:], m1[:, 0:1], ALU.is_ge, -1e30, ALU.mult)
                k.tt(t8[:], t8[:], lg[:], ALU.add)
                k.reduce(m2[:], t8[:], ALU.max)
                k.ts(sl[:], lg[:], m2[:, 0:1], ALU.is_ge)
                k.ts(t8[:], lg[:], m1[:, 0:1], ALU.subtract)
                k.actf(t8[:], t8[:], AF.Exp)
                k.tt(t8[:], t8[:], sl[:], ALU.mult)
                k.reduce(sm[:], t8[:], ALU.add)
                k.recip(sm[:], sm[:])
                k.ts(gts[:], t8[:], sm[:, 0:1], ALU.mult)
                k.transpose(pt[0:8, 0:128], gts[:, :], C.ident[:, :])
                k.copy(gT8[:, tb * 128:(tb + 1) * 128], pt[0:8, 0:128])
        gbc = k.sb([128, NT], stack=st)
        for e in range(8):
            for (t0, n, _) in tiles:
                pg = pss[0]
                k.mm(pg[:, 0:n], sel[:, e, :], gT8[:, t0:t0 + n])
                k.copy(gbc[:, t0:t0 + n], pg[:, 0:n])

            def acc(po, dc, t0, n):
                k.stt(xT[:, dc, t0:t0 + n], po, g2[:, dc, 0:1], xT[:, dc, t0:t0 + n], ALU.mult, ALU.add)
            with k.scope() as st2:
                ffn_simple(k, h2B, tiles, w1d[e], w3d[e], w2d[e], FE, acc, pss, stack=st2, gate=gbc)
        for q in range(4):
            k.dma(xout[:, q * 4:(q + 1) * 4, :], xT[:, q * 4:(q + 1) * 4, :])


def build_mega(stop_after=None):
    k = KB()
    C = Consts()
    xin = k.din("xT", [128, 16, S])
    ccT_d = k.din("ccT", [128, 16, 2]); modw_d = k.din("mod_w", [2, D, 6 * D]); modb_d = k.din("mod_bT", [128, 2, 96])
    n1_d = k.din("n1T", [128, 2, 16]); n2_d = k.din("n2T", [128, 2, 16]); gs_d = k.din("gsT", [128, 2, 4])
    win_d = k.din("w_in", [2, D, DIN]); wout_d = k.din("w_out", [2, D, D])
    ident_d = k.din("ident", [128, 128])
    na_g = k.din("na_g", [128, 2, 2]); na_bt = k.din("na_bt", [128, 2, 2, 2, 14, 64])
    mla_wq = k.din("mla_wq", [2, 2, 128, 4, 384]); mla_wkv = k.din("mla_wkv", [2, 2, 128, 2, 512])
    mla_v = k.din("mla_vec", [128, 2, 10]); rope_c = k.din("cosF", [64, 2048]); rope_s = k.din("sinF", [64, 2048])
    rope_r = k.din("RT", [64, 64])
    ml_b = k.din("ml_b", [128, 2, 2, 8]); ml_gn = k.din("ml_gn", [128, 2, 2, 256]); ul4_d = k.din("UL4", [128, 4, 128])
    ssd_cw = k.din("ssd_cw", [128, 2, 2, 4, 5]); ssd_cb = k.din("ssd_cb", [128, 2, 2, 4])
    ssd_v = k.din("ssd_vec", [128, 2, 2, 16]); ssd_dk = k.din("ssd_dsk", [128, 2, 2, 256]); ul8_d = k.din("UL8", [128, 8, 128])
    if stop_after is None or stop_after[0] != "mix" or stop_after[1] > 0:
        fw1 = k.din("ffn_w1", [D, FD]); fw3 = k.din("ffn_w3", [D, FD]); fw2 = k.din("ffn_w2", [FD, D])
    if stop_after is None:
        rt_d = k.din("router", [128, 16, 8]); sel_d = k.din("sel", [8, 8, 128]); selv_d = k.din("selv", [128, 2])
        mw1 = k.din("moe_w1", [8, D, FE]); mw3 = k.din("moe_w3", [8, D, FE]); mw2 = k.din("moe_w2", [8, FE, D])
        xout = k.dout("xo", [128, 16, 1024])
        XO = k.dram("XO", [128, 16, 1024], F32, "Internal"); MIXO = k.dram("MIXO", [128, 16, 1024], F32, "Internal")
    PT = k.dram("PT", [DIN, S], F32, "Internal"); PM = k.dram("PM", [S, DIN], F32, "Internal")
    MIXT = k.dram("MIXT", [D, S], F32, "Internal"); XR = k.dram("XR", [128, 16, S], F32, "Internal")
    C.ones = k.sb([128, 128]); k.memset(C.ones[:], 1.0)
    C.onesb = k.sb([128, 128], BF16); k.memset(C.onesb[:], 1.0)
    C.ident = k.sb([128, 128]); k.dma(C.ident[:], ident_d[:])
    modsb = k.sb([128, 2, 96, 2])
    n1 = k.sb([128, 2, 16]); n2 = k.sb([128, 2, 16]); gs = k.sb([128, 2, 4])
    k.dma(n1[:], n1_d[:]); k.dma(n2[:], n2_d[:]); k.dma(gs[:], gs_d[:])
    scb = k.sb([128, 16, 2], BF16); bsb = k.sb([128, 2, 96])
    with k.scope() as st:
        pss = [k.ps([128, 512], stack=st) for _ in range(8)]
        phase_mod(k, C, ccT_d, modw_d, modb_d, modsb, scb, bsb, pss)
    for l in range(2):
        modl = lambda which, l=l: modsb[:, l, which * 16:(which + 1) * 16, :]
        xsrc = xin[:] if l == 0 else XR[:]
        with k.scope() as st:
            pss = [k.ps([128, 512], stack=st) for _ in range(8)]
            phase_inproj(k, C, xsrc, n1[:, l, :], modl(1), modl(0), win_d[l], PT, PM, pss,
                         modctx=(modw_d, scb, bsb, modsb) if l == 0 else None)
        for s in range(2):
            body_ml(k, C, PT, PM, s, {"bi": ml_b[:, l, s, 0:4], "bf": ml_b[:, l, s, 4:8], "gn": ml_gn[:, l, s, :],
                                      "UL4": ul4_d[:]}, MIXT)
            body_ssd(k, C, PT, PM, s, {"cw": ssd_cw[:, l, s], "cb": ssd_cb[:, l, s], "dtb": ssd_v[:, l, s, 0:8],
                                       "alog": ssd_v[:, l, s, 8:16], "dskip": ssd_dk[:, l, s, :], "UL8": ul8_d[:]}, MIXT)
            body_mla(k, C, PT, s, {"wq": mla_wq[l, s], "wkv": mla_wkv[l, s], "qn": mla_v[:, l, 0:4], "kvn": mla_v[:, l, 4:6],
                                   "gq": mla_v[:, l, 6:8], "gk": mla_v[:, l, 8:10], "cosF": rope_c[:], "sinF": rope_s[:],
                                   "RT": rope_r[:]}, MIXT, own=(selv_d, MIXO) if (l == 1 and stop_after is None) else None)
            body_na(k, C, PT, PM, s, na_g[:, l, 0:1], na_g[:, l, 1:2], na_bt[:, l, s], MIXT)
        if stop_after == ("mix", l):
            dbg = k.dout("dbg", [D, S])
            with k.scope() as st:
                t = k.sb([128, 16, S], stack=st)
                k.dma(t[:], MIXT[:].rr("(kc p) t -> p kc t", p=128))
                k.dma(dbg[:].rr("(kc p) t -> p kc t", p=128), t[:])
            return k.finish()
        with k.scope() as st:
            pss = [k.ps([128, 512], stack=st) for _ in range(8)]
            if l == 0:
                phase_ffn0(k, C, xin[:], MIXT[:], XR, 0, [(0, 256, 1), (256, 512, 0), (768, 384, 0)], modl, n2[:, l, :],
                           gs[:, l, :], wout_d[l], fw1[:], fw3[:], fw2[:], pss)
                phase_ffn0(k, C, xin[:], MIXT[:], XR, 1152, [(0, 512, 0), (512, 512, 0), (1024, 128, 0)], modl, n2[:, l, :],
                           gs[:, l, :], wout_d[l], fw1[:], fw3[:], fw2[:], pss)
            else:
                phase_select(k, XR[:], MIXT[:].rr("(kc p) t -> p kc t", p=128), XO, MIXO, selv_d, skip_mix_q=(2,))
                phase_moe(k, C, XO[:], MIXO[:], modl, n2[:, l, :], gs[:, l, :], wout_d[l], rt_d[:], sel_d[:],
                          mw1, mw3, mw2, xout, pss)
    return k.finish()


def phase_select(k, xr, mixv, XO, MIXO, selv_d, skip_mix_q=()):
    with k.scope() as st:
        sv = k.sb([128, 2], stack=st)
        k.dma(sv[:], selv_d[:])
        a = [k.sb([128, 4, 1024], stack=st) for _ in range(2)]
        b = [k.sb([128, 4, 1024], stack=st) for _ in range(2)]
        it = 0
        for si, (src, dst) in enumerate(((xr, XO), (mixv, MIXO))):
            for q in range(4):
                if si == 1 and q in skip_mix_q:
                    continue
                ta = a[it % 2]; tb = b[it % 2]; it += 1
                k.dma(ta[:], src[:, q * 4:(q + 1) * 4, 256:1280])
                k.dma(tb[:], src[:, q * 4:(q + 1) * 4, 1280:2304])
                k.ts(ta[:], ta[:], sv[:, 0:1], ALU.mult)
                k.stt(ta[:], tb[:], sv[:, 1:2], ta[:], ALU.mult, ALU.add)
                k.dma(dst[:, q * 4:(q + 1) * 4, :], ta[:], waw=False)


ROPE = rope_tables()
UL4c = tri_consts()
UL8c = tri8()


def _rep(v):
    return np.ascontiguousarray(np.repeat(np.asarray(v, np.float32).reshape(1, -1), 128, 0))


def mega_inputs(z, full=True):
    L = 2
    shared = {}
    shared["mod_w"] = np.ascontiguousarray(z['mod_w'])
    shared["mod_bT"] = np.ascontiguousarray(z['mod_b'].reshape(2, 96, 128).transpose(2, 0, 1))
    shared["n1T"] = np.ascontiguousarray(z['norm1'].reshape(2, 16, 128).transpose(2, 0, 1))
    shared["n2T"] = np.ascontiguousarray(z['norm2'].reshape(2, 16, 128).transpose(2, 0, 1))
    shared["gsT"] = np.ascontiguousarray(z['ssd_norm'].reshape(2, 4, 128).transpose(2, 0, 1))
    shared["w_in"] = np.ascontiguousarray(z['w_in']); shared["w_out"] = np.ascontiguousarray(z['w_out'])
    shared["ident"] = np.eye(128, dtype=np.float32)
    shared["na_g"] = np.ascontiguousarray(np.stack([z['na_gq'], z['na_gk']], -1).transpose(1, 0, 2))
    shared["na_bt"] = np.ascontiguousarray(np.stack(
        [np.stack([na_bias_tiles(z['na_rpb'][l], [2 * s, 2 * s + 1]) for s in range(2)], 1) for l in range(L)], 1))
    shared["mla_wq"] = np.ascontiguousarray(np.stack(
        [np.stack([w_pad(z['mla_w_qb'][l][:, s * 384:(s + 1) * 384], 4) for s in range(2)], 0) for l in range(L)], 0))
    shared["mla_wkv"] = np.ascontiguousarray(np.stack(
        [np.stack([w_pad(z['mla_w_kvb'][l][:, s * 512:(s + 1) * 512], 2) for s in range(2)], 0) for l in range(L)], 0))
    shared["mla_vec"] = np.ascontiguousarray(np.stack(
        [np.concatenate([vec_pad(z['mla_q_norm'][l], 4), vec_pad(z['mla_kv_norm'][l], 2), vec_pad(z['mla_gq'][l], 2),
                         vec_pad(z['mla_gk'][l], 2)], 1) for l in range(L)], 1))
    shared["cosF"], shared["sinF"], shared["RT"] = ROPE
    shared["ml_b"] = np.ascontiguousarray(np.stack(
        [np.stack([_rep(np.concatenate([z['ml_i_bias'][l][:, 2 * s:2 * s + 2].reshape(4),
                                        z['ml_f_bias'][l][:, 2 * s:2 * s + 2].reshape(4)])) for s in range(2)], 1)
         for l in range(L)], 1))
    shared["ml_gn"] = np.ascontiguousarray(np.stack(
        [np.stack([_rep(z['ml_norm'][l][s * 256:(s + 1) * 256]) for s in range(2)], 1) for l in range(L)], 1))
    shared["UL4"] = UL4c; shared["UL8"] = UL8c
    cw_l, cb_l, v_l, dk_l = [], [], [], []
    for l in range(L):
        cw_s, cb_s, v_s, dk_s = [], [], [], []
        for s in range(2):
            chans = np.concatenate([np.arange(s * 256, (s + 1) * 256), 512 + np.arange(s * 128, (s + 1) * 128),
                                    768 + np.arange(s * 128, (s + 1) * 128)])
            cw_s.append(z['ssd_conv_w'][l][:, chans].T.reshape(4, 128, 5).transpose(1, 0, 2))
            cb_s.append(z['ssd_conv_b'][l][chans].reshape(4, 128).T)
            v_s.append(_rep(np.concatenate([z['ssd_dt_bias'][l][:, 4 * s:4 * s + 4].reshape(8),
                                            z['ssd_A_log'][l][:, 4 * s:4 * s + 4].reshape(8)])))
            dk_s.append(_rep(np.repeat(z['ssd_D'][l][4 * s:4 * s + 4], 64)))
        cw_l.append(np.stack(cw_s, 1)); cb_l.append(np.stack(cb_s, 1)); v_l.append(np.stack(v_s, 1)); dk_l.append(np.stack(dk_s, 1))
    shared["ssd_cw"] = np.ascontiguousarray(np.stack(cw_l, 1)); shared["ssd_cb"] = np.ascontiguousarray(np.stack(cb_l, 1))
    shared["ssd_vec"] = np.ascontiguousarray(np.stack(v_l, 1)); shared["ssd_dsk"] = np.ascontiguousarray(np.stack(dk_l, 1))
    if full:
        shared["ffn_w1"] = np.ascontiguousarray(z['ffn_w1'][0]); shared["ffn_w3"] = np.ascontiguousarray(z['ffn_w3'][0])
        shared["ffn_w2"] = np.ascontiguousarray(z['ffn_w2'][0])
        shared["router"] = np.ascontiguousarray(z['moe_router'][0].reshape(16, 128, 8).transpose(1, 0, 2))
        sel = np.zeros((8, 8, 128), np.float32)
        for e in range(8):
            sel[e, e, :] = 1.0
        shared["sel"] = sel
        shared["moe_w1"] = np.ascontiguousarray(z['moe_w1'][0]); shared["moe_w3"] = np.ascontiguousarray(z['moe_w3'][0])
        shared["moe_w2"] = np.ascontiguousarray(z['moe_w2'][0])
    maps = []
    for i in range(NCORES):
        b, s = i // 2, i % 2
        m_ = dict(shared)
        m_["xT"] = to_fm(np.concatenate([z['ctx'][b], z['x'][b]], 0))
        cc = np.stack([z['c'][b], z['c_ctx']], 0)
        m_["ccT"] = np.ascontiguousarray(cc.T.reshape(16, 128, 2).transpose(1, 0, 2))
        if full:
            sv = np.zeros((128, 2), np.float32); sv[:, s] = 1.0
            m_["selv"] = sv
        maps.append(m_)
    return maps


def kernel(**inputs):
    z = {k_: np.asarray(v) for k_, v in inputs.items()}
    res = run(build_mega(), mega_inputs(z))
    out = np.zeros((4, 2048, 2048), np.float32)
    for i in range(NCORES):
        b, s = i // 2, i % 2
        out[b, s * 1024:(s + 1) * 1024] = res[i]["xo"].transpose(2, 1, 0).reshape(1024, 2048)
    return out
```

```python
import contextlib
import numpy as np
import concourse.bass as bass
import concourse.mybir as mybir
from concourse.bass_utils import run_bass_kernel_spmd

F32 = mybir.dt.float32
BF16 = mybir.dt.bfloat16
AF = mybir.ActivationFunctionType
ALU = mybir.AluOpType
AX = mybir.AxisListType
NCORES = 8


class Trk:
    __slots__ = ("w", "r")

    def __init__(self):
        self.w = {}
        self.r = {}


class V:
    __slots__ = ("ap", "trks")

    def __init__(self, ap, trks):
        self.ap = ap
        self.trks = trks

    def __getitem__(self, idx):
        return V(self.ap[idx], self.trks)

    def bc(self, shape):
        return V(self.ap.to_broadcast(shape), self.trks)


class Buf:
    def __init__(self, t, nreg=1):
        self.t = t
        self.regs = [Trk() for _ in range(nreg)]

    def __getitem__(self, idx):
        return V(self.t[idx], self.regs)

    def reg(self, i, idx=None):
        ap = self.t[:] if idx is None else self.t[idx]
        return V(ap, [self.regs[i]])


class Eng:
    def __init__(self, k, name, e):
        self.name = name
        self.e = e
        self.sem = k.newsem("c_" + name)
        self.cnt = 0
        self.waited = {}
        self.dsems = []
        self.dvals = []
        self.dnext = 0


class KB:
    def __init__(self, same_engine_sync=True):
        self.nc = bass.Bass("TRN2", target_bir_lowering=False)
        self.es = contextlib.ExitStack()
        self.es.__enter__()
        self.lp = self.nc.allow_low_precision("bf16 matmul operands, fp32 accumulate")
        self.lp.__enter__()
        self.ncd = self.nc.allow_non_contiguous_dma("tiny per-partition parameter loads")
        self.ncd.__enter__()
        self.nsem = 0
        self.allsems = []
        self.same = same_engine_sync
        nc = self.nc
        self.pe = Eng(self, "pe", nc.tensor)
        self.act = Eng(self, "act", nc.scalar)
        self.dve = Eng(self, "dve", nc.vector)
        self.pool = Eng(self, "pool", nc.gpsimd)
        self.sp = Eng(self, "sp", nc.sync)
        self.engs = [self.pe, self.act, self.dve, self.pool, self.sp]
        for q in (self.sp, self.pool, self.act):
            for i in range(8):
                q.dsems.append(self.newsem("d_%s%d" % (q.name, i)))
                q.dvals.append(0)
        self.nid = 0

    def newsem(self, name):
        s = self.es.enter_context(self.nc.semaphore(name))
        self.allsems.append([s, 0])
        return s

    def dram(self, name, shape, dtype, kind):
        t = self.nc.dram_tensor(name, list(shape), dtype, kind=kind).ap()
        return Buf(t)

    def din(self, name, shape, dtype=F32):
        return self.dram(name, shape, dtype, "ExternalInput")

    def dout(self, name, shape, dtype=F32):
        return self.dram(name, shape, dtype, "ExternalOutput")

    def sb(self, shape, dtype=F32, nreg=1, name=None, stack=None):
        self.nid += 1
        name = name or ("sb%d" % self.nid)
        t = (stack or self.es).enter_context(self.nc.sbuf_tensor(name, list(shape), dtype))
        return Buf(t, nreg)

    def ps(self, shape, dtype=F32, nreg=1, name=None, stack=None):
        self.nid += 1
        name = name or ("ps%d" % self.nid)
        t = (stack or self.es).enter_context(self.nc.psum_tensor(name, list(shape), dtype))
        return Buf(t, nreg)

    @contextlib.contextmanager
    def scope(self):
        st = contextlib.ExitStack()
        with st:
            yield st
            self.barrier()

    def _semval(self, sem):
        for sv in self.allsems:
            if sv[0] is sem:
                return sv
        raise KeyError

    def _wait(self, eng, ev):
        sem, val = ev
        if eng.waited.get(id(sem), 0) >= val:
            return
        if sem is eng.sem and not self.same:
            return
        if sem is eng.sem and eng is self.pe:
            return
        eng.e.wait_ge(sem, val)
        eng.waited[id(sem)] = val

    def emit(self, eng, fn, reads, writes, dma=False, waw=True):
        evs = []
        for v in reads:
            for t in v.trks:
                evs.extend(t.w.values())
        for v in writes:
            for t in v.trks:
                if waw:
                    evs.extend(t.w.values())
                evs.extend(t.r.values())
        if dma:
            i = eng.dnext
            eng.dnext = (i + 1) % len(eng.dsems)
            sem = eng.dsems[i]
            if eng.dvals[i] > 0:
                evs.append((sem, eng.dvals[i]))
        for ev in evs:
            self._wait(eng, ev)
        ins = fn()
        if dma:
            eng.dvals[i] += 16
            ins.then_inc(sem, 16)
            ev = (sem, eng.dvals[i])
            self._semval(sem)[1] = eng.dvals[i]
        else:
            eng.cnt += 1
            ins.then_inc(eng.sem, 1)
            ev = (eng.sem, eng.cnt)
            self._semval(eng.sem)[1] = eng.cnt
        for v in reads:
            for t in v.trks:
                t.r[id(ev[0])] = ev
        for v in writes:
            for t in v.trks:
                if waw:
                    t.w = {}
                    t.r = {}
                t.w[id(ev[0])] = ev
        return ins

    def barrier(self):
        for eng in self.engs:
            for sem, val in self.allsems:
                if val > 0:
                    self._wait_force(eng, (sem, val))

    def _wait_force(self, eng, ev):
        sem, val = ev
        if eng.waited.get(id(sem), 0) >= val:
            return
        eng.e.wait_ge(sem, val)
        eng.waited[id(sem)] = val

    def finish(self):
        self.barrier()
        self.ncd.__exit__(None, None, None)
        self.lp.__exit__(None, None, None)
        self.es.__exit__(None, None, None)
        return self.nc

    def _E(self, eng):
        return {"pe": self.pe, "act": self.act, "dve": self.dve, "pool": self.pool, "sp": self.sp}[eng]

    def dma(self, out, in_, q="sp", waw=True):
        e = self._E(q)
        return self.emit(e, lambda: e.e.dma_start(out=out.ap, in_=in_.ap), [in_], [out], dma=True, waw=waw)

    def mm(self, out, lhsT, rhs, start=True, stop=True):
        return self.emit(self.pe, lambda: self.nc.tensor.matmul(out.ap, lhsT.ap, rhs.ap, start=start, stop=stop),
                         [lhsT, rhs], [out])

    def transpose(self, out, in_, ident):
        return self.emit(self.pe, lambda: self.nc.tensor.transpose(out.ap, in_.ap, ident.ap), [in_, ident], [out])

    def actf(self, out, in_, func, bias=None, scale=1.0, accum=None):
        reads = [in_]
        kw = {}
        if isinstance(bias, V):
            reads.append(bias)
            kw["bias"] = bias.ap
        elif bias is not None:
            kw["bias"] = float(bias)
        if isinstance(scale, V):
            reads.append(scale)
            kw["scale"] = scale.ap
        else:
            kw["scale"] = float(scale)
        writes = [out]
        if accum is not None:
            writes.append(accum)
            kw["accum_out"] = accum.ap
        return self.emit(self.act, lambda: self.nc.scalar.activation(out=out.ap, in_=in_.ap, func=func, **kw),
                         reads, writes)

    def _vec(self, eng):
        e = self._E(eng)
        return e, e.e

    def tt(self, out, in0, in1, op, eng="dve"):
        e, x = self._vec(eng)
        return self.emit(e, lambda: x.tensor_tensor(out=out.ap, in0=in0.ap, in1=in1.ap, op=op), [in0, in1], [out])

    def ts(self, out, in0, s1, op0, s2=None, op1=None, eng="dve", accum=None):
        e, x = self._vec(eng)
        reads = [in0]
        a1 = s1.ap if isinstance(s1, V) else float(s1)
        if isinstance(s1, V):
            reads.append(s1)
        a2 = None
        if s2 is not None:
            a2 = s2.ap if isinstance(s2, V) else float(s2)
            if isinstance(s2, V):
                reads.append(s2)
        kw = {}
        if op1 is not None:
            kw["op1"] = op1
        writes = [out]
        if accum is not None:
            kw["accum_out"] = accum.ap
            writes.append(accum)
        return self.emit(e, lambda: x.tensor_scalar(out=out.ap, in0=in0.ap, scalar1=a1, scalar2=a2, op0=op0, **kw),
                         reads, writes)

    def stt(self, out, in0, scalar, in1, op0, op1, eng="dve"):
        e, x = self._vec(eng)
        reads = [in0, in1]
        sc = scalar.ap if isinstance(scalar, V) else float(scalar)
        if isinstance(scalar, V):
            reads.append(scalar)
        return self.emit(e, lambda: x.scalar_tensor_tensor(out=out.ap, in0=in0.ap, scalar=sc, in1=in1.ap,
                                                           op0=op0, op1=op1), reads, [out])

    def copy(self, out, in_, eng="dve"):
        if eng == "act":
            return self.actf(out, in_, AF.Copy)
        e, x = self._vec(eng)
        return self.emit(e, lambda: x.tensor_copy(out=out.ap, in_=in_.ap), [in_], [out])

    def recip(self, out, in_):
        return self.emit(self.dve, lambda: self.nc.vector.reciprocal(out=out.ap, in_=in_.ap), [in_], [out])

    def reduce(self, out, in_, op, axis=AX.X, eng="dve"):
        e, x = self._vec(eng)
        return self.emit(e, lambda: x.tensor_reduce(out=out.ap, in_=in_.ap, axis=axis, op=op), [in_], [out])

    def memset(self, out, val, eng="dve"):
        e, x = self._vec(eng)
        return self.emit(e, lambda: x.memset(out.ap, val), [], [out])


def run(nc, in_maps):
    res = run_bass_kernel_spmd(nc, in_maps, core_ids=list(range(len(in_maps))))
    return res.results


def _v_rr(self, pattern, **kw):
    return V(self.ap.rearrange(pattern, **kw), self.trks)


V.rr = _v_rr


def _v_unsq(self, axis):
    return V(self.ap.unsqueeze(axis), self.trks)


V.unsq = _v_unsq


D = 2048
DIN = 5824
EPS = 1e-6
TILES = [(0, 512, 0), (512, 512, 0), (1024, 128, 1)]


def norm_mod(k, xT, hT, ones, gT, scT, shT, pss, tmp, tiles=TILES, stack=None):
    A = k.sb([128, 16, 2], stack=stack)
    for c in range(2):
        k.ts(A[:, :, c], scT[:, :, c], 1.0, ALU.add)
        k.tt(A[:, :, c], A[:, :, c], gT[:, :], ALU.mult)
    for (t0, n, col) in tiles:
        ps = pss[0]
        for kc in range(16):
            sq = tmp[kc % 2]
            k.actf(sq[:, 0:n], xT[:, kc, t0:t0 + n], AF.Square)
            k.mm(ps[:, 0:n], ones[:, :], sq[:, 0:n], start=(kc == 0), stop=(kc == 15))
        rs = tmp[2]
        k.ts(rs[:, 0:n], ps[:, 0:n], 1.0 / D, ALU.mult, EPS, ALU.add)
        k.actf(rs[:, 0:n], rs[:, 0:n], AF.Sqrt)
        k.recip(rs[:, 0:n], rs[:, 0:n])
        for kc in range(16):
            t = tmp[kc % 2]
            k.tt(t[:, 0:n], xT[:, kc, t0:t0 + n], rs[:, 0:n], ALU.mult)
            k.ts(hT[:, kc, t0:t0 + n], t[:, 0:n], A[:, kc, col:col + 1], ALU.mult, shT[:, kc, col:col + 1], ALU.add,
                 eng="pool" if kc % 2 else "dve")


def build_k1(NT=1152):
    k = KB()
    xTd = k.din("xT", [128, 16, NT])
    gTd = k.din("gT", [128, 16]); scTd = k.din("scT", [128, 16, 2]); shTd = k.din("shT", [128, 16, 2])
    w = k.din("w", [D, DIN])
    p = k.dout("p", [NT, DIN])
    xT = k.sb([128, 16, NT]); hT = k.sb([128, 16, NT], BF16)
    gT = k.sb([128, 16]); scT = k.sb([128, 16, 2]); shT = k.sb([128, 16, 2])
    ones = k.sb([128, 128]); k.memset(ones[:], 1.0)
    tmp = [k.sb([128, 512]) for _ in range(3)]
    pss = [k.ps([128, 512]) for _ in range(4)]
    for q in range(4):
        k.dma(xT[:, q * 4:(q + 1) * 4, :], xTd[:, q * 4:(q + 1) * 4, :])
    k.dma(gT[:], gTd[:]); k.dma(scT[:], scTd[:]); k.dma(shT[:], shTd[:])
    wsb = [k.sb([128, 16, 512], BF16) for _ in range(2)]
    wv = w[:].rr("(kc p) n -> p kc n", p=128)
    groups = [(c0, min(512, DIN - c0)) for c0 in range(0, DIN, 512)]

    def loadw(gi):
        c0, cn = groups[gi]
        for q in range(4):
            k.dma(wsb[gi % 2][:, q * 4:(q + 1) * 4, 0:cn], wv[:, q * 4:(q + 1) * 4, c0:c0 + cn], q="pool")
    loadw(0)
    norm_mod(k, xT, hT, ones, gT, scT, shT, pss, tmp)
    stg = [k.sb([128, 512]) for _ in range(3)]
    it = 0
    for gi, (c0, cn) in enumerate(groups):
        if gi + 1 < len(groups):
            loadw(gi + 1)
        for tb in range(NT // 128):
            ps = pss[it % 4]; st = stg[it % 3]
            for kc in range(16):
                k.mm(ps[:, 0:cn], hT[:, kc, tb * 128:(tb + 1) * 128], wsb[gi % 2][:, kc, 0:cn],
                     start=(kc == 0), stop=(kc == 15))
            if it % 2:
                k.copy(st[:, 0:cn], ps[:, 0:cn], eng="dve")
            else:
                k.copy(st[:, 0:cn], ps[:, 0:cn], eng="act")
            k.dma(p[tb * 128:(tb + 1) * 128, c0:c0 + cn], st[:, 0:cn], q="sp")
            it += 1
    return k.finish()


def to_fm(a):
    return np.ascontiguousarray(a.T.reshape(16, 128, a.shape[0]).transpose(1, 0, 2))


def vec_fm(v):
    return np.ascontiguousarray(v.reshape(16, 128).T)


def core_tokens(x, xc, i):
    b, s = i // 2, i % 2
    return np.concatenate([x[b, s * 1024:(s + 1) * 1024], xc[b, s * 128:(s + 1) * 128]], 0)


def mod_cols(mod_l, which, b):
    sl = mod_l[:, which * D:(which + 1) * D]
    return np.ascontiguousarray(np.stack([vec_fm(sl[b]), vec_fm(sl[4])], -1))


def run_k1(xT_list, mod_l, norm1_l, w_in_l, nc=None):
    maps = []
    for i in range(NCORES):
        b = i // 2
        maps.append({"xT": xT_list[i], "gT": vec_fm(norm1_l), "scT": mod_cols(mod_l, 1, b),
                     "shT": mod_cols(mod_l, 0, b), "w": w_in_l})
    res = run(nc or build_k1(), maps)
    return [r["p"] for r in res]


D = 2048
EPS = 1e-6


def ffn(k, h2T, tiles, w1d, w3d, w2d, F, acc_fn, pss, FG=256, stack=None, gate=None):
    NT = sum(n for _, n, _ in tiles)
    nfc = FG // 128
    w1s = [k.sb([128, 16, FG], BF16, stack=stack) for _ in range(2)]
    w3s = [k.sb([128, 16, FG], BF16, stack=stack) for _ in range(2)]
    w2s = [k.sb([128, nfc, D], BF16, stack=stack) for _ in range(2)]
    aT = [k.sb([128, nfc, NT], BF16, stack=stack) for _ in range(2)]
    tmp = [k.sb([128, 512], stack=stack) for _ in range(2)]
    w1v = w1d.rr("(kc p) f -> p kc f", p=128)
    w3v = w3d.rr("(kc p) f -> p kc f", p=128)
    w2v = w2d.rr("(fc p) d -> p fc d", p=128)
    ng = F // FG

    def loadA(g):
        f0 = g * FG
        for q in range(2):
            k.dma(w1s[g % 2][:, q * 8:(q + 1) * 8, :], w1v[:, q * 8:(q + 1) * 8, f0:f0 + FG], q="pool")
            k.dma(w3s[g % 2][:, q * 8:(q + 1) * 8, :], w3v[:, q * 8:(q + 1) * 8, f0:f0 + FG], q="pool")

    def loadB(g):
        k.dma(w2s[g % 2][:, :, :], w2v[:, g * nfc:(g + 1) * nfc, :], q="pool")
    cnt = [0, 0]

    def up(g, fc, t0, n):
        it = cnt[0]; cnt[0] += 1
        p1 = pss[(it % 2) * 2]; p3 = pss[(it % 2) * 2 + 1]; tm = tmp[it % 2]
        for kc in range(16):
            k.mm(p1[:, 0:n], w1s[g % 2][:, kc, fc * 128:(fc + 1) * 128], h2T[:, kc, t0:t0 + n], start=(kc == 0), stop=(kc == 15))
        for kc in range(16):
            k.mm(p3[:, 0:n], w3s[g % 2][:, kc, fc * 128:(fc + 1) * 128], h2T[:, kc, t0:t0 + n], start=(kc == 0), stop=(kc == 15))
        k.actf(tm[:, 0:n], p1[:, 0:n], AF.Silu)
        if gate is not None:
            k.tt(tm[:, 0:n], tm[:, 0:n], gate[:, t0:t0 + n], ALU.mult, eng="pool")
        k.tt(aT[g % 2][:, fc, t0:t0 + n], tm[:, 0:n], p3[:, 0:n], ALU.mult)

    def down(g, dc, t0, n):
        jt = cnt[1]; cnt[1] += 1
        po = pss[4 + jt % 4]
        for fc in range(nfc):
            k.mm(po[:, 0:n], w2s[g % 2][:, fc, dc * 128:(dc + 1) * 128], aT[g % 2][:, fc, t0:t0 + n],
                 start=(fc == 0), stop=(fc == nfc - 1))
        acc_fn(po[:, 0:n], dc, t0, n)
    ups = [(fc, t0, n) for fc in range(nfc) for (t0, n, _) in tiles]
    downs = [(dc, t0, n) for dc in range(16) for (t0, n, _) in tiles]
    loadA(0); loadB(0)
    if ng > 1:
        loadA(1)
    for u in ups:
        up(0, *u)
    per = -(-len(downs) // len(ups))
    for g in range(ng):
        if g + 2 < ng:
            loadA(g + 2)
        if g + 1 < ng:
            loadB(g + 1)
        di = 0
        for u in (ups if g + 1 < ng else []):
            up(g + 1, *u)
            for dd in downs[di:di + per]:
                down(g, *dd)
            di += per
        for dd in downs[di:]:
            down(g, *dd)


def ffn_simple(k, h2T, tiles, w1d, w3d, w2d, F, acc_fn, pss, FG=512, stack=None, gate=None):
    NT = sum(n for _, n, _ in tiles)
    nfc = FG // 128
    w1s = [k.sb([128, 16, FG], BF16, stack=stack) for _ in range(2)]
    w3s = [k.sb([128, 16, FG], BF16, stack=stack) for _ in range(2)]
    w2s = k.sb([128, nfc, D], BF16, stack=stack)
    a = k.sb([128, nfc, NT], BF16, stack=stack)
    tmp = [k.sb([128, 512], stack=stack) for _ in range(2)]
    w1v = w1d.rr("(kc p) f -> p kc f", p=128)
    w3v = w3d.rr("(kc p) f -> p kc f", p=128)
    w2v = w2d.rr("(fc p) d -> p fc d", p=128)
    ng = F // FG

    def loadA(g):
        f0 = g * FG
        for q in range(2):
            k.dma(w1s[g % 2][:, q * 8:(q + 1) * 8, :], w1v[:, q * 8:(q + 1) * 8, f0:f0 + FG], q="pool")
            k.dma(w3s[g % 2][:, q * 8:(q + 1) * 8, :], w3v[:, q * 8:(q + 1) * 8, f0:f0 + FG], q="pool")

    def loadB(g):
        for q in range(nfc // 2):
            k.dma(w2s[:, q * 2:(q + 1) * 2, :], w2v[:, g * nfc + q * 2:g * nfc + (q + 1) * 2, :], q="pool")
    loadA(0); loadB(0)
    it = 0
    jt = 0
    for g in range(ng):
        if g + 1 < ng:
            loadA(g + 1)
        for (t0, n, _) in tiles:
            for fc in range(nfc):
                p1 = pss[(it % 2) * 2]; p3 = pss[(it % 2) * 2 + 1]; tm = tmp[it % 2]
                for kc in range(16):
                    k.mm(p1[:, 0:n], w1s[g % 2][:, kc, fc * 128:(fc + 1) * 128], h2T[:, kc, t0:t0 + n],
                         start=(kc == 0), stop=(kc == 15))
                for kc in range(16):
                    k.mm(p3[:, 0:n], w3s[g % 2][:, kc, fc * 128:(fc + 1) * 128], h2T[:, kc, t0:t0 + n],
                         start=(kc == 0), stop=(kc == 15))
                k.actf(tm[:, 0:n], p1[:, 0:n], AF.Silu)
                if gate is not None:
                    k.tt(tm[:, 0:n], tm[:, 0:n], gate[:, t0:t0 + n], ALU.mult, eng="pool")
                k.tt(a[:, fc, t0:t0 + n], tm[:, 0:n], p3[:, 0:n], ALU.mult)
                it += 1
        for (t0, n, _) in tiles:
            for dc in range(16):
                po = pss[4 + jt % 4]
                for fc in range(nfc):
                    k.mm(po[:, 0:n], w2s[:, fc, dc * 128:(dc + 1) * 128], a[:, fc, t0:t0 + n],
                         start=(fc == 0), stop=(fc == nfc - 1))
                acc_fn(po[:, 0:n], dc, t0, n)
                jt += 1
        if g + 1 < ng:
            loadB(g + 1)


def build_k3(NT=1152, F=5632, tiles=TILES):
    k = KB()
    mixd = k.din("mixT", [128, 16, NT])
    xTd = k.din("xT", [128, 16, NT])
    wo = k.din("w_out", [D, D])
    g1d = k.din("g1T", [128, 16, 2]); g2d = k.din("g2T", [128, 16, 2])
    n2d = k.din("n2T", [128, 16]); sc2d = k.din("sc2T", [128, 16, 2]); sh2d = k.din("sh2T", [128, 16, 2])
    gsd = k.din("gsT", [128, 4])
    w1d = k.din("w1", [D, F]); w3d = k.din("w3", [D, F]); w2d = k.din("w2", [F, D])
    xo = k.dout("xo", [128, 16, NT])
    xT = k.sb([128, 16, NT])
    g1 = k.sb([128, 16, 2]); g2 = k.sb([128, 16, 2]); n2 = k.sb([128, 16]); sc2 = k.sb([128, 16, 2])
    sh2 = k.sb([128, 16, 2]); gs = k.sb([128, 4])
    ones = k.sb([128, 128]); k.memset(ones[:], 1.0)
    pss = [k.ps([128, 512]) for _ in range(8)]
    for q in range(4):
        k.dma(xT[:, q * 4:(q + 1) * 4, :], xTd[:, q * 4:(q + 1) * 4, :])
    for a, b in ((g1, g1d), (g2, g2d), (n2, n2d), (sc2, sc2d), (sh2, sh2d), (gs, gsd)):
        k.dma(a[:], b[:])
    with k.scope() as st:
        mixb = k.sb([128, 16, NT], BF16, stack=st)
        ssd = k.sb([128, 4, NT], stack=st)
        tmp = [k.sb([128, 512], stack=st) for _ in range(3)]
        wos = [k.sb([128, 16, 512], BF16, stack=st) for _ in range(2)]
        wov = wo[:].rr("(kc p) n -> p kc n", p=128)

        def loadwo(g):
            for q in range(2):
                k.dma(wos[g % 2][:, q * 8:(q + 1) * 8, :], wov[:, q * 8:(q + 1) * 8, g * 512:(g + 1) * 512], q="pool")
        k.dma(mixb[:, 0:4, :], mixd[:, 0:4, :], q="pool")
        k.dma(mixb[:, 8:12, :], mixd[:, 8:12, :], q="pool")
        k.dma(mixb[:, 12:16, :], mixd[:, 12:16, :], q="pool")
        k.dma(ssd[:], mixd[:, 4:8, :])
        loadwo(0)
        for (t0, n, _) in tiles:
            ps = pss[0]
            for c in range(4):
                sq = tmp[c % 2]
                k.actf(sq[:, 0:n], ssd[:, c, t0:t0 + n], AF.Square)
                k.mm(ps[:, 0:n], ones[:, :], sq[:, 0:n], start=(c == 0), stop=(c == 3))
            rs = tmp[2]
            k.ts(rs[:, 0:n], ps[:, 0:n], 1.0 / 512, ALU.mult, EPS, ALU.add)
            k.actf(rs[:, 0:n], rs[:, 0:n], AF.Sqrt)
            k.recip(rs[:, 0:n], rs[:, 0:n])
            for c in range(4):
                k.stt(mixb[:, 4 + c, t0:t0 + n], ssd[:, c, t0:t0 + n], gs[:, c:c + 1], rs[:, 0:n], ALU.mult, ALU.mult)
        jt = 0
        for g in range(4):
            if g + 1 < 4:
                loadwo(g + 1)
            for dl in range(4):
                dc = g * 4 + dl
                for (t0, n, col) in tiles:
                    po = pss[4 + jt % 4]
                    for kc in range(16):
                        k.mm(po[:, 0:n], wos[g % 2][:, kc, dl * 128:(dl + 1) * 128], mixb[:, kc, t0:t0 + n],
                             start=(kc == 0), stop=(kc == 15))
                    k.stt(xT[:, dc, t0:t0 + n], po[:, 0:n], g1[:, dc, col:col + 1], xT[:, dc, t0:t0 + n],
                          ALU.mult, ALU.add)
                    jt += 1
    h2T = k.sb([128, 16, NT], BF16)
    with k.scope() as st:
        tmp = [k.sb([128, 512], stack=st) for _ in range(3)]
        norm_mod(k, xT, h2T, ones, n2, sc2, sh2, pss, tmp, tiles, stack=st)
    colof = {t0: col for (t0, n, col) in tiles}

    def acc(po, dc, t0, n):
        col = colof[t0]
        k.stt(xT[:, dc, t0:t0 + n], po, g2[:, dc, col:col + 1], xT[:, dc, t0:t0 + n], ALU.mult, ALU.add)
    ffn(k, h2T, tiles, w1d[:], w3d[:], w2d[:], F, acc, pss)
    for q in range(4):
        k.dma(xo[:, q * 4:(q + 1) * 4, :], xT[:, q * 4:(q + 1) * 4, :])
    return k.finish()


def run_k3(mixT_list, xT_list, mod_l, l, z, nc=None):
    maps = []
    for i in range(NCORES):
        b = i // 2
        maps.append({"mixT": mixT_list[i], "xT": xT_list[i], "w_out": np.ascontiguousarray(z['w_out'][l]),
                     "g1T": mod_cols(mod_l, 2, b), "g2T": mod_cols(mod_l, 5, b), "n2T": vec_fm(z['norm2'][l]),
                     "sc2T": mod_cols(mod_l, 4, b), "sh2T": mod_cols(mod_l, 3, b),
                     "gsT": np.ascontiguousarray(z['ssd_norm'][l].reshape(4, 128).T),
                     "w1": np.ascontiguousarray(z['ffn_w1'][0]), "w3": np.ascontiguousarray(z['ffn_w3'][0]),
                     "w2": np.ascontiguousarray(z['ffn_w2'][0])})
    res = run(nc or build_k3(), maps)
    return [r["xo"] for r in res]


S = 2304
TT = [(0, 512), (512, 512), (1024, 512), (1536, 512), (2048, 256)]
EPS = 1e-6


def headnorm_fm(k, srcT, dstT, h, gcol, ones, ps, tmp, extra_scale=1.0, nfeat=128):
    for (t0, n) in TT:
        sq = tmp[0]
        k.actf(sq[:, 0:n], srcT[:, h, t0:t0 + n], AF.Square)
        k.mm(ps[:, 0:n], ones[:, :], sq[:, 0:n])
        rs = tmp[1]
        k.ts(rs[:, 0:n], ps[:, 0:n], 1.0 / nfeat, ALU.mult, EPS, ALU.add)
        k.actf(rs[:, 0:n], rs[:, 0:n], AF.Sqrt)
        k.recip(rs[:, 0:n], rs[:, 0:n])
        k.tt(rs[:, 0:n], srcT[:, h, t0:t0 + n], rs[:, 0:n], ALU.mult)
        k.ts(dstT[:, h, t0:t0 + n], rs[:, 0:n], gcol, ALU.mult, extra_scale, ALU.mult)


def build_na():
    k = KB()
    qd = k.din("qT", [128, 2, S]); kd = k.din("kT", [128, 2, S])
    vAd = k.din("vA", [128, 18, 256]); vBd = k.din("vB", [128, 15, 256])
    gqd = k.din("gq", [128, 1]); gkd = k.din("gk", [128, 1])
    btd = k.din("bt", [128, 2, 14, 64])
    od = k.dout("oT", [128, 2, S])
    qf = k.sb([128, 2, S]); kf = k.sb([128, 2, S])
    qb = k.sb([128, 2, S], BF16); kb = k.sb([128, 2, S], BF16)
    vA = k.sb([128, 18, 256], BF16); vB = k.sb([128, 15, 256], BF16)
    gq = k.sb([128, 1]); gk = k.sb([128, 1]); bt = k.sb([128, 2, 14, 64])
    oT = k.sb([128, 2, S])
    ones = k.sb([128, 128]); k.memset(ones[:], 1.0)
    onesb = k.sb([128, 128], BF16); k.memset(onesb[:], 1.0)
    tmp = [k.sb([128, 512]) for _ in range(3)]
    pss = [k.ps([128, 512]) for _ in range(8)]
    k.dma(qf[:], qd[:]); k.dma(kf[:], kd[:])
    k.dma(vA[:], vAd[:], q="pool"); k.dma(vB[:], vBd[:], q="pool")
    k.dma(gq[:], gqd[:]); k.dma(gk[:], gkd[:]); k.dma(bt[:], btd[:])
    scale = 128 ** -0.5
    for h in range(2):
        headnorm_fm(k, qf, qb, h, gq[:, 0:1], ones, pss[0], tmp, extra_scale=scale)
        headnorm_fm(k, kf, kb, h, gk[:, 0:1], ones, pss[1], tmp)
    pT = [k.sb([128, 384], BF16) for _ in range(3)]
    it = 0
    for h in range(2):
        for rg in range(4):
            po = pss[4 + (it % 2) * 2]; pd = pss[5 + (it % 2) * 2]
            it += 1
            for rl in range(8):
                r = rg * 8 + rl
                r0 = min(max(r - 4, 0), 24)
                q0 = 256 + r * 64
                ps_s = pss[r % 4]
                vblks = []
                for j in range(4):
                    kt0 = 256 + r0 * 64 + 128 * j
                    k.mm(ps_s[:, j * 64:(j + 1) * 64], kb[:, h, kt0:kt0 + 128], qb[:, h, q0:q0 + 64])
                    if r0 % 2 == 0:
                        vblks.append(vA[:, 2 + r0 // 2 + j, h * 128:(h + 1) * 128])
                    else:
                        vblks.append(vB[:, (r0 - 1) // 2 + j, h * 128:(h + 1) * 128])
                for j in range(2):
                    k.mm(ps_s[:, 256 + j * 64:256 + (j + 1) * 64], kb[:, h, j * 128:(j + 1) * 128], qb[:, h, q0:q0 + 64])
                    vblks.append(vA[:, j, h * 128:(h + 1) * 128])
                m0 = r0 - r + 7
                tm = tmp[r % 2]
                k.tt(tm[:, 0:256].rr("p (j c) -> p j c", j=4), ps_s[:, 0:256].rr("p (j c) -> p j c", j=4),
                     bt[:, h, m0:m0 + 7:2, :], ALU.add)
                p = pT[r % 3]
                k.actf(p[:, 0:256], tm[:, 0:256], AF.Exp)
                k.actf(p[:, 256:384], ps_s[:, 256:384], AF.Exp)
                for j in range(6):
                    k.mm(po[:, rl * 64:(rl + 1) * 64], vblks[j], p[:, j * 64:(j + 1) * 64], start=(j == 0), stop=(j == 5))
                for j in range(6):
                    k.mm(pd[:, rl * 64:(rl + 1) * 64], onesb[:, :], p[:, j * 64:(j + 1) * 64], start=(j == 0), stop=(j == 5))
            rd = tmp[2]
            k.recip(rd[:, :], pd[:, :])
            k.tt(oT[:, h, 256 + rg * 512:256 + (rg + 1) * 512], po[:, :], rd[:, :], ALU.mult)
        ps_s = pss[0]; po = pss[4]; pd = pss[5]
        p = k.sb([128, 512], BF16)
        for j in range(2):
            k.mm(ps_s[:, j * 256:(j + 1) * 256], kb[:, h, j * 128:(j + 1) * 128], qb[:, h, 0:256])
        k.actf(p[:, :], ps_s[:, :], AF.Exp)
        for j in range(2):
            k.mm(po[:, 0:256], vA[:, j, h * 128:(h + 1) * 128], p[:, j * 256:(j + 1) * 256], start=(j == 0), stop=(j == 1))
        for j in range(2):
            k.mm(pd[:, 0:256], onesb[:, :], p[:, j * 256:(j + 1) * 256], start=(j == 0), stop=(j == 1))
        rd = tmp[2]
        k.recip(rd[:, 0:256], pd[:, 0:256])
        k.tt(oT[:, h, 0:256], po[:, 0:256], rd[:, 0:256], ALU.mult)
    k.dma(od[:], oT[:])
    return k.finish()


def na_bias_tiles(rpb_l, heads):
    bt = np.full((2, 64, len(heads), 14, 64), -30000.0, np.float32)
    qc = np.arange(64)
    c0 = np.clip(qc - 8, 0, 48)
    for kc in range(64):
        ok = (kc >= c0) & (kc < c0 + 16)
        dc = kc - qc + 15
        for a in range(2):
            for m in range(14):
                for hi, h in enumerate(heads):
                    bt[a, kc, hi, m, ok] = rpb_l[h, m + a, dc[ok]]
    return np.ascontiguousarray(bt.reshape(128, len(heads), 14, 64))


def fm_heads(a, nh=2, hd=128):
    return np.ascontiguousarray(a.reshape(a.shape[0], nh, hd).transpose(2, 1, 0))


def na_inputs(P_b, s, z, l):
    hs = [2 * s, 2 * s + 1]
    c = 4288
    q = P_b[:, c + s * 256: c + (s + 1) * 256]
    kk = P_b[:, c + 512 + s * 256: c + 512 + (s + 1) * 256]
    v = P_b[:, c + 1024 + s * 256: c + 1024 + (s + 1) * 256]
    vA = np.ascontiguousarray(v.reshape(18, 128, 256).transpose(1, 0, 2))
    vB = np.ascontiguousarray(v[256 + 64:256 + 64 + 15 * 128].reshape(15, 128, 256).transpose(1, 0, 2))
    return {"qT": fm_heads(q), "kT": fm_heads(kk), "vA": vA, "vB": vB,
            "gq": np.ascontiguousarray(z['na_gq'][l].reshape(128, 1)), "gk": np.ascontiguousarray(z['na_gk'][l].reshape(128, 1)),
            "bt": na_bias_tiles(z['na_rpb'][l], hs)}


def seq_P(r, l, b):
    return np.concatenate([r[f'p_c{l}'][b], r[f'p_l{l}'][b]], 0)


EPS = 1e-6


def rope_tables():
    t = np.arange(2048)
    row = (t // 64).astype(np.float32); col = (t % 64).astype(np.float32)
    freqs = (10000.0 ** (-np.arange(16, dtype=np.float32) / 16)).astype(np.float32)
    ang = np.concatenate([row[:, None] * freqs, col[:, None] * freqs], -1)
    cos = np.cos(ang).T; sin = np.sin(ang).T
    cosF = np.concatenate([cos, cos], 0).astype(np.float32)
    sinF = np.concatenate([sin, sin], 0).astype(np.float32)
    R = np.zeros((64, 64), np.float32)
    for i in range(32):
        R[i, 32 + i] = -1.0
        R[32 + i, i] = 1.0
    return np.ascontiguousarray(cosF), np.ascontiguousarray(sinF), np.ascontiguousarray(R.T)


def build_mla():
    k = KB()
    cqd = k.din("cqT", [128, 4, S]); ckvd = k.din("ckvT", [128, 2, S]); krd = k.din("krT", [64, S])
    wqd = k.din("wq", [128, 4, 384]); wkvd = k.din("wkv", [128, 2, 512])
    qnd = k.din("qn", [128, 4]); kvnd = k.din("kvn", [128, 2])
    gqd = k.din("gq", [128, 2]); gkd = k.din("gk", [128, 2])
    cosd = k.din("cosF", [64, 2048]); sind = k.din("sinF", [64, 2048]); rtd = k.din("RT", [64, 64])
    od = k.dout("oT", [128, 2, S])
    ones = k.sb([128, 128]); k.memset(ones[:], 1.0)
    onesb = k.sb([128, 128], BF16); k.memset(onesb[:], 1.0)
    wq = k.sb([128, 4, 384], BF16); wkv = k.sb([128, 2, 512], BF16)
    qn = k.sb([128, 4]); kvn = k.sb([128, 2]); gq = k.sb([128, 2]); gk = k.sb([128, 2])
    cosF = k.sb([64, 2048]); sinF = k.sb([64, 2048]); RT = k.sb([64, 64])
    krf = k.sb([64, S])
    cqn = k.sb([128, 4, S], BF16); ckvn = k.sb([128, 2, S], BF16)
    oT = k.sb([128, 2, S])
    pss = [k.ps([128, 512]) for _ in range(8)]
    tmp = [k.sb([128, 512]) for _ in range(4)]
    k.dma(wq[:], wqd[:], q="pool"); k.dma(wkv[:], wkvd[:], q="pool")
    for a, b in ((qn, qnd), (kvn, kvnd), (gq, gqd), (gk, gkd), (cosF, cosd), (sinF, sind), (RT, rtd), (krf, krd)):
        k.dma(a[:], b[:])
    with k.scope() as st:
        cq = k.sb([128, 4, S], stack=st); ckv = k.sb([128, 2, S], stack=st)
        k.dma(cq[:], cqd[:]); k.dma(ckv[:], ckvd[:])
        for (src, dst, nch, nfeat, gn) in ((cq, cqn, 4, 448, qn), (ckv, ckvn, 2, 160, kvn)):
            for (t0, n) in TT:
                ps = pss[0]
                for c in range(nch):
                    sq = tmp[c % 2]
                    k.actf(sq[:, 0:n], src[:, c, t0:t0 + n], AF.Square)
                    k.mm(ps[:, 0:n], ones[:, :], sq[:, 0:n], start=(c == 0), stop=(c == nch - 1))
                rs = tmp[2]
                k.ts(rs[:, 0:n], ps[:, 0:n], 1.0 / nfeat, ALU.mult, EPS, ALU.add)
                k.actf(rs[:, 0:n], rs[:, 0:n], AF.Sqrt)
                k.recip(rs[:, 0:n], rs[:, 0:n])
                for c in range(nch):
                    k.stt(dst[:, c, t0:t0 + n], src[:, c, t0:t0 + n], gn[:, c:c + 1], rs[:, 0:n], ALU.mult, ALU.mult)
    scale = 192 ** -0.5
    qA = k.sb([128, S], BF16); qB = k.sb([64, S], BF16)
    kA = k.sb([128, S], BF16); kB = k.sb([64, S], BF16)
    vT = k.sb([128, 18, 128], BF16)
    pT = [k.sb([128, 512], BF16) for _ in range(3)]
    for h in range(2):
        for which in range(2):
            dA, dB = (qA, qB) if which == 0 else (kA, kB)
            g = gq if which == 0 else gk
            sc = scale if which == 0 else 1.0
            for (t0, n) in TT:
                pa = pss[0]; pb = pss[1]; pn = pss[2]; pr = pss[3]
                if which == 0:
                    for c in range(4):
                        k.mm(pa[:, 0:n], wq[:, c, h * 192:h * 192 + 128], cqn[:, c, t0:t0 + n], start=(c == 0), stop=(c == 3))
                    for c in range(4):
                        k.mm(pb[0:64, 0:n], wq[:, c, h * 192 + 128:h * 192 + 192], cqn[:, c, t0:t0 + n], start=(c == 0), stop=(c == 3))
                    srcB = pb[0:64, 0:n]
                else:
                    for c in range(2):
                        k.mm(pa[:, 0:n], wkv[:, c, h * 256:h * 256 + 128], ckvn[:, c, t0:t0 + n], start=(c == 0), stop=(c == 1))
                    srcB = krf[:, t0:t0 + n]
                sa = tmp[0]; sbq = tmp[1]
                k.actf(sa[:, 0:n], pa[:, 0:n], AF.Square)
                k.actf(sbq[0:64, 0:n], srcB, AF.Square)
                k.mm(pn[:, 0:n], ones[:, :], sa[:, 0:n], start=True, stop=False)
                k.mm(pn[:, 0:n], ones[0:64, :], sbq[0:64, 0:n], start=False, stop=True)
                rs = tmp[2]
                k.ts(rs[:, 0:n], pn[:, 0:n], 1.0 / 192, ALU.mult, EPS, ALU.add)
                k.actf(rs[:, 0:n], rs[:, 0:n], AF.Sqrt)
                k.recip(rs[:, 0:n], rs[:, 0:n])
                k.tt(sa[:, 0:n], pa[:, 0:n], rs[:, 0:n], ALU.mult)
                k.ts(dA[:, t0:t0 + n], sa[:, 0:n], g[:, 0:1], ALU.mult, sc, ALU.mult)
                ub = tmp[3]
                k.tt(sbq[0:64, 0:n], srcB, rs[0:64, 0:n], ALU.mult)
                k.ts(ub[0:64, 0:n], sbq[0:64, 0:n], g[0:64, 1:2], ALU.mult, sc, ALU.mult)
                l0 = max(t0, 256)
                if l0 > t0:
                    k.copy(dB[:, t0:l0], ub[0:64, 0:l0 - t0])
                nl = t0 + n - l0
                o = l0 - t0
                k.mm(pr[0:64, 0:nl], RT[:, :], ub[0:64, o:o + nl])
                k.tt(sbq[0:64, 0:nl], pr[0:64, 0:nl], sinF[:, l0 - 256:l0 - 256 + nl], ALU.mult)
                k.tt(ub[0:64, o:o + nl], ub[0:64, o:o + nl], cosF[:, l0 - 256:l0 - 256 + nl], ALU.mult)
                k.tt(dB[:, l0:l0 + nl], ub[0:64, o:o + nl], sbq[0:64, 0:nl], ALU.add)
        for blk in range(18):
            pv = pss[blk % 2]
            for c in range(2):
                k.mm(pv[:, 0:128], ckvn[:, c, blk * 128:(blk + 1) * 128], wkv[:, c, h * 256 + 128:h * 256 + 256],
                     start=(c == 0), stop=(c == 1))
            k.copy(vT[:, blk, :], pv[:, 0:128])
        jobs = [(256 + qt * 512, 512, list(range(18))) for qt in range(4)] + [(0, 256, [0, 1])]
        for ji, (q0, nq, blks) in enumerate(jobs):
            po = pss[4 + (ji % 2) * 2]; pd = pss[5 + (ji % 2) * 2]
            for bi, blk in enumerate(blks):
                ps_s = pss[bi % 4]
                k.mm(ps_s[:, 0:nq], kA[:, blk * 128:(blk + 1) * 128], qA[:, q0:q0 + nq], start=True, stop=False)
                k.mm(ps_s[:, 0:nq], kB[:, blk * 128:(blk + 1) * 128], qB[:, q0:q0 + nq], start=False, stop=True)
                p = pT[bi % 3]
                k.actf(p[:, 0:nq], ps_s[:, 0:nq], AF.Exp)
                k.mm(po[:, 0:nq], vT[:, blk, :], p[:, 0:nq], start=(bi == 0), stop=(bi == len(blks) - 1))
                k.mm(pd[:, 0:nq], onesb[:, :], p[:, 0:nq], start=(bi == 0), stop=(bi == len(blks) - 1))
            rd = tmp[2]
            k.recip(rd[:, 0:nq], pd[:, 0:nq])
            k.tt(oT[:, h, q0:q0 + nq], po[:, 0:nq], rd[:, 0:nq], ALU.mult)
    k.dma(od[:], oT[:])
    return k.finish()


def fm_pad(a, nch):
    o = np.zeros((nch * 128, a.shape[0]), np.float32)
    o[:a.shape[1]] = a.T
    return np.ascontiguousarray(o.reshape(nch, 128, a.shape[0]).transpose(1, 0, 2))


def vec_pad(v, nch):
    o = np.zeros(nch * 128, np.float32); o[:v.shape[0]] = v
    return np.ascontiguousarray(o.reshape(nch, 128).T)


def w_pad(w, nch):
    o = np.zeros((nch * 128, w.shape[1]), np.float32); o[:w.shape[0]] = w
    return np.ascontiguousarray(o.reshape(nch, 128, w.shape[1]).transpose(1, 0, 2))


ROPE = rope_tables()


def mla_inputs(P_b, s, z, l):
    c = 3616
    cq = P_b[:, c:c + 448]; ckv = P_b[:, c + 448:c + 608]; kr = P_b[:, c + 608:c + 672]
    wq = z['mla_w_qb'][l][:, s * 384:(s + 1) * 384]
    wkv = z['mla_w_kvb'][l][:, s * 512:(s + 1) * 512]
    return {"cqT": fm_pad(cq, 4), "ckvT": fm_pad(ckv, 2), "krT": np.ascontiguousarray(kr.T),
            "wq": w_pad(wq, 4), "wkv": w_pad(wkv, 2),
            "qn": vec_pad(z['mla_q_norm'][l], 4), "kvn": vec_pad(z['mla_kv_norm'][l], 2),
            "gq": vec_pad(z['mla_gq'][l], 2), "gk": vec_pad(z['mla_gk'][l], 2),
            "cosF": ROPE[0], "sinF": ROPE[1], "RT": ROPE[2]}


EPS = 1e-6
NB = 18
FWD_ORDER = list(range(18))
BWD_ORDER = [1, 0] + list(range(17, 1, -1))


def tri_consts():
    r = np.arange(128)
    U = (r[:, None] <= r[None, :]).astype(np.float32)
    L = np.ascontiguousarray(U.T)
    return np.ascontiguousarray(np.stack([U, U, L, L], 1))


def build_ml():
    k = KB()
    qTd = k.din("qT", [128, 2, S]); kTd = k.din("kT", [128, 2, S])
    kMd = k.din("kTM", [128, NB, 256]); vMd = k.din("vTM", [128, NB, 256]); oMd = k.din("oTM", [128, NB, 256])
    gid = k.din("gi", [128, NB, 4]); gfd = k.din("gf", [128, NB, 4])
    bid = k.din("bi", [128, 4]); bfd = k.din("bf", [128, 4])
    gnd = k.din("gn", [128, 256])
    uld = k.din("UL4", [128, 4, 128])
    od = k.dout("oTM_out", [128, NB, 256])
    ones = k.sb([128, 128]); k.memset(ones[:], 1.0)
    UL4 = k.sb([128, 4, 128]); k.dma(UL4[:], uld[:])
    qTb = k.sb([128, 2, S], BF16); kTb = k.sb([128, 2, S], BF16)
    kTM = k.sb([128, NB, 256]); vext = k.sb([128, NB, 2, 129], BF16)
    osg = k.sb([128, NB, 256])
    gi = k.sb([128, NB, 4]); gf = k.sb([128, NB, 4]); bi = k.sb([128, 4]); bfb = k.sb([128, 4]); gn = k.sb([128, 256])
    pss = [k.ps([128, 512]) for _ in range(8)]
    k.dma(kTM[:], kMd[:]); k.dma(osg[:], oMd[:])
    for a, b in ((gi, gid), (gf, gfd), (bi, bid), (bfb, bfd), (gn, gnd)):
        k.dma(a[:], b[:])
    k.memset(vext[:, :, :, 128:129], 1.0)
    scale = 128 ** -0.5
    with k.scope() as st:
        qf = k.sb([128, 2, S], stack=st); vf = k.sb([128, NB, 256], stack=st)
        k.dma(qf[:], qTd[:]); k.dma(vf[:], vMd[:])
        k.dma(kTb[:], kTd[:], q="pool")
        k.ts(qTb[:], qf[:], scale, ALU.mult)
        k.copy(vext[:, :, :, 0:128], vf[:].rr("p b (h d) -> p b h d", h=2))
    k.actf(osg[:], osg[:], AF.Sigmoid)
    lf = k.sb([128, NB, 4]); li = k.sb([128, NB, 4])
    for c in range(4):
        k.ts(lf[:, :, c], gf[:, :, c], bfb[:, c:c + 1], ALU.add)
        k.ts(li[:, :, c], gi[:, :, c], bi[:, c:c + 1], ALU.add)
    k.actf(lf[:], lf[:], AF.Exp, scale=-1.0)
    k.actf(lf[:], lf[:], AF.Ln, bias=1.0)
    k.ts(lf[:], lf[:], -1.0, ALU.mult)
    bcol = k.sb([128, NB, 4]); acol = k.sb([128, NB, 4]); wg = k.sb([128, NB, 4]); ebtot = k.sb([128, NB, 4])
    lf2 = lf[:].rr("p b c -> p (b c)")
    pc = pss[0]
    k.mm(pc[:, 0:72], UL4[:, 0, :], lf2)
    k.mm(pc[:, 128:200], UL4[:, 2, :], lf2)
    k.copy(bcol[:, :, 0:2], pc[:, 0:72].rr("p (b c) -> p b c", c=4)[:, :, 0:2])
    k.copy(bcol[:, :, 2:4], pc[:, 128:200].rr("p (b c) -> p b c", c=4)[:, :, 2:4])
    k.tt(acol[:], li[:], bcol[:], ALU.subtract)
    k.mm(pc[:, 256:328], ones[:, :], lf2)
    btot = pc[:, 256:328].rr("p (b c) -> p b c", c=4)
    k.tt(wg[:], acol[:], btot, ALU.add)
    k.actf(wg[:], wg[:], AF.Exp)
    k.actf(ebtot[:], btot, AF.Exp)
    PT4 = k.sb([128, NB, 4, 128], BF16); QT4 = k.sb([128, NB, 4, 128], BF16)
    X = [k.sb([128, 4, 128]) for _ in range(2)]
    Dm = [k.sb([128, 4, 128]) for _ in range(2)]
    Eb = [k.sb([128, 4, 128]) for _ in range(2)]
    for blk in range(NB):
        x = X[blk % 2]; dm = Dm[blk % 2]; eb = Eb[blk % 2]
        pb = pss[1 + blk % 2]; psc = pss[3 + blk % 2]
        k.tt(x[:], UL4[:], lf[:, blk, :].unsq(2).bc([128, 4, 128]), ALU.mult)
        k.mm(pb[:, :], ones[:, :], x[:].rr("p c t -> p (c t)"))
        pb4 = pb[:, :].rr("p (c t) -> p c t", c=4)
        k.tt(dm[:], pb4, bcol[:, blk, :].unsq(2).bc([128, 4, 128]), ALU.subtract)
        k.tt(dm[:], dm[:], UL4[:], ALU.mult)
        k.tt(dm[:], dm[:], li[:, blk, :].unsq(2).bc([128, 4, 128]), ALU.add)
        k.actf(dm[:], dm[:], AF.Exp)
        k.tt(dm[:], dm[:], UL4[:], ALU.mult)
        for hl in range(2):
            k.mm(psc[:, hl * 128:(hl + 1) * 128], kTb[:, hl, blk * 128:(blk + 1) * 128], qTb[:, hl, blk * 128:(blk + 1) * 128])
        for d in range(2):
            k.tt(PT4[:, blk, d * 2:d * 2 + 2, :], dm[:, d * 2:d * 2 + 2, :],
                 psc[:, 0:256].rr("p (h t) -> p h t", h=2), ALU.mult)
        k.actf(eb[:], pb4, AF.Exp)
        for d in range(2):
            k.tt(QT4[:, blk, d * 2:d * 2 + 2, :], eb[:, d * 2:d * 2 + 2, :],
                 qTb[:, :, blk * 128:(blk + 1) * 128], ALU.mult)
    CT = k.sb([128, 4, 129]); CTb = k.sb([128, 4, 129], BF16)
    k.memset(CT[:], 0.0); k.memset(CTb[:], 0.0)
    H = [k.sb([128, NB, 2, 128]) for _ in range(2)]
    kw = [k.sb([128, 128], BF16) for _ in range(4)]
    dn = [k.sb([128, 1]) for _ in range(4)]
    for step in range(NB):
        for c in range(4):
            d, hl = c // 2, c % 2
            blk = FWD_ORDER[step] if d == 0 else BWD_ORDER[step]
            pn = pss[c % 2]; pcs = pss[2 + c % 2]
            k.mm(pn[:, 0:129], PT4[:, blk, c, :], vext[:, blk, hl, :], start=True, stop=False)
            k.mm(pn[:, 0:129], QT4[:, blk, c, :], CTb[:, c, :], start=False, stop=True)
            k.actf(dn[c][:], pn[:, 128:129], AF.Abs)
            k.ts(dn[c][:], dn[c][:], 1.0, ALU.max)
            k.recip(dn[c][:], dn[c][:])
            k.ts(H[d][:, blk, hl, :], pn[:, 0:128], dn[c][:, 0:1], ALU.mult)
            if step < NB - 1:
                k.ts(kw[c][:], kTM[:, blk, hl * 128:(hl + 1) * 128], wg[:, blk, c:c + 1], ALU.mult, eng="pool")
                k.mm(pcs[:, 0:129], kw[c][:], vext[:, blk, hl, :])
                k.stt(CT[:, c, :], CT[:, c, :], ebtot[:, blk, c:c + 1], pcs[:, 0:129], ALU.mult, ALU.add)
                k.copy(CTb[:, c, :], CT[:, c, :], eng="act")
    Hs = H[0]
    k.tt(Hs[:], H[0][:], H[1][:], ALU.add)
    sq = H[1]
    k.tt(sq[:], Hs[:], Hs[:], ALU.mult)
    ss = k.sb([128, NB * 2])
    k.reduce(ss[:], sq[:].rr("p b h d -> p (b h) d"), ALU.add, AX.X)
    k.ts(ss[:], ss[:], 1.0 / 128, ALU.mult, EPS, ALU.add)
    k.actf(ss[:], ss[:], AF.Sqrt)
    k.recip(ss[:], ss[:])
    outb = sq
    for blk in range(NB):
        for hl in range(2):
            k.stt(outb[:, blk, hl, :], Hs[:, blk, hl, :], ss[:, blk * 2 + hl:blk * 2 + hl + 1],
                  gn[:, hl * 128:(hl + 1) * 128], ALU.mult, ALU.mult)
    k.tt(osg[:], osg[:], outb[:].rr("p b h d -> p b (h d)"), ALU.mult)
    k.dma(od[:], osg[:])
    return k.finish()


def tm_blocks(a):
    return np.ascontiguousarray(a.reshape(NB, 128, a.shape[1]).transpose(1, 0, 2))


UL4 = tri_consts()


def ml_inputs(P_b, s, z, l):
    q = P_b[:, s * 256:(s + 1) * 256]; kk = P_b[:, 512 + s * 256:512 + (s + 1) * 256]
    v = P_b[:, 1024 + s * 256:1024 + (s + 1) * 256]; o = P_b[:, 1536 + s * 256:1536 + (s + 1) * 256]
    ig = P_b[:, 2048:2056].reshape(S, 2, 4)[:, :, 2 * s:2 * s + 2].reshape(S, 4)
    fg = P_b[:, 2056:2064].reshape(S, 2, 4)[:, :, 2 * s:2 * s + 2].reshape(S, 4)
    bi = z['ml_i_bias'][l][:, 2 * s:2 * s + 2].reshape(1, 4); bf = z['ml_f_bias'][l][:, 2 * s:2 * s + 2].reshape(1, 4)
    gn = z['ml_norm'][l][s * 256:(s + 1) * 256].reshape(1, 256)
    return {"qT": fm_heads(q), "kT": fm_heads(kk), "kTM": tm_blocks(kk), "vTM": tm_blocks(v), "oTM": tm_blocks(o),
            "gi": tm_blocks(ig), "gf": tm_blocks(fg),
            "bi": np.ascontiguousarray(np.repeat(bi, 128, 0)), "bf": np.ascontiguousarray(np.repeat(bf, 128, 0)),
            "gn": np.ascontiguousarray(np.repeat(gn, 128, 0)), "UL4": UL4}


EPS = 1e-6


def tri8():
    r = np.arange(128)
    U = (r[:, None] <= r[None, :]).astype(np.float32)
    L = np.ascontiguousarray(U.T)
    return np.ascontiguousarray(np.stack([U] * 4 + [L] * 4, 1))


def build_ssd():
    k = KB()
    xbcd = k.din("xbcT", [128, 4, S]); cwd_ = k.din("cw", [128, 4, 5]); cbd = k.din("cb", [128, 4])
    zd = k.din("zTM", [128, NB, 256]); dtd = k.din("dtr", [128, NB, 8])
    dbd = k.din("dtb", [128, 8]); ald = k.din("alog", [128, 8]); dsd = k.din("dskip", [128, 256])
    uld = k.din("UL8", [128, 8, 128]); idd = k.din("ident", [128, 128])
    od = k.dout("yTM", [128, NB, 256])
    ones = k.sb([128, 128]); k.memset(ones[:], 1.0)
    UL8 = k.sb([128, 8, 128]); ident = k.sb([128, 128])
    cw = k.sb([128, 4, 5]); cb = k.sb([128, 4]); dtb = k.sb([128, 8]); alog = k.sb([128, 8]); dsk = k.sb([128, 256])
    zs = k.sb([128, NB, 256]); dt = k.sb([128, NB, 8])
    for a, b in ((UL8, uld), (ident, idd), (cw, cwd_), (cb, cbd), (dtb, dbd), (alog, ald), (dsk, dsd), (zs, zd), (dt, dtd)):
        k.dma(a[:], b[:])
    pss = [k.ps([128, 512]) for _ in range(8)]
    BTb = k.sb([128, S], BF16); CTb = k.sb([128, S], BF16)
    xTM = k.sb([128, NB, 256]); BTM = k.sb([128, NB, 128])
    with k.scope() as st:
        u = k.sb([128, 4, S], stack=st); acc = k.sb([128, 4, S], stack=st)
        xs = u
        k.dma(u[:], xbcd[:])
        for c in range(4):
            e = "dve" if c % 2 == 0 else "pool"
            k.ts(acc[:, c, :], u[:, c, :], cw[:, c, 2:3], ALU.mult, eng=e)
            for j in (0, 1, 3, 4):
                d = j - 2
                for (a, b) in ((0, 256), (256, S)):
                    lo = a + max(0, -d); hi = b - max(0, d)
                    k.stt(acc[:, c, lo:hi], u[:, c, lo + d:hi + d], cw[:, c, j:j + 1], acc[:, c, lo:hi],
                          ALU.mult, ALU.add)
        for c in range(2):
            k.actf(xs[:, c, :], acc[:, c, :], AF.Silu, bias=cb[:, c:c + 1])
        k.actf(acc[:, 2, :], acc[:, 2, :], AF.Silu, bias=cb[:, 2:3])
        k.actf(CTb[:], acc[:, 3, :], AF.Silu, bias=cb[:, 3:4])
        k.copy(BTb[:], acc[:, 2, :])
        for blk in range(NB):
            pt = pss[blk % 4]
            for c in range(2):
                k.transpose(pt[:, c * 128:(c + 1) * 128], xs[:, c, blk * 128:(blk + 1) * 128], ident[:, :])
            k.transpose(pt[:, 256:384], acc[:, 2, blk * 128:(blk + 1) * 128], ident[:, :])
            k.copy(xTM[:, blk, :], pt[:, 0:256], eng="act")
            k.copy(BTM[:, blk, :], pt[:, 256:384])
    A = k.sb([128, 8])
    k.actf(A[:], alog[:], AF.Exp)
    k.ts(A[:], A[:], -1.0, ALU.mult)
    for c in range(8):
        k.ts(dt[:, :, c], dt[:, :, c], dtb[:, c:c + 1], ALU.add)
    k.actf(dt[:], dt[:], AF.Exp)
    k.actf(dt[:], dt[:], AF.Ln, bias=1.0)
    av = k.sb([128, NB, 8])
    for c in range(8):
        k.ts(av[:, :, c], dt[:, :, c], A[:, c:c + 1], ALU.mult)
    bcol = k.sb([128, NB, 8]); wtail = k.sb([128, NB, 8]); ebtot = k.sb([128, NB, 8])
    a2 = av[:].rr("p b c -> p (b c)")
    pc = pss[0]
    k.mm(pc[:, 0:144], UL8[:, 0, :], a2)
    k.mm(pc[:, 160:304], UL8[:, 4, :], a2)
    k.copy(bcol[:, :, 0:4], pc[:, 0:144].rr("p (b c) -> p b c", c=8)[:, :, 0:4])
    k.copy(bcol[:, :, 4:8], pc[:, 160:304].rr("p (b c) -> p b c", c=8)[:, :, 4:8])
    k.mm(pc[:, 320:464], ones[:, :], a2)
    btot = pc[:, 320:464].rr("p (b c) -> p b c", c=8)
    k.tt(wtail[:], btot, bcol[:], ALU.subtract)
    k.actf(wtail[:], wtail[:], AF.Exp)
    k.actf(ebtot[:], btot, AF.Exp)
    xt = k.sb([128, NB, 8, 64], BF16)
    for blk in range(NB):
        for d in range(2):
            k.tt(xt[:, blk, d * 4:(d + 1) * 4, :], xTM[:, blk, :].rr("p (h e) -> p h e", h=4),
                 dt[:, blk, d * 4:(d + 1) * 4].unsq(2).bc([128, 4, 64]), ALU.mult, eng="pool" if d else "dve")
    PT8 = k.sb([128, NB, 8, 128], BF16); CT8 = k.sb([128, NB, 8, 128], BF16)
    pre = k.scope(); st2 = pre.__enter__()
    X = [k.sb([128, 8, 128], stack=st2) for _ in range(2)]
    Dm = [k.sb([128, 8, 128], stack=st2) for _ in range(2)]
    for blk in range(NB):
        x = X[blk % 2]; dm = Dm[blk % 2]
        pb0 = pss[1 + (blk % 2) * 2]; pb1 = pss[2 + (blk % 2) * 2]; pg = pss[5 + blk % 2]
        k.tt(x[:], UL8[:], av[:, blk, :].unsq(2).bc([128, 8, 128]), ALU.mult)
        k.mm(pb0[:, :], ones[:, :], x[:, 0:4, :].rr("p c t -> p (c t)"))
        k.mm(pb1[:, :], ones[:, :], x[:, 4:8, :].rr("p c t -> p (c t)"))
        k.mm(pg[:, 0:128], BTb[:, blk * 128:(blk + 1) * 128], CTb[:, blk * 128:(blk + 1) * 128])
        for d, pb in enumerate((pb0, pb1)):
            pb4 = pb[:, :].rr("p (c t) -> p c t", c=4)
            sl = slice(d * 4, (d + 1) * 4)
            k.tt(dm[:, sl, :], pb4, bcol[:, blk, sl].unsq(2).bc([128, 4, 128]), ALU.subtract)
            k.tt(dm[:, sl, :], dm[:, sl, :], UL8[:, sl, :], ALU.mult)
            k.actf(dm[:, sl, :], dm[:, sl, :], AF.Exp)
            k.tt(dm[:, sl, :], dm[:, sl, :], UL8[:, sl, :], ALU.mult, eng="pool")
            k.tt(PT8[:, blk, sl, :], dm[:, sl, :], pg[:, 0:128].unsq(1).bc([128, 4, 128]), ALU.mult)
            k.actf(x[:, sl, :], pb4, AF.Exp)
            k.tt(CT8[:, blk, sl, :], x[:, sl, :], CTb[:, blk * 128:(blk + 1) * 128].unsq(1).bc([128, 4, 128]),
                 ALU.mult, eng="pool")
    pre.__exit__(None, None, None)
    HT = k.sb([128, 8, 64]); HTb = k.sb([128, 8, 64], BF16)
    k.memset(HT[:], 0.0); k.memset(HTb[:], 0.0)
    Y = [k.sb([128, NB, 256]) for _ in range(2)]
    Bw = [k.sb([128, 128], BF16) for _ in range(8)]
    for step in range(NB):
        for d in range(2):
            blk = FWD_ORDER[step] if d == 0 else BWD_ORDER[step]
            py = pss[d]
            for hh in range(4):
                c = d * 4 + hh
                k.mm(py[:, hh * 64:(hh + 1) * 64], PT8[:, blk, c, :], xt[:, blk, c, :], start=True, stop=False)
                k.mm(py[:, hh * 64:(hh + 1) * 64], CT8[:, blk, c, :], HTb[:, c, :], start=False, stop=True)
            k.copy(Y[d][:, blk, :], py[:, 0:256], eng="act")
            if step < NB - 1:
                ph = pss[2 + d]
                for hh in range(4):
                    c = d * 4 + hh
                    k.ts(Bw[c][:], BTM[:, blk, :], wtail[:, blk, c:c + 1], ALU.mult, eng="pool")
                    k.mm(ph[:, hh * 64:(hh + 1) * 64], Bw[c][:], xt[:, blk, c, :])
                    k.stt(HT[:, c, :], HT[:, c, :], ebtot[:, blk, c:c + 1], ph[:, hh * 64:(hh + 1) * 64], ALU.mult, ALU.add)
                k.copy(HTb[:, d * 4:(d + 1) * 4, :], HT[:, d * 4:(d + 1) * 4, :], eng="act")
    k.tt(Y[0][:], Y[0][:], Y[1][:], ALU.add)
    k.tt(xTM[:], xTM[:], dsk[:, :].unsq(1).bc([128, NB, 256]), ALU.mult)
    k.tt(Y[0][:], Y[0][:], xTM[:], ALU.add)
    k.actf(zs[:], zs[:], AF.Silu)
    k.tt(Y[0][:], Y[0][:], zs[:], ALU.mult)
    k.dma(od[:], Y[0][:])
    return k.finish()


UL8 = tri8()
IDENT = np.eye(128, dtype=np.float32)


def ssd_inputs(P_b, s, z, l):
    c = 2064
    zz = P_b[:, c + s * 256:c + (s + 1) * 256]
    xb = 2576
    xx = P_b[:, xb + s * 256:xb + (s + 1) * 256]
    Bm = P_b[:, xb + 512 + s * 128:xb + 512 + (s + 1) * 128]
    Cm = P_b[:, xb + 768 + s * 128:xb + 768 + (s + 1) * 128]
    chans = np.concatenate([np.arange(s * 256, (s + 1) * 256), 512 + np.arange(s * 128, (s + 1) * 128),
                            768 + np.arange(s * 128, (s + 1) * 128)])
    xbc = np.concatenate([xx, Bm, Cm], 1)
    xbcT = np.ascontiguousarray(xbc.T.reshape(4, 128, S).transpose(1, 0, 2))
    cw = np.ascontiguousarray(z['ssd_conv_w'][l][:, chans].T.reshape(4, 128, 5).transpose(1, 0, 2))
    cb = np.ascontiguousarray(z['ssd_conv_b'][l][chans].reshape(4, 128).T)
    dtr = P_b[:, 3600:3616].reshape(S, 2, 8)[:, :, 4 * s:4 * s + 4].reshape(S, 8)
    rep = lambda v: np.ascontiguousarray(np.repeat(v.reshape(1, -1), 128, 0).astype(np.float32))
    dtb = rep(z['ssd_dt_bias'][l][:, 4 * s:4 * s + 4].reshape(8))
    alog = rep(z['ssd_A_log'][l][:, 4 * s:4 * s + 4].reshape(8))
    dsk = rep(np.repeat(z['ssd_D'][l][4 * s:4 * s + 4], 64))
    return {"xbcT": xbcT, "cw": cw, "cb": cb, "zTM": tm_blocks(zz), "dtr": tm_blocks(dtr), "dtb": dtb, "alog": alog,
            "dskip": dsk, "UL8": UL8, "ident": IDENT}


D = 2048
DIN = 5824
S = 2304
NB = 18
EPS = 1e-6
FD = 5632
FE = 7168
SEQ_TILES = [(0, 256, 1), (256, 512, 0), (768, 512, 0), (1280, 512, 0), (1792, 512, 0)]
TT5 = [(t0, n) for (t0, n, _) in SEQ_TILES]
CGROUPS = [(0, 512, 1, 0), (512, 512, 1, 1), (1024, 512, 0, 1), (1536, 512, 0, 1), (2048, 512, 0, 1), (2560, 16, 0, 1),
           (2576, 512, 1, 0), (3088, 512, 1, 0), (3600, 16, 0, 1), (3616, 512, 1, 0), (4128, 160, 1, 0),
           (4288, 512, 1, 0), (4800, 512, 1, 0), (5312, 512, 0, 1)]


class Consts:
    pass


def rstd_from_ps(k, rs, ps, n, nfeat):
    k.ts(rs[:, 0:n], ps[:, 0:n], 1.0 / nfeat, ALU.mult, EPS, ALU.add)
    k.actf(rs[:, 0:n], rs[:, 0:n], AF.Sqrt)
    k.recip(rs[:, 0:n], rs[:, 0:n])


MG = 768
MNG = 12288 // MG


def mod_load(k, modw_d, wsb, i):
    l, g = divmod(i, MNG)
    wv = modw_d[l].rr("(kc p) n -> p kc n", p=128)
    for q in range(2):
        k.dma(wsb[i % 2][:, q * 8:(q + 1) * 8, :], wv[:, q * 8:(q + 1) * 8, g * MG:(g + 1) * MG], q="pool")


def mod_compute(k, wsb, scb, bsb, modsb, ps2, i):
    l, g = divmod(i, MNG)
    for j in range(MG // 128):
        p = ps2[j % 2]
        for kc in range(16):
            k.mm(p[:, 0:2], wsb[i % 2][:, kc, j * 128:(j + 1) * 128], scb[:, kc, :], start=(kc == 0), stop=(kc == 15))
        ch = g * (MG // 128) + j
        k.ts(modsb[:, l, ch, :], p[:, 0:2], bsb[:, l, ch:ch + 1], ALU.add)


MOD_FIRST = 6


def phase_mod(k, C, ccT_d, modw_d, modb_d, modsb, scb, bsb, pss):
    with k.scope() as st:
        cc = k.sb([128, 16, 2], stack=st)
        wsb = [k.sb([128, 16, MG], BF16, stack=st) for _ in range(2)]
        k.dma(cc[:], ccT_d[:]); k.dma(bsb[:], modb_d[:])
        k.actf(scb[:], cc[:], AF.Silu)
        mod_load(k, modw_d, wsb, 0)
        for i in range(MOD_FIRST):
            if i + 1 < MOD_FIRST:
                mod_load(k, modw_d, wsb, i + 1)
            mod_compute(k, wsb, scb, bsb, modsb, pss[0:2], i)


def norm_tile(k, C, x, h, n, A, sh, col, ps, tmp):
    for kc in range(16):
        sq = tmp[kc % 2]
        k.actf(sq[:, 0:n], x[:, kc, 0:n], AF.Square)
        k.mm(ps[:, 0:n], C.ones[:, :], sq[:, 0:n], start=(kc == 0), stop=(kc == 15))
    rs = tmp[2]
    rstd_from_ps(k, rs, ps, n, D)
    for kc in range(16):
        t = tmp[kc % 2]
        k.tt(t[:, 0:n], x[:, kc, 0:n], rs[:, 0:n], ALU.mult)
        k.ts(h[:, kc, 0:n], t[:, 0:n], A[:, kc, col:col + 1], ALU.mult, sh[:, kc, col:col + 1], ALU.add)


def make_A(k, A, gT, scT):
    for c in range(2):
        k.ts(A[:, :, c], scT[:, :, c], 1.0, ALU.add)
        k.tt(A[:, :, c], A[:, :, c], gT[:, :], ALU.mult)


def phase_inproj(k, C, xsrc, gT, scT, shT, w_d, PT, PM, pss, modctx=None):
    with k.scope() as st:
        hT = k.sb([128, 16, S], BF16, stack=st)
        A = k.sb([128, 16, 2], stack=st)
        make_A(k, A, gT, scT)
        with k.scope() as st2:
            xt = [k.sb([128, 16, 512], stack=st2) for _ in range(2)]
            tmp = [k.sb([128, 512], stack=st2) for _ in range(3)]
            for ti, (t0, n, col) in enumerate(SEQ_TILES):
                x = xt[ti % 2]
                for q in range(4):
                    k.dma(x[:, q * 4:(q + 1) * 4, 0:n], xsrc[:, q * 4:(q + 1) * 4, t0:t0 + n])
                norm_tile(k, C, x, hT[:, :, t0:t0 + n], n, A, shT, col, pss[ti % 2], tmp)
        wsb = [k.sb([128, 16, 512], BF16, stack=st) for _ in range(2)]
        stg = [k.sb([128, 512], stack=st) for _ in range(4)]
        wv = w_d.rr("(kc p) n -> p kc n", p=128)

        def loadw(gi):
            c0, cn, _, _ = CGROUPS[gi]
            for q in range(4):
                k.dma(wsb[gi % 2][:, q * 4:(q + 1) * 4, 0:cn], wv[:, q * 4:(q + 1) * 4, c0:c0 + cn], q="pool")
        loadw(0)
        if modctx is not None:
            modw_d, scb, bsb, modsb = modctx
            wsbm = [k.sb([128, 16, MG], BF16, stack=st) for _ in range(2)]
            mod_load(k, modw_d, wsbm, MOD_FIRST)
        it = 0
        for gi, (c0, cn, fm, tm) in enumerate(CGROUPS):
            if gi + 1 < len(CGROUPS):
                loadw(gi + 1)
            for mi in ((MOD_FIRST + 2 * gi, MOD_FIRST + 2 * gi + 1) if modctx is not None else ()):
                if mi < 2 * MNG:
                    if mi + 1 < 2 * MNG:
                        mod_load(k, modw_d, wsbm, mi + 1)
                    mod_compute(k, wsbm, scb, bsb, modsb, pss[6:8], mi)
            w = wsb[gi % 2]
            if tm:
                for tb in range(NB):
                    ps = pss[it % 4]; sg = stg[it % 4]
                    for kc in range(16):
                        k.mm(ps[:, 0:cn], hT[:, kc, tb * 128:(tb + 1) * 128], w[:, kc, 0:cn], start=(kc == 0), stop=(kc == 15))
                    k.copy(sg[:, 0:cn], ps[:, 0:cn], eng="act" if it % 2 else "dve")
                    k.dma(PM[tb * 128:(tb + 1) * 128, c0:c0 + cn], sg[:, 0:cn], waw=False)
                    it += 1
            if fm:
                for m0 in range(0, cn, 128):
                    mn = min(128, cn - m0)
                    for (t0, n, _) in SEQ_TILES:
                        ps = pss[it % 4]; sg = stg[it % 4]
                        for kc in range(16):
                            k.mm(ps[0:mn, 0:n], w[:, kc, m0:m0 + mn], hT[:, kc, t0:t0 + n], start=(kc == 0), stop=(kc == 15))
                        k.copy(sg[0:mn, 0:n], ps[0:mn, 0:n], eng="act" if it % 2 else "dve")
                        k.dma(PT[c0 + m0:c0 + m0 + mn, t0:t0 + n], sg[0:mn, 0:n], waw=False)
                        it += 1


def headnorm_fm(k, C, srcT, dstT, h, gcol, ps, tmp, extra_scale=1.0, nfeat=128):
    for (t0, n) in TT5:
        sq = tmp[0]
        k.actf(sq[:, 0:n], srcT[:, h, t0:t0 + n], AF.Square)
        k.mm(ps[:, 0:n], C.ones[:, :], sq[:, 0:n])
        rs = tmp[1]
        rstd_from_ps(k, rs, ps, n, nfeat)
        k.tt(rs[:, 0:n], srcT[:, h, t0:t0 + n], rs[:, 0:n], ALU.mult)
        k.ts(dstT[:, h, t0:t0 + n], rs[:, 0:n], gcol, ALU.mult, extra_scale, ALU.mult)


def body_na(k, C, PT, PM, s, gq_d, gk_d, bt_d, MIXT, skip_ctx=False):
    with k.scope() as st:
        sb = lambda shp, dt=F32: k.sb(shp, dt, stack=st)
        qd = PT[4288 + s * 256:4288 + (s + 1) * 256, :].rr("(h p) t -> p h t", p=128)
        kd = PT[4800 + s * 256:4800 + (s + 1) * 256, :].rr("(h p) t -> p h t", p=128)
        vc = slice(5312 + s * 256, 5312 + (s + 1) * 256)
        vAd = PM[:, vc].rr("(j p) c -> p j c", p=128)
        vBd = PM[320:320 + 15 * 128, vc].rr("(j p) c -> p j c", p=128)
        qf = sb([128, 2, S]); kf = sb([128, 2, S])
        qb = sb([128, 2, S], BF16); kb = sb([128, 2, S], BF16)
        vA = sb([128, 18, 256], BF16); vB = sb([128, 15, 256], BF16)
        gq = sb([128, 1]); gk = sb([128, 1]); bt = sb([128, 2, 14, 64])
        oT = sb([128, 2, S])
        tmp = [sb([128, 512]) for _ in range(3)]
        pss = [k.ps([128, 512], stack=st) for _ in range(8)]
        k.dma(qf[:], qd); k.dma(kf[:], kd)
        k.dma(vA[:], vAd, q="pool"); k.dma(vB[:], vBd, q="pool")
        k.dma(gq[:], gq_d); k.dma(gk[:], gk_d); k.dma(bt[:], bt_d)
        scale = 128 ** -0.5
        for h in range(2):
            headnorm_fm(k, C, qf, qb, h, gq[:, 0:1], pss[0], tmp, extra_scale=scale)
            headnorm_fm(k, C, kf, kb, h, gk[:, 0:1], pss[1], tmp)
        pT = [sb([128, 384], BF16) for _ in range(3)]
        pc = sb([128, 512], BF16)
        it = 0
        for h in range(2):
            def s_stage(r, h=h):
                r0 = min(max(r - 4, 0), 24)
                q0 = 256 + r * 64
                ps_s = pss[r % 4]
                vblks = []
                for j in range(4):
                    kt0 = 256 + r0 * 64 + 128 * j
                    k.mm(ps_s[:, j * 64:(j + 1) * 64], kb[:, h, kt0:kt0 + 128], qb[:, h, q0:q0 + 64])
                    if r0 % 2 == 0:
                        vblks.append(vA[:, 2 + r0 // 2 + j, h * 128:(h + 1) * 128])
                    else:
                        vblks.append(vB[:, (r0 - 1) // 2 + j, h * 128:(h + 1) * 128])
                for j in range(2):
                    k.mm(ps_s[:, 256 + j * 64:256 + (j + 1) * 64], kb[:, h, j * 128:(j + 1) * 128], qb[:, h, q0:q0 + 64])
                    vblks.append(vA[:, j, h * 128:(h + 1) * 128])
                return vblks, r0 - r + 7
            cur = s_stage(0)
            for r in range(32):
                rg, rl = r // 8, r % 8
                if rl == 0:
                    po = pss[4 + (it % 2) * 2]; pd = pss[5 + (it % 2) * 2]
                    it += 1
                vblks, m0 = cur
                ps_s = pss[r % 4]
                tm = tmp[r % 2]
                k.tt(tm[:, 0:256].rr("p (j c) -> p j c", j=4), ps_s[:, 0:256].rr("p (j c) -> p j c", j=4),
                     bt[:, h, m0:m0 + 7:2, :], ALU.add)
                p = pT[r % 3]
                k.actf(p[:, 0:256], tm[:, 0:256], AF.Exp)
                k.actf(p[:, 256:384], ps_s[:, 256:384], AF.Exp)
                if r + 1 < 32:
                    cur = s_stage(r + 1)
                for j in range(6):
                    k.mm(po[:, rl * 64:(rl + 1) * 64], vblks[j], p[:, j * 64:(j + 1) * 64], start=(j == 0), stop=(j == 5))
                for j in range(6):
                    k.mm(pd[:, rl * 64:(rl + 1) * 64], C.onesb[:, :], p[:, j * 64:(j + 1) * 64], start=(j == 0), stop=(j == 5))
                if rl == 7:
                    rd = tmp[2]
                    k.recip(rd[:, :], pd[:, :])
                    k.tt(oT[:, h, 256 + rg * 512:256 + (rg + 1) * 512], po[:, :], rd[:, :], ALU.mult)
            if skip_ctx:
                k.memset(oT[:, h, 0:256], 0.0)
                continue
            ps_s = pss[0]; po = pss[4]; pd = pss[5]
            for j in range(2):
                k.mm(ps_s[:, j * 256:(j + 1) * 256], kb[:, h, j * 128:(j + 1) * 128], qb[:, h, 0:256])
            k.actf(pc[:, :], ps_s[:, :], AF.Exp)
            for j in range(2):
                k.mm(po[:, 0:256], vA[:, j, h * 128:(h + 1) * 128], pc[:, j * 256:(j + 1) * 256], start=(j == 0), stop=(j == 1))
            for j in range(2):
                k.mm(pd[:, 0:256], C.onesb[:, :], pc[:, j * 256:(j + 1) * 256], start=(j == 0), stop=(j == 1))
            rd = tmp[2]
            k.recip(rd[:, 0:256], pd[:, 0:256])
            k.tt(oT[:, h, 0:256], po[:, 0:256], rd[:, 0:256], ALU.mult)
        r0_ = 1536 + s * 256
        k.dma(MIXT[r0_:r0_ + 256, :].rr("(h p) t -> p h t", p=128), oT[:], waw=False)


def body_mla(k, C, PT, s, P, MIXT, own=None):
    with k.scope() as st:
        sb = lambda shp, dt=F32: k.sb(shp, dt, stack=st)
        wq = sb([128, 4, 384], BF16); wkv = sb([128, 2, 512], BF16)
        qn = sb([128, 4]); kvn = sb([128, 2]); gq = sb([128, 2]); gk = sb([128, 2])
        cosF = sb([64, 2048]); sinF = sb([64, 2048]); RT = sb([64, 64])
        krf = sb([64, S])
        cqn = sb([128, 4, S], BF16); ckvn = sb([128, 2, S], BF16)
        oT = sb([128, 2, S])
        pss = [k.ps([128, 512], stack=st) for _ in range(8)]
        tmp = [sb([128, 512]) for _ in range(8)]
        k.dma(wq[:], P["wq"], q="pool"); k.dma(wkv[:], P["wkv"], q="pool")
        for a, b in ((qn, "qn"), (kvn, "kvn"), (gq, "gq"), (gk, "gk"), (cosF, "cosF"), (sinF, "sinF"), (RT, "RT")):
            k.dma(a[:], P[b])
        k.dma(krf[:], PT[4224:4288, :])
        with k.scope() as st2:
            cq = k.sb([128, 4, S], stack=st2); ckv = k.sb([128, 2, S], stack=st2)
            k.memset(cq[:, 3, :], 0.0); k.memset(ckv[:, 1, :], 0.0)
            k.dma(cq[:, 0:3, :], PT[3616:3616 + 384, :].rr("(c p) t -> p c t", p=128))
            k.dma(cq[0:64, 3, :], PT[3616 + 384:3616 + 448, :])
            k.dma(ckv[:, 0, :], PT[4064:4064 + 128, :])
            k.dma(ckv[0:32, 1, :], PT[4064 + 128:4064 + 160, :])
            for (src, dst, nch, nfeat, gn) in ((cq, cqn, 4, 448, qn), (ckv, ckvn, 2, 160, kvn)):
                for (t0, n) in TT5:
                    ps = pss[0]
                    for c in range(nch):
                        sq = tmp[c % 2]
                        k.actf(sq[:, 0:n], src[:, c, t0:t0 + n], AF.Square)
                        k.mm(ps[:, 0:n], C.ones[:, :], sq[:, 0:n], start=(c == 0), stop=(c == nch - 1))
                    rs = tmp[2]
                    rstd_from_ps(k, rs, ps, n, nfeat)
                    for c in range(nch):
                        k.stt(dst[:, c, t0:t0 + n], src[:, c, t0:t0 + n], gn[:, c:c + 1], rs[:, 0:n], ALU.mult, ALU.mult)
        scale = 192 ** -0.5
        qA = sb([128, S], BF16); qB = sb([64, S], BF16)
        kA = sb([128, S], BF16); kB = sb([64, S], BF16)
        vT = sb([128, 18, 128], BF16)
        pT = [sb([128, 512], BF16) for _ in range(3)]
        if own is not None:
            sv = sb([128, 2]); k.dma(sv[:], own[0][:])
            qAo = sb([128, 1024], BF16); qBo = sb([64, 1024], BF16)
        pit = [0]
        for h in range(2):
            for which in range(2):
                dA, dB = (qA, qB) if which == 0 else (kA, kB)
                g = gq if which == 0 else gk
                sc = scale if which == 0 else 1.0
                for (t0, n) in TT5:
                    pit[0] += 1
                    o4 = (pit[0] % 2) * 4
                    pa = pss[o4]; pb = pss[o4 + 1]; pn = pss[o4 + 2]; pr = pss[o4 + 3]
                    if which == 0:
                        for c in range(4):
                            k.mm(pa[:, 0:n], wq[:, c, h * 192:h * 192 + 128], cqn[:, c, t0:t0 + n], start=(c == 0), stop=(c == 3))
                        for c in range(4):
                            k.mm(pb[0:64, 0:n], wq[:, c, h * 192 + 128:h * 192 + 192], cqn[:, c, t0:t0 + n], start=(c == 0), stop=(c == 3))
                        srcB = pb[0:64, 0:n]
                    else:
                        for c in range(2):
                            k.mm(pa[:, 0:n], wkv[:, c, h * 256:h * 256 + 128], ckvn[:, c, t0:t0 + n], start=(c == 0), stop=(c == 1))
                        srcB = krf[:, t0:t0 + n]
                    sa = tmp[o4]; sbq = tmp[o4 + 1]
                    k.actf(sa[:, 0:n], pa[:, 0:n], AF.Square)
                    k.actf(sbq[0:64, 0:n], srcB, AF.Square)
                    k.mm(pn[:, 0:n], C.ones[:, :], sa[:, 0:n], start=True, stop=False)
                    k.mm(pn[:, 0:n], C.ones[0:64, :], sbq[0:64, 0:n], start=False, stop=True)
                    rs = tmp[o4 + 2]
                    rstd_from_ps(k, rs, pn, n, 192)
                    k.tt(sa[:, 0:n], pa[:, 0:n], rs[:, 0:n], ALU.mult)
                    k.ts(dA[:, t0:t0 + n], sa[:, 0:n], g[:, 0:1], ALU.mult, sc, ALU.mult)
                    ub = tmp[o4 + 3]
                    k.tt(sbq[0:64, 0:n], srcB, rs[0:64, 0:n], ALU.mult)
                    k.ts(ub[0:64, 0:n], sbq[0:64, 0:n], g[0:64, 1:2], ALU.mult, sc, ALU.mult)
                    l0 = max(t0, 256)
                    if l0 > t0:
                        k.copy(dB[:, t0:l0], ub[0:64, 0:l0 - t0])
                    nl = t0 + n - l0
                    if nl <= 0:
                        continue
                    o = l0 - t0
                    k.mm(pr[0:64, 0:nl], RT[:, :], ub[0:64, o:o + nl])
                    k.tt(sbq[0:64, 0:nl], pr[0:64, 0:nl], sinF[:, l0 - 256:l0 - 256 + nl], ALU.mult)
                    k.tt(ub[0:64, o:o + nl], ub[0:64, o:o + nl], cosF[:, l0 - 256:l0 - 256 + nl], ALU.mult)
                    k.tt(dB[:, l0:l0 + nl], ub[0:64, o:o + nl], sbq[0:64, 0:nl], ALU.add)
            for blk in range(18):
                pv = pss[blk % 2]
                for c in range(2):
                    k.mm(pv[:, 0:128], ckvn[:, c, blk * 128:(blk + 1) * 128], wkv[:, c, h * 256 + 128:h * 256 + 256],
                         start=(c == 0), stop=(c == 1))
                k.copy(vT[:, blk, :], pv[:, 0:128])
            if own is None:
                jobs = [(256 + qt * 512, 512, list(range(18))) for qt in range(4)] + [(0, 256, [0, 1])]
                qsA, qsB = qA, qB
            else:
                k.ts(qAo[:], qA[:, 256:1280], sv[:, 0:1], ALU.mult)
                k.stt(qAo[:], qA[:, 1280:2304], sv[:, 1:2], qAo[:], ALU.mult, ALU.add)
                k.ts(qBo[:], qB[:, 256:1280], sv[0:64, 0:1], ALU.mult)
                k.stt(qBo[:], qB[:, 1280:2304], sv[0:64, 1:2], qBo[:], ALU.mult, ALU.add)
                jobs = [(qt * 512, 512, list(range(18))) for qt in range(2)]
                qsA, qsB = qAo, qBo
            for ji, (q0, nq, blks) in enumerate(jobs):
                po = pss[4 + (ji % 2) * 2]; pd = pss[5 + (ji % 2) * 2]

                def smm(bi):
                    blk = blks[bi]
                    ps_s = pss[bi % 4]
                    k.mm(ps_s[:, 0:nq], kA[:, blk * 128:(blk + 1) * 128], qsA[:, q0:q0 + nq], start=True, stop=False)
                    k.mm(ps_s[:, 0:nq], kB[:, blk * 128:(blk + 1) * 128], qsB[:, q0:q0 + nq], start=False, stop=True)
                for bi in range(min(2, len(blks))):
                    smm(bi)
                for bi, blk in enumerate(blks):
                    p = pT[bi % 3]
                    k.actf(p[:, 0:nq], pss[bi % 4][:, 0:nq], AF.Exp)
                    if bi + 2 < len(blks):
                        smm(bi + 2)
                    k.mm(po[:, 0:nq], vT[:, blk, :], p[:, 0:nq], start=(bi == 0), stop=(bi == len(blks) - 1))
                    k.mm(pd[:, 0:nq], C.onesb[:, :], p[:, 0:nq], start=(bi == 0), stop=(bi == len(blks) - 1))
                rd = tmp[2]
                k.recip(rd[:, 0:nq], pd[:, 0:nq])
                k.tt(oT[:, h, q0:q0 + nq], po[:, 0:nq], rd[:, 0:nq], ALU.mult)
        if own is not None:
            k.dma(own[1][:, 8 + 2 * s:8 + 2 * s + 2, :], oT[:, :, 0:1024], waw=False)
            return
        r0_ = 1024 + s * 256
        k.dma(MIXT[r0_:r0_ + 256, :].rr("(h p) t -> p h t", p=128), oT[:], waw=False)


def tm_to_mixt(k, C, src, MIXT, r0_, pss, stg):
    it = 0
    for c in range(2):
        for g in range(5):
            nb = min(4, NB - g * 4)
            ps = pss[it % 2]; sg = stg[it % 2]
            for j in range(nb):
                blk = g * 4 + j
                k.transpose(ps[:, j * 128:(j + 1) * 128], src[:, blk, c * 128:(c + 1) * 128], C.ident[:, :])
            k.copy(sg[:, 0:nb * 128], ps[:, 0:nb * 128], eng="act" if it % 2 else "dve")
            k.dma(MIXT[r0_ + c * 128:r0_ + (c + 1) * 128, g * 512:g * 512 + nb * 128], sg[:, 0:nb * 128], waw=False)
            it += 1


def body_ml(k, C, PT, PM, s, P, MIXT):
    with k.scope() as st:
        sb = lambda shp, dt=F32: k.sb(shp, dt, stack=st)
        hv = lambda r: PT[r + s * 256:r + (s + 1) * 256, :].rr("(h p) t -> p h t", p=128)
        tv = lambda c: PM[:, c + s * 256:c + (s + 1) * 256].rr("(j p) c -> p j c", p=128)
        UL4 = sb([128, 4, 128]); k.dma(UL4[:], P["UL4"])
        qTb = sb([128, 2, S], BF16); kTb = sb([128, 2, S], BF16)
        kTM = sb([128, NB, 256]); vext = sb([128, NB, 2, 129], BF16)
        osg = sb([128, NB, 256])
        g16 = sb([128, NB, 16]); bi = sb([128, 4]); bfb = sb([128, 4]); gn = sb([128, 256])
        pss = [k.ps([128, 512], stack=st) for _ in range(8)]
        k.dma(kTM[:], tv(512)); k.dma(osg[:], tv(1536))
        k.dma(g16[:], PM[:, 2048:2064].rr("(j p) c -> p j c", p=128))
        for a, b in ((bi, "bi"), (bfb, "bf"), (gn, "gn")):
            k.dma(a[:], P[b])
        k.memset(vext[:, :, :, 128:129], 1.0)
        scale = 128 ** -0.5
        with k.scope() as st2:
            qf = k.sb([128, 2, S], stack=st2); vf = k.sb([128, NB, 256], stack=st2)
            k.dma(qf[:], hv(0)); k.dma(vf[:], tv(1024))
            k.dma(kTb[:], hv(512), q="pool")
            k.ts(qTb[:], qf[:], scale, ALU.mult)
            k.copy(vext[:, :, :, 0:128], vf[:].rr("p b (h d) -> p b h d", h=2))
        k.actf(osg[:], osg[:], AF.Sigmoid)
        lf = sb([128, NB, 4]); li = sb([128, NB, 4])
        for c in range(4):
            d, hl = c // 2, c % 2
            gc = d * 4 + 2 * s + hl
            k.ts(lf[:, :, c], g16[:, :, 8 + gc], bfb[:, c:c + 1], ALU.add)
            k.ts(li[:, :, c], g16[:, :, gc], bi[:, c:c + 1], ALU.add)
        k.actf(lf[:], lf[:], AF.Exp, scale=-1.0)
        k.actf(lf[:], lf[:], AF.Ln, bias=1.0)
        k.ts(lf[:], lf[:], -1.0, ALU.mult)
        bcol = sb([128, NB, 4]); acol = sb([128, NB, 4]); wg = sb([128, NB, 4]); ebtot = sb([128, NB, 4])
        lf2 = lf[:].rr("p b c -> p (b c)")
        pc = pss[0]
        k.mm(pc[:, 0:72], UL4[:, 0, :], lf2)
        k.mm(pc[:, 128:200], UL4[:, 2, :], lf2)
        k.copy(bcol[:, :, 0:2], pc[:, 0:72].rr("p (b c) -> p b c", c=4)[:, :, 0:2])
        k.copy(bcol[:, :, 2:4], pc[:, 128:200].rr("p (b c) -> p b c", c=4)[:, :, 2:4])
        k.tt(acol[:], li[:], bcol[:], ALU.subtract)
        k.mm(pc[:, 256:328], C.ones[:, :], lf2)
        btot = pc[:, 256:328].rr("p (b c) -> p b c", c=4)
        k.tt(wg[:], acol[:], btot, ALU.add)
        k.actf(wg[:], wg[:], AF.Exp)
        k.actf(ebtot[:], btot, AF.Exp)
        PT4 = sb([128, NB, 4, 128], BF16); QT4 = sb([128, NB, 4, 128], BF16)
        KW = sb([128, NB, 4, 128], BF16)
        with k.scope() as st3:
            X = [k.sb([128, 4, 128], stack=st3) for _ in range(2)]
            Dm = [k.sb([128, 4, 128], stack=st3) for _ in range(2)]
            Eb = [k.sb([128, 4, 128], stack=st3) for _ in range(2)]
            for blk in range(NB):
                x = X[blk % 2]; dm = Dm[blk % 2]; eb = Eb[blk % 2]
                pb = pss[1 + blk % 2]; psc = pss[3 + blk % 2]
                k.tt(x[:], UL4[:], lf[:, blk, :].unsq(2).bc([128, 4, 128]), ALU.mult)
                k.mm(pb[:, :], C.ones[:, :], x[:].rr("p c t -> p (c t)"))
                pb4 = pb[:, :].rr("p (c t) -> p c t", c=4)
                k.tt(dm[:], pb4, bcol[:, blk, :].unsq(2).bc([128, 4, 128]), ALU.subtract)
                k.tt(dm[:], dm[:], UL4[:], ALU.mult)
                k.tt(dm[:], dm[:], li[:, blk, :].unsq(2).bc([128, 4, 128]), ALU.add)
                k.actf(dm[:], dm[:], AF.Exp)
                k.tt(dm[:], dm[:], UL4[:], ALU.mult)
                for hl in range(2):
                    k.mm(psc[:, hl * 128:(hl + 1) * 128], kTb[:, hl, blk * 128:(blk + 1) * 128], qTb[:, hl, blk * 128:(blk + 1) * 128])
                for d in range(2):
                    k.tt(PT4[:, blk, d * 2:d * 2 + 2, :], dm[:, d * 2:d * 2 + 2, :],
                         psc[:, 0:256].rr("p (h t) -> p h t", h=2), ALU.mult)
                k.actf(eb[:], pb4, AF.Exp)
                for d in range(2):
                    k.tt(QT4[:, blk, d * 2:d * 2 + 2, :], eb[:, d * 2:d * 2 + 2, :],
                         qTb[:, :, blk * 128:(blk + 1) * 128], ALU.mult)
                for c in range(4):
                    hl = c % 2
                    k.ts(KW[:, blk, c, :], kTM[:, blk, hl * 128:(hl + 1) * 128], wg[:, blk, c:c + 1], ALU.mult)
        CT = sb([128, 4, 129]); CTb = sb([128, 4, 129], BF16)
        k.memset(CT[:], 0.0); k.memset(CTb[:], 0.0)
        H = [sb([128, NB, 2, 128]) for _ in range(2)]
        dn = [sb([128, 1]) for _ in range(4)]
        chains = [(c, c // 2, c % 2) for c in range(4)]
        for step in range(NB):
            blks = [FWD_ORDER[step] if d == 0 else BWD_ORDER[step] for (c, d, hl) in chains]
            for (c, d, hl), blk in zip(chains, blks):
                pn = pss[c]
                k.mm(pn[:, 0:129], PT4[:, blk, c, :], vext[:, blk, hl, :], start=True, stop=False)
                k.mm(pn[:, 0:129], QT4[:, blk, c, :], CTb[:, c, :], start=False, stop=True)
            if step < NB - 1:
                for (c, d, hl), blk in zip(chains, blks):
                    k.mm(pss[4 + c][:, 0:129], KW[:, blk, c, :], vext[:, blk, hl, :])
                for (c, d, hl), blk in zip(chains, blks):
                    k.stt(CT[:, c, :], CT[:, c, :], ebtot[:, blk, c:c + 1], pss[4 + c][:, 0:129], ALU.mult, ALU.add)
                for (c, d, hl), blk in zip(chains, blks):
                    k.copy(CTb[:, c, :], CT[:, c, :], eng="act")
            for (c, d, hl), blk in zip(chains, blks):
                k.actf(dn[c][:], pss[c][:, 128:129], AF.Abs)
            for (c, d, hl), blk in zip(chains, blks):
                k.ts(dn[c][:], dn[c][:], 1.0, ALU.max)
            for (c, d, hl), blk in zip(chains, blks):
                k.recip(dn[c][:], dn[c][:])
            for (c, d, hl), blk in zip(chains, blks):
                k.ts(H[d][:, blk, hl, :], pss[c][:, 0:128], dn[c][:, 0:1], ALU.mult)
        Hs = H[0]
        k.tt(Hs[:], H[0][:], H[1][:], ALU.add)
        sq = H[1]
        k.tt(sq[:], Hs[:], Hs[:], ALU.mult)
        ss = sb([128, NB * 2])
        k.reduce(ss[:], sq[:].rr("p b h d -> p (b h) d"), ALU.add, AX.X)
        k.ts(ss[:], ss[:], 1.0 / 128, ALU.mult, EPS, ALU.add)
        k.actf(ss[:], ss[:], AF.Sqrt)
        k.recip(ss[:], ss[:])
        outb = sq
        for blk in range(NB):
            for hl in range(2):
                k.stt(outb[:, blk, hl, :], Hs[:, blk, hl, :], ss[:, blk * 2 + hl:blk * 2 + hl + 1],
                      gn[:, hl * 128:(hl + 1) * 128], ALU.mult, ALU.mult)
        k.tt(osg[:], osg[:], outb[:].rr("p b h d -> p b (h d)"), ALU.mult)
        stg = [sb([128, 512]) for _ in range(2)]
        tm_to_mixt(k, C, osg, MIXT, s * 256, pss[4:6], stg)


def body_ssd(k, C, PT, PM, s, P, MIXT):
    with k.scope() as st:
        sb = lambda shp, dt=F32: k.sb(shp, dt, stack=st)
        UL8 = sb([128, 8, 128])
        cw = sb([128, 4, 5]); cb = sb([128, 4]); dtb = sb([128, 8]); alog = sb([128, 8]); dsk = sb([128, 256])
        zs = sb([128, NB, 256]); d16 = sb([128, NB, 16]); dt = sb([128, NB, 8])
        for a, b in ((UL8, "UL8"), (cw, "cw"), (cb, "cb"), (dtb, "dtb"), (alog, "alog"), (dsk, "dskip")):
            k.dma(a[:], P[b])
        k.dma(zs[:], PM[:, 2064 + s * 256:2064 + (s + 1) * 256].rr("(j p) c -> p j c", p=128))
        k.dma(d16[:], PM[:, 3600:3616].rr("(j p) c -> p j c", p=128))
        pss = [k.ps([128, 512], stack=st) for _ in range(8)]
        BTb = sb([128, S], BF16); CTb = sb([128, S], BF16)
        xTM = sb([128, NB, 256]); BTM = sb([128, NB, 128])
        with k.scope() as st2:
            u = k.sb([128, 4, S], stack=st2); acc = k.sb([128, 4, S], stack=st2)
            xs = u
            k.dma(u[:, 0:2, :], PT[2576 + s * 256:2576 + (s + 1) * 256, :].rr("(c p) t -> p c t", p=128))
            k.dma(u[:, 2, :], PT[3088 + s * 128:3088 + (s + 1) * 128, :])
            k.dma(u[:, 3, :], PT[3344 + s * 128:3344 + (s + 1) * 128, :])
            for c in range(4):
                k.ts(acc[:, c, :], u[:, c, :], cw[:, c, 2:3], ALU.mult)
                for j in (0, 1, 3, 4):
                    d = j - 2
                    for (a, b) in ((0, 256), (256, S)):
                        lo = a + max(0, -d); hi = b - max(0, d)
                        k.stt(acc[:, c, lo:hi], u[:, c, lo + d:hi + d], cw[:, c, j:j + 1], acc[:, c, lo:hi], ALU.mult, ALU.add)
            for c in range(2):
                k.actf(xs[:, c, :], acc[:, c, :], AF.Silu, bias=cb[:, c:c + 1])
            k.actf(acc[:, 2, :], acc[:, 2, :], AF.Silu, bias=cb[:, 2:3])
            k.actf(CTb[:], acc[:, 3, :], AF.Silu, bias=cb[:, 3:4])
            k.copy(BTb[:], acc[:, 2, :])
            for blk in range(NB):
                pt = pss[blk % 4]
                for c in range(2):
                    k.transpose(pt[:, c * 128:(c + 1) * 128], xs[:, c, blk * 128:(blk + 1) * 128], C.ident[:, :])
                k.transpose(pt[:, 256:384], acc[:, 2, blk * 128:(blk + 1) * 128], C.ident[:, :])
                k.copy(xTM[:, blk, :], pt[:, 0:256], eng="act")
                k.copy(BTM[:, blk, :], pt[:, 256:384])
        A = sb([128, 8])
        k.actf(A[:], alog[:], AF.Exp)
        k.ts(A[:], A[:], -1.0, ALU.mult)
        for c in range(8):
            d, hh = c // 4, c % 4
            k.ts(dt[:, :, c], d16[:, :, d * 8 + 4 * s + hh], dtb[:, c:c + 1], ALU.add)
        k.actf(dt[:], dt[:], AF.Exp)
        k.actf(dt[:], dt[:], AF.Ln, bias=1.0)
        av = sb([128, NB, 8])
        for c in range(8):
            k.ts(av[:, :, c], dt[:, :, c], A[:, c:c + 1], ALU.mult)
        bcol = sb([128, NB, 8]); wtail = sb([128, NB, 8]); ebtot = sb([128, NB, 8])
        a2 = av[:].rr("p b c -> p (b c)")
        pc = pss[0]
        k.mm(pc[:, 0:144], UL8[:, 0, :], a2)
        k.mm(pc[:, 160:304], UL8[:, 4, :], a2)
        k.copy(bcol[:, :, 0:4], pc[:, 0:144].rr("p (b c) -> p b c", c=8)[:, :, 0:4])
        k.copy(bcol[:, :, 4:8], pc[:, 160:304].rr("p (b c) -> p b c", c=8)[:, :, 4:8])
        k.mm(pc[:, 320:464], C.ones[:, :], a2)
        btot = pc[:, 320:464].rr("p (b c) -> p b c", c=8)
        k.tt(wtail[:], btot, bcol[:], ALU.subtract)
        k.actf(wtail[:], wtail[:], AF.Exp)
        k.actf(ebtot[:], btot, AF.Exp)
        xt = sb([128, NB, 8, 64], BF16)
        for blk in range(NB):
            for d in range(2):
                k.tt(xt[:, blk, d * 4:(d + 1) * 4, :], xTM[:, blk, :].rr("p (h e) -> p h e", h=4),
                     dt[:, blk, d * 4:(d + 1) * 4].unsq(2).bc([128, 4, 64]), ALU.mult, eng="pool" if d else "dve")
        PT8 = sb([128, NB, 8, 128], BF16); CT8 = sb([128, NB, 8, 128], BF16)
        with k.scope() as st3:
            X = [k.sb([128, 8, 128], stack=st3) for _ in range(2)]
            Dm = [k.sb([128, 8, 128], stack=st3) for _ in range(2)]
            for blk in range(NB):
                x = X[blk % 2]; dm = Dm[blk % 2]
                pb0 = pss[1 + (blk % 2) * 2]; pb1 = pss[2 + (blk % 2) * 2]; pg = pss[5 + blk % 2]
                k.tt(x[:], UL8[:], av[:, blk, :].unsq(2).bc([128, 8, 128]), ALU.mult)
                k.mm(pb0[:, :], C.ones[:, :], x[:, 0:4, :].rr("p c t -> p (c t)"))
                k.mm(pb1[:, :], C.ones[:, :], x[:, 4:8, :].rr("p c t -> p (c t)"))
                k.mm(pg[:, 0:128], BTb[:, blk * 128:(blk + 1) * 128], CTb[:, blk * 128:(blk + 1) * 128])
                for d, pb in enumerate((pb0, pb1)):
                    pb4 = pb[:, :].rr("p (c t) -> p c t", c=4)
                    sl = slice(d * 4, (d + 1) * 4)
                    k.tt(dm[:, sl, :], pb4, bcol[:, blk, sl].unsq(2).bc([128, 4, 128]), ALU.subtract)
                    k.tt(dm[:, sl, :], dm[:, sl, :], UL8[:, sl, :], ALU.mult)
                    k.actf(dm[:, sl, :], dm[:, sl, :], AF.Exp)
                    k.tt(dm[:, sl, :], dm[:, sl, :], UL8[:, sl, :], ALU.mult, eng="pool")
                    k.tt(PT8[:, blk, sl, :], dm[:, sl, :], pg[:, 0:128].unsq(1).bc([128, 4, 128]), ALU.mult)
                    k.actf(x[:, sl, :], pb4, AF.Exp)
                    k.tt(CT8[:, blk, sl, :], x[:, sl, :], CTb[:, blk * 128:(blk + 1) * 128].unsq(1).bc([128, 4, 128]),
                         ALU.mult, eng="pool")
        HT = sb([128, 8, 64]); HTb = sb([128, 8, 64], BF16)
        k.memset(HT[:], 0.0); k.memset(HTb[:], 0.0)
        Y = [sb([128, NB, 256]) for _ in range(2)]
        Bw = [sb([128, 128], BF16) for _ in range(8)]
        for step in range(NB):
            bl = [FWD_ORDER[step], BWD_ORDER[step]]
            if step < NB - 1:
                for d in range(2):
                    for hh in range(4):
                        c = d * 4 + hh
                        k.ts(Bw[c][:], BTM[:, bl[d], :], wtail[:, bl[d], c:c + 1], ALU.mult)
            for d in range(2):
                for hh in range(4):
                    c = d * 4 + hh
                    k.mm(pss[d][:, hh * 64:(hh + 1) * 64], PT8[:, bl[d], c, :], xt[:, bl[d], c, :], start=True, stop=False)
                    k.mm(pss[d][:, hh * 64:(hh + 1) * 64], CT8[:, bl[d], c, :], HTb[:, c, :], start=False, stop=True)
            if step < NB - 1:
                for d in range(2):
                    for hh in range(4):
                        c = d * 4 + hh
                        k.mm(pss[2 + d][:, hh * 64:(hh + 1) * 64], Bw[c][:], xt[:, bl[d], c, :])
                for d in range(2):
                    k.tt(HT[:, d * 4:(d + 1) * 4, :], HT[:, d * 4:(d + 1) * 4, :],
                         ebtot[:, bl[d], d * 4:(d + 1) * 4].unsq(2).bc([128, 4, 64]), ALU.mult)
                    k.tt(HT[:, d * 4:(d + 1) * 4, :], HT[:, d * 4:(d + 1) * 4, :],
                         pss[2 + d][:, 0:256].rr("p (h e) -> p h e", h=4), ALU.add)
                for d in range(2):
                    k.copy(HTb[:, d * 4:(d + 1) * 4, :], HT[:, d * 4:(d + 1) * 4, :], eng="act")
            for d in range(2):
                k.copy(Y[d][:, bl[d], :], pss[d][:, 0:256], eng="act")
        k.tt(Y[0][:], Y[0][:], Y[1][:], ALU.add)
        k.tt(xTM[:], xTM[:], dsk[:, :].unsq(1).bc([128, NB, 256]), ALU.mult)
        k.tt(Y[0][:], Y[0][:], xTM[:], ALU.add)
        k.actf(zs[:], zs[:], AF.Silu)
        k.tt(Y[0][:], Y[0][:], zs[:], ALU.mult)
        stg = [Y[1][:, 0:2, :].rr("p a c -> p (a c)"), Y[1][:, 2:4, :].rr("p a c -> p (a c)")]
        tm_to_mixt(k, C, Y[0], MIXT, 512 + s * 256, pss[4:6], stg)


def outproj_tiles(k, C, mixsrc, xT, tiles, NT, wo_d, g1, gs, pss, st):
    mixb = k.sb([128, 16, NT], BF16, stack=st); ssd = k.sb([128, 4, NT], stack=st)
    tmp = [k.sb([128, 512], stack=st) for _ in range(3)]
    wos = [k.sb([128, 16, 512], BF16, stack=st) for _ in range(2)]
    wov = wo_d.rr("(kc p) n -> p kc n", p=128)

    def loadwo(g):
        for q in range(2):
            k.dma(wos[g % 2][:, q * 8:(q + 1) * 8, :], wov[:, q * 8:(q + 1) * 8, g * 512:(g + 1) * 512], q="pool")
    k.dma(mixb[:, 0:4, :], mixsrc[:, 0:4, :], q="pool"); k.dma(mixb[:, 8:12, :], mixsrc[:, 8:12, :], q="pool")
    k.dma(mixb[:, 12:16, :], mixsrc[:, 12:16, :], q="pool"); k.dma(ssd[:], mixsrc[:, 4:8, :])
    loadwo(0)
    for (t0, n, _) in tiles:
        ps = pss[0]
        for c in range(4):
            sq = tmp[c % 2]
            k.actf(sq[:, 0:n], ssd[:, c, t0:t0 + n], AF.Square)
            k.mm(ps[:, 0:n], C.ones[:, :], sq[:, 0:n], start=(c == 0), stop=(c == 3))
        rs = tmp[2]
        rstd_from_ps(k, rs, ps, n, 512)
        for c in range(4):
            k.stt(mixb[:, 4 + c, t0:t0 + n], ssd[:, c, t0:t0 + n], gs[:, c:c + 1], rs[:, 0:n], ALU.mult, ALU.mult)
    jt = 0
    for g in range(4):
        if g + 1 < 4:
            loadwo(g + 1)
        for dl in range(4):
            dc = g * 4 + dl
            for (t0, n, col) in tiles:
                po = pss[4 + jt % 4]
                for kc in range(16):
                    k.mm(po[:, 0:n], wos[g % 2][:, kc, dl * 128:(dl + 1) * 128], mixb[:, kc, t0:t0 + n],
                         start=(kc == 0), stop=(kc == 15))
                k.stt(xT[:, dc, t0:t0 + n], po[:, 0:n], g1[:, dc, col:col + 1], xT[:, dc, t0:t0 + n], ALU.mult, ALU.add)
                jt += 1


def phase_ffn0(k, C, xsrc, MIXT, XR, tok0, tiles, modl, n2, gs, wo_d, w1d, w3d, w2d, pss):
    NT = sum(n for _, n, _ in tiles)
    g1 = modl(2); sh2 = modl(3); sc2 = modl(4); g2 = modl(5)
    mixv = MIXT.rr("(kc p) t -> p kc t", p=128)
    with k.scope() as st:
        xT = k.sb([128, 16, NT], stack=st)
        for q in range(4):
            k.dma(xT[:, q * 4:(q + 1) * 4, :], xsrc[:, q * 4:(q + 1) * 4, tok0:tok0 + NT])
        with k.scope() as st2:
            outproj_tiles(k, C, mixv[:, :, tok0:tok0 + NT], xT, tiles, NT, wo_d, g1, gs, pss, st2)
        h2T = k.sb([128, 16, NT], BF16, stack=st)
        with k.scope() as st2:
            tmp = [k.sb([128, 512], stack=st2) for _ in range(3)]
            A = k.sb([128, 16, 2], stack=st2)
            make_A(k, A, n2, sc2)
            for ti, (t0, n, col) in enumerate(tiles):
                norm_tile(k, C, xT[:, :, t0:t0 + n], h2T[:, :, t0:t0 + n], n, A, sh2, col, pss[ti % 2], tmp)
        colof = {t0: col for (t0, n, col) in tiles}

        def acc(po, dc, t0, n):
            col = colof[t0]
            k.stt(xT[:, dc, t0:t0 + n], po, g2[:, dc, col:col + 1], xT[:, dc, t0:t0 + n], ALU.mult, ALU.add)
        with k.scope() as st2:
            ffn_simple(k, h2T, tiles, w1d, w3d, w2d, FD, acc, pss, stack=st2)
        for q in range(4):
            k.dma(XR[:, q * 4:(q + 1) * 4, tok0:tok0 + NT], xT[:, q * 4:(q + 1) * 4, :], waw=False)


def phase_moe(k, C, XO, MIXO, modl, n2, gs, wo_d, rt_d, sel_d, w1d, w3d, w2d, xout, pss):
    NT = 1024
    tiles = [(0, 512, 0), (512, 512, 0)]
    g1 = modl(2); sh2 = modl(3); sc2 = modl(4); g2 = modl(5)
    with k.scope() as st:
        xT = k.sb([128, 16, NT], stack=st)
        for q in range(4):
            k.dma(xT[:, q * 4:(q + 1) * 4, :], XO[:, q * 4:(q + 1) * 4, :])
        with k.scope() as st2:
            outproj_tiles(k, C, MIXO, xT, tiles, NT, wo_d, g1, gs, pss, st2)
        h2B = k.sb([128, 16, NT], BF16, stack=st)
        gT8 = k.sb([8, NT], stack=st)
        sel = k.sb([8, 8, 128], stack=st)
        k.dma(sel[:], sel_d)
        with k.scope() as st2:
            h2F = k.sb([128, 16, NT], stack=st2)
            tmp = [k.sb([128, 512], stack=st2) for _ in range(3)]
            A = k.sb([128, 16, 2], stack=st2)
            rt = k.sb([128, 16, 8], stack=st2)
            k.dma(rt[:], rt_d)
            make_A(k, A, n2, sc2)
            for ti, (t0, n, col) in enumerate(tiles):
                norm_tile(k, C, xT[:, :, t0:t0 + n], h2F[:, :, t0:t0 + n], n, A, sh2, col, pss[ti % 2], tmp)
            for q in range(4):
                k.copy(h2B[:, q * 4:(q + 1) * 4, :], h2F[:, q * 4:(q + 1) * 4, :], eng="act" if q % 2 else "dve")
            lg = k.sb([128, 8], stack=st2); m1 = k.sb([128, 1], stack=st2); m2 = k.sb([128, 1], stack=st2)
            t8 = k.sb([128, 8], stack=st2); sl = k.sb([128, 8], stack=st2); sm = k.sb([128, 1], stack=st2)
            gts = k.sb([128, 8], stack=st2)
            for tb in range(NT // 128):
                pl = pss[tb % 2]; pt = pss[2 + tb % 2]
                for kc in range(16):
                    k.mm(pl[:, 0:8], h2F[:, kc, tb * 128:(tb + 1) * 128], rt[:, kc, :], start=(kc == 0), stop=(kc == 15))
                k.copy(lg[:], pl[:, 0:8])
                k.reduce(m1[:], lg[:], ALU.max)
                k.ts(t8[:], lg[:], m1[:, 0:1], ALU.is_ge, -1e30, ALU.mult)
                k.tt(t8[:], t8[:], lg[:], ALU.add)
                k.reduce(m2[:], t8[:], ALU.max)
                k.ts(sl[:], lg[:], m2[:, 0:1], ALU.is_ge)
                k.ts(t8[:], lg[:], m1[:, 0:1], ALU.subtract)
                k.actf(t8[:], t8[:], AF.Exp)
                k.tt(t8[:], t8[:], sl[:], ALU.mult)
                k.reduce(sm[:], t8[:], ALU.add)
                k.recip(sm[:], sm[:])
                k.ts(gts[:], t8[:], sm[:, 0:1], ALU.mult)
                k.transpose(pt[0:8, 0:128], gts[:, :], C.ident[:, :])
                k.copy(gT8[:, tb * 128:(tb + 1) * 128], pt[0:8, 0:128])
        gbc = k.sb([128, NT], stack=st)
        for e in range(8):
            for (t0, n, _) in tiles:
                pg = pss[0]
                k.mm(pg[:, 0:n], sel[:, e, :], gT8[:, t0:t0 + n])
                k.copy(gbc[:, t0:t0 + n], pg[:, 0:n])

            def acc(po, dc, t0, n):
                k.stt(xT[:, dc, t0:t0 + n], po, g2[:, dc, 0:1], xT[:, dc, t0:t0 + n], ALU.mult, ALU.add)
            with k.scope() as st2:
                ffn_simple(k, h2B, tiles, w1d[e], w3d[e], w2d[e], FE, acc, pss, stack=st2, gate=gbc)
        for q in range(4):
            k.dma(xout[:, q * 4:(q + 1) * 4, :], xT[:, q * 4:(q + 1) * 4, :])


def build_mega(stop_after=None):
    k = KB()
    C = Consts()
    xin = k.din("xT", [128, 16, S])
    ccT_d = k.din("ccT", [128, 16, 2]); modw_d = k.din("mod_w", [2, D, 6 * D]); modb_d = k.din("mod_bT", [128, 2, 96])
    n1_d = k.din("n1T", [128, 2, 16]); n2_d = k.din("n2T", [128, 2, 16]); gs_d = k.din("gsT", [128, 2, 4])
    win_d = k.din("w_in", [2, D, DIN]); wout_d = k.din("w_out", [2, D, D])
    ident_d = k.din("ident", [128, 128])
    na_g = k.din("na_g", [128, 2, 2]); na_bt = k.din("na_bt", [128, 2, 2, 2, 14, 64])
    mla_wq = k.din("mla_wq", [2, 2, 128, 4, 384]); mla_wkv = k.din("mla_wkv", [2, 2, 128, 2, 512])
    mla_v = k.din("mla_vec", [128, 2, 10]); rope_c = k.din("cosF", [64, 2048]); rope_s = k.din("sinF", [64, 2048])
    rope_r = k.din("RT", [64, 64])
    ml_b = k.din("ml_b", [128, 2, 2, 8]); ml_gn = k.din("ml_gn", [128, 2, 2, 256]); ul4_d = k.din("UL4", [128, 4, 128])
    ssd_cw = k.din("ssd_cw", [128, 2, 2, 4, 5]); ssd_cb = k.din("ssd_cb", [128, 2, 2, 4])
    ssd_v = k.din("ssd_vec", [128, 2, 2, 16]); ssd_dk = k.din("ssd_dsk", [128, 2, 2, 256]); ul8_d = k.din("UL8", [128, 8, 128])
    if stop_after is None or stop_after[0] != "mix" or stop_after[1] > 0:
        fw1 = k.din("ffn_w1", [D, FD]); fw3 = k.din("ffn_w3", [D, FD]); fw2 = k.din("ffn_w2", [FD, D])
    if stop_after is None:
        rt_d = k.din("router", [128, 16, 8]); sel_d = k.din("sel", [8, 8, 128]); selv_d = k.din("selv", [128, 2])
        mw1 = k.din("moe_w1", [8, D, FE]); mw3 = k.din("moe_w3", [8, D, FE]); mw2 = k.din("moe_w2", [8, FE, D])
        xout = k.dout("xo", [128, 16, 1024])
        XO = k.dram("XO", [128, 16, 1024], F32, "Internal"); MIXO = k.dram("MIXO", [128, 16, 1024], F32, "Internal")
    PT = k.dram("PT", [DIN, S], F32, "Internal"); PM = k.dram("PM", [S, DIN], F32, "Internal")
    MIXT = k.dram("MIXT", [D, S], F32, "Internal"); XR = k.dram("XR", [128, 16, S], F32, "Internal")
    C.ones = k.sb([128, 128]); k.memset(C.ones[:], 1.0)
    C.onesb = k.sb([128, 128], BF16); k.memset(C.onesb[:], 1.0)
    C.ident = k.sb([128, 128]); k.dma(C.ident[:], ident_d[:])
    modsb = k.sb([128, 2, 96, 2])
    n1 = k.sb([128, 2, 16]); n2 = k.sb([128, 2, 16]); gs = k.sb([128, 2, 4])
    k.dma(n1[:], n1_d[:]); k.dma(n2[:], n2_d[:]); k.dma(gs[:], gs_d[:])
    scb = k.sb([128, 16, 2], BF16); bsb = k.sb([128, 2, 96])
    with k.scope() as st:
        pss = [k.ps([128, 512], stack=st) for _ in range(8)]
        phase_mod(k, C, ccT_d, modw_d, modb_d, modsb, scb, bsb, pss)
    for l in range(2):
        modl = lambda which, l=l: modsb[:, l, which * 16:(which + 1) * 16, :]
        xsrc = xin[:] if l == 0 else XR[:]
        with k.scope() as st:
            pss = [k.ps([128, 512], stack=st) for _ in range(8)]
            phase_inproj(k, C, xsrc, n1[:, l, :], modl(1), modl(0), win_d[l], PT, PM, pss,
                         modctx=(modw_d, scb, bsb, modsb) if l == 0 else None)
        for s in range(2):
            body_ml(k, C, PT, PM, s, {"bi": ml_b[:, l, s, 0:4], "bf": ml_b[:, l, s, 4:8], "gn": ml_gn[:, l, s, :],
                                      "UL4": ul4_d[:]}, MIXT)
            body_ssd(k, C, PT, PM, s, {"cw": ssd_cw[:, l, s], "cb": ssd_cb[:, l, s], "dtb": ssd_v[:, l, s, 0:8],
                                       "alog": ssd_v[:, l, s, 8:16], "dskip": ssd_dk[:, l, s, :], "UL8": ul8_d[:]}, MIXT)
            body_mla(k, C, PT, s, {"wq": mla_wq[l, s], "wkv": mla_wkv[l, s], "qn": mla_v[:, l, 0:4], "kvn": mla_v[:, l, 4:6],
                                   "gq": mla_v[:, l, 6:8], "gk": mla_v[:, l, 8:10], "cosF": rope_c[:], "sinF": rope_s[:],
                                   "RT": rope_r[:]}, MIXT, own=(selv_d, MIXO) if (l == 1 and stop_after is None) else None)
            body_na(k, C, PT, PM, s, na_g[:, l, 0:1], na_g[:, l, 1:2], na_bt[:, l, s], MIXT, skip_ctx=(l == 1))
        if stop_after == ("mix", l):
            dbg = k.dout("dbg", [D, S])
            with k.scope() as st:
                t = k.sb([128, 16, S], stack=st)
                k.dma(t[:], MIXT[:].rr("(kc p) t -> p kc t", p=128))
                k.dma(dbg[:].rr("(kc p) t -> p kc t", p=128), t[:])
            return k.finish()
        with k.scope() as st:
            pss = [k.ps([128, 512], stack=st) for _ in range(8)]
            if l == 0:
                phase_ffn0(k, C, xin[:], MIXT[:], XR, 0, [(0, 256, 1), (256, 512, 0), (768, 384, 0)], modl, n2[:, l, :],
                           gs[:, l, :], wout_d[l], fw1[:], fw3[:], fw2[:], pss)
                phase_ffn0(k, C, xin[:], MIXT[:], XR, 1152, [(0, 512, 0), (512, 512, 0), (1024, 128, 0)], modl, n2[:, l, :],
                           gs[:, l, :], wout_d[l], fw1[:], fw3[:], fw2[:], pss)
            else:
                phase_select(k, XR[:], MIXT[:].rr("(kc p) t -> p kc t", p=128), XO, MIXO, selv_d, skip_mix_q=(2,))
                phase_moe(k, C, XO[:], MIXO[:], modl, n2[:, l, :], gs[:, l, :], wout_d[l], rt_d[:], sel_d[:],
                          mw1, mw3, mw2, xout, pss)
    return k.finish()


def phase_select(k, xr, mixv, XO, MIXO, selv_d, skip_mix_q=()):
    with k.scope() as st:
        sv = k.sb([128, 2], stack=st)
        k.dma(sv[:], selv_d[:])
        a = [k.sb([128, 4, 1024], stack=st) for _ in range(2)]
        b = [k.sb([128, 4, 1024], stack=st) for _ in range(2)]
        it = 0
        for si, (src, dst) in enumerate(((xr, XO), (mixv, MIXO))):
            for q in range(4):
                if si == 1 and q in skip_mix_q:
                    continue
                ta = a[it % 2]; tb = b[it % 2]; it += 1
                k.dma(ta[:], src[:, q * 4:(q + 1) * 4, 256:1280])
                k.dma(tb[:], src[:, q * 4:(q + 1) * 4, 1280:2304])
                k.ts(ta[:], ta[:], sv[:, 0:1], ALU.mult)
                k.stt(ta[:], tb[:], sv[:, 1:2], ta[:], ALU.mult, ALU.add)
                k.dma(dst[:, q * 4:(q + 1) * 4, :], ta[:], waw=False)


ROPE = rope_tables()
UL4c = tri_consts()
UL8c = tri8()


def _rep(v):
    return np.ascontiguousarray(np.repeat(np.asarray(v, np.float32).reshape(1, -1), 128, 0))


def mega_inputs(z, full=True):
    L = 2
    shared = {}
    shared["mod_w"] = np.ascontiguousarray(z['mod_w'])
    shared["mod_bT"] = np.ascontiguousarray(z['mod_b'].reshape(2, 96, 128).transpose(2, 0, 1))
    shared["n1T"] = np.ascontiguousarray(z['norm1'].reshape(2, 16, 128).transpose(2, 0, 1))
    shared["n2T"] = np.ascontiguousarray(z['norm2'].reshape(2, 16, 128).transpose(2, 0, 1))
    shared["gsT"] = np.ascontiguousarray(z['ssd_norm'].reshape(2, 4, 128).transpose(2, 0, 1))
    shared["w_in"] = np.ascontiguousarray(z['w_in']); shared["w_out"] = np.ascontiguousarray(z['w_out'])
    shared["ident"] = np.eye(128, dtype=np.float32)
    shared["na_g"] = np.ascontiguousarray(np.stack([z['na_gq'], z['na_gk']], -1).transpose(1, 0, 2))
    shared["na_bt"] = np.ascontiguousarray(np.stack(
        [np.stack([na_bias_tiles(z['na_rpb'][l], [2 * s, 2 * s + 1]) for s in range(2)], 1) for l in range(L)], 1))
    shared["mla_wq"] = np.ascontiguousarray(np.stack(
        [np.stack([w_pad(z['mla_w_qb'][l][:, s * 384:(s + 1) * 384], 4) for s in range(2)], 0) for l in range(L)], 0))
    shared["mla_wkv"] = np.ascontiguousarray(np.stack(
        [np.stack([w_pad(z['mla_w_kvb'][l][:, s * 512:(s + 1) * 512], 2) for s in range(2)], 0) for l in range(L)], 0))
    shared["mla_vec"] = np.ascontiguousarray(np.stack(
        [np.concatenate([vec_pad(z['mla_q_norm'][l], 4), vec_pad(z['mla_kv_norm'][l], 2), vec_pad(z['mla_gq'][l], 2),
                         vec_pad(z['mla_gk'][l], 2)], 1) for l in range(L)], 1))
    shared["cosF"], shared["sinF"], shared["RT"] = ROPE
    shared["ml_b"] = np.ascontiguousarray(np.stack(
        [np.stack([_rep(np.concatenate([z['ml_i_bias'][l][:, 2 * s:2 * s + 2].reshape(4),
                                        z['ml_f_bias'][l][:, 2 * s:2 * s + 2].reshape(4)])) for s in range(2)], 1)
         for l in range(L)], 1))
    shared["ml_gn"] = np.ascontiguousarray(np.stack(
        [np.stack([_rep(z['ml_norm'][l][s * 256:(s + 1) * 256]) for s in range(2)], 1) for l in range(L)], 1))
    shared["UL4"] = UL4c; shared["UL8"] = UL8c
    cw_l, cb_l, v_l, dk_l = [], [], [], []
    for l in range(L):
        cw_s, cb_s, v_s, dk_s = [], [], [], []
        for s in range(2):
            chans = np.concatenate([np.arange(s * 256, (s + 1) * 256), 512 + np.arange(s * 128, (s + 1) * 128),
                                    768 + np.arange(s * 128, (s + 1) * 128)])
            cw_s.append(z['ssd_conv_w'][l][:, chans].T.reshape(4, 128, 5).transpose(1, 0, 2))
            cb_s.append(z['ssd_conv_b'][l][chans].reshape(4, 128).T)
            v_s.append(_rep(np.concatenate([z['ssd_dt_bias'][l][:, 4 * s:4 * s + 4].reshape(8),
                                            z['ssd_A_log'][l][:, 4 * s:4 * s + 4].reshape(8)])))
            dk_s.append(_rep(np.repeat(z['ssd_D'][l][4 * s:4 * s + 4], 64)))
        cw_l.append(np.stack(cw_s, 1)); cb_l.append(np.stack(cb_s, 1)); v_l.append(np.stack(v_s, 1)); dk_l.append(np.stack(dk_s, 1))
    shared["ssd_cw"] = np.ascontiguousarray(np.stack(cw_l, 1)); shared["ssd_cb"] = np.ascontiguousarray(np.stack(cb_l, 1))
    shared["ssd_vec"] = np.ascontiguousarray(np.stack(v_l, 1)); shared["ssd_dsk"] = np.ascontiguousarray(np.stack(dk_l, 1))
    if full:
        shared["ffn_w1"] = np.ascontiguousarray(z['ffn_w1'][0]); shared["ffn_w3"] = np.ascontiguousarray(z['ffn_w3'][0])
        shared["ffn_w2"] = np.ascontiguousarray(z['ffn_w2'][0])
        shared["router"] = np.ascontiguousarray(z['moe_router'][0].reshape(16, 128, 8).transpose(1, 0, 2))
        sel = np.zeros((8, 8, 128), np.float32)
        for e in range(8):
            sel[e, e, :] = 1.0
        shared["sel"] = sel
        shared["moe_w1"] = np.ascontiguousarray(z['moe_w1'][0]); shared["moe_w3"] = np.ascontiguousarray(z['moe_w3'][0])
        shared["moe_w2"] = np.ascontiguousarray(z['moe_w2'][0])
    maps = []
    for i in range(NCORES):
        b, s = i // 2, i % 2
        m_ = dict(shared)
        m_["xT"] = to_fm(np.concatenate([z['ctx'][b], z['x'][b]], 0))
        cc = np.stack([z['c'][b], z['c_ctx']], 0)
        m_["ccT"] = np.ascontiguousarray(cc.T.reshape(16, 128, 2).transpose(1, 0, 2))
        if full:
            sv = np.zeros((128, 2), np.float32); sv[:, s] = 1.0
            m_["selv"] = sv
        maps.append(m_)
    return maps


def kernel(**inputs):
    z = {k_: np.asarray(v) for k_, v in inputs.items()}
    res = run(build_mega(), mega_inputs(z))
    out = np.zeros((4, 2048, 2048), np.float32)
    for i in range(NCORES):
        b, s = i // 2, i % 2
        out[b, s * 1024:(s + 1) * 1024] = res[i]["xo"].transpose(2, 1, 0).reshape(1024, 2048)
    return out
```
